# Optimizing a Trainium2 kernel written in Bass

```python
import jax, jax.numpy as jnp
from jax import lax
import numpy as np

D_MODEL = 1024
BATCH = 1
SEQ = 16384
DEPTH = 1

CHUNK = 64
Q_BLOCK = 128
EPS = 1e-6
MAX_STREAM_OFFSET = 4096

D_RNN = D_MODEL
LRU_BLOCK_W = 256
LRU_BLOCKS = D_RNN // LRU_BLOCK_W
CONV_W = 4
LRU_C = 8.0

MLA_HEADS = 16
V_HEAD = D_MODEL // MLA_HEADS
QK_NOPE = 64
QK_ROPE = 32
QK_HEAD = QK_NOPE + QK_ROPE
Q_LORA = 768
KV_LORA = 256
ROPE_THETA = 10000.0

N_BRANCH = 2
IN_SPLITS = (D_RNN, D_RNN, Q_LORA, KV_LORA, QK_ROPE, N_BRANCH * D_MODEL)
D_IN = sum(IN_SPLITS)

N_GROUPS = 4
EXPERTS_PER_GROUP = 8
N_EXPERTS = N_GROUPS * EXPERTS_PER_GROUP
TOP_K = 2
D_EXPERT = 256
MOE_BLOCK = 128

kernel_name = "hybrid_rglru_mla_hiermoe_block"


def rms_norm(x, g):
    xf = x.astype(jnp.float32)
    y = xf * lax.rsqrt(jnp.mean(xf * xf, axis=-1, keepdims=True) + EPS)
    return (y * g.astype(jnp.float32)).astype(x.dtype)


def causal_conv(x, w, b):
    y = lax.conv_general_dilated(
        x, w[:, None, :].astype(x.dtype), window_strides=(1,),
        padding=[(CONV_W - 1, 0)], dimension_numbers=('NWC', 'WIO', 'NWC'),
        feature_group_count=x.shape[-1])
    return y + b.astype(x.dtype)


def rg_lru(x, wa, ba, wx, bx, lam):
    B, S, _ = x.shape
    xb = x.reshape(B, S, LRU_BLOCKS, LRU_BLOCK_W)
    r = jax.nn.sigmoid((jnp.einsum('bshi,hij->bshj', xb, wa).reshape(B, S, D_RNN) + ba).astype(jnp.float32))
    i = jax.nn.sigmoid((jnp.einsum('bshi,hij->bshj', xb, wx).reshape(B, S, D_RNN) + bx).astype(jnp.float32))
    log_a = -LRU_C * r * jax.nn.softplus(-lam.astype(jnp.float32))
    a = jnp.exp(log_a)
    mult = jnp.sqrt(-jnp.expm1(2.0 * log_a))
    b = mult * (i * x.astype(jnp.float32))

    def combine(left, right):
        a_l, b_l = left
        a_r, b_r = right
        return a_l * a_r, a_r * b_l + b_r

    _, h = lax.associative_scan(combine, (a, b), axis=1)
    return h.astype(x.dtype)


def rope(x, pos):
    half = QK_ROPE // 2
    freq = ROPE_THETA ** (-jnp.arange(half, dtype=jnp.float32) / half)
    ang = pos.astype(jnp.float32)[:, :, None, None] * freq
    cos, sin = jnp.cos(ang), jnp.sin(ang)
    x1 = x[..., :half].astype(jnp.float32)
    x2 = x[..., half:].astype(jnp.float32)
    return jnp.concatenate([x1 * cos - x2 * sin, x1 * sin + x2 * cos], axis=-1).astype(x.dtype)


def chunk_causal_attention(q, k, v):
    B, S, H, _ = q.shape
    scale = QK_HEAD ** -0.5
    outs = []
    for blk in range(S // Q_BLOCK):
        q0 = blk * Q_BLOCK
        k_end = q0 + Q_BLOCK
        qb = q[:, q0:k_end]
        s = jnp.einsum('bqhd,bkhd->bhqk', qb, k[:, :k_end]).astype(jnp.float32) * scale
        q_chunk = (q0 + jnp.arange(Q_BLOCK)) // CHUNK
        k_chunk = jnp.arange(k_end) // CHUNK
        mask = k_chunk[None, :] <= q_chunk[:, None]
        s = jnp.where(mask, s, -jnp.inf)
        p = jax.nn.softmax(s, axis=-1).astype(v.dtype)
        outs.append(jnp.einsum('bhqk,bkhd->bqhd', p, v[:, :k_end]))
    return jnp.concatenate(outs, axis=1).reshape(B, S, H * V_HEAD)


def mla(q_c, kv_c, k_pe, pos, q_a_g, w_uq, kv_a_g, w_ukv, q_norm_g, k_norm_g):
    B, S, _ = q_c.shape
    q = (rms_norm(q_c, q_a_g) @ w_uq).reshape(B, S, MLA_HEADS, QK_HEAD)
    kv = (rms_norm(kv_c, kv_a_g) @ w_ukv).reshape(B, S, MLA_HEADS, QK_NOPE + V_HEAD)
    k_nope, v = kv[..., :QK_NOPE], kv[..., QK_NOPE:]
    k_pe_h = jnp.broadcast_to(k_pe[:, :, None, :], (B, S, MLA_HEADS, QK_ROPE))
    k = jnp.concatenate([k_nope, k_pe_h], axis=-1)
    q = rms_norm(q, q_norm_g)
    k = rms_norm(k, k_norm_g)
    q = jnp.concatenate([q[..., :QK_NOPE], rope(q[..., QK_NOPE:], pos)], axis=-1)
    k = jnp.concatenate([k[..., :QK_NOPE], rope(k[..., QK_NOPE:], pos)], axis=-1)
    return chunk_causal_attention(q, k, v)


def hier_moe(x, wg, bg, we, be, w_gate, w_up, w_down):
    B, S, D = x.shape
    T = B * S
    xt = x.reshape(T, D)
    xf = xt.astype(jnp.float32)
    g_logits = xf @ wg.astype(jnp.float32) + bg.astype(jnp.float32)
    g_prob = jax.nn.softmax(g_logits, axis=-1)
    grp = jnp.argmax(g_logits, axis=-1)
    p_grp = jnp.take_along_axis(g_prob, grp[:, None], axis=1)
    e_logits = (xf @ we.astype(jnp.float32) + be.astype(jnp.float32)).reshape(T, N_GROUPS, EXPERTS_PER_GROUP)
    e_in = jnp.take_along_axis(e_logits, grp[:, None, None], axis=1)[:, 0]
    top_v, top_i = lax.top_k(e_in, TOP_K)
    combine = p_grp * jax.nn.softmax(top_v, axis=-1)
    expert = grp[:, None] * EXPERTS_PER_GROUP + top_i

    A = T * TOP_K
    e_flat = expert.reshape(A)
    tok_flat = jnp.arange(A, dtype=jnp.int32) // TOP_K
    c_flat = combine.reshape(A)
    order = jnp.argsort(e_flat)
    e_sorted = e_flat[order]
    counts = jnp.bincount(e_flat, length=N_EXPERTS)
    padded = (counts + MOE_BLOCK - 1) // MOE_BLOCK * MOE_BLOCK
    starts = jnp.cumsum(counts) - counts
    p_ends = jnp.cumsum(padded)
    p_starts = p_ends - padded
    dest = p_starts[e_sorted] + (jnp.arange(A) - starts[e_sorted])
    P = A + N_EXPERTS * MOE_BLOCK
    n_blk = P // MOE_BLOCK
    tok_pad = jnp.full((P,), T, jnp.int32).at[dest].set(tok_flat[order])
    c_pad = jnp.zeros((P,), jnp.float32).at[dest].set(c_flat[order])
    blk_expert = jnp.clip(jnp.searchsorted(p_ends, jnp.arange(n_blk) * MOE_BLOCK, side='right'), 0, N_EXPERTS - 1)
    x_pad = jnp.concatenate([xt, jnp.zeros((1, D), xt.dtype)], axis=0)[tok_pad].reshape(n_blk, MOE_BLOCK, D)

    def expert_block(args):
        xb, e = args
        hb = jax.nn.silu(xb @ w_gate[e]) * (xb @ w_up[e])
        return hb @ w_down[e]

    y = lax.map(expert_block, (x_pad, blk_expert)).reshape(P, D).astype(jnp.float32) * c_pad[:, None]
    out = jax.ops.segment_sum(y, tok_pad, num_segments=T + 1)[:T]
    return out.astype(x.dtype).reshape(B, S, D)


def setup_inputs(seed: int = 0) -> dict:
    key = jax.random.key(seed)
    ks = jax.random.split(key, 28)
    L = DEPTH

    def nrm(k, shape, s):
        return jax.random.normal(k, shape, jnp.float32) * s

    def gain(k, n):
        return 1.0 + 0.05 * jax.random.normal(k, (L, n), jnp.float32)

    x = jax.random.normal(ks[0], (BATCH, SEQ, D_MODEL), jnp.float32)
    offset = jax.random.randint(ks[1], (BATCH, 1), 0, MAX_STREAM_OFFSET, dtype=jnp.int32)
    positions = offset + jnp.arange(SEQ, dtype=jnp.int32)[None, :]
    a0 = jax.random.uniform(ks[2], (L, D_RNN), jnp.float32, 0.9, 0.999)
    s0 = a0 ** (1.0 / LRU_C)
    lru_lambda = jnp.log(s0) - jnp.log1p(-s0)
    return {
        "x": x,
        "positions": positions,
        "norm_mix_g": gain(ks[3], D_MODEL),
        "w_in": nrm(ks[4], (L, D_MODEL, D_IN), D_MODEL ** -0.5),
        "conv_w": nrm(ks[5], (L, CONV_W, D_RNN), CONV_W ** -0.5),
        "conv_b": nrm(ks[6], (L, D_RNN), 0.02),
        "lru_wa": nrm(ks[7], (L, LRU_BLOCKS, LRU_BLOCK_W, LRU_BLOCK_W), LRU_BLOCK_W ** -0.5),
        "lru_ba": nrm(ks[8], (L, D_RNN), 0.02),
        "lru_wx": nrm(ks[9], (L, LRU_BLOCKS, LRU_BLOCK_W, LRU_BLOCK_W), LRU_BLOCK_W ** -0.5),
        "lru_bx": nrm(ks[10], (L, D_RNN), 0.02),
        "lru_lambda": lru_lambda,
        "q_a_g": gain(ks[11], Q_LORA),
        "w_uq": nrm(ks[12], (L, Q_LORA, MLA_HEADS * QK_HEAD), Q_LORA ** -0.5),
        "kv_a_g": gain(ks[13], KV_LORA),
        "w_ukv": nrm(ks[14], (L, KV_LORA, MLA_HEADS * (QK_NOPE + V_HEAD)), KV_LORA ** -0.5),
        "q_norm_g": gain(ks[15], QK_HEAD),
        "k_norm_g": gain(ks[16], QK_HEAD),
        "w_out": nrm(ks[17], (L, D_MODEL, D_MODEL), D_MODEL ** -0.5),
        "norm_ffn_g": gain(ks[18], D_MODEL),
        "router_group_w": nrm(ks[19], (L, D_MODEL, N_GROUPS), D_MODEL ** -0.5),
        "router_group_b": nrm(ks[20], (L, N_GROUPS), 0.01),
        "router_expert_w": nrm(ks[21], (L, D_MODEL, N_EXPERTS), D_MODEL ** -0.5),
        "router_expert_b": nrm(ks[22], (L, N_EXPERTS), 0.01),
        "w_gate": nrm(ks[23], (L, N_EXPERTS, D_MODEL, D_EXPERT), D_MODEL ** -0.5),
        "w_up": nrm(ks[24], (L, N_EXPERTS, D_MODEL, D_EXPERT), D_MODEL ** -0.5),
        "w_down": nrm(ks[25], (L, N_EXPERTS, D_EXPERT, D_MODEL), D_EXPERT ** -0.5),
    }


def reference(x, positions, norm_mix_g, w_in, conv_w, conv_b, lru_wa, lru_ba, lru_wx, lru_bx,
              lru_lambda, q_a_g, w_uq, kv_a_g, w_ukv, q_norm_g, k_norm_g, w_out, norm_ffn_g,
              router_group_w, router_group_b, router_expert_w, router_expert_b,
              w_gate, w_up, w_down):
    B, S, _ = x.shape
    split_points = np.cumsum(IN_SPLITS)[:-1].tolist()
    h = x
    for l in range(DEPTH):
        u = rms_norm(h, norm_mix_g[l])
        proj = u @ w_in[l]
        x_rnn, g_rnn, q_c, kv_c, k_pe, gate_logits = jnp.split(proj, split_points, axis=-1)
        xa = causal_conv(x_rnn, conv_w[l], conv_b[l])
        ya = rg_lru(xa, lru_wa[l], lru_ba[l], lru_wx[l], lru_bx[l], lru_lambda[l]) * jax.nn.gelu(g_rnn)
        yb = mla(q_c, kv_c, k_pe, positions, q_a_g[l], w_uq[l], kv_a_g[l], w_ukv[l], q_norm_g[l], k_norm_g[l])
        gates = jax.nn.sigmoid(gate_logits.astype(jnp.float32)).reshape(B, S, N_BRANCH, D_MODEL)
        merged = gates[:, :, 0] * ya.astype(jnp.float32) + gates[:, :, 1] * yb.astype(jnp.float32)
        h = h + merged.astype(h.dtype) @ w_out[l]
        h = h + hier_moe(rms_norm(h, norm_ffn_g[l]), router_group_w[l], router_group_b[l],
                         router_expert_w[l], router_expert_b[l], w_gate[l], w_up[l], w_down[l])
    return h
```

```python
import os
import math
import numpy as np
import ml_dtypes
from contextlib import ExitStack
import concourse.bass as bass
import concourse.mybir as mybir
from concourse.bass_utils import run_bass_kernel_spmd

F32 = mybir.dt.float32
BF16 = mybir.dt.bfloat16
I32 = mybir.dt.int32
ALU = mybir.AluOpType
AF = mybir.ActivationFunctionType
AX = mybir.AxisListType

NCORES = 8
SEQ = 16384
D = 1024
GT = 512
NG = SEQ // GT
NOWN = 4
TOWN = NOWN * GT
EPS = 1e-6
TWO_PI = 2.0 * math.pi
C1 = 6.28125
C2 = TWO_PI - C1
NEXP = 32
DEXP = 256

PP = {}
_o = 0
for _n, _w in [("gmix", 8), ("convw", 32), ("convb", 8), ("ba", 8), ("bx", 8), ("lam", 8), ("qag", 6),
               ("kvag", 2), ("onehot", 8), ("qng", 1), ("gkfold", 1), ("gkpe", 1), ("freq64", 1),
               ("blockones", 2), ("gffn", 8), ("phase64", 1)]:
    PP[_n] = _o
    _o += _w
PPW = _o


class Buf:
    __slots__ = ("name", "last_w", "readers")

    def __init__(self, name=""):
        self.name = name
        self.last_w = None
        self.readers = []


class Sched:
    ENGS = ("sp", "pe", "act", "dve", "pool")
    DUR = {"pe": 0.25, "act": 0.62, "dve": 0.70, "pool": 1.1}

    def __init__(self, nc, stack, n_dma_sems=32, strict=True):
        self.nc = nc
        self.strict = strict
        self.esem = {e: stack.enter_context(nc.semaphore("prog_" + e)) for e in self.ENGS}
        self.dsems = [stack.enter_context(nc.semaphore("dma_%d" % i)) for i in range(n_dma_sems)]
        self.ops = []
        self.seg = 0

    def _add(self, kind, eng, fn, reads, writes, dur):
        oid = len(self.ops)
        deps = set()
        for b in reads:
            if b.last_w is not None:
                deps.add(b.last_w)
        for b in writes:
            if b.last_w is not None:
                deps.add(b.last_w)
            deps.update(b.readers)
        self.ops.append([kind, eng, fn, sorted(deps), dur, self.seg])
        for b in reads:
            b.readers.append(oid)
            if len(b.readers) > 700:
                b.readers = b.readers[-700:]
        for b in writes:
            b.last_w = oid
            b.readers = []
        return oid

    class _Probe:
        def __init__(self):
            self.name = None
            self.kw = {}

        def __getattr__(self, name):
            def f(*a, **kw):
                self.name = name
                self.kw = kw
                return self
            return f

    def _estimate(self, kind, eng, fn):
        try:
            p = Sched._Probe()
            fn(p)
            kw = p.kw
            if kind == "dma":
                o = kw.get("out")
                if o is None:
                    return 3.0
                n = 1
                for d_ in o.shape:
                    n *= int(d_)
                return 2.0 + n * mybir.dt.size(o.dtype) / 150e3
            o = kw.get("out")
            if eng == "pe":
                if p.name == "transpose":
                    return 0.09
                rhs = kw.get("rhs")
                nfree = 1
                for d_ in rhs.shape[1:]:
                    nfree *= int(d_)
                t = 0.03 + max(nfree, 64) / 2400.0
                if rhs.dtype == F32:
                    t *= 4.0
                return t
            nfree = 1
            for d_ in o.shape[1:]:
                nfree *= int(d_)
            if eng == "act":
                return 0.06 + nfree / 960.0 + (0.1 if kw.get("accum_out") is not None else 0.0)
            i0 = kw.get("in0", kw.get("in_", kw.get("data0", None)))
            b = 4
            try:
                b = max(mybir.dt.size(o.dtype), mybir.dt.size(i0.dtype)) if i0 is not None else mybir.dt.size(o.dtype)
            except Exception:
                pass
            t = 0.08 + nfree * (1.12e-3 if b >= 4 else 0.8e-3)
            if p.name == "tensor_tensor_scan":
                t = 0.08 + nfree * 2.1e-3
            if eng == "pool":
                t *= 1.8
            return t
        except Exception:
            return 3.0 if kind == "dma" else self.DUR[eng]

    def op(self, eng, fn, reads=(), writes=(), dur=None):
        return self._add("op", eng, fn, reads, writes, self._estimate("op", eng, fn) if dur is None else dur)

    def dma(self, eng, fn, reads=(), writes=(), dur=None):
        return self._add("dma", eng, fn, reads, writes, self._estimate("dma", eng, fn) if dur is None else dur)

    def barrier(self):
        self.seg += 1

    def _schedule_segment(self, ids, done_before):
        import heapq
        ops = self.ops
        idset = set(ids)
        ndeps = {}
        users = {}
        for i in ids:
            c = 0
            for d in ops[i][3]:
                if d in idset:
                    c += 1
                    users.setdefault(d, []).append(i)
            ndeps[i] = c
        blevel = {}
        for i in reversed(ids):
            m_ = 0.0
            for u in users.get(i, ()):
                if blevel[u] > m_:
                    m_ = blevel[u]
            blevel[i] = ops[i][4] + m_
        finish = {}
        eng_free = {e: 0.0 for e in self.ENGS}
        ready = {e: [] for e in self.ENGS}
        avail = {e: [] for e in self.ENGS}
        for i in ids:
            if ndeps[i] == 0:
                heapq.heappush(ready[ops[i][1]], (0.0, i))
        order = []
        n = len(ids)
        LAT = 0.12
        while len(order) < n:
            best = None
            for e in self.ENGS:
                h = ready[e]
                while h and h[0][0] <= eng_free[e]:
                    rt, i = heapq.heappop(h)
                    heapq.heappush(avail[e], (-blevel[i], i))
                if avail[e]:
                    cand = (eng_free[e], 0, e)
                elif h:
                    cand = (h[0][0], 1, e)
                else:
                    continue
                if best is None or cand < best:
                    best = cand
            st, which, e = best
            if which == 0:
                _, i = heapq.heappop(avail[e])
            else:
                st, i = heapq.heappop(ready[e])
            kind, _, _, _, dur, _ = ops[i]
            if kind == "dma":
                eng_free[e] = st + 0.08
                fin = st + dur
            else:
                eng_free[e] = st + dur
                fin = st + dur
            finish[i] = fin
            order.append(i)
            for u in users.get(i, ()):
                ndeps[u] -= 1
                if ndeps[u] == 0:
                    rt = 0.0
                    for d in ops[u][3]:
                        if d in finish:
                            lat = LAT if ops[d][1] != ops[u][1] else 0.05
                            rt = max(rt, finish[d] + lat)
                    heapq.heappush(ready[ops[u][1]], (rt, u))
        return order

    def emit(self):
        ops = self.ops
        nseg = self.seg + 1
        segs = [[] for _ in range(nseg)]
        for i, o in enumerate(ops):
            segs[o[5]].append(i)
        order = []
        for k in range(nseg):
            if segs[k]:
                order += self._schedule_segment(segs[k], None)
        ecount = {e: 0 for e in self.ENGS}
        dcount = [0] * len(self.dsems)
        dnext = 0
        tok = {}
        prev_dma_tok = {}
        for i in order:
            kind, eng = ops[i][0], ops[i][1]
            if kind == "op":
                ecount[eng] += 1
                tok[i] = (("e", eng), ecount[eng])
            else:
                j = dnext
                dnext = (dnext + 1) % len(self.dsems)
                if dcount[j] > 0:
                    prev_dma_tok[i] = (("d", j), dcount[j])
                dcount[j] += 16
                tok[i] = (("d", j), dcount[j])
        semobj = {}
        for e in self.ENGS:
            semobj[("e", e)] = self.esem[e]
        for j, s_ in enumerate(self.dsems):
            semobj[("d", j)] = s_
        prog = {e: [] for e in self.ENGS}
        waited = {e: {} for e in self.ENGS}
        last_seg = {e: 0 for e in self.ENGS}
        seg_tokens = []

        def need(eng, t):
            key, val = t
            if key == ("e", eng) and (eng == "pe" or not self.strict):
                return
            if val > waited[eng].get(key, 0):
                waited[eng][key] = val
                prog[eng].append(("w", semobj[key], val))

        seg_end = []
        cur = {}
        pos = 0
        for k in range(nseg):
            for _ in segs[k]:
                i = order[pos]; pos += 1
                key, val = tok[i]
                cur[key] = max(cur.get(key, 0), val)
            seg_end.append(dict(cur))
        for i in order:
            kind, eng, fn, deps, dur, sg = ops[i]
            if sg > last_seg[eng]:
                for key, val in seg_end[sg - 1].items():
                    need(eng, (key, val))
                last_seg[eng] = sg
            for d in deps:
                need(eng, tok[d])
            if i in prev_dma_tok:
                need(eng, prev_dma_tok[i])
            key, val = tok[i]
            prog[eng].append(("i", fn, semobj[key], 1 if kind == "op" else 16))
        for e in self.ENGS:
            for key, val in seg_end[-1].items():
                need(e, (key, val))
        names = {"sp": "sync", "pe": "tensor", "act": "scalar", "dve": "vector", "pool": "gpsimd"}
        with self.nc.Block() as block:
            for e in self.ENGS:
                items = prog[e]
                if not items:
                    continue

                def body(engobj, items=items):
                    for it in items:
                        if it[0] == "w":
                            engobj.wait_ge(it[1], it[2])
                        else:
                            it[1](engobj).then_inc(it[2], it[3])

                getattr(block, names[e])(body)


class PsumRot:
    def __init__(self, tiles):
        self.tiles = tiles
        self.free = list(range(len(tiles)))
        self.i = 0

    def reserve(self, n):
        r = [self.free.pop() for _ in range(n)]
        return [self.tiles[k] for k in r], r

    def release(self, idxs):
        self.free.extend(idxs)

    def next(self):
        k = self.free[self.i % len(self.free)]
        self.i += 1
        return self.tiles[k]


def build_program(debug=False, stop_after=None, phases="AB12CDE"):
    nc = bass.Bass("TRN2", target_bir_lowering=False)
    declared = []
    nc._declared_inputs = declared

    def din(name, shape, dt=F32, ph=None):
        if ph is not None and not any(p in phases for p in ph):
            return None
        declared.append(name)
        return nc.dram_tensor(name, list(shape), dt, kind="ExternalInput").ap()

    def dscr(name, shape, dt=F32):
        return nc.dram_tensor(name, list(shape), dt).ap()

    x_all = din("x_all", [SEQ, D], ph="A")
    x_own = din("x_own", [TOWN, D], ph="12D")
    posb_all = din("posb_all", [64, SEQ], I32, ph="A")
    posb_own = din("posb_own", [64, TOWN], I32, ph="A")
    pp_d = din("pp", [128, PPW])
    ident_d = din("ident", [128, 128])
    rot96_d = din("rot96", [96, 96])
    rot32_d = din("rot32", [32, 32])
    masks_d = din("masks", [128, 32, GT], BF16, ph="C")
    rbias_d = din("rbias", [128, 36], ph="D")
    gffnb_d = din("gffnb", [128, D], ph="D")
    w_in_d = din("w_in", [D, 5152], ph="A12")
    wa_d = din("lru_wa", [4, 256, 256], ph="A")
    wx_d = din("lru_wx", [4, 256, 256], ph="A")
    wuq_d = din("w_uq", [768, 1536], ph="2")
    wukv_d = din("w_ukv", [256, 2048], ph="A")
    wout_d = din("w_out", [D, D], ph="D")
    wr_d = din("w_router", [D, 36], ph="D")
    wg_d = din("w_gate", [NEXP, D, DEXP], ph="E")
    wu_d = din("w_up", [NEXP, D, DEXP], ph="E")
    wd_d = din("w_down", [NEXP, DEXP, D], ph="E")
    out_d = nc.dram_tensor("out", [TOWN, D], F32, kind="ExternalOutput").ap()
    dbg = {}
    if debug:
        for nm, shp in [("hl", [D, TOWN]), ("yb", [D, TOWN]), ("hmid", [TOWN, D]), ("qt", [96, 16 * TOWN]),
                        ("comb", [TOWN, 32])]:
            dbg[nm] = nc.dram_tensor("dbg_" + nm, shp, F32, kind="ExternalOutput").ap()
        dbg["hlb"] = nc.dram_tensor("dbg_hlb", [D, TOWN], BF16, kind="ExternalOutput").ap()
        dbg["rk"] = nc.dram_tensor("dbg_rk", [128, 2048], F32, kind="ExternalOutput").ap()
        dbg["ktn"] = nc.dram_tensor("dbg_ktn", [128, 512], BF16, kind="ExternalOutput").ap()
        dbg["kpe"] = nc.dram_tensor("dbg_kpe", [32, 512], BF16, kind="ExternalOutput").ap()
        dbg["v"] = nc.dram_tensor("dbg_v", [128, 512], BF16, kind="ExternalOutput").ap()

    ktn_d = dscr("ktn", [8 * 128, SEQ], BF16)
    kpe_d = dscr("kpe", [32, SEQ], BF16)
    v_d = dscr("vaug", [16, NG, 128, 4 * 128], BF16)
    ownh_d = dscr("ownh", [128, NOWN * 8 * GT], BF16)
    rk_d = dscr("rk", [128, (SEQ // 128) * 16])
    gaya_d = dscr("gaya", [128, 8, TOWN], BF16)
    sgb_d = dscr("sgb", [128, 8, TOWN], BF16)
    mg_d = dscr("mg", [128, 8, TOWN], BF16)
    qt_d = dscr("qt", [96, 16, TOWN], BF16)
    hmid_d = dscr("hmid", [TOWN, D])
    xgt_d = dscr("xgt", [128, 8, TOWN], BF16)
    combt_d = dscr("combt", [32, TOWN])
    kcs_d = dscr("kcs", [64, SEQ])
    qcs_d = dscr("qcs", [64, TOWN])

    with ExitStack() as top:
        S = Sched(nc, top)
        sb = lambda st, name, shape, dt=F32: st.enter_context(nc.sbuf_tensor("sb_" + name, list(shape), dt))

        ps_tiles = []
        for i in range(8):
            t = top.enter_context(nc.psum_tensor("ps%d" % i, [128, 512], F32))
            ps_tiles.append((t, Buf("ps%d" % i)))
        PS = PsumRot(ps_tiles)

        pp = sb(top, "pp_sb", [128, PPW]); b_pp = Buf("pp")
        ident = sb(top, "ident_sb", [128, 128])
        rot96 = sb(top, "rot96_sb", [96, 96])
        rot32 = sb(top, "rot32_sb", [32, 32])
        ones_f = sb(top, "ones_f", [128, 128])
        b_const = Buf("const")
        b_rk = Buf("rk"); b_ownh = Buf("ownh")
        b_rk_d = Buf("rk_d"); b_ownh_d = Buf("ownh_d"); b_gaya_d = Buf(); b_sgb_d = Buf(); b_mg_d = Buf(); b_qt_d = Buf()
        b_hmid_d = Buf(); b_xgt_d = Buf(); b_combt_d = Buf()
        b_ktn_d = Buf("ktn_d"); b_kpe_d = Buf("kpe_d"); b_v_d = Buf("v_d")
        c12 = sb(top, "c12", [128, 40]); b_c12 = Buf("c12")

        S.dma("sp", lambda e: e.dma_start(out=pp[:], in_=pp_d), writes=[b_pp])
        S.dma("sp", lambda e: e.dma_start(out=ident[:], in_=ident_d), writes=[b_const])
        S.dma("sp", lambda e: e.dma_start(out=rot96[:], in_=rot96_d), writes=[b_const])
        S.dma("sp", lambda e: e.dma_start(out=rot32[:], in_=rot32_d), writes=[b_const])
        S.op("dve", lambda e: e.memset(ones_f[:], 1.0), writes=[b_const])

        def ppc(name, j=0, rows=128, w=1):
            o = PP[name] + j
            return pp[0:rows, o:o + w]

        lam_ap = ppc("lam", 0, 128, 8)
        S.op("act", lambda e: e.activation(out=c12[:, 0:8], in_=lam_ap, func=AF.Exp, scale=-1.0), reads=[b_pp], writes=[b_c12])
        S.op("act", lambda e: e.activation(out=c12[:, 0:8], in_=c12[:, 0:8], func=AF.Ln, bias=1.0), reads=[b_c12], writes=[b_c12])
        S.op("dve", lambda e: e.tensor_scalar(out=c12[:, 8:16], in0=c12[:, 0:8], scalar1=-16.0, scalar2=None, op0=ALU.mult), reads=[b_c12], writes=[b_c12])
        S.op("dve", lambda e: e.tensor_scalar(out=c12[:, 16:24], in0=c12[:, 0:8], scalar1=-4.0, scalar2=None, op0=ALU.mult), reads=[b_c12], writes=[b_c12])
        S.op("dve", lambda e: e.tensor_scalar(out=c12[:, 0:8], in0=c12[:, 0:8], scalar1=-8.0, scalar2=None, op0=ALU.mult), reads=[b_c12], writes=[b_c12])
        S.op("dve", lambda e: e.tensor_scalar(out=c12[:, 24:32], in0=ppc("ba", 0, 128, 8), scalar1=0.5, scalar2=None, op0=ALU.mult), reads=[b_pp], writes=[b_c12])
        S.op("dve", lambda e: e.tensor_scalar(out=c12[:, 32:40], in0=ppc("bx", 0, 128, 8), scalar1=0.5, scalar2=None, op0=ALU.mult), reads=[b_pp], writes=[b_c12])

        def sincos_tables(st, posb_ap, ntok, dst_d, tag):
            CH = 2048
            pi_ = sb(st, tag + "_pi", [64, CH], I32)
            ang = sb(st, tag + "_ang", [64, CH])
            kf = sb(st, tag + "_kf", [64, CH])
            ki = sb(st, tag + "_ki", [64, CH], I32)
            msk = sb(st, tag + "_m", [64, CH])
            b = [Buf() for _ in range(5)]
            fr = ppc("freq64", 0, 64); ph = ppc("phase64", 0, 64)
            for c0 in range(0, ntok, CH):
                S.dma("sp", lambda e, c0=c0: e.dma_start(out=pi_[:], in_=posb_ap[:, c0:c0 + CH]), writes=[b[0]])
                S.op("dve", lambda e: e.tensor_copy(out=ang[:], in_=pi_[:]), reads=[b[0]], writes=[b[1]])
                S.op("dve", lambda e: e.tensor_scalar(out=ang[:], in0=ang[:], scalar1=fr, scalar2=ph, op0=ALU.mult, op1=ALU.add),
                     reads=[b[1], b_pp], writes=[b[1]])
                S.op("dve", lambda e: e.tensor_scalar(out=kf[:], in0=ang[:], scalar1=1.0 / TWO_PI, scalar2=None, op0=ALU.mult),
                     reads=[b[1]], writes=[b[2]])
                S.op("dve", lambda e: e.tensor_copy(out=ki[:], in_=kf[:]), reads=[b[2]], writes=[b[3]])
                S.op("dve", lambda e: e.tensor_copy(out=kf[:], in_=ki[:]), reads=[b[3]], writes=[b[2]])
                S.op("dve", lambda e: e.scalar_tensor_tensor(out=ang[:], in0=kf[:], scalar=-C1, in1=ang[:], op0=ALU.mult, op1=ALU.add),
                     reads=[b[2], b[1]], writes=[b[1]])
                S.op("dve", lambda e: e.scalar_tensor_tensor(out=ang[:], in0=kf[:], scalar=-C2, in1=ang[:], op0=ALU.mult, op1=ALU.add),
                     reads=[b[2], b[1]], writes=[b[1]])
                for thr, cmp_, corr in ((math.pi, ALU.is_gt, -TWO_PI), (-math.pi, ALU.is_lt, TWO_PI)):
                    S.op("dve", lambda e, thr=thr, cmp_=cmp_: e.tensor_single_scalar(out=msk[:], in_=ang[:], scalar=thr, op=cmp_),
                         reads=[b[1]], writes=[b[4]])
                    S.op("dve", lambda e, corr=corr: e.scalar_tensor_tensor(out=ang[:], in0=msk[:], scalar=corr, in1=ang[:], op0=ALU.mult, op1=ALU.add),
                         reads=[b[4], b[1]], writes=[b[1]])
                S.op("dve", lambda e: e.tensor_scalar(out=ang[:], in0=ang[:], scalar1=3.1415925, scalar2=-3.1415925, op0=ALU.min, op1=ALU.max),
                     reads=[b[1]], writes=[b[1]])
                S.op("act", lambda e: e.activation(out=kf[:], in_=ang[:], func=AF.Sin), reads=[b[1]], writes=[b[2]])
                S.dma("sp", lambda e, c0=c0: e.dma_start(out=dst_d[:, c0:c0 + CH], in_=kf[:]), reads=[b[2]], writes=[b_dram_cs])

        b_dram_cs = Buf("dram_cs")

        def load_norm_transpose(xsrc_ap, xt, bx_, ss, bss, uT, buT, t4, gcol=None):
            S.dma("sp", lambda e: e.dma_start(out=xt[:], in_=xsrc_ap), writes=[bx_])
            S.op("act", lambda e: e.activation(out=junk[:], in_=xt[:], func=AF.Square, accum_out=ss[:, 0:1]), reads=[bx_], writes=[bss, b_junk])
            S.op("act", lambda e: e.activation(out=ss[:, 1:2], in_=ss[:, 0:1], func=AF.Sqrt, scale=1.0 / D, bias=eps_t[:, 0:1]), reads=[bss], writes=[bss])
            S.op("dve", lambda e: e.reciprocal(out=ss[:, 2:3], in_=ss[:, 1:2]), reads=[bss], writes=[bss])
            S.op("dve", lambda e: e.tensor_scalar(out=xt[:], in0=xt[:], scalar1=ss[:, 2:3], scalar2=None, op0=ALU.mult), reads=[bss, bx_], writes=[bx_])
            for half in range(2):
                pt, pb = PS.next()
                for q in range(4):
                    kc = half * 4 + q
                    S.op("pe", lambda e, pt=pt, q=q, kc=kc: e.transpose(out=pt[:, q * 128:(q + 1) * 128], in_=xt[:, kc * 128:(kc + 1) * 128], identity=ident[:]),
                         reads=[bx_, b_const], writes=[pb])
                dst = uT[:, half * 4:half * 4 + 4, t4 * 128:(t4 + 1) * 128]
                src = pt[:].rearrange("p (q t) -> p q t", q=4)
                eng = "act" if half == 0 else "dve"
                if eng == "act":
                    S.op("act", lambda e, dst=dst, src=src: e.activation(out=dst, in_=src, func=AF.Copy), reads=[pb], writes=[buT])
                else:
                    S.op("dve", lambda e, dst=dst, src=src: e.tensor_copy(out=dst, in_=src), reads=[pb], writes=[buT])

        junk = sb(top, "junk", [128, D], BF16); b_junk = Buf("junk")
        eps_t = sb(top, "eps_t", [128, 1])
        eps96 = sb(top, "eps96", [128, 1])
        S.op("dve", lambda e: e.memset(eps_t[:], EPS), writes=[b_const])
        S.op("dve", lambda e: e.memset(eps96[:], 96.0 * EPS), writes=[b_const])

        def load_weight_cols(st, dst_bf, bdst, src_d, col_ranges, nk, scale_name, tag, stage_w):
            stg = [sb(st, "%s_stg%d" % (tag, i), [128, stage_w]) for i in range(2)]
            bst = [Buf(), Buf()]
            for kc in range(nk):
                s_ = stg[kc % 2]; bs_ = bst[kc % 2]
                o = 0
                for (a, b_) in col_ranges:
                    S.dma("sp", lambda e, s_=s_, o=o, a=a, b_=b_, kc=kc: e.dma_start(out=s_[:, o:o + (b_ - a)], in_=src_d[kc * 128:(kc + 1) * 128, a:b_]), writes=[bs_])
                    o += b_ - a
                eng = "dve" if kc % 2 == 0 else "pool"
                if scale_name is None:
                    S.op(eng, lambda e, s_=s_, kc=kc, o=o: e.tensor_copy(out=dst_bf[:, kc, 0:o], in_=s_[:, 0:o]), reads=[bs_], writes=[bdst])
                else:
                    sc = ppc(scale_name, kc)
                    S.op(eng, lambda e, s_=s_, kc=kc, o=o, sc=sc: e.tensor_scalar(out=dst_bf[:, kc, 0:o], in0=s_[:, 0:o], scalar1=sc, scalar2=None, op0=ALU.mult),
                         reads=[bs_, b_pp], writes=[bdst])

        with ExitStack() as pa:
          if "A" in phases:
            sincos_st = ExitStack()
            with sincos_st:
                sincos_tables(sincos_st, posb_all, SEQ, kcs_d, "csA")
                sincos_tables(sincos_st, posb_own, TOWN, qcs_d, "csO")
            S.barrier()

            rk_all = sb(pa, "rk_all", [128, SEQ // 128, 16])
            own_hb = [sb(pa, "own_h%d" % i, [128, 8, GT], BF16) for i in range(2)]; b_ownhb = [Buf(), Buf()]
            NA = 1024 + 256 + 32
            winA = sb(pa, "winA", [128, 8, NA], BF16); b_winA = Buf("winA")
            wab = sb(pa, "wab", [128, 8, 256], BF16); wxb = sb(pa, "wxb", [128, 8, 256], BF16)
            b_wab = Buf("wab")
            wukK = sb(pa, "wukK", [128, 2, 1024], BF16); wukV = sb(pa, "wukV", [128, 2, 1024], BF16)
            b_wuk = Buf("wuk")
            with ExitStack() as wp:
                load_weight_cols(wp, winA, b_winA, w_in_d, [(0, 1024), (2816, 3104)], 8, "gmix", "wA", NA)
                wst = sb(wp, "wa_stg", [128, 8, 256])
                b_wst = Buf()
                for (src, dstw) in ((wa_d, wab), (wx_d, wxb)):
                    S.dma("sp", lambda e, src=src: e.dma_start(out=wst[:], in_=src.rearrange("b (ic p) j -> p (b ic) j", p=128)), writes=[b_wst])
                    S.op("dve", lambda e, dstw=dstw: e.tensor_copy(out=dstw[:], in_=wst[:]), reads=[b_wst], writes=[b_wab])
                kst = sb(wp, "wukv_stg", [128, 2048]); b_kst = Buf()
                for kc in range(2):
                    S.dma("sp", lambda e, kc=kc: e.dma_start(out=kst[:], in_=wukv_d[kc * 128:(kc + 1) * 128, :]), writes=[b_kst])
                    sc = ppc("kvag", kc)
                    kv4 = kst[:].rearrange("p (h two d) -> p h two d", h=16, two=2)
                    S.op("dve", lambda e, kc=kc, sc=sc, kv4=kv4: e.tensor_scalar(out=wukK[:, kc, :].rearrange("p (h d) -> p h d", h=16), in0=kv4[:, :, 0, :], scalar1=sc, scalar2=None, op0=ALU.mult),
                         reads=[b_kst, b_pp], writes=[b_wuk])
                    S.op("dve", lambda e, kc=kc, sc=sc, kv4=kv4: e.tensor_scalar(out=wukV[:, kc, :].rearrange("p (h d) -> p h d", h=16), in0=kv4[:, :, 1, :], scalar1=sc, scalar2=None, op0=ALU.mult),
                         reads=[b_kst, b_pp], writes=[b_wuk])
                S.barrier()

            xt = [sb(pa, "xtA%d" % i, [128, D]) for i in range(2)]; b_xt = [Buf(), Buf()]
            ssA = [sb(pa, "ssA%d" % i, [128, 4]) for i in range(2)]; b_ss = [Buf(), Buf()]
            uT = [sb(pa, "uTA%d" % i, [128, 8, GT], BF16) for i in range(2)]; b_uT = [Buf(), Buf()]
            xr = sb(pa, "xr", [128, 8, GT + 3], BF16); b_xr = [Buf() for _ in range(8)]
            diagw = sb(pa, "diagw", [128, 8, 4, 128], BF16); b_diagw = Buf()
            xa = sb(pa, "xa", [128, 8, GT]); b_xa = [Buf() for _ in range(8)]
            xab = sb(pa, "xab", [128, 8, GT], BF16); b_xab = [Buf() for _ in range(8)]
            NT = 2
            tr = [sb(pa, "tr%d" % i, [128, GT]) for i in range(1)] * 2; b_tr = [Buf()] * 2
            ti = [sb(pa, "ti%d" % i, [128, GT]) for i in range(4)]; b_ti = [Buf() for _ in range(4)]
            ta = [sb(pa, "ta%d" % i, [128, GT]) for i in range(4)]; b_ta = [Buf() for _ in range(4)]
            tm = [sb(pa, "tm%d" % i, [128, GT]) for i in range(4)]; b_tm = [Buf() for _ in range(4)]
            hst = sb(pa, "hst", [128, 8]); b_hst = Buf("hst"); b_hst8 = [Buf() for _ in range(8)]
            b_oh8 = [[Buf() for _ in range(8)] for _ in range(2)]
            kvc = sb(pa, "kvc", [128, 2, GT]); b_kvc = Buf()
            kvsq = sb(pa, "kvsq", [128, 2, GT]); b_kvsq = Buf()
            kvn = sb(pa, "kvn", [128, 2, GT], BF16); b_kvn = Buf()
            rbc = sb(pa, "rbc", [128, GT]); b_rbc = Buf()
            kpr = sb(pa, "kpr", [32, GT]); b_kpr = Buf()
            kpsq = sb(pa, "kpsq", [32, GT]); b_kpsq = Buf()
            kpo = sb(pa, "kpo", [32, GT], BF16); b_kpo = Buf()
            kcs = sb(pa, "kcs_sb", [32, 2, GT]); b_kcs = Buf()
            ktn = [sb(pa, "ktn_sb%d" % i, [128, 8, GT], BF16) for i in range(1)] * 2; b_ktn = [Buf()] * 2
            ksq = [sb(pa, "ksq%d" % i, [128, GT]) for i in range(1)] * 2; b_ksq = [Buf()] * 2
            vau = sb(pa, "vau", [128, 16, 4, 128], BF16); b_vau = Buf()
            sstat = sb(pa, "sstat", [128, 16]); b_sstat = Buf()

            S.op("dve", lambda e: e.memset(xr[:], 0.0), writes=b_xr)
            for jc in range(8):
                for j in range(4):
                    S.op("dve", lambda e, jc=jc, j=j: e.tensor_scalar(out=diagw[:, jc, j, :], in0=ident[:, :], scalar1=ppc("convw", jc * 4 + j), scalar2=None, op0=ALU.mult),
                         reads=[b_const, b_pp], writes=[b_diagw])
            S.op("dve", lambda e: e.memset(hst[:], 0.0), writes=[b_hst] + b_hst8)
            S.op("pool", lambda e: e.memset(vau[:], 1.0), writes=[b_vau])

            n_groups_A = NG if stop_after != "A1" else 2
            (pst_l, pst_idx) = PS.reserve(1)
            pst, pbst = pst_l[0]
            def gen_H(G):
                u = uT[G % 2]; bu = b_uT[G % 2]
                for t4 in range(4):
                    i = (G * 4 + t4) % 2
                    r0 = G * GT + t4 * 128
                    load_norm_transpose(x_all[r0:r0 + 128, :], xt[i], b_xt[i], ssA[i], b_ss[i], u, bu, t4)
                    yield
            def emit_M(G):
                u = uT[G % 2]; bu = b_uT[G % 2]
                for jc in range(8):
                    pt, pb = PS.next()
                    for kc in range(8):
                        S.op("pe", lambda e, pt=pt, jc=jc, kc=kc, u=u: e.matmul(pt[:, :], lhsT=winA[:, kc, jc * 128:(jc + 1) * 128], rhs=u[:, kc, :], start=(kc == 0), stop=(kc == 7)),
                             reads=[b_winA, bu], writes=[pb])
                    S.op("act", lambda e, pt=pt, jc=jc: e.activation(out=xr[:, jc, 3:3 + GT], in_=pt[:, :], func=AF.Copy), reads=[pb], writes=[b_xr[jc]])
                for jc in range(2):
                    pt, pb = PS.next()
                    for kc in range(8):
                        S.op("pe", lambda e, pt=pt, jc=jc, kc=kc, u=u: e.matmul(pt[:, :], lhsT=winA[:, kc, 1024 + jc * 128:1024 + (jc + 1) * 128], rhs=u[:, kc, :], start=(kc == 0), stop=(kc == 7)),
                             reads=[b_winA, bu], writes=[pb])
                    S.op("act", lambda e, pt=pt, jc=jc: e.activation(out=kvc[:, jc, :], in_=pt[:, :], func=AF.Copy), reads=[pb], writes=[b_kvc])
                    S.op("dve", lambda e, pt=pt, jc=jc: e.tensor_tensor(out=kvsq[:, jc, :], in0=pt[:, :], in1=kvc[:, jc, :], op=ALU.mult), reads=[pb, b_kvc], writes=[b_kvsq])
                pt, pb = PS.next()
                for kc in range(8):
                    S.op("pe", lambda e, pt=pt, kc=kc, u=u: e.matmul(pt[0:32, :], lhsT=winA[:, kc, 1280:1312], rhs=u[:, kc, :], start=(kc == 0), stop=(kc == 7)),
                         reads=[b_winA, bu], writes=[pb])
                S.op("act", lambda e, pt=pt: e.activation(out=kpr[:, :], in_=pt[0:32, :], func=AF.Copy), reads=[pb], writes=[b_kpr])
                S.op("dve", lambda e, pt=pt: e.tensor_tensor(out=kpsq[:, :], in0=pt[0:32, :], in1=kpr[:, :], op=ALU.mult), reads=[pb, b_kpr], writes=[b_kpsq])
            def gen_KV(G):
                S.dma("sp", lambda e, G=G: e.dma_start(out=kcs[:, 0, :], in_=kcs_d[0:32, G * GT:(G + 1) * GT]), reads=[b_dram_cs], writes=[b_kcs])
                S.dma("sp", lambda e, G=G: e.dma_start(out=kcs[:, 1, :], in_=kcs_d[32:64, G * GT:(G + 1) * GT]), reads=[b_dram_cs], writes=[b_kcs])
                gk = ppc("gkpe", 0, 32)
                S.op("dve", lambda e, gk=gk: e.tensor_scalar(out=kpr[:, :], in0=kpr[:, :], scalar1=gk, scalar2=None, op0=ALU.mult), reads=[b_kpr, b_pp], writes=[b_kpr])
                pt, pb = PS.next()
                S.op("pe", lambda e, pt=pt: e.matmul(pt[0:32, :], lhsT=rot32[:, :], rhs=kpr[:, :], start=True, stop=True), reads=[b_kpr, b_const], writes=[pb])
                S.op("dve", lambda e, pt=pt: e.tensor_tensor(out=kcs[:, 0, :], in0=pt[0:32, :], in1=kcs[:, 0, :], op=ALU.mult), reads=[pb, b_kcs], writes=[b_kcs])
                S.op("dve", lambda e: e.tensor_tensor(out=kcs[:, 1, :], in0=kpr[:, :], in1=kcs[:, 1, :], op=ALU.mult), reads=[b_kpr, b_kcs], writes=[b_kcs])
                S.op("dve", lambda e: e.tensor_tensor(out=kpo[:, :], in0=kcs[:, 0, :], in1=kcs[:, 1, :], op=ALU.add), reads=[b_kcs], writes=[b_kpo])
                S.dma("pool", lambda e, G=G: e.dma_start(out=kpe_d[:, G * GT:(G + 1) * GT], in_=kpo[:, :]), reads=[b_kpo], writes=[b_kpe_d])
                yield
                pt, pb = PS.next()
                for jc in range(2):
                    S.op("pe", lambda e, pt=pt, jc=jc: e.matmul(pt[:, :], lhsT=ones_f[:, :], rhs=kvsq[:, jc, :], start=(jc == 0), stop=(jc == 1)), reads=[b_kvsq, b_const], writes=[pb])
                S.op("act", lambda e, pt=pt: e.activation(out=rbc[:, :], in_=pt[:, :], func=AF.Sqrt, scale=1.0 / 256, bias=eps_t[:, 0:1]), reads=[pb], writes=[b_rbc])
                S.op("dve", lambda e: e.reciprocal(out=rbc[:, :], in_=rbc[:, :]), reads=[b_rbc], writes=[b_rbc])
                yield
                for jc in range(2):
                    S.op("dve", lambda e, jc=jc: e.tensor_tensor(out=kvn[:, jc, :], in0=kvc[:, jc, :], in1=rbc[:, :], op=ALU.mult), reads=[b_kvc, b_rbc], writes=[b_kvn])
                kb = ktn[G % 2]; bkb = b_ktn[G % 2]
                for hp in range(8):
                    pt, pb = PS.next()
                    for kc in range(2):
                        S.op("pe", lambda e, pt=pt, kc=kc, hp=hp: e.matmul(pt[:, :], lhsT=wukK[:, kc, hp * 128:(hp + 1) * 128], rhs=kvn[:, kc, :], start=(kc == 0), stop=(kc == 1)),
                             reads=[b_wuk, b_kvn], writes=[pb])
                    S.op("act", lambda e, pt=pt, hp=hp, kb=kb: e.activation(out=kb[:, hp, :], in_=pt[:, :], func=AF.Copy), reads=[pb], writes=[bkb])
                    q_ = ksq[hp % 2]; bq_ = b_ksq[hp % 2]
                    S.op("pool", lambda e, hp=hp, kb=kb, q_=q_: e.tensor_tensor(out=q_[:, :], in0=kb[:, hp, :], in1=kb[:, hp, :], op=ALU.mult), reads=[bkb], writes=[bq_])
                    bo = ppc("blockones", 0, 128, 2)
                    for t4 in range(4):
                        S.op("pe", lambda e, pst=pst, q_=q_, t4=t4, hp=hp, bo=bo: e.matmul(pst[:, t4 * 16 + 2 * hp:t4 * 16 + 2 * hp + 2], lhsT=q_[:, t4 * 128:(t4 + 1) * 128], rhs=bo, start=True, stop=True, skip_group_check=True),
                             reads=[bq_, b_pp], writes=[pbst])
                    yield
                yield
                for t4 in range(4):
                    S.op("pe", lambda e, t4=t4: e.matmul(pst[:, 64 + t4:65 + t4], lhsT=kpsq[:, t4 * 128:(t4 + 1) * 128], rhs=ones_f[0:32, 0:1], start=True, stop=True, skip_group_check=True),
                         reads=[b_kpsq, b_const], writes=[pbst])
                S.op("dve", lambda e: e.tensor_copy(out=sstat[:, 0:4], in_=pst[:, 64:68]), reads=[pbst], writes=[b_sstat])
                for t4 in range(4):
                    tile_i = G * 4 + t4
                    S.op("dve", lambda e, pst=pst, t4=t4, tile_i=tile_i: e.tensor_scalar(out=rk_all[:, tile_i, :], in0=pst[:, t4 * 16:(t4 + 1) * 16], scalar1=sstat[:, t4:t4 + 1], scalar2=None, op0=ALU.add),
                         reads=[pbst, b_sstat], writes=[b_rk])
                S.dma("pool", lambda e, G=G, kb=kb: e.dma_start(out=ktn_d[:, G * GT:(G + 1) * GT].rearrange("(hp p) t -> p hp t", p=128), in_=kb[:, :, :]), reads=[bkb], writes=[b_ktn_d])
                for t4 in range(4):
                    for half in range(2):
                        pt, pb = PS.next()
                        for kc in range(2):
                            S.op("pe", lambda e, pt=pt, kc=kc, half=half, t4=t4: e.matmul(pt[:, :], lhsT=kvn[:, kc, t4 * 128:(t4 + 1) * 128], rhs=wukV[:, kc, half * 512:(half + 1) * 512], start=(kc == 0), stop=(kc == 1)),
                                 reads=[b_wuk, b_kvn], writes=[pb])
                        dst = vau[:, half * 8:(half + 1) * 8, t4, 0:64]
                        src = pt[:, :].rearrange("p (h d) -> p h d", h=8)
                        if half == 0:
                            S.op("act", lambda e, dst=dst, src=src: e.activation(out=dst, in_=src, func=AF.Copy), reads=[pb], writes=[b_vau])
                        else:
                            S.op("dve", lambda e, dst=dst, src=src: e.tensor_copy(out=dst, in_=src), reads=[pb], writes=[b_vau])
                    yield
                S.dma("pool", lambda e, G=G: e.dma_start(out=v_d[:, G, :, :].rearrange("h p c -> p h c"), in_=vau[:].rearrange("p h b c -> p h (b c)")), reads=[b_vau], writes=[b_v_d])
                yield
            def gen_LRU(G):
                for jc in range(8):
                    ptc, pbc = PS.next()
                    for j in range(4):
                        S.op("pe", lambda e, ptc=ptc, jc=jc, j=j: e.matmul(ptc[:, :], lhsT=diagw[:, jc, j, :], rhs=xr[:, jc, j:j + GT], start=(j == 0), stop=(j == 3)),
                             reads=[b_diagw, b_xr[jc]], writes=[pbc])
                    S.op("dve", lambda e, ptc=ptc, jc=jc: e.tensor_scalar(out=xa[:, jc, :], in0=ptc[:, :], scalar1=ppc("convb", jc), scalar2=None, op0=ALU.add), reads=[pbc, b_pp], writes=[b_xa[jc]])
                    S.op("pool", lambda e, jc=jc: e.tensor_copy(out=xab[:, jc, :], in_=xa[:, jc, :]), reads=[b_xa[jc]], writes=[b_xab[jc]])
                    S.op("pool", lambda e, jc=jc: e.tensor_copy(out=xr[:, jc, 0:3], in_=xr[:, jc, GT:GT + 3]), reads=[b_xr[jc]], writes=[b_xr[jc]])
                    yield
                for quad in range(2):
                    for jq in range(4):
                        jc = quad * 4 + jq
                        blk = jc // 2
                        s2 = jc % 2
                        ptr, pbr = PS.next()
                        for ic in range(2):
                            S.op("pe", lambda e, ptr=ptr, ic=ic, blk=blk, jc=jc: e.matmul(ptr[:, :], lhsT=wab[:, blk * 2 + ic, (jc % 2) * 128:(jc % 2 + 1) * 128], rhs=xab[:, blk * 2 + ic, :], start=(ic == 0), stop=(ic == 1)),
                                 reads=[b_wab, b_xab[blk * 2 + ic]], writes=[pbr])
                        pti, pbi = PS.next()
                        for ic in range(2):
                            S.op("pe", lambda e, pti=pti, ic=ic, blk=blk, jc=jc: e.matmul(pti[:, :], lhsT=wxb[:, blk * 2 + ic, (jc % 2) * 128:(jc % 2 + 1) * 128], rhs=xab[:, blk * 2 + ic, :], start=(ic == 0), stop=(ic == 1)),
                                 reads=[b_wab, b_xab[blk * 2 + ic]], writes=[pbi])
                        S.op("act", lambda e, ptr=ptr, s2=s2, jc=jc: e.activation(out=tr[s2][:, :], in_=ptr[:, :], func=AF.Tanh, scale=0.5, bias=c12[:, 24 + jc:25 + jc]), reads=[pbr, b_c12], writes=[b_tr[s2]])
                        S.op("act", lambda e, pti=pti, jq=jq, jc=jc: e.activation(out=ti[jq][:, :], in_=pti[:, :], func=AF.Tanh, scale=0.5, bias=c12[:, 32 + jc:33 + jc]), reads=[pbi, b_c12], writes=[b_ti[jq]])
                        S.op("act", lambda e, s2=s2, jq=jq, jc=jc: e.activation(out=ta[jq][:, :], in_=tr[s2][:, :], func=AF.Exp, scale=c12[:, 16 + jc:17 + jc], bias=c12[:, 16 + jc:17 + jc]), reads=[b_tr[s2], b_c12], writes=[b_ta[jq]])
                        S.op("act", lambda e, s2=s2, jq=jq, jc=jc: e.activation(out=tm[jq][:, :], in_=tr[s2][:, :], func=AF.Exp, scale=c12[:, jc:jc + 1], bias=c12[:, jc:jc + 1]), reads=[b_tr[s2], b_c12], writes=[b_tm[jq]])
                        S.op("dve", lambda e, jq=jq: e.tensor_scalar(out=tm[jq][:, :], in0=tm[jq][:, :], scalar1=-0.25, scalar2=0.25, op0=ALU.mult, op1=ALU.add), reads=[b_tm[jq]], writes=[b_tm[jq]])
                        S.op("dve", lambda e, jq=jq, jc=jc: e.scalar_tensor_tensor(out=ti[jq][:, :], in0=ti[jq][:, :], scalar=1.0, in1=xa[:, jc, :], op0=ALU.add, op1=ALU.mult), reads=[b_ti[jq], b_xa[jc]], writes=[b_ti[jq]])
                        yield
                    for jq in range(4):
                        S.op("act", lambda e, jq=jq: e.activation(out=tm[jq][:, :], in_=tm[jq][:, :], func=AF.Sqrt), reads=[b_tm[jq]], writes=[b_tm[jq]])
                    yield
                    for jq in range(4):
                        S.op("dve", lambda e, jq=jq: e.tensor_tensor(out=ti[jq][:, :], in0=ti[jq][:, :], in1=tm[jq][:, :], op=ALU.mult), reads=[b_ti[jq], b_tm[jq]], writes=[b_ti[jq]])
                    yield
                    for jq in range(4):
                        jc = quad * 4 + jq
                        S.op("dve", lambda e, jq=jq, jc=jc: e.tensor_tensor_scan(out=tm[jq][:, :], data0=ta[jq][:, :], data1=ti[jq][:, :], initial=hst[:, jc:jc + 1], op0=ALU.mult, op1=ALU.add),
                             reads=[b_ta[jq], b_ti[jq], b_hst8[jc]], writes=[b_tm[jq]])
                    for jq in range(4):
                        jc = quad * 4 + jq
                        S.op("dve", lambda e, jq=jq, jc=jc: e.tensor_copy(out=hst[:, jc:jc + 1], in_=tm[jq][:, GT - 1:GT]), reads=[b_tm[jq]], writes=[b_hst8[jc]])
                    yield
                    for jq in range(4):
                        jc = quad * 4 + jq
                        m = G // 8
                        oh = ppc("onehot", G % 8)
                        ohb = own_hb[m % 2]; bohb = b_ownhb[m % 2]
                        if G % 8 == 0:
                            S.op("dve", lambda e, jq=jq, jc=jc, ohb=ohb, oh=oh: e.tensor_scalar(out=ohb[:, jc, :], in0=tm[jq][:, :], scalar1=oh, scalar2=None, op0=ALU.mult),
                                 reads=[b_tm[jq], b_pp], writes=[b_oh8[m % 2][jc]])
                        else:
                            S.op("dve", lambda e, jq=jq, jc=jc, ohb=ohb, oh=oh: e.scalar_tensor_tensor(out=ohb[:, jc, :], in0=tm[jq][:, :], scalar=oh, in1=ohb[:, jc, :], op0=ALU.mult, op1=ALU.add),
                                 reads=[b_tm[jq], b_pp, b_oh8[m % 2][jc]], writes=[b_oh8[m % 2][jc]])
                if G % 8 == 7:
                    m = G // 8
                    ohb = own_hb[m % 2]; bohb = b_ownhb[m % 2]
                    S.dma("sp", lambda e, m=m, ohb=ohb: e.dma_start(out=ownh_d[:, m * 8 * GT:(m + 1) * 8 * GT], in_=ohb[:].rearrange("p j t -> p (j t)")), reads=b_oh8[m % 2], writes=[b_ownh_d])
                    if debug:
                        S.dma("sp", lambda e, m=m, ohb=ohb: e.dma_start(out=dbg["hlb"][:, m * GT:(m + 1) * GT].rearrange("(jc p) t -> p jc t", p=128), in_=ohb[:]), reads=b_oh8[m % 2], writes=[b_out])
                yield
            def interleave(*gens):
                gens = list(gens)
                while gens:
                    for g in list(gens):
                        try:
                            next(g)
                        except StopIteration:
                            gens.remove(g)
            for _ in gen_H(0):
                pass
            for G in range(n_groups_A):
                emit_M(G)
                gl = [gen_KV(G), gen_LRU(G)]
                if G + 1 < n_groups_A:
                    gl.append(gen_H(G + 1))
                interleave(*gl)
            S.op("act", lambda e: e.activation(out=rk_all[:], in_=rk_all[:], func=AF.Sqrt, bias=eps96[:, 0:1]), reads=[b_rk, b_const], writes=[b_rk])
            S.op("dve", lambda e: e.reciprocal(out=rk_all[:], in_=rk_all[:]), reads=[b_rk], writes=[b_rk])
            PS.release(pst_idx)
            S.dma("sp", lambda e: e.dma_start(out=rk_d, in_=rk_all[:].rearrange("p a b -> p (a b)")), reads=[b_rk], writes=[b_rk_d])
            if debug:
                dtmp = xa; b_dt = Buf()
                S.barrier()
                S.dma("sp", lambda e: e.dma_start(out=dbg["rk"], in_=rk_all[:].rearrange("p a b -> p (a b)")), reads=[b_rk], writes=[b_out])
                S.dma("sp", lambda e: e.dma_start(out=dbg["ktn"], in_=ktn_d[0:128, 0:512]), reads=[b_ktn_d], writes=[b_out])
                S.dma("sp", lambda e: e.dma_start(out=dbg["kpe"], in_=kpe_d[:, 0:512]), reads=[b_kpe_d], writes=[b_out])
                S.dma("sp", lambda e: e.dma_start(out=dbg["v"], in_=v_d[0, 0, :, :]), reads=[b_v_d], writes=[b_out])
            S.barrier()

        def own_group_uT(ph, m, xtb, b_xtb, ssb, b_ssb, uTo, b_uTo):
            for t4 in range(4):
                i = t4 % 2
                r0 = m * GT + t4 * 128
                load_norm_transpose(x_own[r0:r0 + 128, :], xtb[i], b_xtb[i], ssb[i], b_ssb[i], uTo, b_uTo, t4)

        if stop_after not in ("A1", "A") and "1" in phases:
          with ExitStack() as pb1:
            NB1 = 1024 + 2048
            winB = sb(pb1, "winB", [128, 8, NB1], BF16); b_winB = Buf()
            with ExitStack() as wp:
                load_weight_cols(wp, winB, b_winB, w_in_d, [(1024, 2048), (3104, 5152)], 8, "gmix", "wB", NB1)
                S.barrier()
            xtb = [sb(pb1, "xtB%d" % i, [128, D]) for i in range(2)]; b_xtb = [Buf(), Buf()]
            ssb = [sb(pb1, "ssB%d" % i, [128, 4]) for i in range(2)]; b_ssb = [Buf(), Buf()]
            uTo = [sb(pb1, "uTB%d" % i, [128, 8, GT], BF16) for i in range(2)]; b_uTo = [Buf(), Buf()]
            oh = sb(pb1, "ohB", [128, 8, GT], BF16); b_oh = Buf()
            gs = [sb(pb1, "gsB%d" % i, [128, GT]) for i in range(2)]; b_gs = [Buf(), Buf()]
            g2 = [sb(pb1, "g2B%d" % i, [128, GT]) for i in range(2)]; b_g2 = [Buf(), Buf()]
            sa = [sb(pb1, "saB%d" % i, [128, GT]) for i in range(2)]; b_sa = [Buf(), Buf()]
            gao = sb(pb1, "gaoB", [128, 8, GT], BF16); b_gao = Buf()
            sbo = sb(pb1, "sboB", [128, 8, GT], BF16); b_sbo = Buf()
            def interleave(*gens):
                gens = list(gens)
                while gens:
                    for g in list(gens):
                        try:
                            next(g)
                        except StopIteration:
                            gens.remove(g)

            def gen_headB(m, xtb, b_xtb, ssb, b_ssb, uTo, b_uTo):
                u = uTo[m % 2]; bu = b_uTo[m % 2]
                for t4 in range(4):
                    i = t4 % 2
                    r0 = m * GT + t4 * 128
                    load_norm_transpose(x_own[r0:r0 + 128, :], xtb[i], b_xtb[i], ssb[i], b_ssb[i], u, bu, t4)
                    yield

            def gen_chunksB1(m, par):
                u = uTo[m % 2]; bu = b_uTo[m % 2]
                for jc in range(par, 8, 2):
                    s_ = jc % 2
                    pt, pb = PS.next()
                    for kc in range(8):
                        S.op("pe", lambda e, pt=pt, jc=jc, kc=kc, u=u: e.matmul(pt[:, :], lhsT=winB[:, kc, jc * 128:(jc + 1) * 128], rhs=u[:, kc, :], start=(kc == 0), stop=(kc == 7)),
                             reads=[b_winB, bu], writes=[pb])
                    S.op("act", lambda e, pt=pt, s_=s_: e.activation(out=gs[s_][:, :], in_=pt[:, :], func=AF.Copy), reads=[pb], writes=[b_gs[s_]])
                    S.op("act", lambda e, pt=pt, s_=s_: e.activation(out=g2[s_][:, :], in_=pt[:, :], func=AF.Square), reads=[pb], writes=[b_g2[s_]])
                    yield
                    S.op("dve", lambda e, s_=s_: e.tensor_scalar(out=g2[s_][:, :], in0=g2[s_][:, :], scalar1=0.044715, scalar2=1.0, op0=ALU.mult, op1=ALU.add), reads=[b_g2[s_]], writes=[b_g2[s_]])
                    S.op("dve", lambda e, s_=s_: e.tensor_tensor(out=g2[s_][:, :], in0=g2[s_][:, :], in1=gs[s_][:, :], op=ALU.mult), reads=[b_g2[s_], b_gs[s_]], writes=[b_g2[s_]])
                    S.op("act", lambda e, s_=s_: e.activation(out=g2[s_][:, :], in_=g2[s_][:, :], func=AF.Sigmoid, scale=1.5957691216057308), reads=[b_g2[s_]], writes=[b_g2[s_]])
                    S.op("dve", lambda e, s_=s_: e.tensor_tensor(out=gs[s_][:, :], in0=gs[s_][:, :], in1=g2[s_][:, :], op=ALU.mult), reads=[b_g2[s_], b_gs[s_]], writes=[b_gs[s_]])
                    S.op("dve", lambda e, s_=s_, jc=jc: e.tensor_tensor(out=gs[s_][:, :], in0=gs[s_][:, :], in1=oh[:, jc, :], op=ALU.mult), reads=[b_oh, b_gs[s_]], writes=[b_gs[s_]])
                    yield
                    pt, pb = PS.next()
                    for kc in range(8):
                        S.op("pe", lambda e, pt=pt, jc=jc, kc=kc, u=u: e.matmul(pt[:, :], lhsT=winB[:, kc, 1024 + jc * 128:1024 + (jc + 1) * 128], rhs=u[:, kc, :], start=(kc == 0), stop=(kc == 7)),
                             reads=[b_winB, bu], writes=[pb])
                    S.op("act", lambda e, pt=pt, s_=s_: e.activation(out=sa[s_][:, :], in_=pt[:, :], func=AF.Sigmoid), reads=[pb], writes=[b_sa[s_]])
                    S.op("dve", lambda e, s_=s_, jc=jc: e.tensor_tensor(out=gao[:, jc, :], in0=gs[s_][:, :], in1=sa[s_][:, :], op=ALU.mult), reads=[b_sa[s_], b_gs[s_]], writes=[b_gao])
                    yield
                    pt, pb = PS.next()
                    for kc in range(8):
                        S.op("pe", lambda e, pt=pt, jc=jc, kc=kc, u=u: e.matmul(pt[:, :], lhsT=winB[:, kc, 2048 + jc * 128:2048 + (jc + 1) * 128], rhs=u[:, kc, :], start=(kc == 0), stop=(kc == 7)),
                             reads=[b_winB, bu], writes=[pb])
                    S.op("act", lambda e, pt=pt, jc=jc: e.activation(out=sbo[:, jc, :], in_=pt[:, :], func=AF.Sigmoid), reads=[pb], writes=[b_sbo])
                    yield

            for _ in gen_headB(0, xtb, b_xtb, ssb, b_ssb, uTo, b_uTo):
                pass
            for m in range(NOWN):
                S.dma("sp", lambda e, m=m: e.dma_start(out=oh[:].rearrange("p j t -> p (j t)"), in_=ownh_d[:, m * 8 * GT:(m + 1) * 8 * GT]), reads=[b_ownh_d], writes=[b_oh])
                gl = [gen_chunksB1(m, 0), gen_chunksB1(m, 1)]
                if m + 1 < NOWN:
                    gl.append(gen_headB(m + 1, xtb, b_xtb, ssb, b_ssb, uTo, b_uTo))
                interleave(*gl)
                S.dma("pool", lambda e, m=m: e.dma_start(out=gaya_d[:, :, m * GT:(m + 1) * GT], in_=gao[:]), reads=[b_gao], writes=[b_gaya_d])
                S.dma("pool", lambda e, m=m: e.dma_start(out=sgb_d[:, :, m * GT:(m + 1) * GT], in_=sbo[:]), reads=[b_sbo], writes=[b_sgb_d])
            S.barrier()

          with ExitStack() as pb2:
            winQ = sb(pb2, "winQ", [128, 8, 768], BF16); b_winQ = Buf()
            wuq = sb(pb2, "wuq", [128, 6, 1536], BF16); b_wuq = Buf()
            with ExitStack() as wp:
                load_weight_cols(wp, winQ, b_winQ, w_in_d, [(2048, 2816)], 8, "gmix", "wQ", 768)
                load_weight_cols(wp, wuq, b_wuq, wuq_d, [(0, 1536)], 6, "qag", "wUQ", 1536)
                S.barrier()
            xtb = [sb(pb2, "xtQ%d" % i, [128, D]) for i in range(2)]; b_xtb = [Buf(), Buf()]
            ssb = [sb(pb2, "ssQ%d" % i, [128, 4]) for i in range(2)]; b_ssb = [Buf(), Buf()]
            uTo = [sb(pb2, "uTQ%d" % i, [128, 8, GT], BF16) for i in range(2)]; b_uTo = [Buf(), Buf()]
            cosf = sb(pb2, "cosf", [96, TOWN]); sinf = sb(pb2, "sinf", [96, TOWN]); b_cs = Buf()
            S.op("dve", lambda e: e.memset(cosf[0:64, :], 1.0), writes=[b_cs])
            S.op("dve", lambda e: e.memset(sinf[0:64, :], 0.0), writes=[b_cs])
            S.dma("sp", lambda e: e.dma_start(out=sinf[64:96, :], in_=qcs_d[0:32, :]), reads=[b_dram_cs], writes=[b_cs])
            S.dma("sp", lambda e: e.dma_start(out=cosf[64:96, :], in_=qcs_d[32:64, :]), reads=[b_dram_cs], writes=[b_cs])
            qc = sb(pb2, "qc", [128, 6, GT]); b_qc = Buf()
            qsq = sb(pb2, "qsq", [128, 6, GT]); b_qsq = Buf()
            qcn = sb(pb2, "qcn", [128, 6, GT], BF16); b_qcn = Buf()
            rbq = sb(pb2, "rbq", [128, GT]); b_rbq = Buf()
            qs = [sb(pb2, "qs%d" % i, [96, GT]) for i in range(2)]; b_qs = [Buf(), Buf()]
            qq = [sb(pb2, "qq%d" % i, [96, GT]) for i in range(2)]; b_qq = [Buf(), Buf()]
            qr = [sb(pb2, "qr%d" % i, [96, GT]) for i in range(2)]; b_qr = [Buf(), Buf()]
            qn = [sb(pb2, "qn%d" % i, [96, GT]) for i in range(2)]; b_qn = [Buf(), Buf()]
            qto = sb(pb2, "qto", [96, 16, GT], BF16); b_qto = Buf()
            def gen_headsB2(m, par):
                for h in range(par, 16, 2):
                    s_ = h % 2
                    pt, pb = PS.next()
                    for kc in range(6):
                        S.op("pe", lambda e, pt=pt, h=h, kc=kc: e.matmul(pt[0:96, :], lhsT=wuq[:, kc, h * 96:(h + 1) * 96], rhs=qcn[:, kc, :], start=(kc == 0), stop=(kc == 5)),
                             reads=[b_wuq, b_qcn], writes=[pb])
                    S.op("act", lambda e, pt=pt, s_=s_: e.activation(out=qs[s_][:, :], in_=pt[0:96, :], func=AF.Copy), reads=[pb], writes=[b_qs[s_]])
                    S.op("act", lambda e, pt=pt, s_=s_: e.activation(out=qq[s_][:, :], in_=pt[0:96, :], func=AF.Square), reads=[pb], writes=[b_qq[s_]])
                    yield
                    pt2, pb2_ = PS.next()
                    S.op("pe", lambda e, pt2=pt2, s_=s_: e.matmul(pt2[0:96, :], lhsT=ones_f[0:96, 0:96], rhs=qq[s_][:, :], start=True, stop=True), reads=[b_qq[s_], b_const], writes=[pb2_])
                    S.op("act", lambda e, pt2=pt2, s_=s_: e.activation(out=qr[s_][:, :], in_=pt2[0:96, :], func=AF.Sqrt, scale=1.0 / 96, bias=eps_t[0:96, 0:1]), reads=[pb2_], writes=[b_qr[s_]])
                    S.op("dve", lambda e, s_=s_: e.reciprocal(out=qr[s_][:, :], in_=qr[s_][:, :]), reads=[b_qr[s_]], writes=[b_qr[s_]])
                    yield
                    qng = ppc("qng", 0, 96)
                    S.op("dve", lambda e, s_=s_, qng=qng: e.scalar_tensor_tensor(out=qn[s_][:, :], in0=qs[s_][:, :], scalar=qng, in1=qr[s_][:, :], op0=ALU.mult, op1=ALU.mult),
                         reads=[b_qs[s_], b_qr[s_], b_pp], writes=[b_qn[s_]])
                    pt3, pb3 = PS.next()
                    S.op("pe", lambda e, pt3=pt3, s_=s_: e.matmul(pt3[0:96, :], lhsT=rot96[:, :], rhs=qn[s_][:, :], start=True, stop=True), reads=[b_qn[s_], b_const], writes=[pb3])
                    S.op("dve", lambda e, pt3=pt3, s_=s_, m=m: e.tensor_tensor(out=qq[s_][:, :], in0=pt3[0:96, :], in1=sinf[:, m * GT:(m + 1) * GT], op=ALU.mult), reads=[pb3, b_cs], writes=[b_qq[s_]])
                    yield
                    S.op("dve", lambda e, s_=s_, m=m: e.tensor_tensor(out=qn[s_][:, :], in0=qn[s_][:, :], in1=cosf[:, m * GT:(m + 1) * GT], op=ALU.mult), reads=[b_qn[s_], b_cs], writes=[b_qn[s_]])
                    S.op("dve", lambda e, s_=s_: e.tensor_tensor(out=qn[s_][:, :], in0=qn[s_][:, :], in1=qq[s_][:, :], op=ALU.add), reads=[b_qn[s_], b_qq[s_]], writes=[b_qn[s_]])
                    gkf = ppc("gkfold", 0, 96)
                    S.op("act", lambda e, s_=s_, h=h, gkf=gkf: e.activation(out=qto[:, h, :], in_=qn[s_][:, :], func=AF.Copy, scale=gkf), reads=[b_qn[s_], b_pp], writes=[b_qto])
                    yield

            for _ in gen_headB(0, xtb, b_xtb, ssb, b_ssb, uTo, b_uTo):
                pass
            for m in range(NOWN):
                u = uTo[m % 2]; bu = b_uTo[m % 2]
                for jc in range(6):
                    pt, pb = PS.next()
                    for kc in range(8):
                        S.op("pe", lambda e, pt=pt, jc=jc, kc=kc, u=u: e.matmul(pt[:, :], lhsT=winQ[:, kc, jc * 128:(jc + 1) * 128], rhs=u[:, kc, :], start=(kc == 0), stop=(kc == 7)),
                             reads=[b_winQ, bu], writes=[pb])
                    S.op("act", lambda e, pt=pt, jc=jc: e.activation(out=qc[:, jc, :], in_=pt[:, :], func=AF.Copy), reads=[pb], writes=[b_qc])
                    S.op("dve", lambda e, pt=pt, jc=jc: e.tensor_tensor(out=qsq[:, jc, :], in0=pt[:, :], in1=qc[:, jc, :], op=ALU.mult), reads=[pb, b_qc], writes=[b_qsq])
                pt, pb = PS.next()
                for jc in range(6):
                    S.op("pe", lambda e, pt=pt, jc=jc: e.matmul(pt[:, :], lhsT=ones_f[:, :], rhs=qsq[:, jc, :], start=(jc == 0), stop=(jc == 5)), reads=[b_qsq, b_const], writes=[pb])
                S.op("act", lambda e, pt=pt: e.activation(out=rbq[:, :], in_=pt[:, :], func=AF.Sqrt, scale=1.0 / 768, bias=eps_t[:, 0:1]), reads=[pb], writes=[b_rbq])
                S.op("dve", lambda e: e.reciprocal(out=rbq[:, :], in_=rbq[:, :]), reads=[b_rbq], writes=[b_rbq])
                for jc in range(6):
                    S.op("dve", lambda e, jc=jc: e.tensor_tensor(out=qcn[:, jc, :], in0=qc[:, jc, :], in1=rbq[:, :], op=ALU.mult), reads=[b_qc, b_rbq], writes=[b_qcn])
                gl = [gen_headsB2(m, 0), gen_headsB2(m, 1)]
                if m + 1 < NOWN:
                    gl.append(gen_headB(m + 1, xtb, b_xtb, ssb, b_ssb, uTo, b_uTo))
                interleave(*gl)
                S.dma("pool", lambda e, m=m: e.dma_start(out=qt_d[:, :, m * GT:(m + 1) * GT], in_=qto[:]), reads=[b_qto], writes=[b_qt_d])
            S.barrier()

        if stop_after not in ("A1", "A", "B") and "C" in phases:
          with ExitStack() as pc:
            masks = sb(pc, "masks", [128, 32, GT], BF16); b_masks = Buf()
            S.dma("sp", lambda e: e.dma_start(out=masks[:], in_=masks_d), writes=[b_masks])
            rk = sb(pc, "rkC", [128, SEQ // 128, 16]); b_rkc = Buf()
            S.dma("sp", lambda e: e.dma_start(out=rk[:].rearrange("p a b -> p (a b)"), in_=rk_d), reads=[b_rk_d], writes=[b_rkc])
            qth = [sb(pc, "qth%d" % i, [96, TOWN], BF16) for i in range(2)]; b_qth = [Buf(), Buf()]
            NR = 4
            kp = [sb(pc, "kp%d" % i, [96, GT], BF16) for i in range(NR)]; b_kp = [Buf() for _ in range(NR)]
            vp = [sb(pc, "vp%d" % i, [128, 4, 128], BF16) for i in range(NR)]; b_vp = [Buf() for _ in range(NR)]
            NPB = 4
            pbuf = [sb(pc, "pb%d" % i, [128, GT], BF16) for i in range(NPB)]; b_pbuf = [Buf() for _ in range(NPB)]
            gat = sb(pc, "gat", [128, TOWN], BF16); b_gat = Buf()
            sgt = sb(pc, "sgt", [128, TOWN], BF16); b_sgt = Buf()
            mgt = sb(pc, "mgt", [128, TOWN], BF16); b_mgt = Buf()
            rl = sb(pc, "rl", [128, GT]); b_rl = Buf()
            rl2 = sb(pc, "rl2", [128, GT]); b_rl2 = Buf()
            ybn = sb(pc, "ybn", [128, GT]); b_ybn = Buf()
            ybs = sb(pc, "ybs", [128, GT]); b_ybs = Buf()
            (s_banks, s_idx) = PS.reserve(4)
            (o_banks, o_idx) = PS.reserve(4)
            n_heads_c = 16 if stop_after != "C1" else 2
            piece = 0
            stepno = 0
            for h in range(n_heads_c):
                q_ = qth[h % 2]; bq_ = b_qth[h % 2]
                S.dma("sp", lambda e, h=h, q_=q_: e.dma_start(out=q_[:, :], in_=qt_d[:, h, :]), reads=[b_qt_d], writes=[bq_])
                hr = (h % 2) * 64
                kcx = h // 2
                S.dma("sp", lambda e, hr=hr, kcx=kcx: e.dma_start(out=gat[hr:hr + 64, :], in_=gaya_d[hr:hr + 64, kcx, :]), reads=[b_gaya_d], writes=[b_gat])
                S.dma("sp", lambda e, hr=hr, kcx=kcx: e.dma_start(out=sgt[hr:hr + 64, :], in_=sgb_d[hr:hr + 64, kcx, :]), reads=[b_sgb_d], writes=[b_sgt])
                pendq = []

                def emit_pv(t_):
                    (ot2, ob2, vt2, bvt2, blk2, pbf2, bpbf2, f2, l2) = t_
                    S.op("pe", lambda e: e.matmul(ot2[:, :], lhsT=vt2[:, blk2, :], rhs=pbf2[:, :], start=f2, stop=l2),
                         reads=[bvt2, bpbf2], writes=[ob2])
                for G in range(NG):
                    r = piece % NR; piece += 1
                    kt = kp[r]; bkt = b_kp[r]; vt = vp[r]; bvt = b_vp[r]
                    S.dma("sp", lambda e, h=h, G=G, kt=kt: e.dma_start(out=kt[0:64, :], in_=ktn_d[h * 64:(h + 1) * 64, G * GT:(G + 1) * GT]), reads=[b_ktn_d], writes=[bkt])
                    S.dma("sp", lambda e, G=G, kt=kt: e.dma_start(out=kt[64:96, :], in_=kpe_d[:, G * GT:(G + 1) * GT]), reads=[b_kpe_d], writes=[bkt])
                    S.dma("pool", lambda e, h=h, G=G, vt=vt: e.dma_start(out=vt[:].rearrange("p b c -> p (b c)"), in_=v_d[h, G, :, :]), reads=[b_v_d], writes=[bvt])
                    for m in range(G // 8, NOWN):
                        ot, ob = o_banks[m]
                        for blk in range(4):
                            si = stepno % 4; stepno += 1
                            st_, sb_ = s_banks[si]
                            pbf = pbuf[si]; bpbf = b_pbuf[si]
                            S.op("pe", lambda e, st_=st_, kt=kt, blk=blk, q_=q_, m=m: e.matmul(st_[:, :], lhsT=kt[:, blk * 128:(blk + 1) * 128], rhs=q_[:, m * GT:(m + 1) * GT], start=True, stop=True),
                                 reads=[bkt, bq_], writes=[sb_])
                            sc = rk[:, G * 4 + blk, h:h + 1]
                            S.op("act", lambda e, st_=st_, pbf=pbf, sc=sc: e.activation(out=pbf[:, :], in_=st_[:, :], func=AF.Exp, scale=sc), reads=[sb_, b_rkc], writes=[bpbf])
                            if G // 8 == m:
                                mj = (G % 8) * 4 + blk
                                S.op("dve", lambda e, pbf=pbf, mj=mj: e.tensor_tensor(out=pbf[:, :], in0=pbf[:, :], in1=masks[:, mj, :], op=ALU.mult), reads=[bpbf, b_masks], writes=[bpbf])
                            first = (G == 0 and blk == 0)
                            last = (G == 8 * m + 7 and blk == 3)
                            pendq.append((ot, ob, vt, bvt, blk, pbf, bpbf, first, last))
                            if len(pendq) > 2:
                                emit_pv(pendq.pop(0))
                    if G % 8 == 7:
                        while pendq:
                            emit_pv(pendq.pop(0))
                        m = G // 8
                        ot, ob = o_banks[m]
                        cs = slice(m * GT, (m + 1) * GT)
                        S.op("dve", lambda e, ot=ot: e.reciprocal(out=rl[64:128, :], in_=ot[64:128, :]), reads=[ob], writes=[b_rl])
                        S.op("dve", lambda e: e.tensor_copy(out=rl2[0:64, :], in_=rl[64:128, :]), reads=[b_rl], writes=[b_rl2])
                        S.op("dve", lambda e, ot=ot: e.tensor_tensor(out=ybn[0:64, :], in0=ot[0:64, :], in1=rl2[0:64, :], op=ALU.mult), reads=[ob, b_rl2], writes=[b_ybn])
                        if hr == 0:
                            ysrc = ybn; bys = b_ybn
                        else:
                            S.op("dve", lambda e: e.tensor_copy(out=ybs[64:128, :], in_=ybn[0:64, :]), reads=[b_ybn], writes=[b_ybs])
                            ysrc = ybs; bys = b_ybs
                        if debug:
                            S.dma("sp", lambda e, ysrc=ysrc, hr=hr, h=h, cs=cs: e.dma_start(out=dbg["yb"][h * 64:(h + 1) * 64, cs], in_=ysrc[hr:hr + 64, :]), reads=[bys], writes=[b_out])
                        S.op("dve", lambda e, ysrc=ysrc, hr=hr, cs=cs: e.tensor_tensor(out=ysrc[hr:hr + 64, :], in0=ysrc[hr:hr + 64, :], in1=sgt[hr:hr + 64, cs], op=ALU.mult), reads=[bys, b_sgt], writes=[bys])
                        S.op("dve", lambda e, ysrc=ysrc, hr=hr, cs=cs: e.tensor_tensor(out=mgt[hr:hr + 64, cs], in0=ysrc[hr:hr + 64, :], in1=gat[hr:hr + 64, cs], op=ALU.add), reads=[bys, b_gat], writes=[b_mgt])
                S.dma("pool", lambda e, hr=hr, kcx=kcx: e.dma_start(out=mg_d[hr:hr + 64, kcx, :], in_=mgt[hr:hr + 64, :]), reads=[b_mgt], writes=[b_mg_d])
            PS.release(s_idx); PS.release(o_idx)
            S.barrier()

        if stop_after not in ("A1", "A", "B", "C", "C1") and "D" in phases:
          with ExitStack() as pd:
            woutb = sb(pd, "woutb", [128, 8, D], BF16); b_woutb = Buf()
            with ExitStack() as wp:
                load_weight_cols(wp, woutb, b_woutb, wout_d, [(0, D)], 8, None, "wO", D)
                S.barrier()
            wr = sb(pd, "wr", [128, 8, 36]); b_wr = Buf()
            S.dma("sp", lambda e: e.dma_start(out=wr[:], in_=wr_d.rearrange("(kc p) n -> p kc n", p=128)), writes=[b_wr])
            rbias = sb(pd, "rbias", [128, 36]); gfb = sb(pd, "gfb", [128, D]); b_rb = Buf()
            S.dma("sp", lambda e: e.dma_start(out=rbias[:], in_=rbias_d), writes=[b_rb])
            S.dma("sp", lambda e: e.dma_start(out=gfb[:], in_=gffnb_d), writes=[b_rb])
            mgs = [sb(pd, "mgs%d" % i, [128, 8, GT], BF16) for i in range(2)]; b_mgs = [Buf(), Buf()]
            xtd = [sb(pd, "xtD%d" % i, [128, D]) for i in range(2)]; b_xtd = [Buf(), Buf()]
            hm = [sb(pd, "hm%d" % i, [128, D]) for i in range(2)]; b_hm = [Buf(), Buf()]
            xn = [sb(pd, "xn%d" % i, [128, D]) for i in range(2)]; b_xn = [Buf(), Buf()]
            ssd = [sb(pd, "ssD%d" % i, [128, 4]) for i in range(2)]; b_ssd = [Buf(), Buf()]
            xnT = [sb(pd, "xnT%d" % i, [128, 8, 128]) for i in range(2)]; b_xnT = [Buf(), Buf()]
            xgs = sb(pd, "xgs", [128, 8, GT], BF16); b_xgs = Buf()
            cts = sb(pd, "cts", [32, GT]); b_cts = Buf()
            R = {}
            for nm, w in [("lg", 36), ("gmax", 1), ("goh", 4), ("ngm", 1), ("ge", 4), ("gsum", 1), ("pg", 1), ("gpen", 4), ("em", 32),
                          ("t1", 1), ("m1", 32), ("em2", 32), ("t2", 1), ("m2", 32), ("dd", 1), ("ed", 1), ("w1", 1), ("w2", 1), ("comb", 32)]:
                R[nm] = [sb(pd, "r_%s%d" % (nm, i), [128, w]) for i in range(2)]
            b_R = [Buf(), Buf()]
            BIG = 1.0e9
            (ct_l, ct_idx) = PS.reserve(1)
            ctp, ctb = ct_l[0]
            for m in range(NOWN):
                mg_ = mgs[m % 2]; bmg_ = b_mgs[m % 2]
                S.dma("sp", lambda e, m=m, mg_=mg_: e.dma_start(out=mg_[:], in_=mg_d[:, :, m * GT:(m + 1) * GT]), reads=[b_mg_d], writes=[bmg_])
                for t4 in range(4):
                    tt = m * 4 + t4
                    i = tt % 2
                    r0 = tt * 128
                    S.dma("sp", lambda e, r0=r0, i=i: e.dma_start(out=xtd[i][:], in_=x_own[r0:r0 + 128, :]), writes=[b_xtd[i]])
                    for nh in range(2):
                        pt, pb = PS.next()
                        for kc in range(8):
                            S.op("pe", lambda e, pt=pt, kc=kc, nh=nh, t4=t4, mg_=mg_: e.matmul(pt[:, :], lhsT=mg_[:, kc, t4 * 128:(t4 + 1) * 128], rhs=woutb[:, kc, nh * 512:(nh + 1) * 512], start=(kc == 0), stop=(kc == 7)),
                                 reads=[bmg_, b_woutb], writes=[pb])
                        S.op("dve", lambda e, pt=pt, nh=nh, i=i: e.tensor_tensor(out=hm[i][:, nh * 512:(nh + 1) * 512], in0=pt[:, :], in1=xtd[i][:, nh * 512:(nh + 1) * 512], op=ALU.add),
                             reads=[pb, b_xtd[i]], writes=[b_hm[i]])
                    S.dma("sp", lambda e, r0=r0, i=i: e.dma_start(out=hmid_d[r0:r0 + 128, :], in_=hm[i][:]), reads=[b_hm[i]], writes=[b_hmid_d])
                    if debug:
                        S.dma("sp", lambda e, r0=r0, i=i: e.dma_start(out=dbg["hmid"][r0:r0 + 128, :], in_=hm[i][:]), reads=[b_hm[i]], writes=[b_out])
                    ss = ssd[i]; bss = b_ssd[i]
                    S.op("act", lambda e, i=i, ss=ss: e.activation(out=junk[:], in_=hm[i][:], func=AF.Square, accum_out=ss[:, 0:1]), reads=[b_hm[i]], writes=[bss, b_junk])
                    S.op("act", lambda e, ss=ss: e.activation(out=ss[:, 1:2], in_=ss[:, 0:1], func=AF.Sqrt, scale=1.0 / D, bias=eps_t[:, 0:1]), reads=[bss], writes=[bss])
                    S.op("dve", lambda e, ss=ss: e.reciprocal(out=ss[:, 2:3], in_=ss[:, 1:2]), reads=[bss], writes=[bss])
                    S.op("dve", lambda e, ss=ss, i=i: e.scalar_tensor_tensor(out=xn[i][:], in0=hm[i][:], scalar=ss[:, 2:3], in1=gfb[:], op0=ALU.mult, op1=ALU.mult),
                         reads=[bss, b_hm[i], b_rb], writes=[b_xn[i]])
                    for half in range(2):
                        pt, pb = PS.next()
                        for q in range(4):
                            kc = half * 4 + q
                            S.op("pe", lambda e, pt=pt, q=q, kc=kc, i=i: e.transpose(out=pt[:, q * 128:(q + 1) * 128], in_=xn[i][:, kc * 128:(kc + 1) * 128], identity=ident[:]),
                                 reads=[b_xn[i], b_const], writes=[pb])
                        dst = xnT[i][:, half * 4:half * 4 + 4, :]
                        src = pt[:].rearrange("p (q t) -> p q t", q=4)
                        if half == 0:
                            S.op("act", lambda e, dst=dst, src=src: e.activation(out=dst, in_=src, func=AF.Copy), reads=[pb], writes=[b_xnT[i]])
                        else:
                            S.op("dve", lambda e, dst=dst, src=src: e.tensor_copy(out=dst, in_=src), reads=[pb], writes=[b_xnT[i]])
                    S.op("pool", lambda e, i=i, t4=t4: e.tensor_copy(out=xgs[:, :, t4 * 128:(t4 + 1) * 128], in_=xnT[i][:, :, :]), reads=[b_xnT[i]], writes=[b_xgs])
                    pt, pb = PS.next()
                    for kc in range(8):
                        S.op("pe", lambda e, pt=pt, kc=kc, i=i: e.matmul(pt[:, 0:36], lhsT=xnT[i][:, kc, :], rhs=wr[:, kc, :], start=(kc == 0), stop=(kc == 7)),
                             reads=[b_xnT[i], b_wr], writes=[pb])
                    r = {k: v[i] for k, v in R.items()}
                    br = b_R[i]
                    rd = [br, b_rb]
                    S.op("dve", lambda e, pt=pt, r=r: e.tensor_tensor(out=r["lg"][:, :], in0=pt[:, 0:36], in1=rbias[:, :], op=ALU.add), reads=[pb, b_rb, br], writes=[br])
                    S.op("dve", lambda e, r=r: e.tensor_reduce(out=r["gmax"][:, :], in_=r["lg"][:, 0:4], axis=AX.X, op=ALU.max), reads=rd, writes=[br])
                    S.op("dve", lambda e, r=r: e.tensor_scalar(out=r["goh"][:, :], in0=r["lg"][:, 0:4], scalar1=r["gmax"][:, 0:1], scalar2=None, op0=ALU.is_ge), reads=rd, writes=[br])
                    S.op("dve", lambda e, r=r: e.tensor_scalar(out=r["ngm"][:, :], in0=r["gmax"][:, :], scalar1=-1.0, scalar2=None, op0=ALU.mult), reads=rd, writes=[br])
                    S.op("act", lambda e, r=r: e.activation(out=r["ge"][:, :], in_=r["lg"][:, 0:4], func=AF.Exp, bias=r["ngm"][:, 0:1], accum_out=r["gsum"][:, 0:1]), reads=rd, writes=[br])
                    S.op("dve", lambda e, r=r: e.reciprocal(out=r["pg"][:, :], in_=r["gsum"][:, :]), reads=rd, writes=[br])
                    S.op("dve", lambda e, r=r: e.tensor_scalar(out=r["gpen"][:, :], in0=r["goh"][:, :], scalar1=BIG, scalar2=-BIG, op0=ALU.mult, op1=ALU.add), reads=rd, writes=[br])
                    for g in range(4):
                        S.op("dve", lambda e, r=r, g=g: e.tensor_scalar(out=r["em"][:, g * 8:(g + 1) * 8], in0=r["lg"][:, 4 + g * 8:4 + (g + 1) * 8], scalar1=r["gpen"][:, g:g + 1], scalar2=None, op0=ALU.add), reads=rd, writes=[br])
                    S.op("dve", lambda e, r=r: e.tensor_reduce(out=r["t1"][:, :], in_=r["em"][:, :], axis=AX.X, op=ALU.max), reads=rd, writes=[br])
                    S.op("dve", lambda e, r=r: e.tensor_scalar(out=r["m1"][:, :], in0=r["em"][:, :], scalar1=r["t1"][:, 0:1], scalar2=None, op0=ALU.is_ge), reads=rd, writes=[br])
                    S.op("dve", lambda e, r=r: e.scalar_tensor_tensor(out=r["em2"][:, :], in0=r["m1"][:, :], scalar=-BIG, in1=r["em"][:, :], op0=ALU.mult, op1=ALU.add), reads=rd, writes=[br])
                    S.op("dve", lambda e, r=r: e.tensor_reduce(out=r["t2"][:, :], in_=r["em2"][:, :], axis=AX.X, op=ALU.max), reads=rd, writes=[br])
                    S.op("dve", lambda e, r=r: e.tensor_scalar(out=r["m2"][:, :], in0=r["em2"][:, :], scalar1=r["t2"][:, 0:1], scalar2=None, op0=ALU.is_ge), reads=rd, writes=[br])
                    S.op("dve", lambda e, r=r: e.tensor_tensor(out=r["dd"][:, :], in0=r["t2"][:, :], in1=r["t1"][:, :], op=ALU.subtract), reads=rd, writes=[br])
                    S.op("act", lambda e, r=r: e.activation(out=r["ed"][:, :], in_=r["dd"][:, :], func=AF.Exp), reads=rd, writes=[br])
                    S.op("dve", lambda e, r=r: e.tensor_scalar(out=r["w1"][:, :], in0=r["ed"][:, :], scalar1=1.0, scalar2=None, op0=ALU.add), reads=rd, writes=[br])
                    S.op("dve", lambda e, r=r: e.reciprocal(out=r["w1"][:, :], in_=r["w1"][:, :]), reads=rd, writes=[br])
                    S.op("dve", lambda e, r=r: e.tensor_tensor(out=r["w2"][:, :], in0=r["ed"][:, :], in1=r["w1"][:, :], op=ALU.mult), reads=rd, writes=[br])
                    S.op("dve", lambda e, r=r: e.tensor_tensor(out=r["w1"][:, :], in0=r["w1"][:, :], in1=r["pg"][:, :], op=ALU.mult), reads=rd, writes=[br])
                    S.op("dve", lambda e, r=r: e.tensor_tensor(out=r["w2"][:, :], in0=r["w2"][:, :], in1=r["pg"][:, :], op=ALU.mult), reads=rd, writes=[br])
                    S.op("dve", lambda e, r=r: e.tensor_scalar(out=r["comb"][:, :], in0=r["m1"][:, :], scalar1=r["w1"][:, 0:1], scalar2=None, op0=ALU.mult), reads=rd, writes=[br])
                    S.op("dve", lambda e, r=r: e.scalar_tensor_tensor(out=r["comb"][:, :], in0=r["m2"][:, :], scalar=r["w2"][:, 0:1], in1=r["comb"][:, :], op0=ALU.mult, op1=ALU.add), reads=rd, writes=[br])
                    if debug:
                        S.dma("sp", lambda e, r=r, r0=r0: e.dma_start(out=dbg["comb"][r0:r0 + 128, :], in_=r["comb"][:, :]), reads=[br], writes=[b_out])
                    S.op("pe", lambda e, r=r, t4=t4: e.transpose(out=ctp[0:32, t4 * 128:(t4 + 1) * 128], in_=r["comb"][:, 0:32], identity=ident[:]), reads=[br, b_const], writes=[ctb])
                S.op("act", lambda e: e.activation(out=cts[:, :], in_=ctp[0:32, :], func=AF.Copy), reads=[ctb], writes=[b_cts])
                S.dma("sp", lambda e, m=m: e.dma_start(out=combt_d[:, m * GT:(m + 1) * GT], in_=cts[:, :]), reads=[b_cts], writes=[b_combt_d])
                S.dma("sp", lambda e, m=m: e.dma_start(out=xgt_d[:, :, m * GT:(m + 1) * GT], in_=xgs[:]), reads=[b_xgs], writes=[b_xgt_d])
            PS.release(ct_idx)
            S.barrier()

        if stop_after not in ("A1", "A", "B", "C", "C1", "D") and "E" in phases:
          with ExitStack() as pe_:
            yacc = sb(pe_, "yacc", [128, 16, D]); b_yacc = [Buf() for _ in range(16)]
            S.dma("sp", lambda e: e.dma_start(out=yacc[:], in_=hmid_d.rearrange("(t p) f -> p t f", p=128)), reads=[b_hmid_d], writes=b_yacc)
            xg = sb(pe_, "xg", [128, 8, TOWN], BF16); b_xg = Buf()
            S.dma("sp", lambda e: e.dma_start(out=xg[:], in_=xgt_d), reads=[b_xgt_d], writes=[b_xg])
            combT = sb(pe_, "combT", [32, TOWN]); b_combT = Buf()
            S.dma("sp", lambda e: e.dma_start(out=combT[:], in_=combt_d), reads=[b_combt_d], writes=[b_combT])
            sel = [sb(pe_, "sel%d" % i, [32, 128]) for i in range(2)]; b_sel = [Buf(), Buf()]
            wgs2 = [sb(pe_, "wgs%d" % i, [128, 8, DEXP]) for i in range(2)]; wus2 = [sb(pe_, "wus%d" % i, [128, 8, DEXP]) for i in range(2)]
            wds2 = [sb(pe_, "wds%d" % i, [128, 2, D]) for i in range(2)]
            b_wgs2 = [Buf(), Buf()]; b_wus2 = [Buf(), Buf()]; b_wds2 = [Buf(), Buf()]
            wgb = [sb(pe_, "wgb%d" % i, [128, 8, DEXP], BF16) for i in range(2)]
            wub = [sb(pe_, "wub%d" % i, [128, 8, DEXP], BF16) for i in range(2)]
            wdb = [sb(pe_, "wdb%d" % i, [128, 2, D], BF16) for i in range(2)]
            b_wgb = [Buf(), Buf()]; b_wub = [Buf(), Buf()]; b_wdb = [Buf(), Buf()]
            cb = sb(pe_, "cb", [128, GT]); b_cb = Buf()
            sg = [sb(pe_, "sg%d" % i, [128, GT]) for i in range(2)]; b_sg = [Buf(), Buf()]
            tu = [sb(pe_, "tu%d" % i, [128, GT]) for i in range(2)]; b_tu = [Buf(), Buf()]
            hb = [sb(pe_, "hb%d" % i, [128, 2, GT], BF16) for i in range(2)]; b_hb = [Buf(), Buf()]
            n_exp = NEXP if stop_after != "E1" else 2
            for ex in range(n_exp):
                w = ex % 2
                wgs = wgs2[w]; wus = wus2[w]; wds = wds2[w]; b_wgs = b_wgs2[w]; b_wus = b_wus2[w]; b_wds = b_wds2[w]
                S.dma("sp", lambda e, ex=ex, wgs=wgs: e.dma_start(out=wgs[:], in_=wg_d[ex].rearrange("(kc p) j -> p kc j", p=128)), writes=[b_wgs])
                S.dma("sp", lambda e, ex=ex, wus=wus: e.dma_start(out=wus[:], in_=wu_d[ex].rearrange("(kc p) j -> p kc j", p=128)), writes=[b_wus])
                S.dma("sp", lambda e, ex=ex, wds=wds: e.dma_start(out=wds[:], in_=wd_d[ex].rearrange("(jc p) n -> p jc n", p=128)), writes=[b_wds])
                S.op("pool", lambda e, w=w, wgs=wgs: e.tensor_copy(out=wgb[w][:], in_=wgs[:]), reads=[b_wgs], writes=[b_wgb[w]])
                S.op("pool", lambda e, w=w, wus=wus: e.tensor_copy(out=wub[w][:], in_=wus[:]), reads=[b_wus], writes=[b_wub[w]])
                S.op("act", lambda e, w=w, wds=wds: e.activation(out=wdb[w][:], in_=wds[:], func=AF.Copy), reads=[b_wds], writes=[b_wdb[w]])
                S.op("dve", lambda e, w=w, ex=ex: e.tensor_copy(out=sel[w][:, :], in_=ident[0:32, ex:ex + 1].to_broadcast([32, 128])), reads=[b_const], writes=[b_sel[w]])
                for m in range(NOWN):
                    cs = slice(m * GT, (m + 1) * GT)
                    h_ = hb[m % 2]; bh_ = b_hb[m % 2]
                    pt, pb = PS.next()
                    S.op("pe", lambda e, pt=pt, w=w, cs=cs: e.matmul(pt[:, :], lhsT=sel[w][:, :], rhs=combT[:, cs], start=True, stop=True), reads=[b_sel[w], b_combT], writes=[pb])
                    S.op("act", lambda e, pt=pt: e.activation(out=cb[:, :], in_=pt[:, :], func=AF.Copy), reads=[pb], writes=[b_cb])
                    for jc in range(2):
                        s_ = jc
                        ptg, pbg = PS.next()
                        for kc in range(8):
                            S.op("pe", lambda e, ptg=ptg, kc=kc, jc=jc, w=w, cs=cs: e.matmul(ptg[:, :], lhsT=wgb[w][:, kc, jc * 128:(jc + 1) * 128], rhs=xg[:, kc, cs], start=(kc == 0), stop=(kc == 7)),
                                 reads=[b_wgb[w], b_xg], writes=[pbg])
                        ptu, pbu = PS.next()
                        for kc in range(8):
                            S.op("pe", lambda e, ptu=ptu, kc=kc, jc=jc, w=w, cs=cs: e.matmul(ptu[:, :], lhsT=wub[w][:, kc, jc * 128:(jc + 1) * 128], rhs=xg[:, kc, cs], start=(kc == 0), stop=(kc == 7)),
                                 reads=[b_wub[w], b_xg], writes=[pbu])
                        S.op("act", lambda e, ptg=ptg, s_=s_: e.activation(out=sg[s_][:, :], in_=ptg[:, :], func=AF.Silu), reads=[pbg], writes=[b_sg[s_]])
                        S.op("dve", lambda e, ptu=ptu, s_=s_: e.tensor_tensor(out=tu[s_][:, :], in0=ptu[:, :], in1=sg[s_][:, :], op=ALU.mult), reads=[pbu, b_sg[s_]], writes=[b_tu[s_]])
                        S.op("dve", lambda e, s_=s_, jc=jc, h_=h_: e.tensor_tensor(out=h_[:, jc, :], in0=tu[s_][:, :], in1=cb[:, :], op=ALU.mult), reads=[b_tu[s_], b_cb], writes=[bh_])
                    for t4 in range(4):
                        tt = m * 4 + t4
                        for nh in range(2):
                            pty, pby = PS.next()
                            for jc in range(2):
                                S.op("pe", lambda e, pty=pty, jc=jc, t4=t4, nh=nh, w=w, h_=h_: e.matmul(pty[:, :], lhsT=h_[:, jc, t4 * 128:(t4 + 1) * 128], rhs=wdb[w][:, jc, nh * 512:(nh + 1) * 512], start=(jc == 0), stop=(jc == 1)),
                                     reads=[bh_, b_wdb[w]], writes=[pby])
                            S.op("dve", lambda e, pty=pty, tt=tt, nh=nh: e.tensor_tensor(out=yacc[:, tt, nh * 512:(nh + 1) * 512], in0=pty[:, :], in1=yacc[:, tt, nh * 512:(nh + 1) * 512], op=ALU.add),
                                 reads=[pby, b_yacc[tt]], writes=[b_yacc[tt]])
            S.dma("sp", lambda e: e.dma_start(out=out_d.rearrange("(t p) f -> p t f", p=128), in_=yacc[:]), reads=b_yacc, writes=[b_out])
            S.barrier()

        S.barrier()
        for e_ in ("sp", "pool"):
            pass
        S.emit()
    return nc


b_out = Buf("out")


def _pcol(v, nchunk):
    return np.ascontiguousarray(np.asarray(v, np.float32).reshape(nchunk, 128).T)


def _prep_common(inp):
    f = lambda k: np.asarray(inp[k], np.float32)[0]
    pp = np.zeros((128, PPW), np.float32)
    pp[:, PP["gmix"]:PP["gmix"] + 8] = _pcol(f("norm_mix_g"), 8)
    cw = f("conv_w")
    pp[:, PP["convw"]:PP["convw"] + 32] = cw.reshape(4, 8, 128).transpose(2, 1, 0).reshape(128, 32)
    pp[:, PP["convb"]:PP["convb"] + 8] = _pcol(f("conv_b"), 8)
    pp[:, PP["ba"]:PP["ba"] + 8] = _pcol(f("lru_ba"), 8)
    pp[:, PP["bx"]:PP["bx"] + 8] = _pcol(f("lru_bx"), 8)
    pp[:, PP["lam"]:PP["lam"] + 8] = _pcol(f("lru_lambda"), 8)
    pp[:, PP["qag"]:PP["qag"] + 6] = _pcol(f("q_a_g"), 6)
    pp[:, PP["kvag"]:PP["kvag"] + 2] = _pcol(f("kv_a_g"), 2)
    pp[0:96, PP["qng"]] = f("q_norm_g")
    kng = f("k_norm_g")
    pp[0:64, PP["gkfold"]] = kng[0:64]
    pp[64:96, PP["gkfold"]] = 1.0
    pp[0:32, PP["gkpe"]] = kng[64:96]
    freq = (10000.0 ** (-np.arange(16, dtype=np.float32) / 16.0)).astype(np.float32)
    fr32 = np.concatenate([freq, freq])
    pp[0:32, PP["freq64"]] = fr32
    pp[32:64, PP["freq64"]] = fr32
    pp[32:64, PP["phase64"]] = np.float32(math.pi / 2)
    pp[0:64, PP["blockones"]] = 1.0
    pp[64:128, PP["blockones"] + 1] = 1.0
    pp[:, PP["gffn"]:PP["gffn"] + 8] = _pcol(f("norm_ffn_g"), 8)
    ident = np.eye(128, dtype=np.float32)
    rot32 = np.zeros((32, 32), np.float32)
    for m in range(16):
        rot32[m + 16, m] = -1.0
        rot32[m, m + 16] = 1.0
    rot96 = np.zeros((96, 96), np.float32)
    rot96[64:96, 64:96] = rot32
    wr = np.concatenate([f("router_group_w"), f("router_expert_w")], axis=1)
    rb = np.concatenate([f("router_group_b"), f("router_expert_b")])[None, :]
    rbias = np.ascontiguousarray(np.broadcast_to(rb, (128, 36))).astype(np.float32)
    gffnb = np.ascontiguousarray(np.broadcast_to(f("norm_ffn_g")[None, :], (128, D))).astype(np.float32)
    common = dict(gffnb=gffnb, ident=ident, rot96=rot96, rot32=rot32, rbias=rbias, w_in=f("w_in"), lru_wa=f("lru_wa"),
                  lru_wx=f("lru_wx"), w_uq=f("w_uq"), w_ukv=f("w_ukv"), w_out=f("w_out"), w_router=np.ascontiguousarray(wr),
                  w_gate=f("w_gate"), w_up=f("w_up"), w_down=f("w_down"))
    return pp, common


def _masks_for(c):
    m = np.zeros((128, 32, GT), np.float32)
    q = np.arange(GT)[None, :]
    for cp in range(8):
        for i in range(4):
            j = cp * 4 + i
            if cp < c:
                m[:, j, :] = 1.0
            elif cp == c:
                m[0:64, j, :] = (q >= 128 * i)
                m[64:128, j, :] = (q >= 128 * i + 64)
    return m.astype(ml_dtypes.bfloat16)


def make_in_maps(inp, names=None):
    pp, common = _prep_common(inp)
    x = np.asarray(inp["x"], np.float32)[0]
    pos = np.asarray(inp["positions"], np.int32)[0]
    posb_all = np.ascontiguousarray(np.broadcast_to(pos[None, :], (64, SEQ)))
    maps = []
    for c in range(NCORES):
        rows = np.concatenate([np.arange((8 * m + c) * GT, (8 * m + c + 1) * GT) for m in range(NOWN)])
        ppc_ = pp.copy()
        ppc_[:, PP["onehot"] + c] = 1.0
        d = dict(common)
        d.update(x_all=x, x_own=np.ascontiguousarray(x[rows]), posb_all=posb_all,
                 posb_own=np.ascontiguousarray(posb_all[:, rows]), pp=ppc_, masks=_masks_for(c))
        if names is not None:
            d = {k: d[k] for k in names}
        maps.append(d)
    return maps


def own_rows(c):
    return np.concatenate([np.arange((8 * m + c) * GT, (8 * m + c + 1) * GT) for m in range(NOWN)])


def kernel(**inputs):
    nc = build_program()
    maps = make_in_maps(inputs, nc._declared_inputs)
    res = run_bass_kernel_spmd(nc, maps, core_ids=list(range(NCORES)))
    out = np.zeros((1, SEQ, D), np.float32)
    for c in range(NCORES):
        out[0, own_rows(c)] = res.results[c]["out"]
    return out
```

```python
import os
import math
import numpy as np
import ml_dtypes
from contextlib import ExitStack
import concourse.bass as bass
import concourse.mybir as mybir
from concourse.bass_utils import run_bass_kernel_spmd

F32 = mybir.dt.float32
BF16 = mybir.dt.bfloat16
I32 = mybir.dt.int32
ALU = mybir.AluOpType
AF = mybir.ActivationFunctionType
AX = mybir.AxisListType

NCORES = 8
SEQ = 16384
D = 1024
GT = 512
NG = SEQ // GT
NOWN = 4
TOWN = NOWN * GT
EPS = 1e-6
TWO_PI = 2.0 * math.pi
C1 = 6.28125
C2 = TWO_PI - C1
NEXP = 32
DEXP = 256

PP = {}
_o = 0
for _n, _w in [("gmix", 8), ("convw", 32), ("convb", 8), ("ba", 8), ("bx", 8), ("lam", 8), ("qag", 6),
               ("kvag", 2), ("onehot", 8), ("qng", 1), ("gkfold", 1), ("gkpe", 1), ("freq64", 1),
               ("blockones", 2), ("gffn", 8), ("phase64", 1)]:
    PP[_n] = _o
    _o += _w
PPW = _o


class Buf:
    __slots__ = ("name", "last_w", "readers")

    def __init__(self, name=""):
        self.name = name
        self.last_w = None
        self.readers = []


class Sched:
    ENGS = ("sp", "pe", "act", "dve", "pool")
    DUR = {"pe": 0.25, "act": 0.62, "dve": 0.70, "pool": 1.1}

    def __init__(self, nc, stack, n_dma_sems=32, strict=True):
        self.nc = nc
        self.strict = strict
        self.esem = {e: stack.enter_context(nc.semaphore("prog_" + e)) for e in self.ENGS}
        self.dsems = [stack.enter_context(nc.semaphore("dma_%d" % i)) for i in range(n_dma_sems)]
        self.ops = []
        self.seg = 0

    def _add(self, kind, eng, fn, reads, writes, dur):
        oid = len(self.ops)
        deps = set()
        for b in reads:
            if b.last_w is not None:
                deps.add(b.last_w)
        for b in writes:
            if b.last_w is not None:
                deps.add(b.last_w)
            deps.update(b.readers)
        self.ops.append([kind, eng, fn, sorted(deps), dur, self.seg])
        for b in reads:
            b.readers.append(oid)
            if len(b.readers) > 700:
                b.readers = b.readers[-700:]
        for b in writes:
            b.last_w = oid
            b.readers = []
        return oid

    class _Probe:
        def __init__(self):
            self.name = None
            self.kw = {}

        def __getattr__(self, name):
            def f(*a, **kw):
                self.name = name
                self.kw = kw
                return self
            return f

    def _estimate(self, kind, eng, fn):
        try:
            p = Sched._Probe()
            fn(p)
            kw = p.kw
            if kind == "dma":
                o = kw.get("out")
                if o is None:
                    return 3.0
                n = 1
                for d_ in o.shape:
                    n *= int(d_)
                return 2.0 + n * mybir.dt.size(o.dtype) / 150e3
            o = kw.get("out")
            if eng == "pe":
                if p.name == "transpose":
                    return 0.09
                rhs = kw.get("rhs")
                nfree = 1
                for d_ in rhs.shape[1:]:
                    nfree *= int(d_)
                t = 0.03 + max(nfree, 64) / 2400.0
                if rhs.dtype == F32:
                    t *= 4.0
                return t
            nfree = 1
            for d_ in o.shape[1:]:
                nfree *= int(d_)
            if eng == "act":
                return 0.06 + nfree / 960.0 + (0.1 if kw.get("accum_out") is not None else 0.0)
            i0 = kw.get("in0", kw.get("in_", kw.get("data0", None)))
            b = 4
            try:
                b = max(mybir.dt.size(o.dtype), mybir.dt.size(i0.dtype)) if i0 is not None else mybir.dt.size(o.dtype)
            except Exception:
                pass
            t = 0.08 + nfree * (1.12e-3 if b >= 4 else 0.8e-3)
            if p.name == "tensor_tensor_scan":
                t = 0.08 + nfree * 2.1e-3
            if eng == "pool":
                t *= 1.8
            return t
        except Exception:
            return 3.0 if kind == "dma" else self.DUR[eng]

    def op(self, eng, fn, reads=(), writes=(), dur=None):
        return self._add("op", eng, fn, reads, writes, self._estimate("op", eng, fn) if dur is None else dur)

    def dma(self, eng, fn, reads=(), writes=(), dur=None):
        return self._add("dma", eng, fn, reads, writes, self._estimate("dma", eng, fn) if dur is None else dur)

    def barrier(self):
        self.seg += 1

    def _schedule_segment(self, ids, done_before):
        import heapq
        ops = self.ops
        idset = set(ids)
        ndeps = {}
        users = {}
        for i in ids:
            c = 0
            for d in ops[i][3]:
                if d in idset:
                    c += 1
                    users.setdefault(d, []).append(i)
            ndeps[i] = c
        blevel = {}
        for i in reversed(ids):
            m_ = 0.0
            for u in users.get(i, ()):
                if blevel[u] > m_:
                    m_ = blevel[u]
            blevel[i] = ops[i][4] + m_
        finish = {}
        eng_free = {e: 0.0 for e in self.ENGS}
        ready = {e: [] for e in self.ENGS}
        avail = {e: [] for e in self.ENGS}
        for i in ids:
            if ndeps[i] == 0:
                heapq.heappush(ready[ops[i][1]], (0.0, i))
        order = []
        n = len(ids)
        LAT = 0.12
        while len(order) < n:
            best = None
            for e in self.ENGS:
                h = ready[e]
                while h and h[0][0] <= eng_free[e]:
                    rt, i = heapq.heappop(h)
                    heapq.heappush(avail[e], (-blevel[i], i))
                if avail[e]:
                    cand = (eng_free[e], 0, e)
                elif h:
                    cand = (h[0][0], 1, e)
                else:
                    continue
                if best is None or cand < best:
                    best = cand
            st, which, e = best
            if which == 0:
                _, i = heapq.heappop(avail[e])
            else:
                st, i = heapq.heappop(ready[e])
            kind, _, _, _, dur, _ = ops[i]
            if kind == "dma":
                eng_free[e] = st + 0.08
                fin = st + dur
            else:
                eng_free[e] = st + dur
                fin = st + dur
            finish[i] = fin
            order.append(i)
            for u in users.get(i, ()):
                ndeps[u] -= 1
                if ndeps[u] == 0:
                    rt = 0.0
                    for d in ops[u][3]:
                        if d in finish:
                            lat = LAT if ops[d][1] != ops[u][1] else 0.05
                            rt = max(rt, finish[d] + lat)
                    heapq.heappush(ready[ops[u][1]], (rt, u))
        return order

    def emit(self):
        ops = self.ops
        nseg = self.seg + 1
        segs = [[] for _ in range(nseg)]
        for i, o in enumerate(ops):
            segs[o[5]].append(i)
        order = []
        for k in range(nseg):
            if segs[k]:
                order += self._schedule_segment(segs[k], None)
        ecount = {e: 0 for e in self.ENGS}
        dcount = [0] * len(self.dsems)
        dnext = 0
        tok = {}
        prev_dma_tok = {}
        for i in order:
            kind, eng = ops[i][0], ops[i][1]
            if kind == "op":
                ecount[eng] += 1
                tok[i] = (("e", eng), ecount[eng])
            else:
                j = dnext
                dnext = (dnext + 1) % len(self.dsems)
                if dcount[j] > 0:
                    prev_dma_tok[i] = (("d", j), dcount[j])
                dcount[j] += 16
                tok[i] = (("d", j), dcount[j])
        semobj = {}
        for e in self.ENGS:
            semobj[("e", e)] = self.esem[e]
        for j, s_ in enumerate(self.dsems):
            semobj[("d", j)] = s_
        prog = {e: [] for e in self.ENGS}
        waited = {e: {} for e in self.ENGS}
        last_seg = {e: 0 for e in self.ENGS}
        seg_tokens = []

        def need(eng, t):
            key, val = t
            if key == ("e", eng) and (eng == "pe" or not self.strict):
                return
            if val > waited[eng].get(key, 0):
                waited[eng][key] = val
                prog[eng].append(("w", semobj[key], val))

        seg_end = []
        cur = {}
        pos = 0
        for k in range(nseg):
            for _ in segs[k]:
                i = order[pos]; pos += 1
                key, val = tok[i]
                cur[key] = max(cur.get(key, 0), val)
            seg_end.append(dict(cur))
        for i in order:
            kind, eng, fn, deps, dur, sg = ops[i]
            if sg > last_seg[eng]:
                for key, val in seg_end[sg - 1].items():
                    need(eng, (key, val))
                last_seg[eng] = sg
            for d in deps:
                need(eng, tok[d])
            if i in prev_dma_tok:
                need(eng, prev_dma_tok[i])
            key, val = tok[i]
            prog[eng].append(("i", fn, semobj[key], 1 if kind == "op" else 16))
        for e in self.ENGS:
            for key, val in seg_end[-1].items():
                need(e, (key, val))
        names = {"sp": "sync", "pe": "tensor", "act": "scalar", "dve": "vector", "pool": "gpsimd"}
        with self.nc.Block() as block:
            for e in self.ENGS:
                items = prog[e]
                if not items:
                    continue

                def body(engobj, items=items):
                    for it in items:
                        if it[0] == "w":
                            engobj.wait_ge(it[1], it[2])
                        else:
                            it[1](engobj).then_inc(it[2], it[3])

                getattr(block, names[e])(body)


class PsumRot:
    def __init__(self, tiles):
        self.tiles = tiles
        self.free = list(range(len(tiles)))
        self.i = 0

    def reserve(self, n):
        r = [self.free.pop() for _ in range(n)]
        return [self.tiles[k] for k in r], r

    def release(self, idxs):
        self.free.extend(idxs)

    def next(self):
        k = self.free[self.i % len(self.free)]
        self.i += 1
        return self.tiles[k]


def build_program(debug=False, stop_after=None, phases="AB12CDE"):
    nc = bass.Bass("TRN2", target_bir_lowering=False)
    declared = []
    nc._declared_inputs = declared

    def din(name, shape, dt=F32, ph=None):
        if ph is not None and not any(p in phases for p in ph):
            return None
        declared.append(name)
        return nc.dram_tensor(name, list(shape), dt, kind="ExternalInput").ap()

    def dscr(name, shape, dt=F32):
        return nc.dram_tensor(name, list(shape), dt).ap()

    x_all = din("x_all", [SEQ, D], ph="A")
    x_own = din("x_own", [TOWN, D], ph="12D")
    posb_all = din("posb_all", [64, SEQ], I32, ph="A")
    posb_own = din("posb_own", [64, TOWN], I32, ph="A")
    pp_d = din("pp", [128, PPW])
    ident_d = din("ident", [128, 128])
    rot96_d = din("rot96", [96, 96])
    rot32_d = din("rot32", [32, 32])
    masks_d = din("masks", [128, 32, GT], BF16, ph="C")
    rbias_d = din("rbias", [128, 36], ph="D")
    gffnb_d = din("gffnb", [128, D], ph="D")
    w_in_d = din("w_in", [D, 5152], ph="A12")
    wa_d = din("lru_wa", [4, 256, 256], ph="A")
    wx_d = din("lru_wx", [4, 256, 256], ph="A")
    wuq_d = din("w_uq", [768, 1536], ph="2")
    wukv_d = din("w_ukv", [256, 2048], ph="A")
    wout_d = din("w_out", [D, D], ph="D")
    wr_d = din("w_router", [D, 36], ph="D")
    wg_d = din("w_gate", [NEXP, D, DEXP], ph="E")
    wu_d = din("w_up", [NEXP, D, DEXP], ph="E")
    wd_d = din("w_down", [NEXP, DEXP, D], ph="E")
    out_d = nc.dram_tensor("out", [TOWN, D], F32, kind="ExternalOutput").ap()
    dbg = {}
    if debug:
        for nm, shp in [("hl", [D, TOWN]), ("yb", [D, TOWN]), ("hmid", [TOWN, D]), ("qt", [96, 16 * TOWN]),
                        ("comb", [TOWN, 32])]:
            dbg[nm] = nc.dram_tensor("dbg_" + nm, shp, F32, kind="ExternalOutput").ap()
        dbg["hlb"] = nc.dram_tensor("dbg_hlb", [D, TOWN], BF16, kind="ExternalOutput").ap()
        dbg["rk"] = nc.dram_tensor("dbg_rk", [128, 2048], F32, kind="ExternalOutput").ap()
        dbg["ktn"] = nc.dram_tensor("dbg_ktn", [128, 512], BF16, kind="ExternalOutput").ap()
        dbg["kpe"] = nc.dram_tensor("dbg_kpe", [32, 512], BF16, kind="ExternalOutput").ap()
        dbg["v"] = nc.dram_tensor("dbg_v", [128, 512], BF16, kind="ExternalOutput").ap()

    ktn_d = dscr("ktn", [8 * 128, SEQ], BF16)
    kpe_d = dscr("kpe", [32, SEQ], BF16)
    v_d = dscr("vaug", [16, NG, 128, 4 * 128], BF16)
    ownh_d = dscr("ownh", [128, NOWN * 8 * GT], BF16)
    rk_d = dscr("rk", [128, (SEQ // 128) * 16])
    gaya_d = dscr("gaya", [128, 8, TOWN], BF16)
    sgb_d = dscr("sgb", [128, 8, TOWN], BF16)
    mg_d = dscr("mg", [128, 8, TOWN], BF16)
    qt_d = dscr("qt", [96, 16, TOWN], BF16)
    hmid_d = dscr("hmid", [TOWN, D])
    xgt_d = dscr("xgt", [128, 8, TOWN], BF16)
    combt_d = dscr("combt", [32, TOWN])
    kcs_d = dscr("kcs", [64, SEQ])
    qcs_d = dscr("qcs", [64, TOWN])

    with ExitStack() as top:
        S = Sched(nc, top)
        sb = lambda st, name, shape, dt=F32: st.enter_context(nc.sbuf_tensor("sb_" + name, list(shape), dt))

        ps_tiles = []
        for i in range(8):
            t = top.enter_context(nc.psum_tensor("ps%d" % i, [128, 512], F32))
            ps_tiles.append((t, Buf("ps%d" % i)))
        PS = PsumRot(ps_tiles)

        pp = sb(top, "pp_sb", [128, PPW]); b_pp = Buf("pp")
        ident = sb(top, "ident_sb", [128, 128])
        rot96 = sb(top, "rot96_sb", [96, 96])
        rot32 = sb(top, "rot32_sb", [32, 32])
        ones_f = sb(top, "ones_f", [128, 128])
        b_const = Buf("const")
        b_rk = Buf("rk"); b_ownh = Buf("ownh")
        b_rk_d = Buf("rk_d"); b_ownh_d = Buf("ownh_d"); b_gaya_d = Buf(); b_sgb_d = Buf(); b_mg_d = Buf(); b_qt_d = Buf()
        b_hmid_d = Buf(); b_xgt_d = Buf(); b_combt_d = Buf()
        b_ktn_d = Buf("ktn_d"); b_kpe_d = Buf("kpe_d"); b_v_d = Buf("v_d")
        c12 = sb(top, "c12", [128, 40]); b_c12 = Buf("c12")

        S.dma("sp", lambda e: e.dma_start(out=pp[:], in_=pp_d), writes=[b_pp])
        S.dma("sp", lambda e: e.dma_start(out=ident[:], in_=ident_d), writes=[b_const])
        S.dma("sp", lambda e: e.dma_start(out=rot96[:], in_=rot96_d), writes=[b_const])
        S.dma("sp", lambda e: e.dma_start(out=rot32[:], in_=rot32_d), writes=[b_const])
        S.op("dve", lambda e: e.memset(ones_f[:], 1.0), writes=[b_const])

        def ppc(name, j=0, rows=128, w=1):
            o = PP[name] + j
            return pp[0:rows, o:o + w]

        lam_ap = ppc("lam", 0, 128, 8)
        S.op("act", lambda e: e.activation(out=c12[:, 0:8], in_=lam_ap, func=AF.Exp, scale=-1.0), reads=[b_pp], writes=[b_c12])
        S.op("act", lambda e: e.activation(out=c12[:, 0:8], in_=c12[:, 0:8], func=AF.Ln, bias=1.0), reads=[b_c12], writes=[b_c12])
        S.op("dve", lambda e: e.tensor_scalar(out=c12[:, 8:16], in0=c12[:, 0:8], scalar1=-16.0, scalar2=None, op0=ALU.mult), reads=[b_c12], writes=[b_c12])
        S.op("dve", lambda e: e.tensor_scalar(out=c12[:, 16:24], in0=c12[:, 0:8], scalar1=-4.0, scalar2=None, op0=ALU.mult), reads=[b_c12], writes=[b_c12])
        S.op("dve", lambda e: e.tensor_scalar(out=c12[:, 0:8], in0=c12[:, 0:8], scalar1=-8.0, scalar2=None, op0=ALU.mult), reads=[b_c12], writes=[b_c12])
        S.op("dve", lambda e: e.tensor_scalar(out=c12[:, 24:32], in0=ppc("ba", 0, 128, 8), scalar1=0.5, scalar2=None, op0=ALU.mult), reads=[b_pp], writes=[b_c12])
        S.op("dve", lambda e: e.tensor_scalar(out=c12[:, 32:40], in0=ppc("bx", 0, 128, 8), scalar1=0.5, scalar2=None, op0=ALU.mult), reads=[b_pp], writes=[b_c12])

        def sincos_tables(st, posb_ap, ntok, dst_d, tag):
            CH = 2048
            pi_ = sb(st, tag + "_pi", [64, CH], I32)
            ang = sb(st, tag + "_ang", [64, CH])
            kf = sb(st, tag + "_kf", [64, CH])
            ki = sb(st, tag + "_ki", [64, CH], I32)
            msk = sb(st, tag + "_m", [64, CH])
            b = [Buf() for _ in range(5)]
            fr = ppc("freq64", 0, 64); ph = ppc("phase64", 0, 64)
            for c0 in range(0, ntok, CH):
                S.dma("sp", lambda e, c0=c0: e.dma_start(out=pi_[:], in_=posb_ap[:, c0:c0 + CH]), writes=[b[0]])
                S.op("dve", lambda e: e.tensor_copy(out=ang[:], in_=pi_[:]), reads=[b[0]], writes=[b[1]])
                S.op("dve", lambda e: e.tensor_scalar(out=ang[:], in0=ang[:], scalar1=fr, scalar2=ph, op0=ALU.mult, op1=ALU.add),
                     reads=[b[1], b_pp], writes=[b[1]])
                S.op("dve", lambda e: e.tensor_scalar(out=kf[:], in0=ang[:], scalar1=1.0 / TWO_PI, scalar2=None, op0=ALU.mult),
                     reads=[b[1]], writes=[b[2]])
                S.op("dve", lambda e: e.tensor_copy(out=ki[:], in_=kf[:]), reads=[b[2]], writes=[b[3]])
                S.op("dve", lambda e: e.tensor_copy(out=kf[:], in_=ki[:]), reads=[b[3]], writes=[b[2]])
                S.op("dve", lambda e: e.scalar_tensor_tensor(out=ang[:], in0=kf[:], scalar=-C1, in1=ang[:], op0=ALU.mult, op1=ALU.add),
                     reads=[b[2], b[1]], writes=[b[1]])
                S.op("dve", lambda e: e.scalar_tensor_tensor(out=ang[:], in0=kf[:], scalar=-C2, in1=ang[:], op0=ALU.mult, op1=ALU.add),
                     reads=[b[2], b[1]], writes=[b[1]])
                for thr, cmp_, corr in ((math.pi, ALU.is_gt, -TWO_PI), (-math.pi, ALU.is_lt, TWO_PI)):
                    S.op("dve", lambda e, thr=thr, cmp_=cmp_: e.tensor_single_scalar(out=msk[:], in_=ang[:], scalar=thr, op=cmp_),
                         reads=[b[1]], writes=[b[4]])
                    S.op("dve", lambda e, corr=corr: e.scalar_tensor_tensor(out=ang[:], in0=msk[:], scalar=corr, in1=ang[:], op0=ALU.mult, op1=ALU.add),
                         reads=[b[4], b[1]], writes=[b[1]])
                S.op("dve", lambda e: e.tensor_scalar(out=ang[:], in0=ang[:], scalar1=3.1415925, scalar2=-3.1415925, op0=ALU.min, op1=ALU.max),
                     reads=[b[1]], writes=[b[1]])
                S.op("act", lambda e: e.activation(out=kf[:], in_=ang[:], func=AF.Sin), reads=[b[1]], writes=[b[2]])
                S.dma("sp", lambda e, c0=c0: e.dma_start(out=dst_d[:, c0:c0 + CH], in_=kf[:]), reads=[b[2]], writes=[b_dram_cs])

        b_dram_cs = Buf("dram_cs")

        def load_norm_transpose(xsrc_ap, xt, bx_, ss, bss, uT, buT, t4, gcol=None):
            S.dma("sp", lambda e: e.dma_start(out=xt[:], in_=xsrc_ap), writes=[bx_])
            S.op("act", lambda e: e.activation(out=junk[:], in_=xt[:], func=AF.Square, accum_out=ss[:, 0:1]), reads=[bx_], writes=[bss, b_junk])
            S.op("act", lambda e: e.activation(out=ss[:, 1:2], in_=ss[:, 0:1], func=AF.Sqrt, scale=1.0 / D, bias=eps_t[:, 0:1]), reads=[bss], writes=[bss])
            S.op("dve", lambda e: e.reciprocal(out=ss[:, 2:3], in_=ss[:, 1:2]), reads=[bss], writes=[bss])
            S.op("dve", lambda e: e.tensor_scalar(out=xt[:], in0=xt[:], scalar1=ss[:, 2:3], scalar2=None, op0=ALU.mult), reads=[bss, bx_], writes=[bx_])
            for half in range(2):
                pt, pb = PS.next()
                for q in range(4):
                    kc = half * 4 + q
                    S.op("pe", lambda e, pt=pt, q=q, kc=kc: e.transpose(out=pt[:, q * 128:(q + 1) * 128], in_=xt[:, kc * 128:(kc + 1) * 128], identity=ident[:]),
                         reads=[bx_, b_const], writes=[pb])
                dst = uT[:, half * 4:half * 4 + 4, t4 * 128:(t4 + 1) * 128]
                src = pt[:].rearrange("p (q t) -> p q t", q=4)
                eng = "act" if half == 0 else "dve"
                if eng == "act":
                    S.op("act", lambda e, dst=dst, src=src: e.activation(out=dst, in_=src, func=AF.Copy), reads=[pb], writes=[buT])
                else:
                    S.op("dve", lambda e, dst=dst, src=src: e.tensor_copy(out=dst, in_=src), reads=[pb], writes=[buT])

        junk = sb(top, "junk", [128, D], BF16); b_junk = Buf("junk")
        eps_t = sb(top, "eps_t", [128, 1])
        eps96 = sb(top, "eps96", [128, 1])
        S.op("dve", lambda e: e.memset(eps_t[:], EPS), writes=[b_const])
        S.op("dve", lambda e: e.memset(eps96[:], 96.0 * EPS), writes=[b_const])

        def load_weight_cols(st, dst_bf, bdst, src_d, col_ranges, nk, scale_name, tag, stage_w):
            stg = [sb(st, "%s_stg%d" % (tag, i), [128, stage_w]) for i in range(2)]
            bst = [Buf(), Buf()]
            for kc in range(nk):
                s_ = stg[kc % 2]; bs_ = bst[kc % 2]
                o = 0
                for (a, b_) in col_ranges:
                    S.dma("sp", lambda e, s_=s_, o=o, a=a, b_=b_, kc=kc: e.dma_start(out=s_[:, o:o + (b_ - a)], in_=src_d[kc * 128:(kc + 1) * 128, a:b_]), writes=[bs_])
                    o += b_ - a
                eng = "dve" if kc % 2 == 0 else "pool"
                if scale_name is None:
                    S.op(eng, lambda e, s_=s_, kc=kc, o=o: e.tensor_copy(out=dst_bf[:, kc, 0:o], in_=s_[:, 0:o]), reads=[bs_], writes=[bdst])
                else:
                    sc = ppc(scale_name, kc)
                    S.op(eng, lambda e, s_=s_, kc=kc, o=o, sc=sc: e.tensor_scalar(out=dst_bf[:, kc, 0:o], in0=s_[:, 0:o], scalar1=sc, scalar2=None, op0=ALU.mult),
                         reads=[bs_, b_pp], writes=[bdst])

        with ExitStack() as pa:
          if "A" in phases:
            sincos_st = ExitStack()
            with sincos_st:
                sincos_tables(sincos_st, posb_all, SEQ, kcs_d, "csA")
                sincos_tables(sincos_st, posb_own, TOWN, qcs_d, "csO")
            S.barrier()

            rk_all = sb(pa, "rk_all", [128, SEQ // 128, 16])
            own_hb = [sb(pa, "own_h%d" % i, [128, 8, GT], BF16) for i in range(2)]; b_ownhb = [Buf(), Buf()]
            NA = 1024 + 256 + 32
            winA = sb(pa, "winA", [128, 8, NA], BF16); b_winA = Buf("winA")
            wab = sb(pa, "wab", [128, 8, 256], BF16); wxb = sb(pa, "wxb", [128, 8, 256], BF16)
            b_wab = Buf("wab")
            wukK = sb(pa, "wukK", [128, 2, 1024], BF16); wukV = sb(pa, "wukV", [128, 2, 1024], BF16)
            b_wuk = Buf("wuk")
            with ExitStack() as wp:
                load_weight_cols(wp, winA, b_winA, w_in_d, [(0, 1024), (2816, 3104)], 8, "gmix", "wA", NA)
                wst = sb(wp, "wa_stg", [128, 8, 256])
                b_wst = Buf()
                for (src, dstw) in ((wa_d, wab), (wx_d, wxb)):
                    S.dma("sp", lambda e, src=src: e.dma_start(out=wst[:], in_=src.rearrange("b (ic p) j -> p (b ic) j", p=128)), writes=[b_wst])
                    S.op("dve", lambda e, dstw=dstw: e.tensor_copy(out=dstw[:], in_=wst[:]), reads=[b_wst], writes=[b_wab])
                kst = sb(wp, "wukv_stg", [128, 2048]); b_kst = Buf()
                for kc in range(2):
                    S.dma("sp", lambda e, kc=kc: e.dma_start(out=kst[:], in_=wukv_d[kc * 128:(kc + 1) * 128, :]), writes=[b_kst])
                    sc = ppc("kvag", kc)
                    kv4 = kst[:].rearrange("p (h two d) -> p h two d", h=16, two=2)
                    S.op("dve", lambda e, kc=kc, sc=sc, kv4=kv4: e.tensor_scalar(out=wukK[:, kc, :].rearrange("p (h d) -> p h d", h=16), in0=kv4[:, :, 0, :], scalar1=sc, scalar2=None, op0=ALU.mult),
                         reads=[b_kst, b_pp], writes=[b_wuk])
                    S.op("dve", lambda e, kc=kc, sc=sc, kv4=kv4: e.tensor_scalar(out=wukV[:, kc, :].rearrange("p (h d) -> p h d", h=16), in0=kv4[:, :, 1, :], scalar1=sc, scalar2=None, op0=ALU.mult),
                         reads=[b_kst, b_pp], writes=[b_wuk])
                S.barrier()

            xt = [sb(pa, "xtA%d" % i, [128, D]) for i in range(2)]; b_xt = [Buf(), Buf()]
            ssA = [sb(pa, "ssA%d" % i, [128, 4]) for i in range(2)]; b_ss = [Buf(), Buf()]
            uT = [sb(pa, "uTA%d" % i, [128, 8, GT], BF16) for i in range(2)]; b_uT = [Buf(), Buf()]
            xr = sb(pa, "xr", [128, 8, GT + 3], BF16); b_xr = [Buf() for _ in range(8)]
            diagw = sb(pa, "diagw", [128, 8, 4, 128], BF16); b_diagw = Buf()
            xa = sb(pa, "xa", [128, 8, GT]); b_xa = [Buf() for _ in range(8)]
            xab = sb(pa, "xab", [128, 8, GT], BF16); b_xab = [Buf() for _ in range(8)]
            NT = 2
            tr = [sb(pa, "tr%d" % i, [128, GT]) for i in range(1)] * 2; b_tr = [Buf()] * 2
            ti = [sb(pa, "ti%d" % i, [128, GT]) for i in range(4)]; b_ti = [Buf() for _ in range(4)]
            ta = [sb(pa, "ta%d" % i, [128, GT]) for i in range(4)]; b_ta = [Buf() for _ in range(4)]
            tm = [sb(pa, "tm%d" % i, [128, GT]) for i in range(4)]; b_tm = [Buf() for _ in range(4)]
            hst = sb(pa, "hst", [128, 8]); b_hst = Buf("hst"); b_hst8 = [Buf() for _ in range(8)]
            b_oh8 = [[Buf() for _ in range(8)] for _ in range(2)]
            kvc = sb(pa, "kvc", [128, 2, GT]); b_kvc = Buf()
            kvsq = sb(pa, "kvsq", [128, 2, GT]); b_kvsq = Buf()
            kvn = sb(pa, "kvn", [128, 2, GT], BF16); b_kvn = Buf()
            rbc = sb(pa, "rbc", [128, GT]); b_rbc = Buf()
            kpr = sb(pa, "kpr", [32, GT]); b_kpr = Buf()
            kpsq = sb(pa, "kpsq", [32, GT]); b_kpsq = Buf()
            kpo = sb(pa, "kpo", [32, GT], BF16); b_kpo = Buf()
            kcs = sb(pa, "kcs_sb", [32, 2, GT]); b_kcs = Buf()
            ktn = [sb(pa, "ktn_sb%d" % i, [128, 8, GT], BF16) for i in range(1)] * 2; b_ktn = [Buf()] * 2
            ksq = [sb(pa, "ksq%d" % i, [128, GT]) for i in range(1)] * 2; b_ksq = [Buf()] * 2
            vau = sb(pa, "vau", [128, 16, 4, 128], BF16); b_vau = Buf()
            sstat = sb(pa, "sstat", [128, 16]); b_sstat = Buf()

            S.op("dve", lambda e: e.memset(xr[:], 0.0), writes=b_xr)
            for jc in range(8):
                for j in range(4):
                    S.op("dve", lambda e, jc=jc, j=j: e.tensor_scalar(out=diagw[:, jc, j, :], in0=ident[:, :], scalar1=ppc("convw", jc * 4 + j), scalar2=None, op0=ALU.mult),
                         reads=[b_const, b_pp], writes=[b_diagw])
            S.op("dve", lambda e: e.memset(hst[:], 0.0), writes=[b_hst] + b_hst8)
            S.op("pool", lambda e: e.memset(vau[:], 1.0), writes=[b_vau])

            n_groups_A = NG if stop_after != "A1" else 2
            (pst_l, pst_idx) = PS.reserve(1)
            pst, pbst = pst_l[0]
            def gen_H(G):
                u = uT[G % 2]; bu = b_uT[G % 2]
                for t4 in range(4):
                    i = (G * 4 + t4) % 2
                    r0 = G * GT + t4 * 128
                    load_norm_transpose(x_all[r0:r0 + 128, :], xt[i], b_xt[i], ssA[i], b_ss[i], u, bu, t4)
                    yield
            def emit_M(G):
                u = uT[G % 2]; bu = b_uT[G % 2]
                for jc in range(8):
                    pt, pb = PS.next()
                    for kc in range(8):
                        S.op("pe", lambda e, pt=pt, jc=jc, kc=kc, u=u: e.matmul(pt[:, :], lhsT=winA[:, kc, jc * 128:(jc + 1) * 128], rhs=u[:, kc, :], start=(kc == 0), stop=(kc == 7)),
                             reads=[b_winA, bu], writes=[pb])
                    S.op("act", lambda e, pt=pt, jc=jc: e.activation(out=xr[:, jc, 3:3 + GT], in_=pt[:, :], func=AF.Copy), reads=[pb], writes=[b_xr[jc]])
                for jc in range(2):
                    pt, pb = PS.next()
                    for kc in range(8):
                        S.op("pe", lambda e, pt=pt, jc=jc, kc=kc, u=u: e.matmul(pt[:, :], lhsT=winA[:, kc, 1024 + jc * 128:1024 + (jc + 1) * 128], rhs=u[:, kc, :], start=(kc == 0), stop=(kc == 7)),
                             reads=[b_winA, bu], writes=[pb])
                    S.op("act", lambda e, pt=pt, jc=jc: e.activation(out=kvc[:, jc, :], in_=pt[:, :], func=AF.Copy), reads=[pb], writes=[b_kvc])
                    S.op("dve", lambda e, pt=pt, jc=jc: e.tensor_tensor(out=kvsq[:, jc, :], in0=pt[:, :], in1=kvc[:, jc, :], op=ALU.mult), reads=[pb, b_kvc], writes=[b_kvsq])
                pt, pb = PS.next()
                for kc in range(8):
                    S.op("pe", lambda e, pt=pt, kc=kc, u=u: e.matmul(pt[0:32, :], lhsT=winA[:, kc, 1280:1312], rhs=u[:, kc, :], start=(kc == 0), stop=(kc == 7)),
                         reads=[b_winA, bu], writes=[pb])
                S.op("act", lambda e, pt=pt: e.activation(out=kpr[:, :], in_=pt[0:32, :], func=AF.Copy), reads=[pb], writes=[b_kpr])
                S.op("dve", lambda e, pt=pt: e.tensor_tensor(out=kpsq[:, :], in0=pt[0:32, :], in1=kpr[:, :], op=ALU.mult), reads=[pb, b_kpr], writes=[b_kpsq])
            def gen_KV(G):
                S.dma("sp", lambda e, G=G: e.dma_start(out=kcs[:, 0, :], in_=kcs_d[0:32, G * GT:(G + 1) * GT]), reads=[b_dram_cs], writes=[b_kcs])
                S.dma("sp", lambda e, G=G: e.dma_start(out=kcs[:, 1, :], in_=kcs_d[32:64, G * GT:(G + 1) * GT]), reads=[b_dram_cs], writes=[b_kcs])
                gk = ppc("gkpe", 0, 32)
                S.op("dve", lambda e, gk=gk: e.tensor_scalar(out=kpr[:, :], in0=kpr[:, :], scalar1=gk, scalar2=None, op0=ALU.mult), reads=[b_kpr, b_pp], writes=[b_kpr])
                pt, pb = PS.next()
                S.op("pe", lambda e, pt=pt: e.matmul(pt[0:32, :], lhsT=rot32[:, :], rhs=kpr[:, :], start=True, stop=True), reads=[b_kpr, b_const], writes=[pb])
                S.op("dve", lambda e, pt=pt: e.tensor_tensor(out=kcs[:, 0, :], in0=pt[0:32, :], in1=kcs[:, 0, :], op=ALU.mult), reads=[pb, b_kcs], writes=[b_kcs])
                S.op("dve", lambda e: e.tensor_tensor(out=kcs[:, 1, :], in0=kpr[:, :], in1=kcs[:, 1, :], op=ALU.mult), reads=[b_kpr, b_kcs], writes=[b_kcs])
                S.op("dve", lambda e: e.tensor_tensor(out=kpo[:, :], in0=kcs[:, 0, :], in1=kcs[:, 1, :], op=ALU.add), reads=[b_kcs], writes=[b_kpo])
                S.dma("pool", lambda e, G=G: e.dma_start(out=kpe_d[:, G * GT:(G + 1) * GT], in_=kpo[:, :]), reads=[b_kpo], writes=[b_kpe_d])
                yield
                pt, pb = PS.next()
                for jc in range(2):
                    S.op("pe", lambda e, pt=pt, jc=jc: e.matmul(pt[:, :], lhsT=ones_f[:, :], rhs=kvsq[:, jc, :], start=(jc == 0), stop=(jc == 1)), reads=[b_kvsq, b_const], writes=[pb])
                S.op("act", lambda e, pt=pt: e.activation(out=rbc[:, :], in_=pt[:, :], func=AF.Sqrt, scale=1.0 / 256, bias=eps_t[:, 0:1]), reads=[pb], writes=[b_rbc])
                S.op("dve", lambda e: e.reciprocal(out=rbc[:, :], in_=rbc[:, :]), reads=[b_rbc], writes=[b_rbc])
                yield
                for jc in range(2):
                    S.op("dve", lambda e, jc=jc: e.tensor_tensor(out=kvn[:, jc, :], in0=kvc[:, jc, :], in1=rbc[:, :], op=ALU.mult), reads=[b_kvc, b_rbc], writes=[b_kvn])
                kb = ktn[G % 2]; bkb = b_ktn[G % 2]
                for hp in range(8):
                    pt, pb = PS.next()
                    for kc in range(2):
                        S.op("pe", lambda e, pt=pt, kc=kc, hp=hp: e.matmul(pt[:, :], lhsT=wukK[:, kc, hp * 128:(hp + 1) * 128], rhs=kvn[:, kc, :], start=(kc == 0), stop=(kc == 1)),
                             reads=[b_wuk, b_kvn], writes=[pb])
                    S.op("act", lambda e, pt=pt, hp=hp, kb=kb: e.activation(out=kb[:, hp, :], in_=pt[:, :], func=AF.Copy), reads=[pb], writes=[bkb])
                    q_ = ksq[hp % 2]; bq_ = b_ksq[hp % 2]
                    S.op("act", lambda e, pt=pt, q_=q_: e.activation(out=q_[:, :], in_=pt[:, :], func=AF.Square), reads=[pb], writes=[bq_])
                    bo = ppc("blockones", 0, 128, 2)
                    for t4 in range(4):
                        S.op("pe", lambda e, pst=pst, q_=q_, t4=t4, hp=hp, bo=bo: e.matmul(pst[:, t4 * 16 + 2 * hp:t4 * 16 + 2 * hp + 2], lhsT=q_[:, t4 * 128:(t4 + 1) * 128], rhs=bo, start=True, stop=True, skip_group_check=True),
                             reads=[bq_, b_pp], writes=[pbst])
                    yield
                yield
                for t4 in range(4):
                    S.op("pe", lambda e, t4=t4: e.matmul(pst[:, 64 + t4:65 + t4], lhsT=kpsq[:, t4 * 128:(t4 + 1) * 128], rhs=ones_f[0:32, 0:1], start=True, stop=True, skip_group_check=True),
                         reads=[b_kpsq, b_const], writes=[pbst])
                S.op("dve", lambda e: e.tensor_copy(out=sstat[:, 0:4], in_=pst[:, 64:68]), reads=[pbst], writes=[b_sstat])
                for t4 in range(4):
                    tile_i = G * 4 + t4
                    S.op("dve", lambda e, pst=pst, t4=t4, tile_i=tile_i: e.tensor_scalar(out=rk_all[:, tile_i, :], in0=pst[:, t4 * 16:(t4 + 1) * 16], scalar1=sstat[:, t4:t4 + 1], scalar2=None, op0=ALU.add),
                         reads=[pbst, b_sstat], writes=[b_rk])
                S.dma("pool", lambda e, G=G, kb=kb: e.dma_start(out=ktn_d[:, G * GT:(G + 1) * GT].rearrange("(hp p) t -> p hp t", p=128), in_=kb[:, :, :]), reads=[bkb], writes=[b_ktn_d])
                for t4 in range(4):
                    for half in range(2):
                        pt, pb = PS.next()
                        for kc in range(2):
                            S.op("pe", lambda e, pt=pt, kc=kc, half=half, t4=t4: e.matmul(pt[:, :], lhsT=kvn[:, kc, t4 * 128:(t4 + 1) * 128], rhs=wukV[:, kc, half * 512:(half + 1) * 512], start=(kc == 0), stop=(kc == 1)),
                                 reads=[b_wuk, b_kvn], writes=[pb])
                        dst = vau[:, half * 8:(half + 1) * 8, t4, 0:64]
                        src = pt[:, :].rearrange("p (h d) -> p h d", h=8)
                        if half == 0:
                            S.op("act", lambda e, dst=dst, src=src: e.activation(out=dst, in_=src, func=AF.Copy), reads=[pb], writes=[b_vau])
                        else:
                            S.op("dve", lambda e, dst=dst, src=src: e.tensor_copy(out=dst, in_=src), reads=[pb], writes=[b_vau])
                    yield
                S.dma("pool", lambda e, G=G: e.dma_start(out=v_d[:, G, :, :].rearrange("h p c -> p h c"), in_=vau[:].rearrange("p h b c -> p h (b c)")), reads=[b_vau], writes=[b_v_d])
                yield
            def gen_LRU(G):
                for jc in range(8):
                    ptc, pbc = PS.next()
                    for j in range(4):
                        S.op("pe", lambda e, ptc=ptc, jc=jc, j=j: e.matmul(ptc[:, :], lhsT=diagw[:, jc, j, :], rhs=xr[:, jc, j:j + GT], start=(j == 0), stop=(j == 3)),
                             reads=[b_diagw, b_xr[jc]], writes=[pbc])
                    S.op("dve", lambda e, ptc=ptc, jc=jc: e.tensor_scalar(out=xa[:, jc, :], in0=ptc[:, :], scalar1=ppc("convb", jc), scalar2=None, op0=ALU.add), reads=[pbc, b_pp], writes=[b_xa[jc]])
                    S.op("pool", lambda e, jc=jc: e.tensor_copy(out=xab[:, jc, :], in_=xa[:, jc, :]), reads=[b_xa[jc]], writes=[b_xab[jc]])
                    S.op("pool", lambda e, jc=jc: e.tensor_copy(out=xr[:, jc, 0:3], in_=xr[:, jc, GT:GT + 3]), reads=[b_xr[jc]], writes=[b_xr[jc]])
                    yield
                for quad in range(2):
                    for jq in range(4):
                        jc = quad * 4 + jq
                        blk = jc // 2
                        s2 = jc % 2
                        ptr, pbr = PS.next()
                        for ic in range(2):
                            S.op("pe", lambda e, ptr=ptr, ic=ic, blk=blk, jc=jc: e.matmul(ptr[:, :], lhsT=wab[:, blk * 2 + ic, (jc % 2) * 128:(jc % 2 + 1) * 128], rhs=xab[:, blk * 2 + ic, :], start=(ic == 0), stop=(ic == 1)),
                                 reads=[b_wab, b_xab[blk * 2 + ic]], writes=[pbr])
                        pti, pbi = PS.next()
                        for ic in range(2):
                            S.op("pe", lambda e, pti=pti, ic=ic, blk=blk, jc=jc: e.matmul(pti[:, :], lhsT=wxb[:, blk * 2 + ic, (jc % 2) * 128:(jc % 2 + 1) * 128], rhs=xab[:, blk * 2 + ic, :], start=(ic == 0), stop=(ic == 1)),
                                 reads=[b_wab, b_xab[blk * 2 + ic]], writes=[pbi])
                        S.op("act", lambda e, ptr=ptr, s2=s2, jc=jc: e.activation(out=tr[s2][:, :], in_=ptr[:, :], func=AF.Tanh, scale=0.5, bias=c12[:, 24 + jc:25 + jc]), reads=[pbr, b_c12], writes=[b_tr[s2]])
                        S.op("act", lambda e, pti=pti, jq=jq, jc=jc: e.activation(out=ti[jq][:, :], in_=pti[:, :], func=AF.Tanh, scale=0.5, bias=c12[:, 32 + jc:33 + jc]), reads=[pbi, b_c12], writes=[b_ti[jq]])
                        S.op("act", lambda e, s2=s2, jq=jq, jc=jc: e.activation(out=ta[jq][:, :], in_=tr[s2][:, :], func=AF.Exp, scale=c12[:, 16 + jc:17 + jc], bias=c12[:, 16 + jc:17 + jc]), reads=[b_tr[s2], b_c12], writes=[b_ta[jq]])
                        S.op("act", lambda e, s2=s2, jq=jq, jc=jc: e.activation(out=tm[jq][:, :], in_=tr[s2][:, :], func=AF.Exp, scale=c12[:, jc:jc + 1], bias=c12[:, jc:jc + 1]), reads=[b_tr[s2], b_c12], writes=[b_tm[jq]])
                        S.op("dve", lambda e, jq=jq: e.tensor_scalar(out=tm[jq][:, :], in0=tm[jq][:, :], scalar1=-0.25, scalar2=0.25, op0=ALU.mult, op1=ALU.add), reads=[b_tm[jq]], writes=[b_tm[jq]])
                        S.op("dve", lambda e, jq=jq, jc=jc: e.scalar_tensor_tensor(out=ti[jq][:, :], in0=ti[jq][:, :], scalar=1.0, in1=xa[:, jc, :], op0=ALU.add, op1=ALU.mult), reads=[b_ti[jq], b_xa[jc]], writes=[b_ti[jq]])
                        yield
                    for jq in range(4):
                        S.op("act", lambda e, jq=jq: e.activation(out=tm[jq][:, :], in_=tm[jq][:, :], func=AF.Sqrt), reads=[b_tm[jq]], writes=[b_tm[jq]])
                    yield
                    for jq in range(4):
                        S.op("dve", lambda e, jq=jq: e.tensor_tensor(out=ti[jq][:, :], in0=ti[jq][:, :], in1=tm[jq][:, :], op=ALU.mult), reads=[b_ti[jq], b_tm[jq]], writes=[b_ti[jq]])
                    yield
                    for jq in range(4):
                        jc = quad * 4 + jq
                        S.op("dve", lambda e, jq=jq, jc=jc: e.tensor_tensor_scan(out=tm[jq][:, :], data0=ta[jq][:, :], data1=ti[jq][:, :], initial=hst[:, jc:jc + 1], op0=ALU.mult, op1=ALU.add),
                             reads=[b_ta[jq], b_ti[jq], b_hst8[jc]], writes=[b_tm[jq]])
                    for jq in range(4):
                        jc = quad * 4 + jq
                        S.op("dve", lambda e, jq=jq, jc=jc: e.tensor_copy(out=hst[:, jc:jc + 1], in_=tm[jq][:, GT - 1:GT]), reads=[b_tm[jq]], writes=[b_hst8[jc]])
                    yield
                    for jq in range(4):
                        jc = quad * 4 + jq
                        m = G // 8
                        oh = ppc("onehot", G % 8)
                        ohb = own_hb[m % 2]; bohb = b_ownhb[m % 2]
                        if G % 8 == 0:
                            S.op("dve", lambda e, jq=jq, jc=jc, ohb=ohb, oh=oh: e.tensor_scalar(out=ohb[:, jc, :], in0=tm[jq][:, :], scalar1=oh, scalar2=None, op0=ALU.mult),
                                 reads=[b_tm[jq], b_pp], writes=[b_oh8[m % 2][jc]])
                        else:
                            S.op("dve", lambda e, jq=jq, jc=jc, ohb=ohb, oh=oh: e.scalar_tensor_tensor(out=ohb[:, jc, :], in0=tm[jq][:, :], scalar=oh, in1=ohb[:, jc, :], op0=ALU.mult, op1=ALU.add),
                                 reads=[b_tm[jq], b_pp, b_oh8[m % 2][jc]], writes=[b_oh8[m % 2][jc]])
                if G % 8 == 7:
                    m = G // 8
                    ohb = own_hb[m % 2]; bohb = b_ownhb[m % 2]
                    S.dma("sp", lambda e, m=m, ohb=ohb: e.dma_start(out=ownh_d[:, m * 8 * GT:(m + 1) * 8 * GT], in_=ohb[:].rearrange("p j t -> p (j t)")), reads=b_oh8[m % 2], writes=[b_ownh_d])
                    if debug:
                        S.dma("sp", lambda e, m=m, ohb=ohb: e.dma_start(out=dbg["hlb"][:, m * GT:(m + 1) * GT].rearrange("(jc p) t -> p jc t", p=128), in_=ohb[:]), reads=b_oh8[m % 2], writes=[b_out])
                yield
            def interleave(*gens):
                gens = list(gens)
                while gens:
                    for g in list(gens):
                        try:
                            next(g)
                        except StopIteration:
                            gens.remove(g)
            for _ in gen_H(0):
                pass
            for G in range(n_groups_A):
                emit_M(G)
                gl = [gen_KV(G), gen_LRU(G)]
                if G + 1 < n_groups_A:
                    gl.append(gen_H(G + 1))
                interleave(*gl)
            S.op("act", lambda e: e.activation(out=rk_all[:], in_=rk_all[:], func=AF.Sqrt, bias=eps96[:, 0:1]), reads=[b_rk, b_const], writes=[b_rk])
            S.op("dve", lambda e: e.reciprocal(out=rk_all[:], in_=rk_all[:]), reads=[b_rk], writes=[b_rk])
            PS.release(pst_idx)
            S.dma("sp", lambda e: e.dma_start(out=rk_d, in_=rk_all[:].rearrange("p a b -> p (a b)")), reads=[b_rk], writes=[b_rk_d])
            if debug:
                dtmp = xa; b_dt = Buf()
                S.barrier()
                S.dma("sp", lambda e: e.dma_start(out=dbg["rk"], in_=rk_all[:].rearrange("p a b -> p (a b)")), reads=[b_rk], writes=[b_out])
                S.dma("sp", lambda e: e.dma_start(out=dbg["ktn"], in_=ktn_d[0:128, 0:512]), reads=[b_ktn_d], writes=[b_out])
                S.dma("sp", lambda e: e.dma_start(out=dbg["kpe"], in_=kpe_d[:, 0:512]), reads=[b_kpe_d], writes=[b_out])
                S.dma("sp", lambda e: e.dma_start(out=dbg["v"], in_=v_d[0, 0, :, :]), reads=[b_v_d], writes=[b_out])
            S.barrier()

        def own_group_uT(ph, m, xtb, b_xtb, ssb, b_ssb, uTo, b_uTo):
            for t4 in range(4):
                i = t4 % 2
                r0 = m * GT + t4 * 128
                load_norm_transpose(x_own[r0:r0 + 128, :], xtb[i], b_xtb[i], ssb[i], b_ssb[i], uTo, b_uTo, t4)

        if stop_after not in ("A1", "A") and "1" in phases:
          with ExitStack() as pb1:
            NB1 = 1024 + 2048
            winB = sb(pb1, "winB", [128, 8, NB1], BF16); b_winB = Buf()
            with ExitStack() as wp:
                load_weight_cols(wp, winB, b_winB, w_in_d, [(1024, 2048), (3104, 5152)], 8, "gmix", "wB", NB1)
                S.barrier()
            xtb = [sb(pb1, "xtB%d" % i, [128, D]) for i in range(2)]; b_xtb = [Buf(), Buf()]
            ssb = [sb(pb1, "ssB%d" % i, [128, 4]) for i in range(2)]; b_ssb = [Buf(), Buf()]
            uTo = [sb(pb1, "uTB%d" % i, [128, 8, GT], BF16) for i in range(2)]; b_uTo = [Buf(), Buf()]
            oh = sb(pb1, "ohB", [128, 8, GT], BF16); b_oh = Buf()
            gs = [sb(pb1, "gsB%d" % i, [128, GT]) for i in range(2)]; b_gs = [Buf(), Buf()]
            g2 = [sb(pb1, "g2B%d" % i, [128, GT]) for i in range(2)]; b_g2 = [Buf(), Buf()]
            sa = [sb(pb1, "saB%d" % i, [128, GT]) for i in range(2)]; b_sa = [Buf(), Buf()]
            gao = sb(pb1, "gaoB", [128, 8, GT], BF16); b_gao = Buf()
            sbo = sb(pb1, "sboB", [128, 8, GT], BF16); b_sbo = Buf()
            def interleave(*gens):
                gens = list(gens)
                while gens:
                    for g in list(gens):
                        try:
                            next(g)
                        except StopIteration:
                            gens.remove(g)

            def gen_headB(m, xtb, b_xtb, ssb, b_ssb, uTo, b_uTo):
                u = uTo[m % 2]; bu = b_uTo[m % 2]
                for t4 in range(4):
                    i = t4 % 2
                    r0 = m * GT + t4 * 128
                    load_norm_transpose(x_own[r0:r0 + 128, :], xtb[i], b_xtb[i], ssb[i], b_ssb[i], u, bu, t4)
                    yield

            def gen_chunksB1(m, par):
                u = uTo[m % 2]; bu = b_uTo[m % 2]
                for jc in range(par, 8, 2):
                    s_ = jc % 2
                    pt, pb = PS.next()
                    for kc in range(8):
                        S.op("pe", lambda e, pt=pt, jc=jc, kc=kc, u=u: e.matmul(pt[:, :], lhsT=winB[:, kc, jc * 128:(jc + 1) * 128], rhs=u[:, kc, :], start=(kc == 0), stop=(kc == 7)),
                             reads=[b_winB, bu], writes=[pb])
                    S.op("act", lambda e, pt=pt, s_=s_: e.activation(out=gs[s_][:, :], in_=pt[:, :], func=AF.Copy), reads=[pb], writes=[b_gs[s_]])
                    S.op("act", lambda e, pt=pt, s_=s_: e.activation(out=g2[s_][:, :], in_=pt[:, :], func=AF.Square), reads=[pb], writes=[b_g2[s_]])
                    yield
                    S.op("dve", lambda e, s_=s_: e.tensor_scalar(out=g2[s_][:, :], in0=g2[s_][:, :], scalar1=0.044715, scalar2=1.0, op0=ALU.mult, op1=ALU.add), reads=[b_g2[s_]], writes=[b_g2[s_]])
                    S.op("dve", lambda e, s_=s_: e.tensor_tensor(out=g2[s_][:, :], in0=g2[s_][:, :], in1=gs[s_][:, :], op=ALU.mult), reads=[b_g2[s_], b_gs[s_]], writes=[b_g2[s_]])
                    S.op("act", lambda e, s_=s_: e.activation(out=g2[s_][:, :], in_=g2[s_][:, :], func=AF.Sigmoid, scale=1.5957691216057308), reads=[b_g2[s_]], writes=[b_g2[s_]])
                    S.op("dve", lambda e, s_=s_: e.tensor_tensor(out=gs[s_][:, :], in0=gs[s_][:, :], in1=g2[s_][:, :], op=ALU.mult), reads=[b_g2[s_], b_gs[s_]], writes=[b_gs[s_]])
                    S.op("dve", lambda e, s_=s_, jc=jc: e.tensor_tensor(out=gs[s_][:, :], in0=gs[s_][:, :], in1=oh[:, jc, :], op=ALU.mult), reads=[b_oh, b_gs[s_]], writes=[b_gs[s_]])
                    yield
                    pt, pb = PS.next()
                    for kc in range(8):
                        S.op("pe", lambda e, pt=pt, jc=jc, kc=kc, u=u: e.matmul(pt[:, :], lhsT=winB[:, kc, 1024 + jc * 128:1024 + (jc + 1) * 128], rhs=u[:, kc, :], start=(kc == 0), stop=(kc == 7)),
                             reads=[b_winB, bu], writes=[pb])
                    S.op("act", lambda e, pt=pt, s_=s_: e.activation(out=sa[s_][:, :], in_=pt[:, :], func=AF.Sigmoid), reads=[pb], writes=[b_sa[s_]])
                    S.op("dve", lambda e, s_=s_, jc=jc: e.tensor_tensor(out=gao[:, jc, :], in0=gs[s_][:, :], in1=sa[s_][:, :], op=ALU.mult), reads=[b_sa[s_], b_gs[s_]], writes=[b_gao])
                    yield
                    pt, pb = PS.next()
                    for kc in range(8):
                        S.op("pe", lambda e, pt=pt, jc=jc, kc=kc, u=u: e.matmul(pt[:, :], lhsT=winB[:, kc, 2048 + jc * 128:2048 + (jc + 1) * 128], rhs=u[:, kc, :], start=(kc == 0), stop=(kc == 7)),
                             reads=[b_winB, bu], writes=[pb])
                    S.op("act", lambda e, pt=pt, jc=jc: e.activation(out=sbo[:, jc, :], in_=pt[:, :], func=AF.Sigmoid), reads=[pb], writes=[b_sbo])
                    yield

            for _ in gen_headB(0, xtb, b_xtb, ssb, b_ssb, uTo, b_uTo):
                pass
            for m in range(NOWN):
                S.dma("sp", lambda e, m=m: e.dma_start(out=oh[:].rearrange("p j t -> p (j t)"), in_=ownh_d[:, m * 8 * GT:(m + 1) * 8 * GT]), reads=[b_ownh_d], writes=[b_oh])
                gl = [gen_chunksB1(m, 0), gen_chunksB1(m, 1)]
                if m + 1 < NOWN:
                    gl.append(gen_headB(m + 1, xtb, b_xtb, ssb, b_ssb, uTo, b_uTo))
                interleave(*gl)
                S.dma("pool", lambda e, m=m: e.dma_start(out=gaya_d[:, :, m * GT:(m + 1) * GT], in_=gao[:]), reads=[b_gao], writes=[b_gaya_d])
                S.dma("pool", lambda e, m=m: e.dma_start(out=sgb_d[:, :, m * GT:(m + 1) * GT], in_=sbo[:]), reads=[b_sbo], writes=[b_sgb_d])
            S.barrier()

          with ExitStack() as pb2:
            winQ = sb(pb2, "winQ", [128, 8, 768], BF16); b_winQ = Buf()
            wuq = sb(pb2, "wuq", [128, 6, 1536], BF16); b_wuq = Buf()
            with ExitStack() as wp:
                load_weight_cols(wp, winQ, b_winQ, w_in_d, [(2048, 2816)], 8, "gmix", "wQ", 768)
                load_weight_cols(wp, wuq, b_wuq, wuq_d, [(0, 1536)], 6, "qag", "wUQ", 1536)
                S.barrier()
            xtb = [sb(pb2, "xtQ%d" % i, [128, D]) for i in range(2)]; b_xtb = [Buf(), Buf()]
            ssb = [sb(pb2, "ssQ%d" % i, [128, 4]) for i in range(2)]; b_ssb = [Buf(), Buf()]
            uTo = [sb(pb2, "uTQ%d" % i, [128, 8, GT], BF16) for i in range(2)]; b_uTo = [Buf(), Buf()]
            cosf = sb(pb2, "cosf", [96, TOWN]); sinf = sb(pb2, "sinf", [96, TOWN]); b_cs = Buf()
            S.op("dve", lambda e: e.memset(cosf[0:64, :], 1.0), writes=[b_cs])
            S.op("dve", lambda e: e.memset(sinf[0:64, :], 0.0), writes=[b_cs])
            S.dma("sp", lambda e: e.dma_start(out=sinf[64:96, :], in_=qcs_d[0:32, :]), reads=[b_dram_cs], writes=[b_cs])
            S.dma("sp", lambda e: e.dma_start(out=cosf[64:96, :], in_=qcs_d[32:64, :]), reads=[b_dram_cs], writes=[b_cs])
            qc = sb(pb2, "qc", [128, 6, GT]); b_qc = Buf()
            qsq = sb(pb2, "qsq", [128, 6, GT]); b_qsq = Buf()
            qcn = sb(pb2, "qcn", [128, 6, GT], BF16); b_qcn = Buf()
            rbq = sb(pb2, "rbq", [128, GT]); b_rbq = Buf()
            qs = [sb(pb2, "qs%d" % i, [96, GT]) for i in range(2)]; b_qs = [Buf(), Buf()]
            qq = [sb(pb2, "qq%d" % i, [96, GT]) for i in range(2)]; b_qq = [Buf(), Buf()]
            qr = [sb(pb2, "qr%d" % i, [96, GT]) for i in range(2)]; b_qr = [Buf(), Buf()]
            qn = [sb(pb2, "qn%d" % i, [96, GT]) for i in range(2)]; b_qn = [Buf(), Buf()]
            qto = sb(pb2, "qto", [96, 16, GT], BF16); b_qto = Buf()
            def gen_headsB2(m, par):
                for h in range(par, 16, 2):
                    s_ = h % 2
                    pt, pb = PS.next()
                    for kc in range(6):
                        S.op("pe", lambda e, pt=pt, h=h, kc=kc: e.matmul(pt[0:96, :], lhsT=wuq[:, kc, h * 96:(h + 1) * 96], rhs=qcn[:, kc, :], start=(kc == 0), stop=(kc == 5)),
                             reads=[b_wuq, b_qcn], writes=[pb])
                    S.op("act", lambda e, pt=pt, s_=s_: e.activation(out=qs[s_][:, :], in_=pt[0:96, :], func=AF.Copy), reads=[pb], writes=[b_qs[s_]])
                    S.op("act", lambda e, pt=pt, s_=s_: e.activation(out=qq[s_][:, :], in_=pt[0:96, :], func=AF.Square), reads=[pb], writes=[b_qq[s_]])
                    yield
                    pt2, pb2_ = PS.next()
                    S.op("pe", lambda e, pt2=pt2, s_=s_: e.matmul(pt2[0:96, :], lhsT=ones_f[0:96, 0:96], rhs=qq[s_][:, :], start=True, stop=True), reads=[b_qq[s_], b_const], writes=[pb2_])
                    S.op("act", lambda e, pt2=pt2, s_=s_: e.activation(out=qr[s_][:, :], in_=pt2[0:96, :], func=AF.Sqrt, scale=1.0 / 96, bias=eps_t[0:96, 0:1]), reads=[pb2_], writes=[b_qr[s_]])
                    S.op("dve", lambda e, s_=s_: e.reciprocal(out=qr[s_][:, :], in_=qr[s_][:, :]), reads=[b_qr[s_]], writes=[b_qr[s_]])
                    yield
                    qng = ppc("qng", 0, 96)
                    S.op("dve", lambda e, s_=s_, qng=qng: e.scalar_tensor_tensor(out=qn[s_][:, :], in0=qs[s_][:, :], scalar=qng, in1=qr[s_][:, :], op0=ALU.mult, op1=ALU.mult),
                         reads=[b_qs[s_], b_qr[s_], b_pp], writes=[b_qn[s_]])
                    pt3, pb3 = PS.next()
                    S.op("pe", lambda e, pt3=pt3, s_=s_: e.matmul(pt3[0:96, :], lhsT=rot96[:, :], rhs=qn[s_][:, :], start=True, stop=True), reads=[b_qn[s_], b_const], writes=[pb3])
                    S.op("dve", lambda e, pt3=pt3, s_=s_, m=m: e.tensor_tensor(out=qq[s_][:, :], in0=pt3[0:96, :], in1=sinf[:, m * GT:(m + 1) * GT], op=ALU.mult), reads=[pb3, b_cs], writes=[b_qq[s_]])
                    yield
                    S.op("dve", lambda e, s_=s_, m=m: e.tensor_tensor(out=qn[s_][:, :], in0=qn[s_][:, :], in1=cosf[:, m * GT:(m + 1) * GT], op=ALU.mult), reads=[b_qn[s_], b_cs], writes=[b_qn[s_]])
                    S.op("dve", lambda e, s_=s_: e.tensor_tensor(out=qn[s_][:, :], in0=qn[s_][:, :], in1=qq[s_][:, :], op=ALU.add), reads=[b_qn[s_], b_qq[s_]], writes=[b_qn[s_]])
                    gkf = ppc("gkfold", 0, 96)
                    S.op("act", lambda e, s_=s_, h=h, gkf=gkf: e.activation(out=qto[:, h, :], in_=qn[s_][:, :], func=AF.Copy, scale=gkf), reads=[b_qn[s_], b_pp], writes=[b_qto])
                    yield

            for _ in gen_headB(0, xtb, b_xtb, ssb, b_ssb, uTo, b_uTo):
                pass
            for m in range(NOWN):
                u = uTo[m % 2]; bu = b_uTo[m % 2]
                for jc in range(6):
                    pt, pb = PS.next()
                    for kc in range(8):
                        S.op("pe", lambda e, pt=pt, jc=jc, kc=kc, u=u: e.matmul(pt[:, :], lhsT=winQ[:, kc, jc * 128:(jc + 1) * 128], rhs=u[:, kc, :], start=(kc == 0), stop=(kc == 7)),
                             reads=[b_winQ, bu], writes=[pb])
                    S.op("act", lambda e, pt=pt, jc=jc: e.activation(out=qc[:, jc, :], in_=pt[:, :], func=AF.Copy), reads=[pb], writes=[b_qc])
                    S.op("dve", lambda e, pt=pt, jc=jc: e.tensor_tensor(out=qsq[:, jc, :], in0=pt[:, :], in1=qc[:, jc, :], op=ALU.mult), reads=[pb, b_qc], writes=[b_qsq])
                pt, pb = PS.next()
                for jc in range(6):
                    S.op("pe", lambda e, pt=pt, jc=jc: e.matmul(pt[:, :], lhsT=ones_f[:, :], rhs=qsq[:, jc, :], start=(jc == 0), stop=(jc == 5)), reads=[b_qsq, b_const], writes=[pb])
                S.op("act", lambda e, pt=pt: e.activation(out=rbq[:, :], in_=pt[:, :], func=AF.Sqrt, scale=1.0 / 768, bias=eps_t[:, 0:1]), reads=[pb], writes=[b_rbq])
                S.op("dve", lambda e: e.reciprocal(out=rbq[:, :], in_=rbq[:, :]), reads=[b_rbq], writes=[b_rbq])
                for jc in range(6):
                    S.op("dve", lambda e, jc=jc: e.tensor_tensor(out=qcn[:, jc, :], in0=qc[:, jc, :], in1=rbq[:, :], op=ALU.mult), reads=[b_qc, b_rbq], writes=[b_qcn])
                gl = [gen_headsB2(m, 0), gen_headsB2(m, 1)]
                if m + 1 < NOWN:
                    gl.append(gen_headB(m + 1, xtb, b_xtb, ssb, b_ssb, uTo, b_uTo))
                interleave(*gl)
                S.dma("pool", lambda e, m=m: e.dma_start(out=qt_d[:, :, m * GT:(m + 1) * GT], in_=qto[:]), reads=[b_qto], writes=[b_qt_d])
            S.barrier()

        if stop_after not in ("A1", "A", "B") and "C" in phases:
          with ExitStack() as pc:
            masks = sb(pc, "masks", [128, 32, GT], BF16); b_masks = Buf()
            S.dma("sp", lambda e: e.dma_start(out=masks[:], in_=masks_d), writes=[b_masks])
            rk = sb(pc, "rkC", [128, SEQ // 128, 16]); b_rkc = Buf()
            S.dma("sp", lambda e: e.dma_start(out=rk[:].rearrange("p a b -> p (a b)"), in_=rk_d), reads=[b_rk_d], writes=[b_rkc])
            qth = [sb(pc, "qth%d" % i, [96, TOWN], BF16) for i in range(2)]; b_qth = [Buf(), Buf()]
            NR = 4
            kp = [sb(pc, "kp%d" % i, [96, GT], BF16) for i in range(NR)]; b_kp = [Buf() for _ in range(NR)]
            vp = [sb(pc, "vp%d" % i, [128, 4, 128], BF16) for i in range(NR)]; b_vp = [Buf() for _ in range(NR)]
            NPB = 4
            pbuf = [sb(pc, "pb%d" % i, [128, GT], BF16) for i in range(NPB)]; b_pbuf = [Buf() for _ in range(NPB)]
            gat = sb(pc, "gat", [128, TOWN], BF16); b_gat = Buf()
            sgt = sb(pc, "sgt", [128, TOWN], BF16); b_sgt = Buf()
            mgt = sb(pc, "mgt", [128, TOWN], BF16); b_mgt = Buf()
            rl = sb(pc, "rl", [128, GT]); b_rl = Buf()
            rl2 = sb(pc, "rl2", [128, GT]); b_rl2 = Buf()
            ybn = sb(pc, "ybn", [128, GT]); b_ybn = Buf()
            ybs = sb(pc, "ybs", [128, GT]); b_ybs = Buf()
            (s_banks, s_idx) = PS.reserve(4)
            (o_banks, o_idx) = PS.reserve(4)
            n_heads_c = 16 if stop_after != "C1" else 2
            piece = 0
            stepno = 0
            for h in range(n_heads_c):
                q_ = qth[h % 2]; bq_ = b_qth[h % 2]
                S.dma("sp", lambda e, h=h, q_=q_: e.dma_start(out=q_[:, :], in_=qt_d[:, h, :]), reads=[b_qt_d], writes=[bq_])
                hr = (h % 2) * 64
                kcx = h // 2
                S.dma("sp", lambda e, hr=hr, kcx=kcx: e.dma_start(out=gat[hr:hr + 64, :], in_=gaya_d[hr:hr + 64, kcx, :]), reads=[b_gaya_d], writes=[b_gat])
                S.dma("sp", lambda e, hr=hr, kcx=kcx: e.dma_start(out=sgt[hr:hr + 64, :], in_=sgb_d[hr:hr + 64, kcx, :]), reads=[b_sgb_d], writes=[b_sgt])
                pendq = []

                def emit_pv(t_):
                    (ot2, ob2, vt2, bvt2, blk2, pbf2, bpbf2, f2, l2) = t_
                    S.op("pe", lambda e: e.matmul(ot2[:, :], lhsT=vt2[:, blk2, :], rhs=pbf2[:, :], start=f2, stop=l2),
                         reads=[bvt2, bpbf2], writes=[ob2])
                for G in range(NG):
                    r = piece % NR; piece += 1
                    kt = kp[r]; bkt = b_kp[r]; vt = vp[r]; bvt = b_vp[r]
                    S.dma("sp", lambda e, h=h, G=G, kt=kt: e.dma_start(out=kt[0:64, :], in_=ktn_d[h * 64:(h + 1) * 64, G * GT:(G + 1) * GT]), reads=[b_ktn_d], writes=[bkt])
                    S.dma("sp", lambda e, G=G, kt=kt: e.dma_start(out=kt[64:96, :], in_=kpe_d[:, G * GT:(G + 1) * GT]), reads=[b_kpe_d], writes=[bkt])
                    S.dma("pool", lambda e, h=h, G=G, vt=vt: e.dma_start(out=vt[:].rearrange("p b c -> p (b c)"), in_=v_d[h, G, :, :]), reads=[b_v_d], writes=[bvt])
                    for m in range(G // 8, NOWN):
                        ot, ob = o_banks[m]
                        for blk in range(4):
                            si = stepno % 4; stepno += 1
                            st_, sb_ = s_banks[si]
                            pbf = pbuf[si]; bpbf = b_pbuf[si]
                            S.op("pe", lambda e, st_=st_, kt=kt, blk=blk, q_=q_, m=m: e.matmul(st_[:, :], lhsT=kt[:, blk * 128:(blk + 1) * 128], rhs=q_[:, m * GT:(m + 1) * GT], start=True, stop=True),
                                 reads=[bkt, bq_], writes=[sb_])
                            sc = rk[:, G * 4 + blk, h:h + 1]
                            S.op("act", lambda e, st_=st_, pbf=pbf, sc=sc: e.activation(out=pbf[:, :], in_=st_[:, :], func=AF.Exp, scale=sc), reads=[sb_, b_rkc], writes=[bpbf])
                            if G // 8 == m:
                                mj = (G % 8) * 4 + blk
                                S.op("dve", lambda e, pbf=pbf, mj=mj: e.tensor_tensor(out=pbf[:, :], in0=pbf[:, :], in1=masks[:, mj, :], op=ALU.mult), reads=[bpbf, b_masks], writes=[bpbf])
                            first = (G == 0 and blk == 0)
                            last = (G == 8 * m + 7 and blk == 3)
                            pendq.append((ot, ob, vt, bvt, blk, pbf, bpbf, first, last))
                            if len(pendq) > 2:
                                emit_pv(pendq.pop(0))
                    if G % 8 == 7:
                        while pendq:
                            emit_pv(pendq.pop(0))
                        m = G // 8
                        ot, ob = o_banks[m]
                        cs = slice(m * GT, (m + 1) * GT)
                        S.op("dve", lambda e, ot=ot: e.reciprocal(out=rl[64:128, :], in_=ot[64:128, :]), reads=[ob], writes=[b_rl])
                        S.op("dve", lambda e: e.tensor_copy(out=rl2[0:64, :], in_=rl[64:128, :]), reads=[b_rl], writes=[b_rl2])
                        S.op("dve", lambda e, ot=ot: e.tensor_tensor(out=ybn[0:64, :], in0=ot[0:64, :], in1=rl2[0:64, :], op=ALU.mult), reads=[ob, b_rl2], writes=[b_ybn])
                        if hr == 0:
                            ysrc = ybn; bys = b_ybn
                        else:
                            S.op("dve", lambda e: e.tensor_copy(out=ybs[64:128, :], in_=ybn[0:64, :]), reads=[b_ybn], writes=[b_ybs])
                            ysrc = ybs; bys = b_ybs
                        if debug:
                            S.dma("sp", lambda e, ysrc=ysrc, hr=hr, h=h, cs=cs: e.dma_start(out=dbg["yb"][h * 64:(h + 1) * 64, cs], in_=ysrc[hr:hr + 64, :]), reads=[bys], writes=[b_out])
                        S.op("dve", lambda e, ysrc=ysrc, hr=hr, cs=cs: e.tensor_tensor(out=ysrc[hr:hr + 64, :], in0=ysrc[hr:hr + 64, :], in1=sgt[hr:hr + 64, cs], op=ALU.mult), reads=[bys, b_sgt], writes=[bys])
                        S.op("dve", lambda e, ysrc=ysrc, hr=hr, cs=cs: e.tensor_tensor(out=mgt[hr:hr + 64, cs], in0=ysrc[hr:hr + 64, :], in1=gat[hr:hr + 64, cs], op=ALU.add), reads=[bys, b_gat], writes=[b_mgt])
                S.dma("pool", lambda e, hr=hr, kcx=kcx: e.dma_start(out=mg_d[hr:hr + 64, kcx, :], in_=mgt[hr:hr + 64, :]), reads=[b_mgt], writes=[b_mg_d])
            PS.release(s_idx); PS.release(o_idx)
            S.barrier()

        if stop_after not in ("A1", "A", "B", "C", "C1") and "D" in phases:
          with ExitStack() as pd:
            woutb = sb(pd, "woutb", [128, 8, D], BF16); b_woutb = Buf()
            with ExitStack() as wp:
                load_weight_cols(wp, woutb, b_woutb, wout_d, [(0, D)], 8, None, "wO", D)
                S.barrier()
            wr = sb(pd, "wr", [128, 8, 36]); b_wr = Buf()
            S.dma("sp", lambda e: e.dma_start(out=wr[:], in_=wr_d.rearrange("(kc p) n -> p kc n", p=128)), writes=[b_wr])
            rbias = sb(pd, "rbias", [128, 36]); gfb = sb(pd, "gfb", [128, D]); b_rb = Buf()
            S.dma("sp", lambda e: e.dma_start(out=rbias[:], in_=rbias_d), writes=[b_rb])
            S.dma("sp", lambda e: e.dma_start(out=gfb[:], in_=gffnb_d), writes=[b_rb])
            mgs = [sb(pd, "mgs%d" % i, [128, 8, GT], BF16) for i in range(2)]; b_mgs = [Buf(), Buf()]
            xtd = [sb(pd, "xtD%d" % i, [128, D]) for i in range(2)]; b_xtd = [Buf(), Buf()]
            hm = [sb(pd, "hm%d" % i, [128, D]) for i in range(2)]; b_hm = [Buf(), Buf()]
            xn = [sb(pd, "xn%d" % i, [128, D]) for i in range(2)]; b_xn = [Buf(), Buf()]
            ssd = [sb(pd, "ssD%d" % i, [128, 4]) for i in range(2)]; b_ssd = [Buf(), Buf()]
            xnT = [sb(pd, "xnT%d" % i, [128, 8, 128]) for i in range(2)]; b_xnT = [Buf(), Buf()]
            xgs = sb(pd, "xgs", [128, 8, GT], BF16); b_xgs = Buf()
            cts = sb(pd, "cts", [32, GT]); b_cts = Buf()
            R = {}
            for nm, w in [("lg", 36), ("gmax", 1), ("goh", 4), ("ngm", 1), ("ge", 4), ("gsum", 1), ("pg", 1), ("gpen", 4), ("em", 32),
                          ("t1", 1), ("m1", 32), ("em2", 32), ("t2", 1), ("m2", 32), ("dd", 1), ("ed", 1), ("w1", 1), ("w2", 1), ("comb", 32)]:
                R[nm] = [sb(pd, "r_%s%d" % (nm, i), [128, w]) for i in range(2)]
            b_R = [Buf(), Buf()]
            BIG = 1.0e9
            (ct_l, ct_idx) = PS.reserve(1)
            ctp, ctb = ct_l[0]
            for m in range(NOWN):
                mg_ = mgs[m % 2]; bmg_ = b_mgs[m % 2]
                S.dma("sp", lambda e, m=m, mg_=mg_: e.dma_start(out=mg_[:], in_=mg_d[:, :, m * GT:(m + 1) * GT]), reads=[b_mg_d], writes=[bmg_])
                for t4 in range(4):
                    tt = m * 4 + t4
                    i = tt % 2
                    r0 = tt * 128
                    S.dma("sp", lambda e, r0=r0, i=i: e.dma_start(out=xtd[i][:], in_=x_own[r0:r0 + 128, :]), writes=[b_xtd[i]])
                    for nh in range(2):
                        pt, pb = PS.next()
                        for kc in range(8):
                            S.op("pe", lambda e, pt=pt, kc=kc, nh=nh, t4=t4, mg_=mg_: e.matmul(pt[:, :], lhsT=mg_[:, kc, t4 * 128:(t4 + 1) * 128], rhs=woutb[:, kc, nh * 512:(nh + 1) * 512], start=(kc == 0), stop=(kc == 7)),
                                 reads=[bmg_, b_woutb], writes=[pb])
                        S.op("dve", lambda e, pt=pt, nh=nh, i=i: e.tensor_tensor(out=hm[i][:, nh * 512:(nh + 1) * 512], in0=pt[:, :], in1=xtd[i][:, nh * 512:(nh + 1) * 512], op=ALU.add),
                             reads=[pb, b_xtd[i]], writes=[b_hm[i]])
                    S.dma("sp", lambda e, r0=r0, i=i: e.dma_start(out=hmid_d[r0:r0 + 128, :], in_=hm[i][:]), reads=[b_hm[i]], writes=[b_hmid_d])
                    if debug:
                        S.dma("sp", lambda e, r0=r0, i=i: e.dma_start(out=dbg["hmid"][r0:r0 + 128, :], in_=hm[i][:]), reads=[b_hm[i]], writes=[b_out])
                    ss = ssd[i]; bss = b_ssd[i]
                    S.op("act", lambda e, i=i, ss=ss: e.activation(out=junk[:], in_=hm[i][:], func=AF.Square, accum_out=ss[:, 0:1]), reads=[b_hm[i]], writes=[bss, b_junk])
                    S.op("act", lambda e, ss=ss: e.activation(out=ss[:, 1:2], in_=ss[:, 0:1], func=AF.Sqrt, scale=1.0 / D, bias=eps_t[:, 0:1]), reads=[bss], writes=[bss])
                    S.op("dve", lambda e, ss=ss: e.reciprocal(out=ss[:, 2:3], in_=ss[:, 1:2]), reads=[bss], writes=[bss])
                    S.op("dve", lambda e, ss=ss, i=i: e.scalar_tensor_tensor(out=xn[i][:], in0=hm[i][:], scalar=ss[:, 2:3], in1=gfb[:], op0=ALU.mult, op1=ALU.mult),
                         reads=[bss, b_hm[i], b_rb], writes=[b_xn[i]])
                    for half in range(2):
                        pt, pb = PS.next()
                        for q in range(4):
                            kc = half * 4 + q
                            S.op("pe", lambda e, pt=pt, q=q, kc=kc, i=i: e.transpose(out=pt[:, q * 128:(q + 1) * 128], in_=xn[i][:, kc * 128:(kc + 1) * 128], identity=ident[:]),
                                 reads=[b_xn[i], b_const], writes=[pb])
                        dst = xnT[i][:, half * 4:half * 4 + 4, :]
                        src = pt[:].rearrange("p (q t) -> p q t", q=4)
                        if half == 0:
                            S.op("act", lambda e, dst=dst, src=src: e.activation(out=dst, in_=src, func=AF.Copy), reads=[pb], writes=[b_xnT[i]])
                        else:
                            S.op("dve", lambda e, dst=dst, src=src: e.tensor_copy(out=dst, in_=src), reads=[pb], writes=[b_xnT[i]])
                    S.op("pool", lambda e, i=i, t4=t4: e.tensor_copy(out=xgs[:, :, t4 * 128:(t4 + 1) * 128], in_=xnT[i][:, :, :]), reads=[b_xnT[i]], writes=[b_xgs])
                    pt, pb = PS.next()
                    for kc in range(8):
                        S.op("pe", lambda e, pt=pt, kc=kc, i=i: e.matmul(pt[:, 0:36], lhsT=xnT[i][:, kc, :], rhs=wr[:, kc, :], start=(kc == 0), stop=(kc == 7)),
                             reads=[b_xnT[i], b_wr], writes=[pb])
                    r = {k: v[i] for k, v in R.items()}
                    br = b_R[i]
                    rd = [br, b_rb]
                    S.op("dve", lambda e, pt=pt, r=r: e.tensor_tensor(out=r["lg"][:, :], in0=pt[:, 0:36], in1=rbias[:, :], op=ALU.add), reads=[pb, b_rb, br], writes=[br])
                    S.op("dve", lambda e, r=r: e.tensor_reduce(out=r["gmax"][:, :], in_=r["lg"][:, 0:4], axis=AX.X, op=ALU.max), reads=rd, writes=[br])
                    S.op("dve", lambda e, r=r: e.tensor_scalar(out=r["goh"][:, :], in0=r["lg"][:, 0:4], scalar1=r["gmax"][:, 0:1], scalar2=None, op0=ALU.is_ge), reads=rd, writes=[br])
                    S.op("dve", lambda e, r=r: e.tensor_scalar(out=r["ngm"][:, :], in0=r["gmax"][:, :], scalar1=-1.0, scalar2=None, op0=ALU.mult), reads=rd, writes=[br])
                    S.op("act", lambda e, r=r: e.activation(out=r["ge"][:, :], in_=r["lg"][:, 0:4], func=AF.Exp, bias=r["ngm"][:, 0:1], accum_out=r["gsum"][:, 0:1]), reads=rd, writes=[br])
                    S.op("dve", lambda e, r=r: e.reciprocal(out=r["pg"][:, :], in_=r["gsum"][:, :]), reads=rd, writes=[br])
                    S.op("dve", lambda e, r=r: e.tensor_scalar(out=r["gpen"][:, :], in0=r["goh"][:, :], scalar1=BIG, scalar2=-BIG, op0=ALU.mult, op1=ALU.add), reads=rd, writes=[br])
                    for g in range(4):
                        S.op("dve", lambda e, r=r, g=g: e.tensor_scalar(out=r["em"][:, g * 8:(g + 1) * 8], in0=r["lg"][:, 4 + g * 8:4 + (g + 1) * 8], scalar1=r["gpen"][:, g:g + 1], scalar2=None, op0=ALU.add), reads=rd, writes=[br])
                    S.op("dve", lambda e, r=r: e.tensor_reduce(out=r["t1"][:, :], in_=r["em"][:, :], axis=AX.X, op=ALU.max), reads=rd, writes=[br])
                    S.op("dve", lambda e, r=r: e.tensor_scalar(out=r["m1"][:, :], in0=r["em"][:, :], scalar1=r["t1"][:, 0:1], scalar2=None, op0=ALU.is_ge), reads=rd, writes=[br])
                    S.op("dve", lambda e, r=r: e.scalar_tensor_tensor(out=r["em2"][:, :], in0=r["m1"][:, :], scalar=-BIG, in1=r["em"][:, :], op0=ALU.mult, op1=ALU.add), reads=rd, writes=[br])
                    S.op("dve", lambda e, r=r: e.tensor_reduce(out=r["t2"][:, :], in_=r["em2"][:, :], axis=AX.X, op=ALU.max), reads=rd, writes=[br])
                    S.op("dve", lambda e, r=r: e.tensor_scalar(out=r["m2"][:, :], in0=r["em2"][:, :], scalar1=r["t2"][:, 0:1], scalar2=None, op0=ALU.is_ge), reads=rd, writes=[br])
                    S.op("dve", lambda e, r=r: e.tensor_tensor(out=r["dd"][:, :], in0=r["t2"][:, :], in1=r["t1"][:, :], op=ALU.subtract), reads=rd, writes=[br])
                    S.op("act", lambda e, r=r: e.activation(out=r["ed"][:, :], in_=r["dd"][:, :], func=AF.Exp), reads=rd, writes=[br])
                    S.op("dve", lambda e, r=r: e.tensor_scalar(out=r["w1"][:, :], in0=r["ed"][:, :], scalar1=1.0, scalar2=None, op0=ALU.add), reads=rd, writes=[br])
                    S.op("dve", lambda e, r=r: e.reciprocal(out=r["w1"][:, :], in_=r["w1"][:, :]), reads=rd, writes=[br])
                    S.op("dve", lambda e, r=r: e.tensor_tensor(out=r["w2"][:, :], in0=r["ed"][:, :], in1=r["w1"][:, :], op=ALU.mult), reads=rd, writes=[br])
                    S.op("dve", lambda e, r=r: e.tensor_tensor(out=r["w1"][:, :], in0=r["w1"][:, :], in1=r["pg"][:, :], op=ALU.mult), reads=rd, writes=[br])
                    S.op("dve", lambda e, r=r: e.tensor_tensor(out=r["w2"][:, :], in0=r["w2"][:, :], in1=r["pg"][:, :], op=ALU.mult), reads=rd, writes=[br])
                    S.op("dve", lambda e, r=r: e.tensor_scalar(out=r["comb"][:, :], in0=r["m1"][:, :], scalar1=r["w1"][:, 0:1], scalar2=None, op0=ALU.mult), reads=rd, writes=[br])
                    S.op("dve", lambda e, r=r: e.scalar_tensor_tensor(out=r["comb"][:, :], in0=r["m2"][:, :], scalar=r["w2"][:, 0:1], in1=r["comb"][:, :], op0=ALU.mult, op1=ALU.add), reads=rd, writes=[br])
                    if debug:
                        S.dma("sp", lambda e, r=r, r0=r0: e.dma_start(out=dbg["comb"][r0:r0 + 128, :], in_=r["comb"][:, :]), reads=[br], writes=[b_out])
                    S.op("pe", lambda e, r=r, t4=t4: e.transpose(out=ctp[0:32, t4 * 128:(t4 + 1) * 128], in_=r["comb"][:, 0:32], identity=ident[:]), reads=[br, b_const], writes=[ctb])
                S.op("act", lambda e: e.activation(out=cts[:, :], in_=ctp[0:32, :], func=AF.Copy), reads=[ctb], writes=[b_cts])
                S.dma("sp", lambda e, m=m: e.dma_start(out=combt_d[:, m * GT:(m + 1) * GT], in_=cts[:, :]), reads=[b_cts], writes=[b_combt_d])
                S.dma("sp", lambda e, m=m: e.dma_start(out=xgt_d[:, :, m * GT:(m + 1) * GT], in_=xgs[:]), reads=[b_xgs], writes=[b_xgt_d])
            PS.release(ct_idx)
            S.barrier()

        if stop_after not in ("A1", "A", "B", "C", "C1", "D") and "E" in phases:
          with ExitStack() as pe_:
            yacc = sb(pe_, "yacc", [128, 16, D]); b_yacc = [Buf() for _ in range(16)]
            S.dma("sp", lambda e: e.dma_start(out=yacc[:], in_=hmid_d.rearrange("(t p) f -> p t f", p=128)), reads=[b_hmid_d], writes=b_yacc)
            xg = sb(pe_, "xg", [128, 8, TOWN], BF16); b_xg = Buf()
            S.dma("sp", lambda e: e.dma_start(out=xg[:], in_=xgt_d), reads=[b_xgt_d], writes=[b_xg])
            combT = sb(pe_, "combT", [32, TOWN]); b_combT = Buf()
            S.dma("sp", lambda e: e.dma_start(out=combT[:], in_=combt_d), reads=[b_combt_d], writes=[b_combT])
            sel = [sb(pe_, "sel%d" % i, [32, 128]) for i in range(2)]; b_sel = [Buf(), Buf()]
            wgs2 = [sb(pe_, "wgs%d" % i, [128, 8, DEXP]) for i in range(2)]; wus2 = [sb(pe_, "wus%d" % i, [128, 8, DEXP]) for i in range(2)]
            wds2 = [sb(pe_, "wds%d" % i, [128, 2, D]) for i in range(2)]
            b_wgs2 = [Buf(), Buf()]; b_wus2 = [Buf(), Buf()]; b_wds2 = [Buf(), Buf()]
            wgb = [sb(pe_, "wgb%d" % i, [128, 8, DEXP], BF16) for i in range(2)]
            wub = [sb(pe_, "wub%d" % i, [128, 8, DEXP], BF16) for i in range(2)]
            wdb = [sb(pe_, "wdb%d" % i, [128, 2, D], BF16) for i in range(2)]
            b_wgb = [Buf(), Buf()]; b_wub = [Buf(), Buf()]; b_wdb = [Buf(), Buf()]
            cb = sb(pe_, "cb", [128, GT]); b_cb = Buf()
            sg = [sb(pe_, "sg%d" % i, [128, GT]) for i in range(2)]; b_sg = [Buf(), Buf()]
            tu = [sb(pe_, "tu%d" % i, [128, GT]) for i in range(2)]; b_tu = [Buf(), Buf()]
            hb = [sb(pe_, "hb%d" % i, [128, 2, GT], BF16) for i in range(2)]; b_hb = [Buf(), Buf()]
            n_exp = NEXP if stop_after != "E1" else 2
            for ex in range(n_exp):
                w = ex % 2
                wgs = wgs2[w]; wus = wus2[w]; wds = wds2[w]; b_wgs = b_wgs2[w]; b_wus = b_wus2[w]; b_wds = b_wds2[w]
                S.dma("sp", lambda e, ex=ex, wgs=wgs: e.dma_start(out=wgs[:], in_=wg_d[ex].rearrange("(kc p) j -> p kc j", p=128)), writes=[b_wgs])
                S.dma("sp", lambda e, ex=ex, wus=wus: e.dma_start(out=wus[:], in_=wu_d[ex].rearrange("(kc p) j -> p kc j", p=128)), writes=[b_wus])
                S.dma("sp", lambda e, ex=ex, wds=wds: e.dma_start(out=wds[:], in_=wd_d[ex].rearrange("(jc p) n -> p jc n", p=128)), writes=[b_wds])
                S.op("act", lambda e, w=w, wgs=wgs: e.activation(out=wgb[w][:], in_=wgs[:], func=AF.Copy), reads=[b_wgs], writes=[b_wgb[w]])
                S.op("act", lambda e, w=w, wus=wus: e.activation(out=wub[w][:], in_=wus[:], func=AF.Copy), reads=[b_wus], writes=[b_wub[w]])
                S.op("act", lambda e, w=w, wds=wds: e.activation(out=wdb[w][:], in_=wds[:], func=AF.Copy), reads=[b_wds], writes=[b_wdb[w]])
                S.op("dve", lambda e, w=w, ex=ex: e.tensor_copy(out=sel[w][:, :], in_=ident[0:32, ex:ex + 1].to_broadcast([32, 128])), reads=[b_const], writes=[b_sel[w]])
                for m in range(NOWN):
                    cs = slice(m * GT, (m + 1) * GT)
                    h_ = hb[m % 2]; bh_ = b_hb[m % 2]
                    pt, pb = PS.next()
                    S.op("pe", lambda e, pt=pt, w=w, cs=cs: e.matmul(pt[:, :], lhsT=sel[w][:, :], rhs=combT[:, cs], start=True, stop=True), reads=[b_sel[w], b_combT], writes=[pb])
                    S.op("act", lambda e, pt=pt: e.activation(out=cb[:, :], in_=pt[:, :], func=AF.Copy), reads=[pb], writes=[b_cb])
                    for jc in range(2):
                        s_ = jc
                        ptg, pbg = PS.next()
                        for kc in range(8):
                            S.op("pe", lambda e, ptg=ptg, kc=kc, jc=jc, w=w, cs=cs: e.matmul(ptg[:, :], lhsT=wgb[w][:, kc, jc * 128:(jc + 1) * 128], rhs=xg[:, kc, cs], start=(kc == 0), stop=(kc == 7)),
                                 reads=[b_wgb[w], b_xg], writes=[pbg])
                        ptu, pbu = PS.next()
                        for kc in range(8):
                            S.op("pe", lambda e, ptu=ptu, kc=kc, jc=jc, w=w, cs=cs: e.matmul(ptu[:, :], lhsT=wub[w][:, kc, jc * 128:(jc + 1) * 128], rhs=xg[:, kc, cs], start=(kc == 0), stop=(kc == 7)),
                                 reads=[b_wub[w], b_xg], writes=[pbu])
                        S.op("act", lambda e, ptg=ptg, s_=s_: e.activation(out=sg[s_][:, :], in_=ptg[:, :], func=AF.Silu), reads=[pbg], writes=[b_sg[s_]])
                        S.op("dve", lambda e, ptu=ptu, s_=s_: e.tensor_tensor(out=tu[s_][:, :], in0=ptu[:, :], in1=sg[s_][:, :], op=ALU.mult), reads=[pbu, b_sg[s_]], writes=[b_tu[s_]])
                        S.op("dve", lambda e, s_=s_, jc=jc, h_=h_: e.tensor_tensor(out=h_[:, jc, :], in0=tu[s_][:, :], in1=cb[:, :], op=ALU.mult), reads=[b_tu[s_], b_cb], writes=[bh_])
                    for t4 in range(4):
                        tt = m * 4 + t4
                        for nh in range(2):
                            pty, pby = PS.next()
                            for jc in range(2):
                                S.op("pe", lambda e, pty=pty, jc=jc, t4=t4, nh=nh, w=w, h_=h_: e.matmul(pty[:, :], lhsT=h_[:, jc, t4 * 128:(t4 + 1) * 128], rhs=wdb[w][:, jc, nh * 512:(nh + 1) * 512], start=(jc == 0), stop=(jc == 1)),
                                     reads=[bh_, b_wdb[w]], writes=[pby])
                            S.op("dve", lambda e, pty=pty, tt=tt, nh=nh: e.tensor_tensor(out=yacc[:, tt, nh * 512:(nh + 1) * 512], in0=pty[:, :], in1=yacc[:, tt, nh * 512:(nh + 1) * 512], op=ALU.add),
                                 reads=[pby, b_yacc[tt]], writes=[b_yacc[tt]])
            S.dma("sp", lambda e: e.dma_start(out=out_d.rearrange("(t p) f -> p t f", p=128), in_=yacc[:]), reads=b_yacc, writes=[b_out])
            S.barrier()

        S.barrier()
        for e_ in ("sp", "pool"):
            pass
        S.emit()
    return nc


b_out = Buf("out")


def _pcol(v, nchunk):
    return np.ascontiguousarray(np.asarray(v, np.float32).reshape(nchunk, 128).T)


def _prep_common(inp):
    f = lambda k: np.asarray(inp[k], np.float32)[0]
    pp = np.zeros((128, PPW), np.float32)
    pp[:, PP["gmix"]:PP["gmix"] + 8] = _pcol(f("norm_mix_g"), 8)
    cw = f("conv_w")
    pp[:, PP["convw"]:PP["convw"] + 32] = cw.reshape(4, 8, 128).transpose(2, 1, 0).reshape(128, 32)
    pp[:, PP["convb"]:PP["convb"] + 8] = _pcol(f("conv_b"), 8)
    pp[:, PP["ba"]:PP["ba"] + 8] = _pcol(f("lru_ba"), 8)
    pp[:, PP["bx"]:PP["bx"] + 8] = _pcol(f("lru_bx"), 8)
    pp[:, PP["lam"]:PP["lam"] + 8] = _pcol(f("lru_lambda"), 8)
    pp[:, PP["qag"]:PP["qag"] + 6] = _pcol(f("q_a_g"), 6)
    pp[:, PP["kvag"]:PP["kvag"] + 2] = _pcol(f("kv_a_g"), 2)
    pp[0:96, PP["qng"]] = f("q_norm_g")
    kng = f("k_norm_g")
    pp[0:64, PP["gkfold"]] = kng[0:64]
    pp[64:96, PP["gkfold"]] = 1.0
    pp[0:32, PP["gkpe"]] = kng[64:96]
    freq = (10000.0 ** (-np.arange(16, dtype=np.float32) / 16.0)).astype(np.float32)
    fr32 = np.concatenate([freq, freq])
    pp[0:32, PP["freq64"]] = fr32
    pp[32:64, PP["freq64"]] = fr32
    pp[32:64, PP["phase64"]] = np.float32(math.pi / 2)
    pp[0:64, PP["blockones"]] = 1.0
    pp[64:128, PP["blockones"] + 1] = 1.0
    pp[:, PP["gffn"]:PP["gffn"] + 8] = _pcol(f("norm_ffn_g"), 8)
    ident = np.eye(128, dtype=np.float32)
    rot32 = np.zeros((32, 32), np.float32)
    for m in range(16):
        rot32[m + 16, m] = -1.0
        rot32[m, m + 16] = 1.0
    rot96 = np.zeros((96, 96), np.float32)
    rot96[64:96, 64:96] = rot32
    wr = np.concatenate([f("router_group_w"), f("router_expert_w")], axis=1)
    rb = np.concatenate([f("router_group_b"), f("router_expert_b")])[None, :]
    rbias = np.ascontiguousarray(np.broadcast_to(rb, (128, 36))).astype(np.float32)
    gffnb = np.ascontiguousarray(np.broadcast_to(f("norm_ffn_g")[None, :], (128, D))).astype(np.float32)
    common = dict(gffnb=gffnb, ident=ident, rot96=rot96, rot32=rot32, rbias=rbias, w_in=f("w_in"), lru_wa=f("lru_wa"),
                  lru_wx=f("lru_wx"), w_uq=f("w_uq"), w_ukv=f("w_ukv"), w_out=f("w_out"), w_router=np.ascontiguousarray(wr),
                  w_gate=f("w_gate"), w_up=f("w_up"), w_down=f("w_down"))
    return pp, common


def _masks_for(c):
    m = np.zeros((128, 32, GT), np.float32)
    q = np.arange(GT)[None, :]
    for cp in range(8):
        for i in range(4):
            j = cp * 4 + i
            if cp < c:
                m[:, j, :] = 1.0
            elif cp == c:
                m[0:64, j, :] = (q >= 128 * i)
                m[64:128, j, :] = (q >= 128 * i + 64)
    return m.astype(ml_dtypes.bfloat16)


def make_in_maps(inp, names=None):
    pp, common = _prep_common(inp)
    x = np.asarray(inp["x"], np.float32)[0]
    pos = np.asarray(inp["positions"], np.int32)[0]
    posb_all = np.ascontiguousarray(np.broadcast_to(pos[None, :], (64, SEQ)))
    maps = []
    for c in range(NCORES):
        rows = np.concatenate([np.arange((8 * m + c) * GT, (8 * m + c + 1) * GT) for m in range(NOWN)])
        ppc_ = pp.copy()
        ppc_[:, PP["onehot"] + c] = 1.0
        d = dict(common)
        d.update(x_all=x, x_own=np.ascontiguousarray(x[rows]), posb_all=posb_all,
                 posb_own=np.ascontiguousarray(posb_all[:, rows]), pp=ppc_, masks=_masks_for(c))
        if names is not None:
            d = {k: d[k] for k in names}
        maps.append(d)
    return maps


def own_rows(c):
    return np.concatenate([np.arange((8 * m + c) * GT, (8 * m + c + 1) * GT) for m in range(NOWN)])


def kernel(**inputs):
    nc = build_program()
    maps = make_in_maps(inputs, nc._declared_inputs)
    res = run_bass_kernel_spmd(nc, maps, core_ids=list(range(NCORES)))
    out = np.zeros((1, SEQ, D), np.float32)
    for c in range(NCORES):
        out[0, own_rows(c)] = res.results[c]["out"]
    return out
```

```python
import os
import math
import numpy as np
import ml_dtypes
from contextlib import ExitStack
import concourse.bass as bass
import concourse.mybir as mybir
from concourse.bass_utils import run_bass_kernel_spmd

F32 = mybir.dt.float32
BF16 = mybir.dt.bfloat16
I32 = mybir.dt.int32
ALU = mybir.AluOpType
AF = mybir.ActivationFunctionType
AX = mybir.AxisListType

NCORES = 8
SEQ = 16384
D = 1024
GT = 512
NG = SEQ // GT
NOWN = 4
TOWN = NOWN * GT
EPS = 1e-6
TWO_PI = 2.0 * math.pi
C1 = 6.28125
C2 = TWO_PI - C1
NEXP = 32
DEXP = 256

PP = {}
_o = 0
for _n, _w in [("gmix", 8), ("convw", 32), ("convb", 8), ("ba", 8), ("bx", 8), ("lam", 8), ("qag", 6),
               ("kvag", 2), ("onehot", 8), ("qng", 1), ("gkfold", 1), ("gkpe", 1), ("freq64", 1),
               ("blockones", 2), ("gffn", 8), ("phase64", 1)]:
    PP[_n] = _o
    _o += _w
PPW = _o


class Buf:
    __slots__ = ("name", "last_w", "readers")

    def __init__(self, name=""):
        self.name = name
        self.last_w = None
        self.readers = []


class Sched:
    ENGS = ("sp", "pe", "act", "dve", "pool")
    DUR = {"pe": 0.25, "act": 0.62, "dve": 0.70, "pool": 1.1}

    def __init__(self, nc, stack, n_dma_sems=32, strict=True):
        self.nc = nc
        self.strict = strict
        self.esem = {e: stack.enter_context(nc.semaphore("prog_" + e)) for e in self.ENGS}
        self.dsems = [stack.enter_context(nc.semaphore("dma_%d" % i)) for i in range(n_dma_sems)]
        self.ops = []
        self.seg = 0

    def _add(self, kind, eng, fn, reads, writes, dur):
        oid = len(self.ops)
        deps = set()
        for b in reads:
            if b.last_w is not None:
                deps.add(b.last_w)
        for b in writes:
            if b.last_w is not None:
                deps.add(b.last_w)
            deps.update(b.readers)
        self.ops.append([kind, eng, fn, sorted(deps), dur, self.seg])
        for b in reads:
            b.readers.append(oid)
            if len(b.readers) > 700:
                b.readers = b.readers[-700:]
        for b in writes:
            b.last_w = oid
            b.readers = []
        return oid

    class _Probe:
        def __init__(self):
            self.name = None
            self.kw = {}

        def __getattr__(self, name):
            def f(*a, **kw):
                self.name = name
                self.kw = kw
                return self
            return f

    def _estimate(self, kind, eng, fn):
        try:
            p = Sched._Probe()
            fn(p)
            kw = p.kw
            if kind == "dma":
                o = kw.get("out")
                if o is None:
                    return 3.0
                n = 1
                for d_ in o.shape:
                    n *= int(d_)
                return 2.0 + n * mybir.dt.size(o.dtype) / 150e3
            o = kw.get("out")
            if eng == "pe":
                if p.name == "transpose":
                    return 0.09
                rhs = kw.get("rhs")
                nfree = 1
                for d_ in rhs.shape[1:]:
                    nfree *= int(d_)
                t = 0.03 + max(nfree, 64) / 2400.0
                if rhs.dtype == F32:
                    t *= 4.0
                return t
            nfree = 1
            for d_ in o.shape[1:]:
                nfree *= int(d_)
            if eng == "act":
                return 0.06 + nfree / 960.0 + (0.1 if kw.get("accum_out") is not None else 0.0)
            i0 = kw.get("in0", kw.get("in_", kw.get("data0", None)))
            b = 4
            try:
                b = max(mybir.dt.size(o.dtype), mybir.dt.size(i0.dtype)) if i0 is not None else mybir.dt.size(o.dtype)
            except Exception:
                pass
            t = 0.08 + nfree * (1.12e-3 if b >= 4 else 0.8e-3)
            if p.name == "tensor_tensor_scan":
                t = 0.08 + nfree * 2.1e-3
            if eng == "pool":
                t *= 1.8
            return t
        except Exception:
            return 3.0 if kind == "dma" else self.DUR[eng]

    def op(self, eng, fn, reads=(), writes=(), dur=None):
        return self._add("op", eng, fn, reads, writes, self._estimate("op", eng, fn) if dur is None else dur)

    def dma(self, eng, fn, reads=(), writes=(), dur=None):
        return self._add("dma", eng, fn, reads, writes, self._estimate("dma", eng, fn) if dur is None else dur)

    def barrier(self):
        self.seg += 1

    def _schedule_segment(self, ids, done_before):
        import heapq
        ops = self.ops
        idset = set(ids)
        ndeps = {}
        users = {}
        for i in ids:
            c = 0
            for d in ops[i][3]:
                if d in idset:
                    c += 1
                    users.setdefault(d, []).append(i)
            ndeps[i] = c
        blevel = {}
        for i in reversed(ids):
            m_ = 0.0
            for u in users.get(i, ()):
                if blevel[u] > m_:
                    m_ = blevel[u]
            blevel[i] = ops[i][4] + m_
        finish = {}
        eng_free = {e: 0.0 for e in self.ENGS}
        ready = {e: [] for e in self.ENGS}
        avail = {e: [] for e in self.ENGS}
        for i in ids:
            if ndeps[i] == 0:
                heapq.heappush(ready[ops[i][1]], (0.0, i))
        order = []
        n = len(ids)
        LAT = 0.12
        while len(order) < n:
            best = None
            for e in self.ENGS:
                h = ready[e]
                while h and h[0][0] <= eng_free[e]:
                    rt, i = heapq.heappop(h)
                    heapq.heappush(avail[e], (-blevel[i], i))
                if avail[e]:
                    cand = (eng_free[e], 0, e)
                elif h:
                    cand = (h[0][0], 1, e)
                else:
                    continue
                if best is None or cand < best:
                    best = cand
            st, which, e = best
            if which == 0:
                _, i = heapq.heappop(avail[e])
            else:
                st, i = heapq.heappop(ready[e])
            kind, _, _, _, dur, _ = ops[i]
            if kind == "dma":
                eng_free[e] = st + 0.08
                fin = st + dur
            else:
                eng_free[e] = st + dur
                fin = st + dur
            finish[i] = fin
            order.append(i)
            for u in users.get(i, ()):
                ndeps[u] -= 1
                if ndeps[u] == 0:
                    rt = 0.0
                    for d in ops[u][3]:
                        if d in finish:
                            lat = LAT if ops[d][1] != ops[u][1] else 0.05
                            rt = max(rt, finish[d] + lat)
                    heapq.heappush(ready[ops[u][1]], (rt, u))
        return order

    def emit(self):
        ops = self.ops
        nseg = self.seg + 1
        segs = [[] for _ in range(nseg)]
        for i, o in enumerate(ops):
            segs[o[5]].append(i)
        order = []
        for k in range(nseg):
            if segs[k]:
                order += self._schedule_segment(segs[k], None)
        ecount = {e: 0 for e in self.ENGS}
        dcount = [0] * len(self.dsems)
        dnext = 0
        tok = {}
        prev_dma_tok = {}
        for i in order:
            kind, eng = ops[i][0], ops[i][1]
            if kind == "op":
                ecount[eng] += 1
                tok[i] = (("e", eng), ecount[eng])
            else:
                j = dnext
                dnext = (dnext + 1) % len(self.dsems)
                if dcount[j] > 0:
                    prev_dma_tok[i] = (("d", j), dcount[j])
                dcount[j] += 16
                tok[i] = (("d", j), dcount[j])
        semobj = {}
        for e in self.ENGS:
            semobj[("e", e)] = self.esem[e]
        for j, s_ in enumerate(self.dsems):
            semobj[("d", j)] = s_
        prog = {e: [] for e in self.ENGS}
        waited = {e: {} for e in self.ENGS}
        last_seg = {e: 0 for e in self.ENGS}
        seg_tokens = []

        def need(eng, t):
            key, val = t
            if key == ("e", eng) and (eng == "pe" or not self.strict):
                return
            if val > waited[eng].get(key, 0):
                waited[eng][key] = val
                prog[eng].append(("w", semobj[key], val))

        seg_end = []
        cur = {}
        pos = 0
        for k in range(nseg):
            for _ in segs[k]:
                i = order[pos]; pos += 1
                key, val = tok[i]
                cur[key] = max(cur.get(key, 0), val)
            seg_end.append(dict(cur))
        for i in order:
            kind, eng, fn, deps, dur, sg = ops[i]
            if sg > last_seg[eng]:
                for key, val in seg_end[sg - 1].items():
                    need(eng, (key, val))
                last_seg[eng] = sg
            for d in deps:
                need(eng, tok[d])
            if i in prev_dma_tok:
                need(eng, prev_dma_tok[i])
            key, val = tok[i]
            prog[eng].append(("i", fn, semobj[key], 1 if kind == "op" else 16))
        for e in self.ENGS:
            for key, val in seg_end[-1].items():
                need(e, (key, val))
        names = {"sp": "sync", "pe": "tensor", "act": "scalar", "dve": "vector", "pool": "gpsimd"}
        with self.nc.Block() as block:
            for e in self.ENGS:
                items = prog[e]
                if not items:
                    continue

                def body(engobj, items=items):
                    for it in items:
                        if it[0] == "w":
                            engobj.wait_ge(it[1], it[2])
                        else:
                            it[1](engobj).then_inc(it[2], it[3])

                getattr(block, names[e])(body)


class PsumRot:
    def __init__(self, tiles):
        self.tiles = tiles
        self.free = list(range(len(tiles)))
        self.i = 0

    def reserve(self, n):
        r = [self.free.pop() for _ in range(n)]
        return [self.tiles[k] for k in r], r

    def release(self, idxs):
        self.free.extend(idxs)

    def next(self):
        k = self.free[self.i % len(self.free)]
        self.i += 1
        return self.tiles[k]


def build_program(debug=False, stop_after=None, phases="AB12CDE"):
    nc = bass.Bass("TRN2", target_bir_lowering=False)
    declared = []
    nc._declared_inputs = declared

    def din(name, shape, dt=F32, ph=None):
        if ph is not None and not any(p in phases for p in ph):
            return None
        declared.append(name)
        return nc.dram_tensor(name, list(shape), dt, kind="ExternalInput").ap()

    def dscr(name, shape, dt=F32):
        return nc.dram_tensor(name, list(shape), dt).ap()

    x_all = din("x_all", [SEQ, D], ph="A")
    x_own = din("x_own", [TOWN, D], ph="12D")
    posb_all = din("posb_all", [64, SEQ], I32, ph="A")
    posb_own = din("posb_own", [64, TOWN], I32, ph="A")
    pp_d = din("pp", [128, PPW])
    ident_d = din("ident", [128, 128])
    rot96_d = din("rot96", [96, 96])
    rot32_d = din("rot32", [32, 32])
    masks_d = din("masks", [128, 32, GT], BF16, ph="C")
    rbias_d = din("rbias", [128, 36], ph="D")
    gffnb_d = din("gffnb", [128, D], ph="D")
    w_in_d = din("w_in", [D, 5152], ph="A12")
    wa_d = din("lru_wa", [4, 256, 256], ph="A")
    wx_d = din("lru_wx", [4, 256, 256], ph="A")
    wuq_d = din("w_uq", [768, 1536], ph="2")
    wukv_d = din("w_ukv", [256, 2048], ph="A")
    wout_d = din("w_out", [D, D], ph="D")
    wr_d = din("w_router", [D, 36], ph="D")
    wg_d = din("w_gate", [NEXP, D, DEXP], ph="E")
    wu_d = din("w_up", [NEXP, D, DEXP], ph="E")
    wd_d = din("w_down", [NEXP, DEXP, D], ph="E")
    out_d = nc.dram_tensor("out", [TOWN, D], F32, kind="ExternalOutput").ap()
    dbg = {}
    if debug:
        for nm, shp in [("hl", [D, TOWN]), ("yb", [D, TOWN]), ("hmid", [TOWN, D]), ("qt", [96, 16 * TOWN]),
                        ("comb", [TOWN, 32])]:
            dbg[nm] = nc.dram_tensor("dbg_" + nm, shp, F32, kind="ExternalOutput").ap()
        dbg["hlb"] = nc.dram_tensor("dbg_hlb", [D, TOWN], BF16, kind="ExternalOutput").ap()
        dbg["rk"] = nc.dram_tensor("dbg_rk", [128, 2048], F32, kind="ExternalOutput").ap()
        dbg["ktn"] = nc.dram_tensor("dbg_ktn", [128, 512], BF16, kind="ExternalOutput").ap()
        dbg["kpe"] = nc.dram_tensor("dbg_kpe", [32, 512], BF16, kind="ExternalOutput").ap()
        dbg["v"] = nc.dram_tensor("dbg_v", [128, 512], BF16, kind="ExternalOutput").ap()

    ktn_d = dscr("ktn", [8 * 128, SEQ], BF16)
    kpe_d = dscr("kpe", [32, SEQ], BF16)
    v_d = dscr("vaug", [16, NG, 128, 4 * 128], BF16)
    ownh_d = dscr("ownh", [128, NOWN * 8 * GT], BF16)
    rk_d = dscr("rk", [128, (SEQ // 128) * 16])
    gaya_d = dscr("gaya", [128, 8, TOWN], BF16)
    sgb_d = dscr("sgb", [128, 8, TOWN], BF16)
    mg_d = dscr("mg", [128, 8, TOWN], BF16)
    qt_d = dscr("qt", [96, 16, TOWN], BF16)
    hmid_d = dscr("hmid", [TOWN, D])
    xgt_d = dscr("xgt", [128, 8, TOWN], BF16)
    combt_d = dscr("combt", [32, TOWN])
    kcs_d = dscr("kcs", [64, SEQ])
    qcs_d = dscr("qcs", [64, TOWN])

    with ExitStack() as top:
        S = Sched(nc, top)
        sb = lambda st, name, shape, dt=F32: st.enter_context(nc.sbuf_tensor("sb_" + name, list(shape), dt))

        ps_tiles = []
        for i in range(8):
            t = top.enter_context(nc.psum_tensor("ps%d" % i, [128, 512], F32))
            ps_tiles.append((t, Buf("ps%d" % i)))
        PS = PsumRot(ps_tiles)

        pp = sb(top, "pp_sb", [128, PPW]); b_pp = Buf("pp")
        ident = sb(top, "ident_sb", [128, 128])
        rot96 = sb(top, "rot96_sb", [96, 96])
        rot32 = sb(top, "rot32_sb", [32, 32])
        ones_f = sb(top, "ones_f", [128, 128])
        b_const = Buf("const")
        b_rk = Buf("rk"); b_ownh = Buf("ownh")
        b_rk_d = Buf("rk_d"); b_ownh_d = Buf("ownh_d"); b_gaya_d = Buf(); b_sgb_d = Buf(); b_mg_d = Buf(); b_qt_d = Buf()
        b_hmid_d = Buf(); b_xgt_d = Buf(); b_combt_d = Buf()
        b_ktn_d = Buf("ktn_d"); b_kpe_d = Buf("kpe_d"); b_v_d = Buf("v_d")
        c12 = sb(top, "c12", [128, 40]); b_c12 = Buf("c12")

        S.dma("sp", lambda e: e.dma_start(out=pp[:], in_=pp_d), writes=[b_pp])
        S.dma("sp", lambda e: e.dma_start(out=ident[:], in_=ident_d), writes=[b_const])
        S.dma("sp", lambda e: e.dma_start(out=rot96[:], in_=rot96_d), writes=[b_const])
        S.dma("sp", lambda e: e.dma_start(out=rot32[:], in_=rot32_d), writes=[b_const])
        S.op("dve", lambda e: e.memset(ones_f[:], 1.0), writes=[b_const])

        def ppc(name, j=0, rows=128, w=1):
            o = PP[name] + j
            return pp[0:rows, o:o + w]

        lam_ap = ppc("lam", 0, 128, 8)
        S.op("act", lambda e: e.activation(out=c12[:, 0:8], in_=lam_ap, func=AF.Exp, scale=-1.0), reads=[b_pp], writes=[b_c12])
        S.op("act", lambda e: e.activation(out=c12[:, 0:8], in_=c12[:, 0:8], func=AF.Ln, bias=1.0), reads=[b_c12], writes=[b_c12])
        S.op("dve", lambda e: e.tensor_scalar(out=c12[:, 8:16], in0=c12[:, 0:8], scalar1=-16.0, scalar2=None, op0=ALU.mult), reads=[b_c12], writes=[b_c12])
        S.op("dve", lambda e: e.tensor_scalar(out=c12[:, 16:24], in0=c12[:, 0:8], scalar1=-4.0, scalar2=None, op0=ALU.mult), reads=[b_c12], writes=[b_c12])
        S.op("dve", lambda e: e.tensor_scalar(out=c12[:, 0:8], in0=c12[:, 0:8], scalar1=-8.0, scalar2=None, op0=ALU.mult), reads=[b_c12], writes=[b_c12])
        S.op("dve", lambda e: e.tensor_scalar(out=c12[:, 24:32], in0=ppc("ba", 0, 128, 8), scalar1=0.5, scalar2=None, op0=ALU.mult), reads=[b_pp], writes=[b_c12])
        S.op("dve", lambda e: e.tensor_scalar(out=c12[:, 32:40], in0=ppc("bx", 0, 128, 8), scalar1=0.5, scalar2=None, op0=ALU.mult), reads=[b_pp], writes=[b_c12])

        def sincos_tables(st, posb_ap, ntok, dst_d, tag):
            CH = 2048
            pi_ = sb(st, tag + "_pi", [64, CH], I32)
            ang = sb(st, tag + "_ang", [64, CH])
            kf = sb(st, tag + "_kf", [64, CH])
            ki = sb(st, tag + "_ki", [64, CH], I32)
            msk = sb(st, tag + "_m", [64, CH])
            b = [Buf() for _ in range(5)]
            fr = ppc("freq64", 0, 64); ph = ppc("phase64", 0, 64)
            for c0 in range(0, ntok, CH):
                S.dma("sp", lambda e, c0=c0: e.dma_start(out=pi_[:], in_=posb_ap[:, c0:c0 + CH]), writes=[b[0]])
                S.op("dve", lambda e: e.tensor_copy(out=ang[:], in_=pi_[:]), reads=[b[0]], writes=[b[1]])
                S.op("dve", lambda e: e.tensor_scalar(out=ang[:], in0=ang[:], scalar1=fr, scalar2=ph, op0=ALU.mult, op1=ALU.add),
                     reads=[b[1], b_pp], writes=[b[1]])
                S.op("dve", lambda e: e.tensor_scalar(out=kf[:], in0=ang[:], scalar1=1.0 / TWO_PI, scalar2=None, op0=ALU.mult),
                     reads=[b[1]], writes=[b[2]])
                S.op("dve", lambda e: e.tensor_copy(out=ki[:], in_=kf[:]), reads=[b[2]], writes=[b[3]])
                S.op("dve", lambda e: e.tensor_copy(out=kf[:], in_=ki[:]), reads=[b[3]], writes=[b[2]])
                S.op("dve", lambda e: e.scalar_tensor_tensor(out=ang[:], in0=kf[:], scalar=-C1, in1=ang[:], op0=ALU.mult, op1=ALU.add),
                     reads=[b[2], b[1]], writes=[b[1]])
                S.op("dve", lambda e: e.scalar_tensor_tensor(out=ang[:], in0=kf[:], scalar=-C2, in1=ang[:], op0=ALU.mult, op1=ALU.add),
                     reads=[b[2], b[1]], writes=[b[1]])
                for thr, cmp_, corr in ((math.pi, ALU.is_gt, -TWO_PI), (-math.pi, ALU.is_lt, TWO_PI)):
                    S.op("dve", lambda e, thr=thr, cmp_=cmp_: e.tensor_single_scalar(out=msk[:], in_=ang[:], scalar=thr, op=cmp_),
                         reads=[b[1]], writes=[b[4]])
                    S.op("dve", lambda e, corr=corr: e.scalar_tensor_tensor(out=ang[:], in0=msk[:], scalar=corr, in1=ang[:], op0=ALU.mult, op1=ALU.add),
                         reads=[b[4], b[1]], writes=[b[1]])
                S.op("dve", lambda e: e.tensor_scalar(out=ang[:], in0=ang[:], scalar1=3.1415925, scalar2=-3.1415925, op0=ALU.min, op1=ALU.max),
                     reads=[b[1]], writes=[b[1]])
                S.op("act", lambda e: e.activation(out=kf[:], in_=ang[:], func=AF.Sin), reads=[b[1]], writes=[b[2]])
                S.dma("sp", lambda e, c0=c0: e.dma_start(out=dst_d[:, c0:c0 + CH], in_=kf[:]), reads=[b[2]], writes=[b_dram_cs])

        b_dram_cs = Buf("dram_cs")

        def load_norm_transpose(xsrc_ap, xt, bx_, ss, bss, uT, buT, t4, gcol=None):
            S.dma("sp", lambda e: e.dma_start(out=xt[:], in_=xsrc_ap), writes=[bx_])
            S.op("act", lambda e: e.activation(out=junk[:], in_=xt[:], func=AF.Square, accum_out=ss[:, 0:1]), reads=[bx_], writes=[bss, b_junk])
            S.op("act", lambda e: e.activation(out=ss[:, 1:2], in_=ss[:, 0:1], func=AF.Sqrt, scale=1.0 / D, bias=eps_t[:, 0:1]), reads=[bss], writes=[bss])
            S.op("dve", lambda e: e.reciprocal(out=ss[:, 2:3], in_=ss[:, 1:2]), reads=[bss], writes=[bss])
            S.op("dve", lambda e: e.tensor_scalar(out=xt[:], in0=xt[:], scalar1=ss[:, 2:3], scalar2=None, op0=ALU.mult), reads=[bss, bx_], writes=[bx_])
            for half in range(2):
                pt, pb = PS.next()
                for q in range(4):
                    kc = half * 4 + q
                    S.op("pe", lambda e, pt=pt, q=q, kc=kc: e.transpose(out=pt[:, q * 128:(q + 1) * 128], in_=xt[:, kc * 128:(kc + 1) * 128], identity=ident[:]),
                         reads=[bx_, b_const], writes=[pb])
                dst = uT[:, half * 4:half * 4 + 4, t4 * 128:(t4 + 1) * 128]
                src = pt[:].rearrange("p (q t) -> p q t", q=4)
                eng = "act" if half == 0 else "dve"
                if eng == "act":
                    S.op("act", lambda e, dst=dst, src=src: e.activation(out=dst, in_=src, func=AF.Copy), reads=[pb], writes=[buT])
                else:
                    S.op("dve", lambda e, dst=dst, src=src: e.tensor_copy(out=dst, in_=src), reads=[pb], writes=[buT])

        junk = sb(top, "junk", [128, D], BF16); b_junk = Buf("junk")
        eps_t = sb(top, "eps_t", [128, 1])
        eps96 = sb(top, "eps96", [128, 1])
        S.op("dve", lambda e: e.memset(eps_t[:], EPS), writes=[b_const])
        S.op("dve", lambda e: e.memset(eps96[:], 96.0 * EPS), writes=[b_const])

        def load_weight_cols(st, dst_bf, bdst, src_d, col_ranges, nk, scale_name, tag, stage_w):
            stg = [sb(st, "%s_stg%d" % (tag, i), [128, stage_w]) for i in range(2)]
            bst = [Buf(), Buf()]
            for kc in range(nk):
                s_ = stg[kc % 2]; bs_ = bst[kc % 2]
                o = 0
                for (a, b_) in col_ranges:
                    S.dma("sp", lambda e, s_=s_, o=o, a=a, b_=b_, kc=kc: e.dma_start(out=s_[:, o:o + (b_ - a)], in_=src_d[kc * 128:(kc + 1) * 128, a:b_]), writes=[bs_])
                    o += b_ - a
                eng = "dve" if kc % 2 == 0 else "pool"
                if scale_name is None:
                    S.op(eng, lambda e, s_=s_, kc=kc, o=o: e.tensor_copy(out=dst_bf[:, kc, 0:o], in_=s_[:, 0:o]), reads=[bs_], writes=[bdst])
                else:
                    sc = ppc(scale_name, kc)
                    S.op(eng, lambda e, s_=s_, kc=kc, o=o, sc=sc: e.tensor_scalar(out=dst_bf[:, kc, 0:o], in0=s_[:, 0:o], scalar1=sc, scalar2=None, op0=ALU.mult),
                         reads=[bs_, b_pp], writes=[bdst])

        with ExitStack() as pa:
          if "A" in phases:
            sincos_st = ExitStack()
            with sincos_st:
                sincos_tables(sincos_st, posb_all, SEQ, kcs_d, "csA")
                sincos_tables(sincos_st, posb_own, TOWN, qcs_d, "csO")
            S.barrier()

            rk_all = sb(pa, "rk_all", [128, SEQ // 128, 16])
            own_hb = [sb(pa, "own_h%d" % i, [128, 8, GT], BF16) for i in range(2)]; b_ownhb = [Buf(), Buf()]
            NA = 1024 + 256 + 32
            winA = sb(pa, "winA", [128, 8, NA], BF16); b_winA = Buf("winA")
            wab = sb(pa, "wab", [128, 8, 256], BF16); wxb = sb(pa, "wxb", [128, 8, 256], BF16)
            b_wab = Buf("wab")
            wukK = sb(pa, "wukK", [128, 2, 1024], BF16); wukV = sb(pa, "wukV", [128, 2, 1024], BF16)
            b_wuk = Buf("wuk")
            with ExitStack() as wp:
                load_weight_cols(wp, winA, b_winA, w_in_d, [(0, 1024), (2816, 3104)], 8, "gmix", "wA", NA)
                wst = sb(wp, "wa_stg", [128, 8, 256])
                b_wst = Buf()
                for (src, dstw) in ((wa_d, wab), (wx_d, wxb)):
                    S.dma("sp", lambda e, src=src: e.dma_start(out=wst[:], in_=src.rearrange("b (ic p) j -> p (b ic) j", p=128)), writes=[b_wst])
                    S.op("dve", lambda e, dstw=dstw: e.tensor_copy(out=dstw[:], in_=wst[:]), reads=[b_wst], writes=[b_wab])
                kst = sb(wp, "wukv_stg", [128, 2048]); b_kst = Buf()
                for kc in range(2):
                    S.dma("sp", lambda e, kc=kc: e.dma_start(out=kst[:], in_=wukv_d[kc * 128:(kc + 1) * 128, :]), writes=[b_kst])
                    sc = ppc("kvag", kc)
                    kv4 = kst[:].rearrange("p (h two d) -> p h two d", h=16, two=2)
                    S.op("dve", lambda e, kc=kc, sc=sc, kv4=kv4: e.tensor_scalar(out=wukK[:, kc, :].rearrange("p (h d) -> p h d", h=16), in0=kv4[:, :, 0, :], scalar1=sc, scalar2=None, op0=ALU.mult),
                         reads=[b_kst, b_pp], writes=[b_wuk])
                    S.op("dve", lambda e, kc=kc, sc=sc, kv4=kv4: e.tensor_scalar(out=wukV[:, kc, :].rearrange("p (h d) -> p h d", h=16), in0=kv4[:, :, 1, :], scalar1=sc, scalar2=None, op0=ALU.mult),
                         reads=[b_kst, b_pp], writes=[b_wuk])
                S.barrier()

            xt = [sb(pa, "xtA%d" % i, [128, D]) for i in range(2)]; b_xt = [Buf(), Buf()]
            ssA = [sb(pa, "ssA%d" % i, [128, 4]) for i in range(2)]; b_ss = [Buf(), Buf()]
            uT = [sb(pa, "uTA%d" % i, [128, 8, GT], BF16) for i in range(2)]; b_uT = [Buf(), Buf()]
            xr = sb(pa, "xr", [128, 8, GT + 3], BF16); b_xr = [Buf() for _ in range(8)]
            diagw = sb(pa, "diagw", [128, 8, 4, 128], BF16); b_diagw = Buf()
            xa = sb(pa, "xa", [128, 8, GT]); b_xa = [Buf() for _ in range(8)]
            xab = sb(pa, "xab", [128, 8, GT], BF16); b_xab = [Buf() for _ in range(8)]
            NT = 2
            tr = [sb(pa, "tr%d" % i, [128, GT]) for i in range(1)] * 2; b_tr = [Buf()] * 2
            ti = [sb(pa, "ti%d" % i, [128, GT]) for i in range(4)]; b_ti = [Buf() for _ in range(4)]
            ta = [sb(pa, "ta%d" % i, [128, GT]) for i in range(4)]; b_ta = [Buf() for _ in range(4)]
            tm = [sb(pa, "tm%d" % i, [128, GT]) for i in range(4)]; b_tm = [Buf() for _ in range(4)]
            hst = sb(pa, "hst", [128, 8]); b_hst = Buf("hst"); b_hst8 = [Buf() for _ in range(8)]
            b_oh8 = [[Buf() for _ in range(8)] for _ in range(2)]
            kvc = sb(pa, "kvc", [128, 2, GT]); b_kvc = Buf()
            kvsq = sb(pa, "kvsq", [128, 2, GT]); b_kvsq = Buf()
            kvn = sb(pa, "kvn", [128, 2, GT], BF16); b_kvn = Buf()
            rbc = sb(pa, "rbc", [128, GT]); b_rbc = Buf()
            kpr = sb(pa, "kpr", [32, GT]); b_kpr = Buf()
            kpsq = sb(pa, "kpsq", [32, GT]); b_kpsq = Buf()
            kpo = sb(pa, "kpo", [32, GT], BF16); b_kpo = Buf()
            kcs = sb(pa, "kcs_sb", [32, 2, GT]); b_kcs = Buf()
            ktn = [sb(pa, "ktn_sb%d" % i, [128, 8, GT], BF16) for i in range(1)] * 2; b_ktn = [Buf()] * 2
            ksq = [sb(pa, "ksq%d" % i, [128, GT]) for i in range(1)] * 2; b_ksq = [Buf()] * 2
            vau = sb(pa, "vau", [128, 16, 4, 128], BF16); b_vau = Buf()
            sstat = sb(pa, "sstat", [128, 16]); b_sstat = Buf()

            S.op("dve", lambda e: e.memset(xr[:], 0.0), writes=b_xr)
            for jc in range(8):
                for j in range(4):
                    S.op("dve", lambda e, jc=jc, j=j: e.tensor_scalar(out=diagw[:, jc, j, :], in0=ident[:, :], scalar1=ppc("convw", jc * 4 + j), scalar2=None, op0=ALU.mult),
                         reads=[b_const, b_pp], writes=[b_diagw])
            S.op("dve", lambda e: e.memset(hst[:], 0.0), writes=[b_hst] + b_hst8)
            S.op("pool", lambda e: e.memset(vau[:], 1.0), writes=[b_vau])

            n_groups_A = NG if stop_after != "A1" else 2
            (pst_l, pst_idx) = PS.reserve(1)
            pst, pbst = pst_l[0]
            def gen_H(G):
                u = uT[G % 2]; bu = b_uT[G % 2]
                for t4 in range(4):
                    i = (G * 4 + t4) % 2
                    r0 = G * GT + t4 * 128
                    load_norm_transpose(x_all[r0:r0 + 128, :], xt[i], b_xt[i], ssA[i], b_ss[i], u, bu, t4)
                    yield
            def emit_M(G):
                u = uT[G % 2]; bu = b_uT[G % 2]
                for jc in range(8):
                    pt, pb = PS.next()
                    for kc in range(8):
                        S.op("pe", lambda e, pt=pt, jc=jc, kc=kc, u=u: e.matmul(pt[:, :], lhsT=winA[:, kc, jc * 128:(jc + 1) * 128], rhs=u[:, kc, :], start=(kc == 0), stop=(kc == 7)),
                             reads=[b_winA, bu], writes=[pb])
                    S.op("act", lambda e, pt=pt, jc=jc: e.activation(out=xr[:, jc, 3:3 + GT], in_=pt[:, :], func=AF.Copy), reads=[pb], writes=[b_xr[jc]])
                for jc in range(2):
                    pt, pb = PS.next()
                    for kc in range(8):
                        S.op("pe", lambda e, pt=pt, jc=jc, kc=kc, u=u: e.matmul(pt[:, :], lhsT=winA[:, kc, 1024 + jc * 128:1024 + (jc + 1) * 128], rhs=u[:, kc, :], start=(kc == 0), stop=(kc == 7)),
                             reads=[b_winA, bu], writes=[pb])
                    S.op("act", lambda e, pt=pt, jc=jc: e.activation(out=kvc[:, jc, :], in_=pt[:, :], func=AF.Copy), reads=[pb], writes=[b_kvc])
                    S.op("dve", lambda e, pt=pt, jc=jc: e.tensor_tensor(out=kvsq[:, jc, :], in0=pt[:, :], in1=kvc[:, jc, :], op=ALU.mult), reads=[pb, b_kvc], writes=[b_kvsq])
                pt, pb = PS.next()
                for kc in range(8):
                    S.op("pe", lambda e, pt=pt, kc=kc, u=u: e.matmul(pt[0:32, :], lhsT=winA[:, kc, 1280:1312], rhs=u[:, kc, :], start=(kc == 0), stop=(kc == 7)),
                         reads=[b_winA, bu], writes=[pb])
                S.op("act", lambda e, pt=pt: e.activation(out=kpr[:, :], in_=pt[0:32, :], func=AF.Copy), reads=[pb], writes=[b_kpr])
                S.op("dve", lambda e, pt=pt: e.tensor_tensor(out=kpsq[:, :], in0=pt[0:32, :], in1=kpr[:, :], op=ALU.mult), reads=[pb, b_kpr], writes=[b_kpsq])
            def gen_KV(G):
                S.dma("sp", lambda e, G=G: e.dma_start(out=kcs[:, 0, :], in_=kcs_d[0:32, G * GT:(G + 1) * GT]), reads=[b_dram_cs], writes=[b_kcs])
                S.dma("sp", lambda e, G=G: e.dma_start(out=kcs[:, 1, :], in_=kcs_d[32:64, G * GT:(G + 1) * GT]), reads=[b_dram_cs], writes=[b_kcs])
                gk = ppc("gkpe", 0, 32)
                S.op("dve", lambda e, gk=gk: e.tensor_scalar(out=kpr[:, :], in0=kpr[:, :], scalar1=gk, scalar2=None, op0=ALU.mult), reads=[b_kpr, b_pp], writes=[b_kpr])
                pt, pb = PS.next()
                S.op("pe", lambda e, pt=pt: e.matmul(pt[0:32, :], lhsT=rot32[:, :], rhs=kpr[:, :], start=True, stop=True), reads=[b_kpr, b_const], writes=[pb])
                S.op("dve", lambda e, pt=pt: e.tensor_tensor(out=kcs[:, 0, :], in0=pt[0:32, :], in1=kcs[:, 0, :], op=ALU.mult), reads=[pb, b_kcs], writes=[b_kcs])
                S.op("dve", lambda e: e.tensor_tensor(out=kcs[:, 1, :], in0=kpr[:, :], in1=kcs[:, 1, :], op=ALU.mult), reads=[b_kpr, b_kcs], writes=[b_kcs])
                S.op("dve", lambda e: e.tensor_tensor(out=kpo[:, :], in0=kcs[:, 0, :], in1=kcs[:, 1, :], op=ALU.add), reads=[b_kcs], writes=[b_kpo])
                S.dma("sp", lambda e, G=G: e.dma_start(out=kpe_d[:, G * GT:(G + 1) * GT], in_=kpo[:, :]), reads=[b_kpo], writes=[b_kpe_d])
                yield
                pt, pb = PS.next()
                for jc in range(2):
                    S.op("pe", lambda e, pt=pt, jc=jc: e.matmul(pt[:, :], lhsT=ones_f[:, :], rhs=kvsq[:, jc, :], start=(jc == 0), stop=(jc == 1)), reads=[b_kvsq, b_const], writes=[pb])
                S.op("act", lambda e, pt=pt: e.activation(out=rbc[:, :], in_=pt[:, :], func=AF.Sqrt, scale=1.0 / 256, bias=eps_t[:, 0:1]), reads=[pb], writes=[b_rbc])
                S.op("dve", lambda e: e.reciprocal(out=rbc[:, :], in_=rbc[:, :]), reads=[b_rbc], writes=[b_rbc])
                yield
                for jc in range(2):
                    S.op("dve", lambda e, jc=jc: e.tensor_tensor(out=kvn[:, jc, :], in0=kvc[:, jc, :], in1=rbc[:, :], op=ALU.mult), reads=[b_kvc, b_rbc], writes=[b_kvn])
                kb = ktn[G % 2]; bkb = b_ktn[G % 2]
                for hp in range(8):
                    pt, pb = PS.next()
                    for kc in range(2):
                        S.op("pe", lambda e, pt=pt, kc=kc, hp=hp: e.matmul(pt[:, :], lhsT=wukK[:, kc, hp * 128:(hp + 1) * 128], rhs=kvn[:, kc, :], start=(kc == 0), stop=(kc == 1)),
                             reads=[b_wuk, b_kvn], writes=[pb])
                    S.op("act", lambda e, pt=pt, hp=hp, kb=kb: e.activation(out=kb[:, hp, :], in_=pt[:, :], func=AF.Copy), reads=[pb], writes=[bkb])
                    q_ = ksq[hp % 2]; bq_ = b_ksq[hp % 2]
                    S.op("act", lambda e, pt=pt, q_=q_: e.activation(out=q_[:, :], in_=pt[:, :], func=AF.Square), reads=[pb], writes=[bq_])
                    bo = ppc("blockones", 0, 128, 2)
                    for t4 in range(4):
                        S.op("pe", lambda e, pst=pst, q_=q_, t4=t4, hp=hp, bo=bo: e.matmul(pst[:, t4 * 16 + 2 * hp:t4 * 16 + 2 * hp + 2], lhsT=q_[:, t4 * 128:(t4 + 1) * 128], rhs=bo, start=True, stop=True, skip_group_check=True),
                             reads=[bq_, b_pp], writes=[pbst])
                    yield
                yield
                for t4 in range(4):
                    S.op("pe", lambda e, t4=t4: e.matmul(pst[:, 64 + t4:65 + t4], lhsT=kpsq[:, t4 * 128:(t4 + 1) * 128], rhs=ones_f[0:32, 0:1], start=True, stop=True, skip_group_check=True),
                         reads=[b_kpsq, b_const], writes=[pbst])
                S.op("dve", lambda e: e.tensor_copy(out=sstat[:, 0:4], in_=pst[:, 64:68]), reads=[pbst], writes=[b_sstat])
                for t4 in range(4):
                    tile_i = G * 4 + t4
                    S.op("dve", lambda e, pst=pst, t4=t4, tile_i=tile_i: e.tensor_scalar(out=rk_all[:, tile_i, :], in0=pst[:, t4 * 16:(t4 + 1) * 16], scalar1=sstat[:, t4:t4 + 1], scalar2=None, op0=ALU.add),
                         reads=[pbst, b_sstat], writes=[b_rk])
                S.dma("sp", lambda e, G=G, kb=kb: e.dma_start(out=ktn_d[:, G * GT:(G + 1) * GT].rearrange("(hp p) t -> p hp t", p=128), in_=kb[:, :, :]), reads=[bkb], writes=[b_ktn_d])
                for t4 in range(4):
                    for half in range(2):
                        pt, pb = PS.next()
                        for kc in range(2):
                            S.op("pe", lambda e, pt=pt, kc=kc, half=half, t4=t4: e.matmul(pt[:, :], lhsT=kvn[:, kc, t4 * 128:(t4 + 1) * 128], rhs=wukV[:, kc, half * 512:(half + 1) * 512], start=(kc == 0), stop=(kc == 1)),
                                 reads=[b_wuk, b_kvn], writes=[pb])
                        dst = vau[:, half * 8:(half + 1) * 8, t4, 0:64]
                        src = pt[:, :].rearrange("p (h d) -> p h d", h=8)
                        if half == 0:
                            S.op("act", lambda e, dst=dst, src=src: e.activation(out=dst, in_=src, func=AF.Copy), reads=[pb], writes=[b_vau])
                        else:
                            S.op("dve", lambda e, dst=dst, src=src: e.tensor_copy(out=dst, in_=src), reads=[pb], writes=[b_vau])
                    yield
                S.dma("sp", lambda e, G=G: e.dma_start(out=v_d[:, G, :, :].rearrange("h p c -> p h c"), in_=vau[:].rearrange("p h b c -> p h (b c)")), reads=[b_vau], writes=[b_v_d])
                yield
            def gen_LRU(G):
                for jc in range(8):
                    ptc, pbc = PS.next()
                    for j in range(4):
                        S.op("pe", lambda e, ptc=ptc, jc=jc, j=j: e.matmul(ptc[:, :], lhsT=diagw[:, jc, j, :], rhs=xr[:, jc, j:j + GT], start=(j == 0), stop=(j == 3)),
                             reads=[b_diagw, b_xr[jc]], writes=[pbc])
                    S.op("dve", lambda e, ptc=ptc, jc=jc: e.tensor_scalar(out=xa[:, jc, :], in0=ptc[:, :], scalar1=ppc("convb", jc), scalar2=None, op0=ALU.add), reads=[pbc, b_pp], writes=[b_xa[jc]])
                    S.op("dve", lambda e, jc=jc: e.tensor_copy(out=xab[:, jc, :], in_=xa[:, jc, :]), reads=[b_xa[jc]], writes=[b_xab[jc]])
                    S.op("dve", lambda e, jc=jc: e.tensor_copy(out=xr[:, jc, 0:3], in_=xr[:, jc, GT:GT + 3]), reads=[b_xr[jc]], writes=[b_xr[jc]])
                    yield
                for quad in range(2):
                    for jq in range(4):
                        jc = quad * 4 + jq
                        blk = jc // 2
                        s2 = jc % 2
                        ptr, pbr = PS.next()
                        for ic in range(2):
                            S.op("pe", lambda e, ptr=ptr, ic=ic, blk=blk, jc=jc: e.matmul(ptr[:, :], lhsT=wab[:, blk * 2 + ic, (jc % 2) * 128:(jc % 2 + 1) * 128], rhs=xab[:, blk * 2 + ic, :], start=(ic == 0), stop=(ic == 1)),
                                 reads=[b_wab, b_xab[blk * 2 + ic]], writes=[pbr])
                        pti, pbi = PS.next()
                        for ic in range(2):
                            S.op("pe", lambda e, pti=pti, ic=ic, blk=blk, jc=jc: e.matmul(pti[:, :], lhsT=wxb[:, blk * 2 + ic, (jc % 2) * 128:(jc % 2 + 1) * 128], rhs=xab[:, blk * 2 + ic, :], start=(ic == 0), stop=(ic == 1)),
                                 reads=[b_wab, b_xab[blk * 2 + ic]], writes=[pbi])
                        S.op("act", lambda e, ptr=ptr, s2=s2, jc=jc: e.activation(out=tr[s2][:, :], in_=ptr[:, :], func=AF.Tanh, scale=0.5, bias=c12[:, 24 + jc:25 + jc]), reads=[pbr, b_c12], writes=[b_tr[s2]])
                        S.op("act", lambda e, pti=pti, jq=jq, jc=jc: e.activation(out=ti[jq][:, :], in_=pti[:, :], func=AF.Tanh, scale=0.5, bias=c12[:, 32 + jc:33 + jc]), reads=[pbi, b_c12], writes=[b_ti[jq]])
                        S.op("act", lambda e, s2=s2, jq=jq, jc=jc: e.activation(out=ta[jq][:, :], in_=tr[s2][:, :], func=AF.Exp, scale=c12[:, 16 + jc:17 + jc], bias=c12[:, 16 + jc:17 + jc]), reads=[b_tr[s2], b_c12], writes=[b_ta[jq]])
                        S.op("act", lambda e, s2=s2, jq=jq, jc=jc: e.activation(out=tm[jq][:, :], in_=tr[s2][:, :], func=AF.Exp, scale=c12[:, jc:jc + 1], bias=c12[:, jc:jc + 1]), reads=[b_tr[s2], b_c12], writes=[b_tm[jq]])
                        S.op("dve", lambda e, jq=jq: e.tensor_scalar(out=tm[jq][:, :], in0=tm[jq][:, :], scalar1=-0.25, scalar2=0.25, op0=ALU.mult, op1=ALU.add), reads=[b_tm[jq]], writes=[b_tm[jq]])
                        S.op("dve", lambda e, jq=jq, jc=jc: e.scalar_tensor_tensor(out=ti[jq][:, :], in0=ti[jq][:, :], scalar=1.0, in1=xa[:, jc, :], op0=ALU.add, op1=ALU.mult), reads=[b_ti[jq], b_xa[jc]], writes=[b_ti[jq]])
                        yield
                    for jq in range(4):
                        S.op("act", lambda e, jq=jq: e.activation(out=tm[jq][:, :], in_=tm[jq][:, :], func=AF.Sqrt), reads=[b_tm[jq]], writes=[b_tm[jq]])
                    yield
                    for jq in range(4):
                        S.op("dve", lambda e, jq=jq: e.tensor_tensor(out=ti[jq][:, :], in0=ti[jq][:, :], in1=tm[jq][:, :], op=ALU.mult), reads=[b_ti[jq], b_tm[jq]], writes=[b_ti[jq]])
                    yield
                    for jq in range(4):
                        jc = quad * 4 + jq
                        S.op("dve", lambda e, jq=jq, jc=jc: e.tensor_tensor_scan(out=tm[jq][:, :], data0=ta[jq][:, :], data1=ti[jq][:, :], initial=hst[:, jc:jc + 1], op0=ALU.mult, op1=ALU.add),
                             reads=[b_ta[jq], b_ti[jq], b_hst8[jc]], writes=[b_tm[jq]])
                    for jq in range(4):
                        jc = quad * 4 + jq
                        S.op("dve", lambda e, jq=jq, jc=jc: e.tensor_copy(out=hst[:, jc:jc + 1], in_=tm[jq][:, GT - 1:GT]), reads=[b_tm[jq]], writes=[b_hst8[jc]])
                    yield
                    for jq in range(4):
                        jc = quad * 4 + jq
                        m = G // 8
                        oh = ppc("onehot", G % 8)
                        ohb = own_hb[m % 2]; bohb = b_ownhb[m % 2]
                        if G % 8 == 0:
                            S.op("dve", lambda e, jq=jq, jc=jc, ohb=ohb, oh=oh: e.tensor_scalar(out=ohb[:, jc, :], in0=tm[jq][:, :], scalar1=oh, scalar2=None, op0=ALU.mult),
                                 reads=[b_tm[jq], b_pp], writes=[b_oh8[m % 2][jc]])
                        else:
                            S.op("dve", lambda e, jq=jq, jc=jc, ohb=ohb, oh=oh: e.scalar_tensor_tensor(out=ohb[:, jc, :], in0=tm[jq][:, :], scalar=oh, in1=ohb[:, jc, :], op0=ALU.mult, op1=ALU.add),
                                 reads=[b_tm[jq], b_pp, b_oh8[m % 2][jc]], writes=[b_oh8[m % 2][jc]])
                if G % 8 == 7:
                    m = G // 8
                    ohb = own_hb[m % 2]; bohb = b_ownhb[m % 2]
                    S.dma("sp", lambda e, m=m, ohb=ohb: e.dma_start(out=ownh_d[:, m * 8 * GT:(m + 1) * 8 * GT], in_=ohb[:].rearrange("p j t -> p (j t)")), reads=b_oh8[m % 2], writes=[b_ownh_d])
                    if debug:
                        S.dma("sp", lambda e, m=m, ohb=ohb: e.dma_start(out=dbg["hlb"][:, m * GT:(m + 1) * GT].rearrange("(jc p) t -> p jc t", p=128), in_=ohb[:]), reads=b_oh8[m % 2], writes=[b_out])
                yield
            def interleave(*gens):
                gens = list(gens)
                while gens:
                    for g in list(gens):
                        try:
                            next(g)
                        except StopIteration:
                            gens.remove(g)
            for _ in gen_H(0):
                pass
            for G in range(n_groups_A):
                emit_M(G)
                gl = [gen_KV(G), gen_LRU(G)]
                if G + 1 < n_groups_A:
                    gl.append(gen_H(G + 1))
                interleave(*gl)
            S.op("act", lambda e: e.activation(out=rk_all[:], in_=rk_all[:], func=AF.Sqrt, bias=eps96[:, 0:1]), reads=[b_rk, b_const], writes=[b_rk])
            S.op("dve", lambda e: e.reciprocal(out=rk_all[:], in_=rk_all[:]), reads=[b_rk], writes=[b_rk])
            PS.release(pst_idx)
            S.dma("sp", lambda e: e.dma_start(out=rk_d, in_=rk_all[:].rearrange("p a b -> p (a b)")), reads=[b_rk], writes=[b_rk_d])
            if debug:
                dtmp = xa; b_dt = Buf()
                S.barrier()
                S.dma("sp", lambda e: e.dma_start(out=dbg["rk"], in_=rk_all[:].rearrange("p a b -> p (a b)")), reads=[b_rk], writes=[b_out])
                S.dma("sp", lambda e: e.dma_start(out=dbg["ktn"], in_=ktn_d[0:128, 0:512]), reads=[b_ktn_d], writes=[b_out])
                S.dma("sp", lambda e: e.dma_start(out=dbg["kpe"], in_=kpe_d[:, 0:512]), reads=[b_kpe_d], writes=[b_out])
                S.dma("sp", lambda e: e.dma_start(out=dbg["v"], in_=v_d[0, 0, :, :]), reads=[b_v_d], writes=[b_out])
            S.barrier()

        def own_group_uT(ph, m, xtb, b_xtb, ssb, b_ssb, uTo, b_uTo):
            for t4 in range(4):
                i = t4 % 2
                r0 = m * GT + t4 * 128
                load_norm_transpose(x_own[r0:r0 + 128, :], xtb[i], b_xtb[i], ssb[i], b_ssb[i], uTo, b_uTo, t4)

        if stop_after not in ("A1", "A") and "1" in phases:
          with ExitStack() as pb1:
            NB1 = 1024 + 2048
            winB = sb(pb1, "winB", [128, 8, NB1], BF16); b_winB = Buf()
            with ExitStack() as wp:
                load_weight_cols(wp, winB, b_winB, w_in_d, [(1024, 2048), (3104, 5152)], 8, "gmix", "wB", NB1)
                S.barrier()
            xtb = [sb(pb1, "xtB%d" % i, [128, D]) for i in range(2)]; b_xtb = [Buf(), Buf()]
            ssb = [sb(pb1, "ssB%d" % i, [128, 4]) for i in range(2)]; b_ssb = [Buf(), Buf()]
            uTo = [sb(pb1, "uTB%d" % i, [128, 8, GT], BF16) for i in range(2)]; b_uTo = [Buf(), Buf()]
            oh = sb(pb1, "ohB", [128, 8, GT], BF16); b_oh = Buf()
            gs = [sb(pb1, "gsB%d" % i, [128, GT]) for i in range(2)]; b_gs = [Buf(), Buf()]
            g2 = [sb(pb1, "g2B%d" % i, [128, GT]) for i in range(2)]; b_g2 = [Buf(), Buf()]
            sa = [sb(pb1, "saB%d" % i, [128, GT]) for i in range(2)]; b_sa = [Buf(), Buf()]
            gao = sb(pb1, "gaoB", [128, 8, GT], BF16); b_gao = Buf()
            sbo = sb(pb1, "sboB", [128, 8, GT], BF16); b_sbo = Buf()
            def interleave(*gens):
                gens = list(gens)
                while gens:
                    for g in list(gens):
                        try:
                            next(g)
                        except StopIteration:
                            gens.remove(g)

            def gen_headB(m, xtb, b_xtb, ssb, b_ssb, uTo, b_uTo):
                u = uTo[m % 2]; bu = b_uTo[m % 2]
                for t4 in range(4):
                    i = t4 % 2
                    r0 = m * GT + t4 * 128
                    load_norm_transpose(x_own[r0:r0 + 128, :], xtb[i], b_xtb[i], ssb[i], b_ssb[i], u, bu, t4)
                    yield

            def gen_chunksB1(m, par):
                u = uTo[m % 2]; bu = b_uTo[m % 2]
                for jc in range(par, 8, 2):
                    s_ = jc % 2
                    pt, pb = PS.next()
                    for kc in range(8):
                        S.op("pe", lambda e, pt=pt, jc=jc, kc=kc, u=u: e.matmul(pt[:, :], lhsT=winB[:, kc, jc * 128:(jc + 1) * 128], rhs=u[:, kc, :], start=(kc == 0), stop=(kc == 7)),
                             reads=[b_winB, bu], writes=[pb])
                    S.op("act", lambda e, pt=pt, s_=s_: e.activation(out=gs[s_][:, :], in_=pt[:, :], func=AF.Copy), reads=[pb], writes=[b_gs[s_]])
                    S.op("act", lambda e, pt=pt, s_=s_: e.activation(out=g2[s_][:, :], in_=pt[:, :], func=AF.Square), reads=[pb], writes=[b_g2[s_]])
                    yield
                    S.op("dve", lambda e, s_=s_: e.tensor_scalar(out=g2[s_][:, :], in0=g2[s_][:, :], scalar1=0.044715, scalar2=1.0, op0=ALU.mult, op1=ALU.add), reads=[b_g2[s_]], writes=[b_g2[s_]])
                    S.op("dve", lambda e, s_=s_: e.tensor_tensor(out=g2[s_][:, :], in0=g2[s_][:, :], in1=gs[s_][:, :], op=ALU.mult), reads=[b_g2[s_], b_gs[s_]], writes=[b_g2[s_]])
                    S.op("act", lambda e, s_=s_: e.activation(out=g2[s_][:, :], in_=g2[s_][:, :], func=AF.Sigmoid, scale=1.5957691216057308), reads=[b_g2[s_]], writes=[b_g2[s_]])
                    S.op("dve", lambda e, s_=s_: e.tensor_tensor(out=gs[s_][:, :], in0=gs[s_][:, :], in1=g2[s_][:, :], op=ALU.mult), reads=[b_g2[s_], b_gs[s_]], writes=[b_gs[s_]])
                    S.op("dve", lambda e, s_=s_, jc=jc: e.tensor_tensor(out=gs[s_][:, :], in0=gs[s_][:, :], in1=oh[:, jc, :], op=ALU.mult), reads=[b_oh, b_gs[s_]], writes=[b_gs[s_]])
                    yield
                    pt, pb = PS.next()
                    for kc in range(8):
                        S.op("pe", lambda e, pt=pt, jc=jc, kc=kc, u=u: e.matmul(pt[:, :], lhsT=winB[:, kc, 1024 + jc * 128:1024 + (jc + 1) * 128], rhs=u[:, kc, :], start=(kc == 0), stop=(kc == 7)),
                             reads=[b_winB, bu], writes=[pb])
                    S.op("act", lambda e, pt=pt, s_=s_: e.activation(out=sa[s_][:, :], in_=pt[:, :], func=AF.Sigmoid), reads=[pb], writes=[b_sa[s_]])
                    S.op("dve", lambda e, s_=s_, jc=jc: e.tensor_tensor(out=gao[:, jc, :], in0=gs[s_][:, :], in1=sa[s_][:, :], op=ALU.mult), reads=[b_sa[s_], b_gs[s_]], writes=[b_gao])
                    yield
                    pt, pb = PS.next()
                    for kc in range(8):
                        S.op("pe", lambda e, pt=pt, jc=jc, kc=kc, u=u: e.matmul(pt[:, :], lhsT=winB[:, kc, 2048 + jc * 128:2048 + (jc + 1) * 128], rhs=u[:, kc, :], start=(kc == 0), stop=(kc == 7)),
                             reads=[b_winB, bu], writes=[pb])
                    S.op("act", lambda e, pt=pt, jc=jc: e.activation(out=sbo[:, jc, :], in_=pt[:, :], func=AF.Sigmoid), reads=[pb], writes=[b_sbo])
                    yield

            for _ in gen_headB(0, xtb, b_xtb, ssb, b_ssb, uTo, b_uTo):
                pass
            for m in range(NOWN):
                S.dma("sp", lambda e, m=m: e.dma_start(out=oh[:].rearrange("p j t -> p (j t)"), in_=ownh_d[:, m * 8 * GT:(m + 1) * 8 * GT]), reads=[b_ownh_d], writes=[b_oh])
                gl = [gen_chunksB1(m, 0), gen_chunksB1(m, 1)]
                if m + 1 < NOWN:
                    gl.append(gen_headB(m + 1, xtb, b_xtb, ssb, b_ssb, uTo, b_uTo))
                interleave(*gl)
                S.dma("sp", lambda e, m=m: e.dma_start(out=gaya_d[:, :, m * GT:(m + 1) * GT], in_=gao[:]), reads=[b_gao], writes=[b_gaya_d])
                S.dma("sp", lambda e, m=m: e.dma_start(out=sgb_d[:, :, m * GT:(m + 1) * GT], in_=sbo[:]), reads=[b_sbo], writes=[b_sgb_d])
            S.barrier()

          with ExitStack() as pb2:
            winQ = sb(pb2, "winQ", [128, 8, 768], BF16); b_winQ = Buf()
            wuq = sb(pb2, "wuq", [128, 6, 1536], BF16); b_wuq = Buf()
            with ExitStack() as wp:
                load_weight_cols(wp, winQ, b_winQ, w_in_d, [(2048, 2816)], 8, "gmix", "wQ", 768)
                load_weight_cols(wp, wuq, b_wuq, wuq_d, [(0, 1536)], 6, "qag", "wUQ", 1536)
                S.barrier()
            xtb = [sb(pb2, "xtQ%d" % i, [128, D]) for i in range(2)]; b_xtb = [Buf(), Buf()]
            ssb = [sb(pb2, "ssQ%d" % i, [128, 4]) for i in range(2)]; b_ssb = [Buf(), Buf()]
            uTo = [sb(pb2, "uTQ%d" % i, [128, 8, GT], BF16) for i in range(2)]; b_uTo = [Buf(), Buf()]
            cosf = sb(pb2, "cosf", [96, TOWN]); sinf = sb(pb2, "sinf", [96, TOWN]); b_cs = Buf()
            S.op("dve", lambda e: e.memset(cosf[0:64, :], 1.0), writes=[b_cs])
            S.op("dve", lambda e: e.memset(sinf[0:64, :], 0.0), writes=[b_cs])
            S.dma("sp", lambda e: e.dma_start(out=sinf[64:96, :], in_=qcs_d[0:32, :]), reads=[b_dram_cs], writes=[b_cs])
            S.dma("sp", lambda e: e.dma_start(out=cosf[64:96, :], in_=qcs_d[32:64, :]), reads=[b_dram_cs], writes=[b_cs])
            qc = sb(pb2, "qc", [128, 6, GT]); b_qc = Buf()
            qsq = sb(pb2, "qsq", [128, 6, GT]); b_qsq = Buf()
            qcn = sb(pb2, "qcn", [128, 6, GT], BF16); b_qcn = Buf()
            rbq = sb(pb2, "rbq", [128, GT]); b_rbq = Buf()
            qs = [sb(pb2, "qs%d" % i, [96, GT]) for i in range(2)]; b_qs = [Buf(), Buf()]
            qq = [sb(pb2, "qq%d" % i, [96, GT]) for i in range(2)]; b_qq = [Buf(), Buf()]
            qr = [sb(pb2, "qr%d" % i, [96, GT]) for i in range(2)]; b_qr = [Buf(), Buf()]
            qn = [sb(pb2, "qn%d" % i, [96, GT]) for i in range(2)]; b_qn = [Buf(), Buf()]
            qto = sb(pb2, "qto", [96, 16, GT], BF16); b_qto = Buf()
            def gen_headsB2(m, par):
                for h in range(par, 16, 2):
                    s_ = h % 2
                    pt, pb = PS.next()
                    for kc in range(6):
                        S.op("pe", lambda e, pt=pt, h=h, kc=kc: e.matmul(pt[0:96, :], lhsT=wuq[:, kc, h * 96:(h + 1) * 96], rhs=qcn[:, kc, :], start=(kc == 0), stop=(kc == 5)),
                             reads=[b_wuq, b_qcn], writes=[pb])
                    S.op("act", lambda e, pt=pt, s_=s_: e.activation(out=qs[s_][:, :], in_=pt[0:96, :], func=AF.Copy), reads=[pb], writes=[b_qs[s_]])
                    S.op("act", lambda e, pt=pt, s_=s_: e.activation(out=qq[s_][:, :], in_=pt[0:96, :], func=AF.Square), reads=[pb], writes=[b_qq[s_]])
                    yield
                    pt2, pb2_ = PS.next()
                    S.op("pe", lambda e, pt2=pt2, s_=s_: e.matmul(pt2[0:96, :], lhsT=ones_f[0:96, 0:96], rhs=qq[s_][:, :], start=True, stop=True), reads=[b_qq[s_], b_const], writes=[pb2_])
                    S.op("act", lambda e, pt2=pt2, s_=s_: e.activation(out=qr[s_][:, :], in_=pt2[0:96, :], func=AF.Sqrt, scale=1.0 / 96, bias=eps_t[0:96, 0:1]), reads=[pb2_], writes=[b_qr[s_]])
                    S.op("dve", lambda e, s_=s_: e.reciprocal(out=qr[s_][:, :], in_=qr[s_][:, :]), reads=[b_qr[s_]], writes=[b_qr[s_]])
                    yield
                    qng = ppc("qng", 0, 96)
                    S.op("dve", lambda e, s_=s_, qng=qng: e.scalar_tensor_tensor(out=qn[s_][:, :], in0=qs[s_][:, :], scalar=qng, in1=qr[s_][:, :], op0=ALU.mult, op1=ALU.mult),
                         reads=[b_qs[s_], b_qr[s_], b_pp], writes=[b_qn[s_]])
                    pt3, pb3 = PS.next()
                    S.op("pe", lambda e, pt3=pt3, s_=s_: e.matmul(pt3[0:96, :], lhsT=rot96[:, :], rhs=qn[s_][:, :], start=True, stop=True), reads=[b_qn[s_], b_const], writes=[pb3])
                    S.op("dve", lambda e, pt3=pt3, s_=s_, m=m: e.tensor_tensor(out=qq[s_][:, :], in0=pt3[0:96, :], in1=sinf[:, m * GT:(m + 1) * GT], op=ALU.mult), reads=[pb3, b_cs], writes=[b_qq[s_]])
                    yield
                    S.op("dve", lambda e, s_=s_, m=m: e.tensor_tensor(out=qn[s_][:, :], in0=qn[s_][:, :], in1=cosf[:, m * GT:(m + 1) * GT], op=ALU.mult), reads=[b_qn[s_], b_cs], writes=[b_qn[s_]])
                    S.op("dve", lambda e, s_=s_: e.tensor_tensor(out=qn[s_][:, :], in0=qn[s_][:, :], in1=qq[s_][:, :], op=ALU.add), reads=[b_qn[s_], b_qq[s_]], writes=[b_qn[s_]])
                    gkf = ppc("gkfold", 0, 96)
                    S.op("act", lambda e, s_=s_, h=h, gkf=gkf: e.activation(out=qto[:, h, :], in_=qn[s_][:, :], func=AF.Copy, scale=gkf), reads=[b_qn[s_], b_pp], writes=[b_qto])
                    yield

            for _ in gen_headB(0, xtb, b_xtb, ssb, b_ssb, uTo, b_uTo):
                pass
            for m in range(NOWN):
                u = uTo[m % 2]; bu = b_uTo[m % 2]
                for jc in range(6):
                    pt, pb = PS.next()
                    for kc in range(8):
                        S.op("pe", lambda e, pt=pt, jc=jc, kc=kc, u=u: e.matmul(pt[:, :], lhsT=winQ[:, kc, jc * 128:(jc + 1) * 128], rhs=u[:, kc, :], start=(kc == 0), stop=(kc == 7)),
                             reads=[b_winQ, bu], writes=[pb])
                    S.op("act", lambda e, pt=pt, jc=jc: e.activation(out=qc[:, jc, :], in_=pt[:, :], func=AF.Copy), reads=[pb], writes=[b_qc])
                    S.op("dve", lambda e, pt=pt, jc=jc: e.tensor_tensor(out=qsq[:, jc, :], in0=pt[:, :], in1=qc[:, jc, :], op=ALU.mult), reads=[pb, b_qc], writes=[b_qsq])
                pt, pb = PS.next()
                for jc in range(6):
                    S.op("pe", lambda e, pt=pt, jc=jc: e.matmul(pt[:, :], lhsT=ones_f[:, :], rhs=qsq[:, jc, :], start=(jc == 0), stop=(jc == 5)), reads=[b_qsq, b_const], writes=[pb])
                S.op("act", lambda e, pt=pt: e.activation(out=rbq[:, :], in_=pt[:, :], func=AF.Sqrt, scale=1.0 / 768, bias=eps_t[:, 0:1]), reads=[pb], writes=[b_rbq])
                S.op("dve", lambda e: e.reciprocal(out=rbq[:, :], in_=rbq[:, :]), reads=[b_rbq], writes=[b_rbq])
                for jc in range(6):
                    S.op("dve", lambda e, jc=jc: e.tensor_tensor(out=qcn[:, jc, :], in0=qc[:, jc, :], in1=rbq[:, :], op=ALU.mult), reads=[b_qc, b_rbq], writes=[b_qcn])
                gl = [gen_headsB2(m, 0), gen_headsB2(m, 1)]
                if m + 1 < NOWN:
                    gl.append(gen_headB(m + 1, xtb, b_xtb, ssb, b_ssb, uTo, b_uTo))
                interleave(*gl)
                S.dma("sp", lambda e, m=m: e.dma_start(out=qt_d[:, :, m * GT:(m + 1) * GT], in_=qto[:]), reads=[b_qto], writes=[b_qt_d])
            S.barrier()

        if stop_after not in ("A1", "A", "B") and "C" in phases:
          with ExitStack() as pc:
            masks = sb(pc, "masks", [128, 32, GT], BF16); b_masks = Buf()
            S.dma("sp", lambda e: e.dma_start(out=masks[:], in_=masks_d), writes=[b_masks])
            rk = sb(pc, "rkC", [128, SEQ // 128, 16]); b_rkc = Buf()
            S.dma("sp", lambda e: e.dma_start(out=rk[:].rearrange("p a b -> p (a b)"), in_=rk_d), reads=[b_rk_d], writes=[b_rkc])
            qth = [sb(pc, "qth%d" % i, [96, TOWN], BF16) for i in range(2)]; b_qth = [Buf(), Buf()]
            NR = 4
            kp = [sb(pc, "kp%d" % i, [96, GT], BF16) for i in range(NR)]; b_kp = [Buf() for _ in range(NR)]
            vp = [sb(pc, "vp%d" % i, [128, 4, 128], BF16) for i in range(NR)]; b_vp = [Buf() for _ in range(NR)]
            NPB = 4
            pbuf = [sb(pc, "pb%d" % i, [128, GT], BF16) for i in range(NPB)]; b_pbuf = [Buf() for _ in range(NPB)]
            gat = sb(pc, "gat", [128, TOWN], BF16); b_gat = Buf()
            sgt = sb(pc, "sgt", [128, TOWN], BF16); b_sgt = Buf()
            mgt = sb(pc, "mgt", [128, TOWN], BF16); b_mgt = Buf()
            rl = sb(pc, "rl", [128, GT]); b_rl = Buf()
            rl2 = sb(pc, "rl2", [128, GT]); b_rl2 = Buf()
            ybn = sb(pc, "ybn", [128, GT]); b_ybn = Buf()
            ybs = sb(pc, "ybs", [128, GT]); b_ybs = Buf()
            (s_banks, s_idx) = PS.reserve(4)
            (o_banks, o_idx) = PS.reserve(4)
            n_heads_c = 16 if stop_after != "C1" else 2
            piece = 0
            stepno = 0
            for h in range(n_heads_c):
                q_ = qth[h % 2]; bq_ = b_qth[h % 2]
                S.dma("sp", lambda e, h=h, q_=q_: e.dma_start(out=q_[:, :], in_=qt_d[:, h, :]), reads=[b_qt_d], writes=[bq_])
                hr = (h % 2) * 64
                kcx = h // 2
                S.dma("sp", lambda e, hr=hr, kcx=kcx: e.dma_start(out=gat[hr:hr + 64, :], in_=gaya_d[hr:hr + 64, kcx, :]), reads=[b_gaya_d], writes=[b_gat])
                S.dma("sp", lambda e, hr=hr, kcx=kcx: e.dma_start(out=sgt[hr:hr + 64, :], in_=sgb_d[hr:hr + 64, kcx, :]), reads=[b_sgb_d], writes=[b_sgt])
                pendq = []

                def emit_pv(t_):
                    (ot2, ob2, vt2, bvt2, blk2, pbf2, bpbf2, f2, l2) = t_
                    S.op("pe", lambda e: e.matmul(ot2[:, :], lhsT=vt2[:, blk2, :], rhs=pbf2[:, :], start=f2, stop=l2),
                         reads=[bvt2, bpbf2], writes=[ob2])
                for G in range(NG):
                    r = piece % NR; piece += 1
                    kt = kp[r]; bkt = b_kp[r]; vt = vp[r]; bvt = b_vp[r]
                    S.dma("sp", lambda e, h=h, G=G, kt=kt: e.dma_start(out=kt[0:64, :], in_=ktn_d[h * 64:(h + 1) * 64, G * GT:(G + 1) * GT]), reads=[b_ktn_d], writes=[bkt])
                    S.dma("sp", lambda e, G=G, kt=kt: e.dma_start(out=kt[64:96, :], in_=kpe_d[:, G * GT:(G + 1) * GT]), reads=[b_kpe_d], writes=[bkt])
                    S.dma("sp", lambda e, h=h, G=G, vt=vt: e.dma_start(out=vt[:].rearrange("p b c -> p (b c)"), in_=v_d[h, G, :, :]), reads=[b_v_d], writes=[bvt])
                    for m in range(G // 8, NOWN):
                        ot, ob = o_banks[m]
                        for blk in range(4):
                            si = stepno % 4; stepno += 1
                            st_, sb_ = s_banks[si]
                            pbf = pbuf[si]; bpbf = b_pbuf[si]
                            S.op("pe", lambda e, st_=st_, kt=kt, blk=blk, q_=q_, m=m: e.matmul(st_[:, :], lhsT=kt[:, blk * 128:(blk + 1) * 128], rhs=q_[:, m * GT:(m + 1) * GT], start=True, stop=True),
                                 reads=[bkt, bq_], writes=[sb_])
                            sc = rk[:, G * 4 + blk, h:h + 1]
                            S.op("act", lambda e, st_=st_, pbf=pbf, sc=sc: e.activation(out=pbf[:, :], in_=st_[:, :], func=AF.Exp, scale=sc), reads=[sb_, b_rkc], writes=[bpbf])
                            if G // 8 == m:
                                mj = (G % 8) * 4 + blk
                                S.op("dve", lambda e, pbf=pbf, mj=mj: e.tensor_tensor(out=pbf[:, :], in0=pbf[:, :], in1=masks[:, mj, :], op=ALU.mult), reads=[bpbf, b_masks], writes=[bpbf])
                            first = (G == 0 and blk == 0)
                            last = (G == 8 * m + 7 and blk == 3)
                            pendq.append((ot, ob, vt, bvt, blk, pbf, bpbf, first, last))
                            if len(pendq) > 2:
                                emit_pv(pendq.pop(0))
                    if G % 8 == 7:
                        while pendq:
                            emit_pv(pendq.pop(0))
                        m = G // 8
                        ot, ob = o_banks[m]
                        cs = slice(m * GT, (m + 1) * GT)
                        S.op("dve", lambda e, ot=ot: e.reciprocal(out=rl[64:128, :], in_=ot[64:128, :]), reads=[ob], writes=[b_rl])
                        S.op("dve", lambda e: e.tensor_copy(out=rl2[0:64, :], in_=rl[64:128, :]), reads=[b_rl], writes=[b_rl2])
                        S.op("dve", lambda e, ot=ot: e.tensor_tensor(out=ybn[0:64, :], in0=ot[0:64, :], in1=rl2[0:64, :], op=ALU.mult), reads=[ob, b_rl2], writes=[b_ybn])
                        if hr == 0:
                            ysrc = ybn; bys = b_ybn
                        else:
                            S.op("dve", lambda e: e.tensor_copy(out=ybs[64:128, :], in_=ybn[0:64, :]), reads=[b_ybn], writes=[b_ybs])
                            ysrc = ybs; bys = b_ybs
                        if debug:
                            S.dma("sp", lambda e, ysrc=ysrc, hr=hr, h=h, cs=cs: e.dma_start(out=dbg["yb"][h * 64:(h + 1) * 64, cs], in_=ysrc[hr:hr + 64, :]), reads=[bys], writes=[b_out])
                        S.op("dve", lambda e, ysrc=ysrc, hr=hr, cs=cs: e.tensor_tensor(out=ysrc[hr:hr + 64, :], in0=ysrc[hr:hr + 64, :], in1=sgt[hr:hr + 64, cs], op=ALU.mult), reads=[bys, b_sgt], writes=[bys])
                        S.op("dve", lambda e, ysrc=ysrc, hr=hr, cs=cs: e.tensor_tensor(out=mgt[hr:hr + 64, cs], in0=ysrc[hr:hr + 64, :], in1=gat[hr:hr + 64, cs], op=ALU.add), reads=[bys, b_gat], writes=[b_mgt])
                S.dma("sp", lambda e, hr=hr, kcx=kcx: e.dma_start(out=mg_d[hr:hr + 64, kcx, :], in_=mgt[hr:hr + 64, :]), reads=[b_mgt], writes=[b_mg_d])
            PS.release(s_idx); PS.release(o_idx)
            S.barrier()

        if stop_after not in ("A1", "A", "B", "C", "C1") and "D" in phases:
          with ExitStack() as pd:
            woutb = sb(pd, "woutb", [128, 8, D], BF16); b_woutb = Buf()
            with ExitStack() as wp:
                load_weight_cols(wp, woutb, b_woutb, wout_d, [(0, D)], 8, None, "wO", D)
                S.barrier()
            wr = sb(pd, "wr", [128, 8, 36]); b_wr = Buf()
            S.dma("sp", lambda e: e.dma_start(out=wr[:], in_=wr_d.rearrange("(kc p) n -> p kc n", p=128)), writes=[b_wr])
            rbias = sb(pd, "rbias", [128, 36]); gfb = sb(pd, "gfb", [128, D]); b_rb = Buf()
            S.dma("sp", lambda e: e.dma_start(out=rbias[:], in_=rbias_d), writes=[b_rb])
            S.dma("sp", lambda e: e.dma_start(out=gfb[:], in_=gffnb_d), writes=[b_rb])
            mgs = [sb(pd, "mgs%d" % i, [128, 8, GT], BF16) for i in range(2)]; b_mgs = [Buf(), Buf()]
            xtd = [sb(pd, "xtD%d" % i, [128, D]) for i in range(2)]; b_xtd = [Buf(), Buf()]
            hm = [sb(pd, "hm%d" % i, [128, D]) for i in range(2)]; b_hm = [Buf(), Buf()]
            xn = [sb(pd, "xn%d" % i, [128, D]) for i in range(2)]; b_xn = [Buf(), Buf()]
            ssd = [sb(pd, "ssD%d" % i, [128, 4]) for i in range(2)]; b_ssd = [Buf(), Buf()]
            xnT = [sb(pd, "xnT%d" % i, [128, 8, 128]) for i in range(2)]; b_xnT = [Buf(), Buf()]
            xgs = sb(pd, "xgs", [128, 8, GT], BF16); b_xgs = Buf()
            cts = sb(pd, "cts", [32, GT]); b_cts = Buf()
            R = {}
            for nm, w in [("lg", 36), ("gmax", 1), ("goh", 4), ("ngm", 1), ("ge", 4), ("gsum", 1), ("pg", 1), ("gpen", 4), ("em", 32),
                          ("t1", 1), ("m1", 32), ("em2", 32), ("t2", 1), ("m2", 32), ("dd", 1), ("ed", 1), ("w1", 1), ("w2", 1), ("comb", 32)]:
                R[nm] = [sb(pd, "r_%s%d" % (nm, i), [128, w]) for i in range(2)]
            b_R = [Buf(), Buf()]
            BIG = 1.0e9
            (ct_l, ct_idx) = PS.reserve(1)
            ctp, ctb = ct_l[0]
            for m in range(NOWN):
                mg_ = mgs[m % 2]; bmg_ = b_mgs[m % 2]
                S.dma("sp", lambda e, m=m, mg_=mg_: e.dma_start(out=mg_[:], in_=mg_d[:, :, m * GT:(m + 1) * GT]), reads=[b_mg_d], writes=[bmg_])
                for t4 in range(4):
                    tt = m * 4 + t4
                    i = tt % 2
                    r0 = tt * 128
                    S.dma("sp", lambda e, r0=r0, i=i: e.dma_start(out=xtd[i][:], in_=x_own[r0:r0 + 128, :]), writes=[b_xtd[i]])
                    for nh in range(2):
                        pt, pb = PS.next()
                        for kc in range(8):
                            S.op("pe", lambda e, pt=pt, kc=kc, nh=nh, t4=t4, mg_=mg_: e.matmul(pt[:, :], lhsT=mg_[:, kc, t4 * 128:(t4 + 1) * 128], rhs=woutb[:, kc, nh * 512:(nh + 1) * 512], start=(kc == 0), stop=(kc == 7)),
                                 reads=[bmg_, b_woutb], writes=[pb])
                        S.op("dve", lambda e, pt=pt, nh=nh, i=i: e.tensor_tensor(out=hm[i][:, nh * 512:(nh + 1) * 512], in0=pt[:, :], in1=xtd[i][:, nh * 512:(nh + 1) * 512], op=ALU.add),
                             reads=[pb, b_xtd[i]], writes=[b_hm[i]])
                    S.dma("sp", lambda e, r0=r0, i=i: e.dma_start(out=hmid_d[r0:r0 + 128, :], in_=hm[i][:]), reads=[b_hm[i]], writes=[b_hmid_d])
                    if debug:
                        S.dma("sp", lambda e, r0=r0, i=i: e.dma_start(out=dbg["hmid"][r0:r0 + 128, :], in_=hm[i][:]), reads=[b_hm[i]], writes=[b_out])
                    ss = ssd[i]; bss = b_ssd[i]
                    S.op("act", lambda e, i=i, ss=ss: e.activation(out=junk[:], in_=hm[i][:], func=AF.Square, accum_out=ss[:, 0:1]), reads=[b_hm[i]], writes=[bss, b_junk])
                    S.op("act", lambda e, ss=ss: e.activation(out=ss[:, 1:2], in_=ss[:, 0:1], func=AF.Sqrt, scale=1.0 / D, bias=eps_t[:, 0:1]), reads=[bss], writes=[bss])
                    S.op("dve", lambda e, ss=ss: e.reciprocal(out=ss[:, 2:3], in_=ss[:, 1:2]), reads=[bss], writes=[bss])
                    S.op("dve", lambda e, ss=ss, i=i: e.scalar_tensor_tensor(out=xn[i][:], in0=hm[i][:], scalar=ss[:, 2:3], in1=gfb[:], op0=ALU.mult, op1=ALU.mult),
                         reads=[bss, b_hm[i], b_rb], writes=[b_xn[i]])
                    for half in range(2):
                        pt, pb = PS.next()
                        for q in range(4):
                            kc = half * 4 + q
                            S.op("pe", lambda e, pt=pt, q=q, kc=kc, i=i: e.transpose(out=pt[:, q * 128:(q + 1) * 128], in_=xn[i][:, kc * 128:(kc + 1) * 128], identity=ident[:]),
                                 reads=[b_xn[i], b_const], writes=[pb])
                        dst = xnT[i][:, half * 4:half * 4 + 4, :]
                        src = pt[:].rearrange("p (q t) -> p q t", q=4)
                        if half == 0:
                            S.op("act", lambda e, dst=dst, src=src: e.activation(out=dst, in_=src, func=AF.Copy), reads=[pb], writes=[b_xnT[i]])
                        else:
                            S.op("dve", lambda e, dst=dst, src=src: e.tensor_copy(out=dst, in_=src), reads=[pb], writes=[b_xnT[i]])
                    S.op("pool", lambda e, i=i, t4=t4: e.tensor_copy(out=xgs[:, :, t4 * 128:(t4 + 1) * 128], in_=xnT[i][:, :, :]), reads=[b_xnT[i]], writes=[b_xgs])
                    pt, pb = PS.next()
                    for kc in range(8):
                        S.op("pe", lambda e, pt=pt, kc=kc, i=i: e.matmul(pt[:, 0:36], lhsT=xnT[i][:, kc, :], rhs=wr[:, kc, :], start=(kc == 0), stop=(kc == 7)),
                             reads=[b_xnT[i], b_wr], writes=[pb])
                    r = {k: v[i] for k, v in R.items()}
                    br = b_R[i]
                    rd = [br, b_rb]
                    S.op("dve", lambda e, pt=pt, r=r: e.tensor_tensor(out=r["lg"][:, :], in0=pt[:, 0:36], in1=rbias[:, :], op=ALU.add), reads=[pb, b_rb, br], writes=[br])
                    S.op("dve", lambda e, r=r: e.tensor_reduce(out=r["gmax"][:, :], in_=r["lg"][:, 0:4], axis=AX.X, op=ALU.max), reads=rd, writes=[br])
                    S.op("dve", lambda e, r=r: e.tensor_scalar(out=r["goh"][:, :], in0=r["lg"][:, 0:4], scalar1=r["gmax"][:, 0:1], scalar2=None, op0=ALU.is_ge), reads=rd, writes=[br])
                    S.op("dve", lambda e, r=r: e.tensor_scalar(out=r["ngm"][:, :], in0=r["gmax"][:, :], scalar1=-1.0, scalar2=None, op0=ALU.mult), reads=rd, writes=[br])
                    S.op("act", lambda e, r=r: e.activation(out=r["ge"][:, :], in_=r["lg"][:, 0:4], func=AF.Exp, bias=r["ngm"][:, 0:1], accum_out=r["gsum"][:, 0:1]), reads=rd, writes=[br])
                    S.op("dve", lambda e, r=r: e.reciprocal(out=r["pg"][:, :], in_=r["gsum"][:, :]), reads=rd, writes=[br])
                    S.op("dve", lambda e, r=r: e.tensor_scalar(out=r["gpen"][:, :], in0=r["goh"][:, :], scalar1=BIG, scalar2=-BIG, op0=ALU.mult, op1=ALU.add), reads=rd, writes=[br])
                    for g in range(4):
                        S.op("dve", lambda e, r=r, g=g: e.tensor_scalar(out=r["em"][:, g * 8:(g + 1) * 8], in0=r["lg"][:, 4 + g * 8:4 + (g + 1) * 8], scalar1=r["gpen"][:, g:g + 1], scalar2=None, op0=ALU.add), reads=rd, writes=[br])
                    S.op("dve", lambda e, r=r: e.tensor_reduce(out=r["t1"][:, :], in_=r["em"][:, :], axis=AX.X, op=ALU.max), reads=rd, writes=[br])
                    S.op("dve", lambda e, r=r: e.tensor_scalar(out=r["m1"][:, :], in0=r["em"][:, :], scalar1=r["t1"][:, 0:1], scalar2=None, op0=ALU.is_ge), reads=rd, writes=[br])
                    S.op("dve", lambda e, r=r: e.scalar_tensor_tensor(out=r["em2"][:, :], in0=r["m1"][:, :], scalar=-BIG, in1=r["em"][:, :], op0=ALU.mult, op1=ALU.add), reads=rd, writes=[br])
                    S.op("dve", lambda e, r=r: e.tensor_reduce(out=r["t2"][:, :], in_=r["em2"][:, :], axis=AX.X, op=ALU.max), reads=rd, writes=[br])
                    S.op("dve", lambda e, r=r: e.tensor_scalar(out=r["m2"][:, :], in0=r["em2"][:, :], scalar1=r["t2"][:, 0:1], scalar2=None, op0=ALU.is_ge), reads=rd, writes=[br])
                    S.op("dve", lambda e, r=r: e.tensor_tensor(out=r["dd"][:, :], in0=r["t2"][:, :], in1=r["t1"][:, :], op=ALU.subtract), reads=rd, writes=[br])
                    S.op("act", lambda e, r=r: e.activation(out=r["ed"][:, :], in_=r["dd"][:, :], func=AF.Exp), reads=rd, writes=[br])
                    S.op("dve", lambda e, r=r: e.tensor_scalar(out=r["w1"][:, :], in0=r["ed"][:, :], scalar1=1.0, scalar2=None, op0=ALU.add), reads=rd, writes=[br])
                    S.op("dve", lambda e, r=r: e.reciprocal(out=r["w1"][:, :], in_=r["w1"][:, :]), reads=rd, writes=[br])
                    S.op("dve", lambda e, r=r: e.tensor_tensor(out=r["w2"][:, :], in0=r["ed"][:, :], in1=r["w1"][:, :], op=ALU.mult), reads=rd, writes=[br])
                    S.op("dve", lambda e, r=r: e.tensor_tensor(out=r["w1"][:, :], in0=r["w1"][:, :], in1=r["pg"][:, :], op=ALU.mult), reads=rd, writes=[br])
                    S.op("dve", lambda e, r=r: e.tensor_tensor(out=r["w2"][:, :], in0=r["w2"][:, :], in1=r["pg"][:, :], op=ALU.mult), reads=rd, writes=[br])
                    S.op("dve", lambda e, r=r: e.tensor_scalar(out=r["comb"][:, :], in0=r["m1"][:, :], scalar1=r["w1"][:, 0:1], scalar2=None, op0=ALU.mult), reads=rd, writes=[br])
                    S.op("dve", lambda e, r=r: e.scalar_tensor_tensor(out=r["comb"][:, :], in0=r["m2"][:, :], scalar=r["w2"][:, 0:1], in1=r["comb"][:, :], op0=ALU.mult, op1=ALU.add), reads=rd, writes=[br])
                    if debug:
                        S.dma("sp", lambda e, r=r, r0=r0: e.dma_start(out=dbg["comb"][r0:r0 + 128, :], in_=r["comb"][:, :]), reads=[br], writes=[b_out])
                    S.op("pe", lambda e, r=r, t4=t4: e.transpose(out=ctp[0:32, t4 * 128:(t4 + 1) * 128], in_=r["comb"][:, 0:32], identity=ident[:]), reads=[br, b_const], writes=[ctb])
                S.op("act", lambda e: e.activation(out=cts[:, :], in_=ctp[0:32, :], func=AF.Copy), reads=[ctb], writes=[b_cts])
                S.dma("sp", lambda e, m=m: e.dma_start(out=combt_d[:, m * GT:(m + 1) * GT], in_=cts[:, :]), reads=[b_cts], writes=[b_combt_d])
                S.dma("sp", lambda e, m=m: e.dma_start(out=xgt_d[:, :, m * GT:(m + 1) * GT], in_=xgs[:]), reads=[b_xgs], writes=[b_xgt_d])
            PS.release(ct_idx)
            S.barrier()

        if stop_after not in ("A1", "A", "B", "C", "C1", "D") and "E" in phases:
          with ExitStack() as pe_:
            yacc = sb(pe_, "yacc", [128, 16, D]); b_yacc = [Buf() for _ in range(16)]
            S.dma("sp", lambda e: e.dma_start(out=yacc[:], in_=hmid_d.rearrange("(t p) f -> p t f", p=128)), reads=[b_hmid_d], writes=b_yacc)
            xg = sb(pe_, "xg", [128, 8, TOWN], BF16); b_xg = Buf()
            S.dma("sp", lambda e: e.dma_start(out=xg[:], in_=xgt_d), reads=[b_xgt_d], writes=[b_xg])
            combT = sb(pe_, "combT", [32, TOWN]); b_combT = Buf()
            S.dma("sp", lambda e: e.dma_start(out=combT[:], in_=combt_d), reads=[b_combt_d], writes=[b_combT])
            sel = [sb(pe_, "sel%d" % i, [32, 128]) for i in range(2)]; b_sel = [Buf(), Buf()]
            wgs2 = [sb(pe_, "wgs%d" % i, [128, 8, DEXP]) for i in range(2)]; wus2 = [sb(pe_, "wus%d" % i, [128, 8, DEXP]) for i in range(2)]
            wds2 = [sb(pe_, "wds%d" % i, [128, 2, D]) for i in range(2)]
            b_wgs2 = [Buf(), Buf()]; b_wus2 = [Buf(), Buf()]; b_wds2 = [Buf(), Buf()]
            wgb = [sb(pe_, "wgb%d" % i, [128, 8, DEXP], BF16) for i in range(2)]
            wub = [sb(pe_, "wub%d" % i, [128, 8, DEXP], BF16) for i in range(2)]
            wdb = [sb(pe_, "wdb%d" % i, [128, 2, D], BF16) for i in range(2)]
            b_wgb = [Buf(), Buf()]; b_wub = [Buf(), Buf()]; b_wdb = [Buf(), Buf()]
            cb = sb(pe_, "cb", [128, GT]); b_cb = Buf()
            sg = [sb(pe_, "sg%d" % i, [128, GT]) for i in range(2)]; b_sg = [Buf(), Buf()]
            tu = [sb(pe_, "tu%d" % i, [128, GT]) for i in range(2)]; b_tu = [Buf(), Buf()]
            hb = [sb(pe_, "hb%d" % i, [128, 2, GT], BF16) for i in range(2)]; b_hb = [Buf(), Buf()]
            n_exp = NEXP if stop_after != "E1" else 2
            for ex in range(n_exp):
                w = ex % 2
                wgs = wgs2[w]; wus = wus2[w]; wds = wds2[w]; b_wgs = b_wgs2[w]; b_wus = b_wus2[w]; b_wds = b_wds2[w]
                S.dma("sp", lambda e, ex=ex, wgs=wgs: e.dma_start(out=wgs[:], in_=wg_d[ex].rearrange("(kc p) j -> p kc j", p=128)), writes=[b_wgs])
                S.dma("sp", lambda e, ex=ex, wus=wus: e.dma_start(out=wus[:], in_=wu_d[ex].rearrange("(kc p) j -> p kc j", p=128)), writes=[b_wus])
                S.dma("sp", lambda e, ex=ex, wds=wds: e.dma_start(out=wds[:], in_=wd_d[ex].rearrange("(jc p) n -> p jc n", p=128)), writes=[b_wds])
                S.op("act", lambda e, w=w, wgs=wgs: e.activation(out=wgb[w][:], in_=wgs[:], func=AF.Copy), reads=[b_wgs], writes=[b_wgb[w]])
                S.op("act", lambda e, w=w, wus=wus: e.activation(out=wub[w][:], in_=wus[:], func=AF.Copy), reads=[b_wus], writes=[b_wub[w]])
                S.op("act", lambda e, w=w, wds=wds: e.activation(out=wdb[w][:], in_=wds[:], func=AF.Copy), reads=[b_wds], writes=[b_wdb[w]])
                S.op("dve", lambda e, w=w, ex=ex: e.tensor_copy(out=sel[w][:, :], in_=ident[0:32, ex:ex + 1].to_broadcast([32, 128])), reads=[b_const], writes=[b_sel[w]])
                for m in range(NOWN):
                    cs = slice(m * GT, (m + 1) * GT)
                    h_ = hb[m % 2]; bh_ = b_hb[m % 2]
                    pt, pb = PS.next()
                    S.op("pe", lambda e, pt=pt, w=w, cs=cs: e.matmul(pt[:, :], lhsT=sel[w][:, :], rhs=combT[:, cs], start=True, stop=True), reads=[b_sel[w], b_combT], writes=[pb])
                    S.op("act", lambda e, pt=pt: e.activation(out=cb[:, :], in_=pt[:, :], func=AF.Copy), reads=[pb], writes=[b_cb])
                    for jc in range(2):
                        s_ = jc
                        ptg, pbg = PS.next()
                        for kc in range(8):
                            S.op("pe", lambda e, ptg=ptg, kc=kc, jc=jc, w=w, cs=cs: e.matmul(ptg[:, :], lhsT=wgb[w][:, kc, jc * 128:(jc + 1) * 128], rhs=xg[:, kc, cs], start=(kc == 0), stop=(kc == 7)),
                                 reads=[b_wgb[w], b_xg], writes=[pbg])
                        ptu, pbu = PS.next()
                        for kc in range(8):
                            S.op("pe", lambda e, ptu=ptu, kc=kc, jc=jc, w=w, cs=cs: e.matmul(ptu[:, :], lhsT=wub[w][:, kc, jc * 128:(jc + 1) * 128], rhs=xg[:, kc, cs], start=(kc == 0), stop=(kc == 7)),
                                 reads=[b_wub[w], b_xg], writes=[pbu])
                        S.op("act", lambda e, ptg=ptg, s_=s_: e.activation(out=sg[s_][:, :], in_=ptg[:, :], func=AF.Silu), reads=[pbg], writes=[b_sg[s_]])
                        S.op("dve", lambda e, ptu=ptu, s_=s_: e.tensor_tensor(out=tu[s_][:, :], in0=ptu[:, :], in1=sg[s_][:, :], op=ALU.mult), reads=[pbu, b_sg[s_]], writes=[b_tu[s_]])
                        S.op("dve", lambda e, s_=s_, jc=jc, h_=h_: e.tensor_tensor(out=h_[:, jc, :], in0=tu[s_][:, :], in1=cb[:, :], op=ALU.mult), reads=[b_tu[s_], b_cb], writes=[bh_])
                    for t4 in range(4):
                        tt = m * 4 + t4
                        for nh in range(2):
                            pty, pby = PS.next()
                            for jc in range(2):
                                S.op("pe", lambda e, pty=pty, jc=jc, t4=t4, nh=nh, w=w, h_=h_: e.matmul(pty[:, :], lhsT=h_[:, jc, t4 * 128:(t4 + 1) * 128], rhs=wdb[w][:, jc, nh * 512:(nh + 1) * 512], start=(jc == 0), stop=(jc == 1)),
                                     reads=[bh_, b_wdb[w]], writes=[pby])
                            S.op("dve", lambda e, pty=pty, tt=tt, nh=nh: e.tensor_tensor(out=yacc[:, tt, nh * 512:(nh + 1) * 512], in0=pty[:, :], in1=yacc[:, tt, nh * 512:(nh + 1) * 512], op=ALU.add),
                                 reads=[pby, b_yacc[tt]], writes=[b_yacc[tt]])
            S.dma("sp", lambda e: e.dma_start(out=out_d.rearrange("(t p) f -> p t f", p=128), in_=yacc[:]), reads=b_yacc, writes=[b_out])
            S.barrier()

        S.barrier()
        for e_ in ("sp", "pool"):
            pass
        S.emit()
    return nc


b_out = Buf("out")


def _pcol(v, nchunk):
    return np.ascontiguousarray(np.asarray(v, np.float32).reshape(nchunk, 128).T)


def _prep_common(inp):
    f = lambda k: np.asarray(inp[k], np.float32)[0]
    pp = np.zeros((128, PPW), np.float32)
    pp[:, PP["gmix"]:PP["gmix"] + 8] = _pcol(f("norm_mix_g"), 8)
    cw = f("conv_w")
    pp[:, PP["convw"]:PP["convw"] + 32] = cw.reshape(4, 8, 128).transpose(2, 1, 0).reshape(128, 32)
    pp[:, PP["convb"]:PP["convb"] + 8] = _pcol(f("conv_b"), 8)
    pp[:, PP["ba"]:PP["ba"] + 8] = _pcol(f("lru_ba"), 8)
    pp[:, PP["bx"]:PP["bx"] + 8] = _pcol(f("lru_bx"), 8)
    pp[:, PP["lam"]:PP["lam"] + 8] = _pcol(f("lru_lambda"), 8)
    pp[:, PP["qag"]:PP["qag"] + 6] = _pcol(f("q_a_g"), 6)
    pp[:, PP["kvag"]:PP["kvag"] + 2] = _pcol(f("kv_a_g"), 2)
    pp[0:96, PP["qng"]] = f("q_norm_g")
    kng = f("k_norm_g")
    pp[0:64, PP["gkfold"]] = kng[0:64]
    pp[64:96, PP["gkfold"]] = 1.0
    pp[0:32, PP["gkpe"]] = kng[64:96]
    freq = (10000.0 ** (-np.arange(16, dtype=np.float32) / 16.0)).astype(np.float32)
    fr32 = np.concatenate([freq, freq])
    pp[0:32, PP["freq64"]] = fr32
    pp[32:64, PP["freq64"]] = fr32
    pp[32:64, PP["phase64"]] = np.float32(math.pi / 2)
    pp[0:64, PP["blockones"]] = 1.0
    pp[64:128, PP["blockones"] + 1] = 1.0
    pp[:, PP["gffn"]:PP["gffn"] + 8] = _pcol(f("norm_ffn_g"), 8)
    ident = np.eye(128, dtype=np.float32)
    rot32 = np.zeros((32, 32), np.float32)
    for m in range(16):
        rot32[m + 16, m] = -1.0
        rot32[m, m + 16] = 1.0
    rot96 = np.zeros((96, 96), np.float32)
    rot96[64:96, 64:96] = rot32
    wr = np.concatenate([f("router_group_w"), f("router_expert_w")], axis=1)
    rb = np.concatenate([f("router_group_b"), f("router_expert_b")])[None, :]
    rbias = np.ascontiguousarray(np.broadcast_to(rb, (128, 36))).astype(np.float32)
    gffnb = np.ascontiguousarray(np.broadcast_to(f("norm_ffn_g")[None, :], (128, D))).astype(np.float32)
    common = dict(gffnb=gffnb, ident=ident, rot96=rot96, rot32=rot32, rbias=rbias, w_in=f("w_in"), lru_wa=f("lru_wa"),
                  lru_wx=f("lru_wx"), w_uq=f("w_uq"), w_ukv=f("w_ukv"), w_out=f("w_out"), w_router=np.ascontiguousarray(wr),
                  w_gate=f("w_gate"), w_up=f("w_up"), w_down=f("w_down"))
    return pp, common


def _masks_for(c):
    m = np.zeros((128, 32, GT), np.float32)
    q = np.arange(GT)[None, :]
    for cp in range(8):
        for i in range(4):
            j = cp * 4 + i
            if cp < c:
                m[:, j, :] = 1.0
            elif cp == c:
                m[0:64, j, :] = (q >= 128 * i)
                m[64:128, j, :] = (q >= 128 * i + 64)
    return m.astype(ml_dtypes.bfloat16)


def make_in_maps(inp, names=None):
    pp, common = _prep_common(inp)
    x = np.asarray(inp["x"], np.float32)[0]
    pos = np.asarray(inp["positions"], np.int32)[0]
    posb_all = np.ascontiguousarray(np.broadcast_to(pos[None, :], (64, SEQ)))
    maps = []
    for c in range(NCORES):
        rows = np.concatenate([np.arange((8 * m + c) * GT, (8 * m + c + 1) * GT) for m in range(NOWN)])
        ppc_ = pp.copy()
        ppc_[:, PP["onehot"] + c] = 1.0
        d = dict(common)
        d.update(x_all=x, x_own=np.ascontiguousarray(x[rows]), posb_all=posb_all,
                 posb_own=np.ascontiguousarray(posb_all[:, rows]), pp=ppc_, masks=_masks_for(c))
        if names is not None:
            d = {k: d[k] for k in names}
        maps.append(d)
    return maps


def own_rows(c):
    return np.concatenate([np.arange((8 * m + c) * GT, (8 * m + c + 1) * GT) for m in range(NOWN)])


def kernel(**inputs):
    nc = build_program()
    maps = make_in_maps(inputs, nc._declared_inputs)
    res = run_bass_kernel_spmd(nc, maps, core_ids=list(range(NCORES)))
    out = np.zeros((1, SEQ, D), np.float32)
    for c in range(NCORES):
        out[0, own_rows(c)] = res.results[c]["out"]
    return out
```

```python
import os
import math
import numpy as np
import ml_dtypes
from contextlib import ExitStack
import concourse.bass as bass
import concourse.mybir as mybir
from concourse.bass_utils import run_bass_kernel_spmd

F32 = mybir.dt.float32
BF16 = mybir.dt.bfloat16
I32 = mybir.dt.int32
ALU = mybir.AluOpType
AF = mybir.ActivationFunctionType
AX = mybir.AxisListType

NCORES = 8
SEQ = 16384
D = 1024
GT = 512
NG = SEQ // GT
NOWN = 4
TOWN = NOWN * GT
EPS = 1e-6
TWO_PI = 2.0 * math.pi
C1 = 6.28125
C2 = TWO_PI - C1
NEXP = 32
DEXP = 256

PP = {}
_o = 0
for _n, _w in [("gmix", 8), ("convw", 32), ("convb", 8), ("ba", 8), ("bx", 8), ("lam", 8), ("qag", 6),
               ("kvag", 2), ("onehot", 8), ("qng", 1), ("gkfold", 1), ("gkpe", 1), ("freq64", 1),
               ("blockones", 2), ("gffn", 8), ("phase64", 1)]:
    PP[_n] = _o
    _o += _w
PPW = _o


class Buf:
    __slots__ = ("name", "last_w", "readers")

    def __init__(self, name=""):
        self.name = name
        self.last_w = None
        self.readers = []


class Sched:
    ENGS = ("sp", "pe", "act", "dve", "pool")
    DUR = {"pe": 0.25, "act": 0.62, "dve": 0.70, "pool": 1.1}

    def __init__(self, nc, stack, n_dma_sems=32, strict=True):
        self.nc = nc
        self.strict = strict
        self.esem = {e: stack.enter_context(nc.semaphore("prog_" + e)) for e in self.ENGS}
        self.dsems = [stack.enter_context(nc.semaphore("dma_%d" % i)) for i in range(n_dma_sems)]
        self.ops = []
        self.seg = 0

    def _add(self, kind, eng, fn, reads, writes, dur):
        oid = len(self.ops)
        deps = set()
        for b in reads:
            if b.last_w is not None:
                deps.add(b.last_w)
        for b in writes:
            if b.last_w is not None:
                deps.add(b.last_w)
            deps.update(b.readers)
        self.ops.append([kind, eng, fn, sorted(deps), dur, self.seg])
        for b in reads:
            b.readers.append(oid)
            if len(b.readers) > 700:
                b.readers = b.readers[-700:]
        for b in writes:
            b.last_w = oid
            b.readers = []
        return oid

    class _Probe:
        def __init__(self):
            self.name = None
            self.kw = {}

        def __getattr__(self, name):
            def f(*a, **kw):
                self.name = name
                self.kw = kw
                return self
            return f

    def _estimate(self, kind, eng, fn):
        try:
            p = Sched._Probe()
            fn(p)
            kw = p.kw
            if kind == "dma":
                o = kw.get("out")
                if o is None:
                    return 3.0
                n = 1
                for d_ in o.shape:
                    n *= int(d_)
                return 2.0 + n * mybir.dt.size(o.dtype) / 150e3
            o = kw.get("out")
            if eng == "pe":
                if p.name == "transpose":
                    return 0.09
                rhs = kw.get("rhs")
                nfree = 1
                for d_ in rhs.shape[1:]:
                    nfree *= int(d_)
                t = 0.03 + max(nfree, 64) / 2400.0
                if rhs.dtype == F32:
                    t *= 4.0
                return t
            nfree = 1
            for d_ in o.shape[1:]:
                nfree *= int(d_)
            if eng == "act":
                return 0.06 + nfree / 960.0 + (0.1 if kw.get("accum_out") is not None else 0.0)
            i0 = kw.get("in0", kw.get("in_", kw.get("data0", None)))
            b = 4
            try:
                b = max(mybir.dt.size(o.dtype), mybir.dt.size(i0.dtype)) if i0 is not None else mybir.dt.size(o.dtype)
            except Exception:
                pass
            t = 0.08 + nfree * (1.12e-3 if b >= 4 else 0.8e-3)
            if p.name == "tensor_tensor_scan":
                t = 0.08 + nfree * 2.1e-3
            if eng == "pool":
                t *= 1.8
            return t
        except Exception:
            return 3.0 if kind == "dma" else self.DUR[eng]

    def op(self, eng, fn, reads=(), writes=(), dur=None):
        return self._add("op", eng, fn, reads, writes, self._estimate("op", eng, fn) if dur is None else dur)

    def dma(self, eng, fn, reads=(), writes=(), dur=None):
        return self._add("dma", eng, fn, reads, writes, self._estimate("dma", eng, fn) if dur is None else dur)

    def barrier(self):
        self.seg += 1

    def _schedule_segment(self, ids, done_before):
        import heapq
        ops = self.ops
        idset = set(ids)
        ndeps = {}
        users = {}
        for i in ids:
            c = 0
            for d in ops[i][3]:
                if d in idset:
                    c += 1
                    users.setdefault(d, []).append(i)
            ndeps[i] = c
        blevel = {}
        for i in reversed(ids):
            m_ = 0.0
            for u in users.get(i, ()):
                if blevel[u] > m_:
                    m_ = blevel[u]
            blevel[i] = ops[i][4] + m_
        finish = {}
        eng_free = {e: 0.0 for e in self.ENGS}
        ready = {e: [] for e in self.ENGS}
        avail = {e: [] for e in self.ENGS}
        for i in ids:
            if ndeps[i] == 0:
                heapq.heappush(ready[ops[i][1]], (0.0, i))
        order = []
        n = len(ids)
        LAT = 0.12
        while len(order) < n:
            best = None
            for e in self.ENGS:
                h = ready[e]
                while h and h[0][0] <= eng_free[e]:
                    rt, i = heapq.heappop(h)
                    heapq.heappush(avail[e], (-blevel[i], i))
                if avail[e]:
                    cand = (eng_free[e], 0, e)
                elif h:
                    cand = (h[0][0], 1, e)
                else:
                    continue
                if best is None or cand < best:
                    best = cand
            st, which, e = best
            if which == 0:
                _, i = heapq.heappop(avail[e])
            else:
                st, i = heapq.heappop(ready[e])
            kind, _, _, _, dur, _ = ops[i]
            if kind == "dma":
                eng_free[e] = st + 0.08
                fin = st + dur
            else:
                eng_free[e] = st + dur
                fin = st + dur
            finish[i] = fin
            order.append(i)
            for u in users.get(i, ()):
                ndeps[u] -= 1
                if ndeps[u] == 0:
                    rt = 0.0
                    for d in ops[u][3]:
                        if d in finish:
                            lat = LAT if ops[d][1] != ops[u][1] else 0.05
                            rt = max(rt, finish[d] + lat)
                    heapq.heappush(ready[ops[u][1]], (rt, u))
        return order

    def emit(self):
        ops = self.ops
        nseg = self.seg + 1
        segs = [[] for _ in range(nseg)]
        for i, o in enumerate(ops):
            segs[o[5]].append(i)
        order = []
        for k in range(nseg):
            if segs[k]:
                order += self._schedule_segment(segs[k], None)
        ecount = {e: 0 for e in self.ENGS}
        dcount = [0] * len(self.dsems)
        dnext = 0
        tok = {}
        prev_dma_tok = {}
        for i in order:
            kind, eng = ops[i][0], ops[i][1]
            if kind == "op":
                ecount[eng] += 1
                tok[i] = (("e", eng), ecount[eng])
            else:
                j = dnext
                dnext = (dnext + 1) % len(self.dsems)
                if dcount[j] > 0:
                    prev_dma_tok[i] = (("d", j), dcount[j])
                dcount[j] += 16
                tok[i] = (("d", j), dcount[j])
        semobj = {}
        for e in self.ENGS:
            semobj[("e", e)] = self.esem[e]
        for j, s_ in enumerate(self.dsems):
            semobj[("d", j)] = s_
        prog = {e: [] for e in self.ENGS}
        waited = {e: {} for e in self.ENGS}
        last_seg = {e: 0 for e in self.ENGS}
        seg_tokens = []

        def need(eng, t):
            key, val = t
            if key == ("e", eng) and (eng == "pe" or not self.strict):
                return
            if val > waited[eng].get(key, 0):
                waited[eng][key] = val
                prog[eng].append(("w", semobj[key], val))

        seg_end = []
        cur = {}
        pos = 0
        for k in range(nseg):
            for _ in segs[k]:
                i = order[pos]; pos += 1
                key, val = tok[i]
                cur[key] = max(cur.get(key, 0), val)
            seg_end.append(dict(cur))
        for i in order:
            kind, eng, fn, deps, dur, sg = ops[i]
            if sg > last_seg[eng]:
                for key, val in seg_end[sg - 1].items():
                    need(eng, (key, val))
                last_seg[eng] = sg
            for d in deps:
                need(eng, tok[d])
            if i in prev_dma_tok:
                need(eng, prev_dma_tok[i])
            key, val = tok[i]
            prog[eng].append(("i", fn, semobj[key], 1 if kind == "op" else 16))
        for e in self.ENGS:
            for key, val in seg_end[-1].items():
                need(e, (key, val))
        names = {"sp": "sync", "pe": "tensor", "act": "scalar", "dve": "vector", "pool": "gpsimd"}
        with self.nc.Block() as block:
            for e in self.ENGS:
                items = prog[e]
                if not items:
                    continue

                def body(engobj, items=items):
                    for it in items:
                        if it[0] == "w":
                            engobj.wait_ge(it[1], it[2])
                        else:
                            it[1](engobj).then_inc(it[2], it[3])

                getattr(block, names[e])(body)


class PsumRot:
    def __init__(self, tiles):
        self.tiles = tiles
        self.free = list(range(len(tiles)))
        self.i = 0

    def reserve(self, n):
        r = [self.free.pop() for _ in range(n)]
        return [self.tiles[k] for k in r], r

    def release(self, idxs):
        self.free.extend(idxs)

    def next(self):
        k = self.free[self.i % len(self.free)]
        self.i += 1
        return self.tiles[k]


def build_program(debug=False, stop_after=None, phases="AB12CDE"):
    nc = bass.Bass("TRN2", target_bir_lowering=False)
    declared = []
    nc._declared_inputs = declared

    def din(name, shape, dt=F32, ph=None):
        if ph is not None and not any(p in phases for p in ph):
            return None
        declared.append(name)
        return nc.dram_tensor(name, list(shape), dt, kind="ExternalInput").ap()

    def dscr(name, shape, dt=F32):
        return nc.dram_tensor(name, list(shape), dt).ap()

    x_all = din("x_all", [SEQ, D], ph="A")
    x_own = din("x_own", [TOWN, D], ph="12D")
    posb_all = din("posb_all", [64, SEQ], I32, ph="A")
    posb_own = din("posb_own", [64, TOWN], I32, ph="A")
    pp_d = din("pp", [128, PPW])
    ident_d = din("ident", [128, 128])
    rot96_d = din("rot96", [96, 96])
    rot32_d = din("rot32", [32, 32])
    masks_d = din("masks", [128, 32, GT], BF16, ph="C")
    rbias_d = din("rbias", [128, 36], ph="D")
    gffnb_d = din("gffnb", [128, D], ph="D")
    w_in_d = din("w_in", [D, 5152], ph="A12")
    wa_d = din("lru_wa", [4, 256, 256], ph="A")
    wx_d = din("lru_wx", [4, 256, 256], ph="A")
    wuq_d = din("w_uq", [768, 1536], ph="2")
    wukv_d = din("w_ukv", [256, 2048], ph="A")
    wout_d = din("w_out", [D, D], ph="D")
    wr_d = din("w_router", [D, 36], ph="D")
    wg_d = din("w_gate", [NEXP, D, DEXP], ph="E")
    wu_d = din("w_up", [NEXP, D, DEXP], ph="E")
    wd_d = din("w_down", [NEXP, DEXP, D], ph="E")
    out_d = nc.dram_tensor("out", [TOWN, D], F32, kind="ExternalOutput").ap()
    dbg = {}
    if debug:
        for nm, shp in [("hl", [D, TOWN]), ("yb", [D, TOWN]), ("hmid", [TOWN, D]), ("qt", [96, 16 * TOWN]),
                        ("comb", [TOWN, 32])]:
            dbg[nm] = nc.dram_tensor("dbg_" + nm, shp, F32, kind="ExternalOutput").ap()
        dbg["hlb"] = nc.dram_tensor("dbg_hlb", [D, TOWN], BF16, kind="ExternalOutput").ap()
        dbg["rk"] = nc.dram_tensor("dbg_rk", [128, 2048], F32, kind="ExternalOutput").ap()
        dbg["ktn"] = nc.dram_tensor("dbg_ktn", [128, 512], BF16, kind="ExternalOutput").ap()
        dbg["kpe"] = nc.dram_tensor("dbg_kpe", [32, 512], BF16, kind="ExternalOutput").ap()
        dbg["v"] = nc.dram_tensor("dbg_v", [128, 512], BF16, kind="ExternalOutput").ap()

    ktn_d = dscr("ktn", [8 * 128, SEQ], BF16)
    kpe_d = dscr("kpe", [32, SEQ], BF16)
    v_d = dscr("vaug", [16, NG, 128, 4 * 128], BF16)
    ownh_d = dscr("ownh", [128, NOWN * 8 * GT], BF16)
    rk_d = dscr("rk", [128, (SEQ // 128) * 16])
    gaya_d = dscr("gaya", [128, 8, TOWN], BF16)
    sgb_d = dscr("sgb", [128, 8, TOWN], BF16)
    mg_d = dscr("mg", [128, 8, TOWN], BF16)
    qt_d = dscr("qt", [96, 16, TOWN], BF16)
    hmid_d = dscr("hmid", [TOWN, D])
    xgt_d = dscr("xgt", [128, 8, TOWN], BF16)
    combt_d = dscr("combt", [32, TOWN])
    kcs_d = dscr("kcs", [64, SEQ])
    qcs_d = dscr("qcs", [64, TOWN])

    with ExitStack() as top:
        S = Sched(nc, top)
        sb = lambda st, name, shape, dt=F32: st.enter_context(nc.sbuf_tensor("sb_" + name, list(shape), dt))

        ps_tiles = []
        for i in range(8):
            t = top.enter_context(nc.psum_tensor("ps%d" % i, [128, 512], F32))
            ps_tiles.append((t, Buf("ps%d" % i)))
        PS = PsumRot(ps_tiles)

        pp = sb(top, "pp_sb", [128, PPW]); b_pp = Buf("pp")
        ident = sb(top, "ident_sb", [128, 128])
        rot96 = sb(top, "rot96_sb", [96, 96])
        rot32 = sb(top, "rot32_sb", [32, 32])
        ones_f = sb(top, "ones_f", [128, 128])
        b_const = Buf("const")
        b_rk = Buf("rk"); b_ownh = Buf("ownh")
        b_rk_d = Buf("rk_d"); b_ownh_d = Buf("ownh_d"); b_gaya_d = Buf(); b_sgb_d = Buf(); b_mg_d = Buf(); b_qt_d = Buf()
        b_hmid_d = Buf(); b_xgt_d = Buf(); b_combt_d = Buf()
        b_ktn_d = Buf("ktn_d"); b_kpe_d = Buf("kpe_d"); b_v_d = Buf("v_d")
        c12 = sb(top, "c12", [128, 40]); b_c12 = Buf("c12")

        S.dma("sp", lambda e: e.dma_start(out=pp[:], in_=pp_d), writes=[b_pp])
        S.dma("sp", lambda e: e.dma_start(out=ident[:], in_=ident_d), writes=[b_const])
        S.dma("sp", lambda e: e.dma_start(out=rot96[:], in_=rot96_d), writes=[b_const])
        S.dma("sp", lambda e: e.dma_start(out=rot32[:], in_=rot32_d), writes=[b_const])
        S.op("dve", lambda e: e.memset(ones_f[:], 1.0), writes=[b_const])

        def ppc(name, j=0, rows=128, w=1):
            o = PP[name] + j
            return pp[0:rows, o:o + w]

        lam_ap = ppc("lam", 0, 128, 8)
        S.op("act", lambda e: e.activation(out=c12[:, 0:8], in_=lam_ap, func=AF.Exp, scale=-1.0), reads=[b_pp], writes=[b_c12])
        S.op("act", lambda e: e.activation(out=c12[:, 0:8], in_=c12[:, 0:8], func=AF.Ln, bias=1.0), reads=[b_c12], writes=[b_c12])
        S.op("dve", lambda e: e.tensor_scalar(out=c12[:, 8:16], in0=c12[:, 0:8], scalar1=-16.0, scalar2=None, op0=ALU.mult), reads=[b_c12], writes=[b_c12])
        S.op("dve", lambda e: e.tensor_scalar(out=c12[:, 16:24], in0=c12[:, 0:8], scalar1=-4.0, scalar2=None, op0=ALU.mult), reads=[b_c12], writes=[b_c12])
        S.op("dve", lambda e: e.tensor_scalar(out=c12[:, 0:8], in0=c12[:, 0:8], scalar1=-8.0, scalar2=None, op0=ALU.mult), reads=[b_c12], writes=[b_c12])
        S.op("dve", lambda e: e.tensor_scalar(out=c12[:, 24:32], in0=ppc("ba", 0, 128, 8), scalar1=0.5, scalar2=None, op0=ALU.mult), reads=[b_pp], writes=[b_c12])
        S.op("dve", lambda e: e.tensor_scalar(out=c12[:, 32:40], in0=ppc("bx", 0, 128, 8), scalar1=0.5, scalar2=None, op0=ALU.mult), reads=[b_pp], writes=[b_c12])

        def sincos_tables(st, posb_ap, ntok, dst_d, tag):
            CH = 2048
            pi_ = sb(st, tag + "_pi", [64, CH], I32)
            ang = sb(st, tag + "_ang", [64, CH])
            kf = sb(st, tag + "_kf", [64, CH])
            ki = sb(st, tag + "_ki", [64, CH], I32)
            msk = sb(st, tag + "_m", [64, CH])
            b = [Buf() for _ in range(5)]
            fr = ppc("freq64", 0, 64); ph = ppc("phase64", 0, 64)
            for c0 in range(0, ntok, CH):
                S.dma("sp", lambda e, c0=c0: e.dma_start(out=pi_[:], in_=posb_ap[:, c0:c0 + CH]), writes=[b[0]])
                S.op("dve", lambda e: e.tensor_copy(out=ang[:], in_=pi_[:]), reads=[b[0]], writes=[b[1]])
                S.op("dve", lambda e: e.tensor_scalar(out=ang[:], in0=ang[:], scalar1=fr, scalar2=ph, op0=ALU.mult, op1=ALU.add),
                     reads=[b[1], b_pp], writes=[b[1]])
                S.op("dve", lambda e: e.tensor_scalar(out=kf[:], in0=ang[:], scalar1=1.0 / TWO_PI, scalar2=None, op0=ALU.mult),
                     reads=[b[1]], writes=[b[2]])
                S.op("dve", lambda e: e.tensor_copy(out=ki[:], in_=kf[:]), reads=[b[2]], writes=[b[3]])
                S.op("dve", lambda e: e.tensor_copy(out=kf[:], in_=ki[:]), reads=[b[3]], writes=[b[2]])
                S.op("dve", lambda e: e.scalar_tensor_tensor(out=ang[:], in0=kf[:], scalar=-C1, in1=ang[:], op0=ALU.mult, op1=ALU.add),
                     reads=[b[2], b[1]], writes=[b[1]])
                S.op("dve", lambda e: e.scalar_tensor_tensor(out=ang[:], in0=kf[:], scalar=-C2, in1=ang[:], op0=ALU.mult, op1=ALU.add),
                     reads=[b[2], b[1]], writes=[b[1]])
                for thr, cmp_, corr in ((math.pi, ALU.is_gt, -TWO_PI), (-math.pi, ALU.is_lt, TWO_PI)):
                    S.op("dve", lambda e, thr=thr, cmp_=cmp_: e.tensor_single_scalar(out=msk[:], in_=ang[:], scalar=thr, op=cmp_),
                         reads=[b[1]], writes=[b[4]])
                    S.op("dve", lambda e, corr=corr: e.scalar_tensor_tensor(out=ang[:], in0=msk[:], scalar=corr, in1=ang[:], op0=ALU.mult, op1=ALU.add),
                         reads=[b[4], b[1]], writes=[b[1]])
                S.op("dve", lambda e: e.tensor_scalar(out=ang[:], in0=ang[:], scalar1=3.1415925, scalar2=-3.1415925, op0=ALU.min, op1=ALU.max),
                     reads=[b[1]], writes=[b[1]])
                S.op("act", lambda e: e.activation(out=kf[:], in_=ang[:], func=AF.Sin), reads=[b[1]], writes=[b[2]])
                S.dma("sp", lambda e, c0=c0: e.dma_start(out=dst_d[:, c0:c0 + CH], in_=kf[:]), reads=[b[2]], writes=[b_dram_cs])

        b_dram_cs = Buf("dram_cs")

        def load_norm_transpose(xsrc_ap, xt, bx_, ss, bss, uT, buT, t4, gcol=None):
            S.dma("sp", lambda e: e.dma_start(out=xt[:], in_=xsrc_ap), writes=[bx_])
            S.op("act", lambda e: e.activation(out=junk[:], in_=xt[:], func=AF.Square, accum_out=ss[:, 0:1]), reads=[bx_], writes=[bss, b_junk])
            S.op("act", lambda e: e.activation(out=ss[:, 1:2], in_=ss[:, 0:1], func=AF.Sqrt, scale=1.0 / D, bias=eps_t[:, 0:1]), reads=[bss], writes=[bss])
            S.op("dve", lambda e: e.reciprocal(out=ss[:, 2:3], in_=ss[:, 1:2]), reads=[bss], writes=[bss])
            S.op("dve", lambda e: e.tensor_scalar(out=xt[:], in0=xt[:], scalar1=ss[:, 2:3], scalar2=None, op0=ALU.mult), reads=[bss, bx_], writes=[bx_])
            for half in range(2):
                pt, pb = PS.next()
                for q in range(4):
                    kc = half * 4 + q
                    S.op("pe", lambda e, pt=pt, q=q, kc=kc: e.transpose(out=pt[:, q * 128:(q + 1) * 128], in_=xt[:, kc * 128:(kc + 1) * 128], identity=ident[:]),
                         reads=[bx_, b_const], writes=[pb])
                dst = uT[:, half * 4:half * 4 + 4, t4 * 128:(t4 + 1) * 128]
                src = pt[:].rearrange("p (q t) -> p q t", q=4)
                eng = "act" if half == 0 else "dve"
                if eng == "act":
                    S.op("act", lambda e, dst=dst, src=src: e.activation(out=dst, in_=src, func=AF.Copy), reads=[pb], writes=[buT])
                else:
                    S.op("dve", lambda e, dst=dst, src=src: e.tensor_copy(out=dst, in_=src), reads=[pb], writes=[buT])

        junk = sb(top, "junk", [128, D], BF16); b_junk = Buf("junk")
        eps_t = sb(top, "eps_t", [128, 1])
        eps96 = sb(top, "eps96", [128, 1])
        S.op("dve", lambda e: e.memset(eps_t[:], EPS), writes=[b_const])
        S.op("dve", lambda e: e.memset(eps96[:], 96.0 * EPS), writes=[b_const])

        def load_weight_cols(st, dst_bf, bdst, src_d, col_ranges, nk, scale_name, tag, stage_w):
            stg = [sb(st, "%s_stg%d" % (tag, i), [128, stage_w]) for i in range(2)]
            bst = [Buf(), Buf()]
            for kc in range(nk):
                s_ = stg[kc % 2]; bs_ = bst[kc % 2]
                o = 0
                for (a, b_) in col_ranges:
                    S.dma("sp", lambda e, s_=s_, o=o, a=a, b_=b_, kc=kc: e.dma_start(out=s_[:, o:o + (b_ - a)], in_=src_d[kc * 128:(kc + 1) * 128, a:b_]), writes=[bs_])
                    o += b_ - a
                if kc % 2 == 0:
                    if scale_name is None:
                        S.op("dve", lambda e, s_=s_, kc=kc, o=o: e.tensor_copy(out=dst_bf[:, kc, 0:o], in_=s_[:, 0:o]), reads=[bs_], writes=[bdst])
                    else:
                        sc = ppc(scale_name, kc)
                        S.op("dve", lambda e, s_=s_, kc=kc, o=o, sc=sc: e.tensor_scalar(out=dst_bf[:, kc, 0:o], in0=s_[:, 0:o], scalar1=sc, scalar2=None, op0=ALU.mult),
                             reads=[bs_, b_pp], writes=[bdst])
                else:
                    if scale_name is None:
                        S.op("act", lambda e, s_=s_, kc=kc, o=o: e.activation(out=dst_bf[:, kc, 0:o], in_=s_[:, 0:o], func=AF.Copy), reads=[bs_], writes=[bdst])
                    else:
                        sc = ppc(scale_name, kc)
                        S.op("act", lambda e, s_=s_, kc=kc, o=o, sc=sc: e.activation(out=dst_bf[:, kc, 0:o], in_=s_[:, 0:o], func=AF.Copy, scale=sc),
                             reads=[bs_, b_pp], writes=[bdst])

        with ExitStack() as pa:
          if "A" in phases:
            sincos_st = ExitStack()
            with sincos_st:
                sincos_tables(sincos_st, posb_all, SEQ, kcs_d, "csA")
                sincos_tables(sincos_st, posb_own, TOWN, qcs_d, "csO")
            S.barrier()

            rk_all = sb(pa, "rk_all", [128, SEQ // 128, 16])
            own_hb = [sb(pa, "own_h%d" % i, [128, 8, GT], BF16) for i in range(2)]; b_ownhb = [Buf(), Buf()]
            NA = 1024 + 256 + 32
            winA = sb(pa, "winA", [128, 8, NA], BF16); b_winA = Buf("winA")
            wab = sb(pa, "wab", [128, 8, 256], BF16); wxb = sb(pa, "wxb", [128, 8, 256], BF16)
            b_wab = Buf("wab")
            wukK = sb(pa, "wukK", [128, 2, 1024], BF16); wukV = sb(pa, "wukV", [128, 2, 1024], BF16)
            b_wuk = Buf("wuk")
            with ExitStack() as wp:
                load_weight_cols(wp, winA, b_winA, w_in_d, [(0, 1024), (2816, 3104)], 8, "gmix", "wA", NA)
                wst = sb(wp, "wa_stg", [128, 8, 256])
                b_wst = Buf()
                for (src, dstw) in ((wa_d, wab), (wx_d, wxb)):
                    S.dma("sp", lambda e, src=src: e.dma_start(out=wst[:], in_=src.rearrange("b (ic p) j -> p (b ic) j", p=128)), writes=[b_wst])
                    S.op("dve", lambda e, dstw=dstw: e.tensor_copy(out=dstw[:], in_=wst[:]), reads=[b_wst], writes=[b_wab])
                kst = sb(wp, "wukv_stg", [128, 2048]); b_kst = Buf()
                for kc in range(2):
                    S.dma("sp", lambda e, kc=kc: e.dma_start(out=kst[:], in_=wukv_d[kc * 128:(kc + 1) * 128, :]), writes=[b_kst])
                    sc = ppc("kvag", kc)
                    kv4 = kst[:].rearrange("p (h two d) -> p h two d", h=16, two=2)
                    S.op("dve", lambda e, kc=kc, sc=sc, kv4=kv4: e.tensor_scalar(out=wukK[:, kc, :].rearrange("p (h d) -> p h d", h=16), in0=kv4[:, :, 0, :], scalar1=sc, scalar2=None, op0=ALU.mult),
                         reads=[b_kst, b_pp], writes=[b_wuk])
                    S.op("dve", lambda e, kc=kc, sc=sc, kv4=kv4: e.tensor_scalar(out=wukV[:, kc, :].rearrange("p (h d) -> p h d", h=16), in0=kv4[:, :, 1, :], scalar1=sc, scalar2=None, op0=ALU.mult),
                         reads=[b_kst, b_pp], writes=[b_wuk])
                S.barrier()

            xt = [sb(pa, "xtA%d" % i, [128, D]) for i in range(2)]; b_xt = [Buf(), Buf()]
            ssA = [sb(pa, "ssA%d" % i, [128, 4]) for i in range(2)]; b_ss = [Buf(), Buf()]
            uT = [sb(pa, "uTA%d" % i, [128, 8, GT], BF16) for i in range(2)]; b_uT = [Buf(), Buf()]
            xr = sb(pa, "xr", [128, 8, GT + 3], BF16); b_xr = [Buf() for _ in range(8)]
            diagw = sb(pa, "diagw", [128, 8, 4, 128], BF16); b_diagw = Buf()
            xa = sb(pa, "xa", [128, 8, GT]); b_xa = [Buf() for _ in range(8)]
            xab = sb(pa, "xab", [128, 8, GT], BF16); b_xab = [Buf() for _ in range(8)]
            NT = 2
            tr = [sb(pa, "tr%d" % i, [128, GT]) for i in range(1)] * 2; b_tr = [Buf()] * 2
            ti = [sb(pa, "ti%d" % i, [128, GT]) for i in range(4)]; b_ti = [Buf() for _ in range(4)]
            ta = [sb(pa, "ta%d" % i, [128, GT]) for i in range(4)]; b_ta = [Buf() for _ in range(4)]
            tm = [sb(pa, "tm%d" % i, [128, GT]) for i in range(4)]; b_tm = [Buf() for _ in range(4)]
            hst = sb(pa, "hst", [128, 8]); b_hst = Buf("hst"); b_hst8 = [Buf() for _ in range(8)]
            b_oh8 = [[Buf() for _ in range(8)] for _ in range(2)]
            kvc = sb(pa, "kvc", [128, 2, GT]); b_kvc = Buf()
            kvsq = sb(pa, "kvsq", [128, 2, GT]); b_kvsq = Buf()
            kvn = sb(pa, "kvn", [128, 2, GT], BF16); b_kvn = Buf()
            rbc = sb(pa, "rbc", [128, GT]); b_rbc = Buf()
            kpr = sb(pa, "kpr", [32, GT]); b_kpr = Buf()
            kpsq = sb(pa, "kpsq", [32, GT]); b_kpsq = Buf()
            kpo = sb(pa, "kpo", [32, GT], BF16); b_kpo = Buf()
            kcs = sb(pa, "kcs_sb", [32, 2, GT]); b_kcs = Buf()
            ktn = [sb(pa, "ktn_sb%d" % i, [128, 8, GT], BF16) for i in range(1)] * 2; b_ktn = [Buf()] * 2
            ksq = [sb(pa, "ksq%d" % i, [128, GT]) for i in range(1)] * 2; b_ksq = [Buf()] * 2
            vau = sb(pa, "vau", [128, 16, 4, 128], BF16); b_vau = Buf()
            sstat = sb(pa, "sstat", [128, 16]); b_sstat = Buf()

            S.op("dve", lambda e: e.memset(xr[:], 0.0), writes=b_xr)
            for jc in range(8):
                for j in range(4):
                    S.op("dve", lambda e, jc=jc, j=j: e.tensor_scalar(out=diagw[:, jc, j, :], in0=ident[:, :], scalar1=ppc("convw", jc * 4 + j), scalar2=None, op0=ALU.mult),
                         reads=[b_const, b_pp], writes=[b_diagw])
            S.op("dve", lambda e: e.memset(hst[:], 0.0), writes=[b_hst] + b_hst8)
            S.op("pool", lambda e: e.memset(vau[:], 1.0), writes=[b_vau])

            n_groups_A = NG if stop_after != "A1" else 2
            (pst_l, pst_idx) = PS.reserve(1)
            pst, pbst = pst_l[0]
            def gen_H(G):
                u = uT[G % 2]; bu = b_uT[G % 2]
                for t4 in range(4):
                    i = (G * 4 + t4) % 2
                    r0 = G * GT + t4 * 128
                    load_norm_transpose(x_all[r0:r0 + 128, :], xt[i], b_xt[i], ssA[i], b_ss[i], u, bu, t4)
                    yield
            def emit_M(G):
                u = uT[G % 2]; bu = b_uT[G % 2]
                for jc in range(8):
                    pt, pb = PS.next()
                    for kc in range(8):
                        S.op("pe", lambda e, pt=pt, jc=jc, kc=kc, u=u: e.matmul(pt[:, :], lhsT=winA[:, kc, jc * 128:(jc + 1) * 128], rhs=u[:, kc, :], start=(kc == 0), stop=(kc == 7)),
                             reads=[b_winA, bu], writes=[pb])
                    S.op("act", lambda e, pt=pt, jc=jc: e.activation(out=xr[:, jc, 3:3 + GT], in_=pt[:, :], func=AF.Copy), reads=[pb], writes=[b_xr[jc]])
                for jc in range(2):
                    pt, pb = PS.next()
                    for kc in range(8):
                        S.op("pe", lambda e, pt=pt, jc=jc, kc=kc, u=u: e.matmul(pt[:, :], lhsT=winA[:, kc, 1024 + jc * 128:1024 + (jc + 1) * 128], rhs=u[:, kc, :], start=(kc == 0), stop=(kc == 7)),
                             reads=[b_winA, bu], writes=[pb])
                    S.op("act", lambda e, pt=pt, jc=jc: e.activation(out=kvc[:, jc, :], in_=pt[:, :], func=AF.Copy), reads=[pb], writes=[b_kvc])
                    S.op("dve", lambda e, pt=pt, jc=jc: e.tensor_tensor(out=kvsq[:, jc, :], in0=pt[:, :], in1=kvc[:, jc, :], op=ALU.mult), reads=[pb, b_kvc], writes=[b_kvsq])
                pt, pb = PS.next()
                for kc in range(8):
                    S.op("pe", lambda e, pt=pt, kc=kc, u=u: e.matmul(pt[0:32, :], lhsT=winA[:, kc, 1280:1312], rhs=u[:, kc, :], start=(kc == 0), stop=(kc == 7)),
                         reads=[b_winA, bu], writes=[pb])
                S.op("act", lambda e, pt=pt: e.activation(out=kpr[:, :], in_=pt[0:32, :], func=AF.Copy), reads=[pb], writes=[b_kpr])
                S.op("dve", lambda e, pt=pt: e.tensor_tensor(out=kpsq[:, :], in0=pt[0:32, :], in1=kpr[:, :], op=ALU.mult), reads=[pb, b_kpr], writes=[b_kpsq])
            def gen_KV(G):
                S.dma("sp", lambda e, G=G: e.dma_start(out=kcs[:, 0, :], in_=kcs_d[0:32, G * GT:(G + 1) * GT]), reads=[b_dram_cs], writes=[b_kcs])
                S.dma("sp", lambda e, G=G: e.dma_start(out=kcs[:, 1, :], in_=kcs_d[32:64, G * GT:(G + 1) * GT]), reads=[b_dram_cs], writes=[b_kcs])
                gk = ppc("gkpe", 0, 32)
                S.op("dve", lambda e, gk=gk: e.tensor_scalar(out=kpr[:, :], in0=kpr[:, :], scalar1=gk, scalar2=None, op0=ALU.mult), reads=[b_kpr, b_pp], writes=[b_kpr])
                pt, pb = PS.next()
                S.op("pe", lambda e, pt=pt: e.matmul(pt[0:32, :], lhsT=rot32[:, :], rhs=kpr[:, :], start=True, stop=True), reads=[b_kpr, b_const], writes=[pb])
                S.op("dve", lambda e, pt=pt: e.tensor_tensor(out=kcs[:, 0, :], in0=pt[0:32, :], in1=kcs[:, 0, :], op=ALU.mult), reads=[pb, b_kcs], writes=[b_kcs])
                S.op("dve", lambda e: e.tensor_tensor(out=kcs[:, 1, :], in0=kpr[:, :], in1=kcs[:, 1, :], op=ALU.mult), reads=[b_kpr, b_kcs], writes=[b_kcs])
                S.op("dve", lambda e: e.tensor_tensor(out=kpo[:, :], in0=kcs[:, 0, :], in1=kcs[:, 1, :], op=ALU.add), reads=[b_kcs], writes=[b_kpo])
                S.dma("sp", lambda e, G=G: e.dma_start(out=kpe_d[:, G * GT:(G + 1) * GT], in_=kpo[:, :]), reads=[b_kpo], writes=[b_kpe_d])
                yield
                pt, pb = PS.next()
                for jc in range(2):
                    S.op("pe", lambda e, pt=pt, jc=jc: e.matmul(pt[:, :], lhsT=ones_f[:, :], rhs=kvsq[:, jc, :], start=(jc == 0), stop=(jc == 1)), reads=[b_kvsq, b_const], writes=[pb])
                S.op("act", lambda e, pt=pt: e.activation(out=rbc[:, :], in_=pt[:, :], func=AF.Sqrt, scale=1.0 / 256, bias=eps_t[:, 0:1]), reads=[pb], writes=[b_rbc])
                S.op("dve", lambda e: e.reciprocal(out=rbc[:, :], in_=rbc[:, :]), reads=[b_rbc], writes=[b_rbc])
                yield
                for jc in range(2):
                    S.op("dve", lambda e, jc=jc: e.tensor_tensor(out=kvn[:, jc, :], in0=kvc[:, jc, :], in1=rbc[:, :], op=ALU.mult), reads=[b_kvc, b_rbc], writes=[b_kvn])
                kb = ktn[G % 2]; bkb = b_ktn[G % 2]
                for hp in range(8):
                    pt, pb = PS.next()
                    for kc in range(2):
                        S.op("pe", lambda e, pt=pt, kc=kc, hp=hp: e.matmul(pt[:, :], lhsT=wukK[:, kc, hp * 128:(hp + 1) * 128], rhs=kvn[:, kc, :], start=(kc == 0), stop=(kc == 1)),
                             reads=[b_wuk, b_kvn], writes=[pb])
                    S.op("act", lambda e, pt=pt, hp=hp, kb=kb: e.activation(out=kb[:, hp, :], in_=pt[:, :], func=AF.Copy), reads=[pb], writes=[bkb])
                    q_ = ksq[hp % 2]; bq_ = b_ksq[hp % 2]
                    S.op("act", lambda e, pt=pt, q_=q_: e.activation(out=q_[:, :], in_=pt[:, :], func=AF.Square), reads=[pb], writes=[bq_])
                    bo = ppc("blockones", 0, 128, 2)
                    for t4 in range(4):
                        S.op("pe", lambda e, pst=pst, q_=q_, t4=t4, hp=hp, bo=bo: e.matmul(pst[:, t4 * 16 + 2 * hp:t4 * 16 + 2 * hp + 2], lhsT=q_[:, t4 * 128:(t4 + 1) * 128], rhs=bo, start=True, stop=True, skip_group_check=True),
                             reads=[bq_, b_pp], writes=[pbst])
                    yield
                yield
                for t4 in range(4):
                    S.op("pe", lambda e, t4=t4: e.matmul(pst[:, 64 + t4:65 + t4], lhsT=kpsq[:, t4 * 128:(t4 + 1) * 128], rhs=ones_f[0:32, 0:1], start=True, stop=True, skip_group_check=True),
                         reads=[b_kpsq, b_const], writes=[pbst])
                S.op("dve", lambda e: e.tensor_copy(out=sstat[:, 0:4], in_=pst[:, 64:68]), reads=[pbst], writes=[b_sstat])
                for t4 in range(4):
                    tile_i = G * 4 + t4
                    S.op("dve", lambda e, pst=pst, t4=t4, tile_i=tile_i: e.tensor_scalar(out=rk_all[:, tile_i, :], in0=pst[:, t4 * 16:(t4 + 1) * 16], scalar1=sstat[:, t4:t4 + 1], scalar2=None, op0=ALU.add),
                         reads=[pbst, b_sstat], writes=[b_rk])
                S.dma("sp", lambda e, G=G, kb=kb: e.dma_start(out=ktn_d[:, G * GT:(G + 1) * GT].rearrange("(hp p) t -> p hp t", p=128), in_=kb[:, :, :]), reads=[bkb], writes=[b_ktn_d])
                for t4 in range(4):
                    for half in range(2):
                        pt, pb = PS.next()
                        for kc in range(2):
                            S.op("pe", lambda e, pt=pt, kc=kc, half=half, t4=t4: e.matmul(pt[:, :], lhsT=kvn[:, kc, t4 * 128:(t4 + 1) * 128], rhs=wukV[:, kc, half * 512:(half + 1) * 512], start=(kc == 0), stop=(kc == 1)),
                                 reads=[b_wuk, b_kvn], writes=[pb])
                        dst = vau[:, half * 8:(half + 1) * 8, t4, 0:64]
                        src = pt[:, :].rearrange("p (h d) -> p h d", h=8)
                        if half == 0:
                            S.op("act", lambda e, dst=dst, src=src: e.activation(out=dst, in_=src, func=AF.Copy), reads=[pb], writes=[b_vau])
                        else:
                            S.op("dve", lambda e, dst=dst, src=src: e.tensor_copy(out=dst, in_=src), reads=[pb], writes=[b_vau])
                    yield
                S.dma("sp", lambda e, G=G: e.dma_start(out=v_d[:, G, :, :].rearrange("h p c -> p h c"), in_=vau[:].rearrange("p h b c -> p h (b c)")), reads=[b_vau], writes=[b_v_d])
                yield
            def gen_LRU(G):
                for jc in range(8):
                    ptc, pbc = PS.next()
                    for j in range(4):
                        S.op("pe", lambda e, ptc=ptc, jc=jc, j=j: e.matmul(ptc[:, :], lhsT=diagw[:, jc, j, :], rhs=xr[:, jc, j:j + GT], start=(j == 0), stop=(j == 3)),
                             reads=[b_diagw, b_xr[jc]], writes=[pbc])
                    S.op("dve", lambda e, ptc=ptc, jc=jc: e.tensor_scalar(out=xa[:, jc, :], in0=ptc[:, :], scalar1=ppc("convb", jc), scalar2=None, op0=ALU.add), reads=[pbc, b_pp], writes=[b_xa[jc]])
                    S.op("dve", lambda e, jc=jc: e.tensor_copy(out=xab[:, jc, :], in_=xa[:, jc, :]), reads=[b_xa[jc]], writes=[b_xab[jc]])
                    S.op("dve", lambda e, jc=jc: e.tensor_copy(out=xr[:, jc, 0:3], in_=xr[:, jc, GT:GT + 3]), reads=[b_xr[jc]], writes=[b_xr[jc]])
                    yield
                for quad in range(2):
                    for jq in range(4):
                        jc = quad * 4 + jq
                        blk = jc // 2
                        s2 = jc % 2
                        ptr, pbr = PS.next()
                        for ic in range(2):
                            S.op("pe", lambda e, ptr=ptr, ic=ic, blk=blk, jc=jc: e.matmul(ptr[:, :], lhsT=wab[:, blk * 2 + ic, (jc % 2) * 128:(jc % 2 + 1) * 128], rhs=xab[:, blk * 2 + ic, :], start=(ic == 0), stop=(ic == 1)),
                                 reads=[b_wab, b_xab[blk * 2 + ic]], writes=[pbr])
                        pti, pbi = PS.next()
                        for ic in range(2):
                            S.op("pe", lambda e, pti=pti, ic=ic, blk=blk, jc=jc: e.matmul(pti[:, :], lhsT=wxb[:, blk * 2 + ic, (jc % 2) * 128:(jc % 2 + 1) * 128], rhs=xab[:, blk * 2 + ic, :], start=(ic == 0), stop=(ic == 1)),
                                 reads=[b_wab, b_xab[blk * 2 + ic]], writes=[pbi])
                        S.op("act", lambda e, ptr=ptr, s2=s2, jc=jc: e.activation(out=tr[s2][:, :], in_=ptr[:, :], func=AF.Tanh, scale=0.5, bias=c12[:, 24 + jc:25 + jc]), reads=[pbr, b_c12], writes=[b_tr[s2]])
                        S.op("act", lambda e, pti=pti, jq=jq, jc=jc: e.activation(out=ti[jq][:, :], in_=pti[:, :], func=AF.Tanh, scale=0.5, bias=c12[:, 32 + jc:33 + jc]), reads=[pbi, b_c12], writes=[b_ti[jq]])
                        S.op("act", lambda e, s2=s2, jq=jq, jc=jc: e.activation(out=ta[jq][:, :], in_=tr[s2][:, :], func=AF.Exp, scale=c12[:, 16 + jc:17 + jc], bias=c12[:, 16 + jc:17 + jc]), reads=[b_tr[s2], b_c12], writes=[b_ta[jq]])
                        S.op("act", lambda e, s2=s2, jq=jq, jc=jc: e.activation(out=tm[jq][:, :], in_=tr[s2][:, :], func=AF.Exp, scale=c12[:, jc:jc + 1], bias=c12[:, jc:jc + 1]), reads=[b_tr[s2], b_c12], writes=[b_tm[jq]])
                        S.op("dve", lambda e, jq=jq: e.tensor_scalar(out=tm[jq][:, :], in0=tm[jq][:, :], scalar1=-0.25, scalar2=0.25, op0=ALU.mult, op1=ALU.add), reads=[b_tm[jq]], writes=[b_tm[jq]])
                        S.op("dve", lambda e, jq=jq, jc=jc: e.scalar_tensor_tensor(out=ti[jq][:, :], in0=ti[jq][:, :], scalar=1.0, in1=xa[:, jc, :], op0=ALU.add, op1=ALU.mult), reads=[b_ti[jq], b_xa[jc]], writes=[b_ti[jq]])
                        yield
                    for jq in range(4):
                        S.op("act", lambda e, jq=jq: e.activation(out=tm[jq][:, :], in_=tm[jq][:, :], func=AF.Sqrt), reads=[b_tm[jq]], writes=[b_tm[jq]])
                    yield
                    for jq in range(4):
                        S.op("dve", lambda e, jq=jq: e.tensor_tensor(out=ti[jq][:, :], in0=ti[jq][:, :], in1=tm[jq][:, :], op=ALU.mult), reads=[b_ti[jq], b_tm[jq]], writes=[b_ti[jq]])
                    yield
                    for jq in range(4):
                        jc = quad * 4 + jq
                        S.op("dve", lambda e, jq=jq, jc=jc: e.tensor_tensor_scan(out=tm[jq][:, :], data0=ta[jq][:, :], data1=ti[jq][:, :], initial=hst[:, jc:jc + 1], op0=ALU.mult, op1=ALU.add),
                             reads=[b_ta[jq], b_ti[jq], b_hst8[jc]], writes=[b_tm[jq]])
                    for jq in range(4):
                        jc = quad * 4 + jq
                        S.op("dve", lambda e, jq=jq, jc=jc: e.tensor_copy(out=hst[:, jc:jc + 1], in_=tm[jq][:, GT - 1:GT]), reads=[b_tm[jq]], writes=[b_hst8[jc]])
                    yield
                    for jq in range(4):
                        jc = quad * 4 + jq
                        m = G // 8
                        oh = ppc("onehot", G % 8)
                        ohb = own_hb[m % 2]; bohb = b_ownhb[m % 2]
                        if G % 8 == 0:
                            S.op("dve", lambda e, jq=jq, jc=jc, ohb=ohb, oh=oh: e.tensor_scalar(out=ohb[:, jc, :], in0=tm[jq][:, :], scalar1=oh, scalar2=None, op0=ALU.mult),
                                 reads=[b_tm[jq], b_pp], writes=[b_oh8[m % 2][jc]])
                        else:
                            S.op("dve", lambda e, jq=jq, jc=jc, ohb=ohb, oh=oh: e.scalar_tensor_tensor(out=ohb[:, jc, :], in0=tm[jq][:, :], scalar=oh, in1=ohb[:, jc, :], op0=ALU.mult, op1=ALU.add),
                                 reads=[b_tm[jq], b_pp, b_oh8[m % 2][jc]], writes=[b_oh8[m % 2][jc]])
                if G % 8 == 7:
                    m = G // 8
                    ohb = own_hb[m % 2]; bohb = b_ownhb[m % 2]
                    S.dma("sp", lambda e, m=m, ohb=ohb: e.dma_start(out=ownh_d[:, m * 8 * GT:(m + 1) * 8 * GT], in_=ohb[:].rearrange("p j t -> p (j t)")), reads=b_oh8[m % 2], writes=[b_ownh_d])
                    if debug:
                        S.dma("sp", lambda e, m=m, ohb=ohb: e.dma_start(out=dbg["hlb"][:, m * GT:(m + 1) * GT].rearrange("(jc p) t -> p jc t", p=128), in_=ohb[:]), reads=b_oh8[m % 2], writes=[b_out])
                yield
            def interleave(*gens):
                gens = list(gens)
                while gens:
                    for g in list(gens):
                        try:
                            next(g)
                        except StopIteration:
                            gens.remove(g)
            for _ in gen_H(0):
                pass
            for G in range(n_groups_A):
                emit_M(G)
                gl = [gen_KV(G), gen_LRU(G)]
                if G + 1 < n_groups_A:
                    gl.append(gen_H(G + 1))
                interleave(*gl)
            S.op("act", lambda e: e.activation(out=rk_all[:], in_=rk_all[:], func=AF.Sqrt, bias=eps96[:, 0:1]), reads=[b_rk, b_const], writes=[b_rk])
            S.op("dve", lambda e: e.reciprocal(out=rk_all[:], in_=rk_all[:]), reads=[b_rk], writes=[b_rk])
            PS.release(pst_idx)
            S.dma("sp", lambda e: e.dma_start(out=rk_d, in_=rk_all[:].rearrange("p a b -> p (a b)")), reads=[b_rk], writes=[b_rk_d])
            if debug:
                dtmp = xa; b_dt = Buf()
                S.barrier()
                S.dma("sp", lambda e: e.dma_start(out=dbg["rk"], in_=rk_all[:].rearrange("p a b -> p (a b)")), reads=[b_rk], writes=[b_out])
                S.dma("sp", lambda e: e.dma_start(out=dbg["ktn"], in_=ktn_d[0:128, 0:512]), reads=[b_ktn_d], writes=[b_out])
                S.dma("sp", lambda e: e.dma_start(out=dbg["kpe"], in_=kpe_d[:, 0:512]), reads=[b_kpe_d], writes=[b_out])
                S.dma("sp", lambda e: e.dma_start(out=dbg["v"], in_=v_d[0, 0, :, :]), reads=[b_v_d], writes=[b_out])
            S.barrier()

        def own_group_uT(ph, m, xtb, b_xtb, ssb, b_ssb, uTo, b_uTo):
            for t4 in range(4):
                i = t4 % 2
                r0 = m * GT + t4 * 128
                load_norm_transpose(x_own[r0:r0 + 128, :], xtb[i], b_xtb[i], ssb[i], b_ssb[i], uTo, b_uTo, t4)

        if stop_after not in ("A1", "A") and "1" in phases:
          with ExitStack() as pb1:
            NB1 = 1024 + 2048
            winB = sb(pb1, "winB", [128, 8, NB1], BF16); b_winB = Buf()
            with ExitStack() as wp:
                load_weight_cols(wp, winB, b_winB, w_in_d, [(1024, 2048), (3104, 5152)], 8, "gmix", "wB", NB1)
                S.barrier()
            xtb = [sb(pb1, "xtB%d" % i, [128, D]) for i in range(2)]; b_xtb = [Buf(), Buf()]
            ssb = [sb(pb1, "ssB%d" % i, [128, 4]) for i in range(2)]; b_ssb = [Buf(), Buf()]
            uTo = [sb(pb1, "uTB%d" % i, [128, 8, GT], BF16) for i in range(2)]; b_uTo = [Buf(), Buf()]
            oh = sb(pb1, "ohB", [128, 8, GT], BF16); b_oh = Buf()
            gs = [sb(pb1, "gsB%d" % i, [128, GT]) for i in range(2)]; b_gs = [Buf(), Buf()]
            g2 = [sb(pb1, "g2B%d" % i, [128, GT]) for i in range(2)]; b_g2 = [Buf(), Buf()]
            sa = [sb(pb1, "saB%d" % i, [128, GT]) for i in range(2)]; b_sa = [Buf(), Buf()]
            gao = sb(pb1, "gaoB", [128, 8, GT], BF16); b_gao = Buf()
            sbo = sb(pb1, "sboB", [128, 8, GT], BF16); b_sbo = Buf()
            def interleave(*gens):
                gens = list(gens)
                while gens:
                    for g in list(gens):
                        try:
                            next(g)
                        except StopIteration:
                            gens.remove(g)

            def gen_headB(m, xtb, b_xtb, ssb, b_ssb, uTo, b_uTo):
                u = uTo[m % 2]; bu = b_uTo[m % 2]
                for t4 in range(4):
                    i = t4 % 2
                    r0 = m * GT + t4 * 128
                    load_norm_transpose(x_own[r0:r0 + 128, :], xtb[i], b_xtb[i], ssb[i], b_ssb[i], u, bu, t4)
                    yield

            def gen_chunksB1(m, par):
                u = uTo[m % 2]; bu = b_uTo[m % 2]
                for jc in range(par, 8, 2):
                    s_ = jc % 2
                    pt, pb = PS.next()
                    for kc in range(8):
                        S.op("pe", lambda e, pt=pt, jc=jc, kc=kc, u=u: e.matmul(pt[:, :], lhsT=winB[:, kc, jc * 128:(jc + 1) * 128], rhs=u[:, kc, :], start=(kc == 0), stop=(kc == 7)),
                             reads=[b_winB, bu], writes=[pb])
                    S.op("act", lambda e, pt=pt, s_=s_: e.activation(out=gs[s_][:, :], in_=pt[:, :], func=AF.Copy), reads=[pb], writes=[b_gs[s_]])
                    S.op("act", lambda e, pt=pt, s_=s_: e.activation(out=g2[s_][:, :], in_=pt[:, :], func=AF.Square), reads=[pb], writes=[b_g2[s_]])
                    yield
                    S.op("dve", lambda e, s_=s_: e.tensor_scalar(out=g2[s_][:, :], in0=g2[s_][:, :], scalar1=0.044715, scalar2=1.0, op0=ALU.mult, op1=ALU.add), reads=[b_g2[s_]], writes=[b_g2[s_]])
                    S.op("dve", lambda e, s_=s_: e.tensor_tensor(out=g2[s_][:, :], in0=g2[s_][:, :], in1=gs[s_][:, :], op=ALU.mult), reads=[b_g2[s_], b_gs[s_]], writes=[b_g2[s_]])
                    S.op("act", lambda e, s_=s_: e.activation(out=g2[s_][:, :], in_=g2[s_][:, :], func=AF.Sigmoid, scale=1.5957691216057308), reads=[b_g2[s_]], writes=[b_g2[s_]])
                    S.op("dve", lambda e, s_=s_: e.tensor_tensor(out=gs[s_][:, :], in0=gs[s_][:, :], in1=g2[s_][:, :], op=ALU.mult), reads=[b_g2[s_], b_gs[s_]], writes=[b_gs[s_]])
                    S.op("dve", lambda e, s_=s_, jc=jc: e.tensor_tensor(out=gs[s_][:, :], in0=gs[s_][:, :], in1=oh[:, jc, :], op=ALU.mult), reads=[b_oh, b_gs[s_]], writes=[b_gs[s_]])
                    yield
                    pt, pb = PS.next()
                    for kc in range(8):
                        S.op("pe", lambda e, pt=pt, jc=jc, kc=kc, u=u: e.matmul(pt[:, :], lhsT=winB[:, kc, 1024 + jc * 128:1024 + (jc + 1) * 128], rhs=u[:, kc, :], start=(kc == 0), stop=(kc == 7)),
                             reads=[b_winB, bu], writes=[pb])
                    S.op("act", lambda e, pt=pt, s_=s_: e.activation(out=sa[s_][:, :], in_=pt[:, :], func=AF.Sigmoid), reads=[pb], writes=[b_sa[s_]])
                    S.op("dve", lambda e, s_=s_, jc=jc: e.tensor_tensor(out=gao[:, jc, :], in0=gs[s_][:, :], in1=sa[s_][:, :], op=ALU.mult), reads=[b_sa[s_], b_gs[s_]], writes=[b_gao])
                    yield
                    pt, pb = PS.next()
                    for kc in range(8):
                        S.op("pe", lambda e, pt=pt, jc=jc, kc=kc, u=u: e.matmul(pt[:, :], lhsT=winB[:, kc, 2048 + jc * 128:2048 + (jc + 1) * 128], rhs=u[:, kc, :], start=(kc == 0), stop=(kc == 7)),
                             reads=[b_winB, bu], writes=[pb])
                    S.op("act", lambda e, pt=pt, jc=jc: e.activation(out=sbo[:, jc, :], in_=pt[:, :], func=AF.Sigmoid), reads=[pb], writes=[b_sbo])
                    yield

            for _ in gen_headB(0, xtb, b_xtb, ssb, b_ssb, uTo, b_uTo):
                pass
            for m in range(NOWN):
                S.dma("sp", lambda e, m=m: e.dma_start(out=oh[:].rearrange("p j t -> p (j t)"), in_=ownh_d[:, m * 8 * GT:(m + 1) * 8 * GT]), reads=[b_ownh_d], writes=[b_oh])
                gl = [gen_chunksB1(m, 0), gen_chunksB1(m, 1)]
                if m + 1 < NOWN:
                    gl.append(gen_headB(m + 1, xtb, b_xtb, ssb, b_ssb, uTo, b_uTo))
                interleave(*gl)
                S.dma("sp", lambda e, m=m: e.dma_start(out=gaya_d[:, :, m * GT:(m + 1) * GT], in_=gao[:]), reads=[b_gao], writes=[b_gaya_d])
                S.dma("sp", lambda e, m=m: e.dma_start(out=sgb_d[:, :, m * GT:(m + 1) * GT], in_=sbo[:]), reads=[b_sbo], writes=[b_sgb_d])
            S.barrier()

          with ExitStack() as pb2:
            winQ = sb(pb2, "winQ", [128, 8, 768], BF16); b_winQ = Buf()
            wuq = sb(pb2, "wuq", [128, 6, 1536], BF16); b_wuq = Buf()
            with ExitStack() as wp:
                load_weight_cols(wp, winQ, b_winQ, w_in_d, [(2048, 2816)], 8, "gmix", "wQ", 768)
                load_weight_cols(wp, wuq, b_wuq, wuq_d, [(0, 1536)], 6, "qag", "wUQ", 1536)
                S.barrier()
            xtb = [sb(pb2, "xtQ%d" % i, [128, D]) for i in range(2)]; b_xtb = [Buf(), Buf()]
            ssb = [sb(pb2, "ssQ%d" % i, [128, 4]) for i in range(2)]; b_ssb = [Buf(), Buf()]
            uTo = [sb(pb2, "uTQ%d" % i, [128, 8, GT], BF16) for i in range(2)]; b_uTo = [Buf(), Buf()]
            cosf = sb(pb2, "cosf", [96, TOWN]); sinf = sb(pb2, "sinf", [96, TOWN]); b_cs = Buf()
            S.op("dve", lambda e: e.memset(cosf[0:64, :], 1.0), writes=[b_cs])
            S.op("dve", lambda e: e.memset(sinf[0:64, :], 0.0), writes=[b_cs])
            S.dma("sp", lambda e: e.dma_start(out=sinf[64:96, :], in_=qcs_d[0:32, :]), reads=[b_dram_cs], writes=[b_cs])
            S.dma("sp", lambda e: e.dma_start(out=cosf[64:96, :], in_=qcs_d[32:64, :]), reads=[b_dram_cs], writes=[b_cs])
            qc = sb(pb2, "qc", [128, 6, GT]); b_qc = Buf()
            qsq = sb(pb2, "qsq", [128, 6, GT]); b_qsq = Buf()
            qcn = sb(pb2, "qcn", [128, 6, GT], BF16); b_qcn = Buf()
            rbq = sb(pb2, "rbq", [128, GT]); b_rbq = Buf()
            qs = [sb(pb2, "qs%d" % i, [96, GT]) for i in range(2)]; b_qs = [Buf(), Buf()]
            qq = [sb(pb2, "qq%d" % i, [96, GT]) for i in range(2)]; b_qq = [Buf(), Buf()]
            qr = [sb(pb2, "qr%d" % i, [96, GT]) for i in range(2)]; b_qr = [Buf(), Buf()]
            qn = [sb(pb2, "qn%d" % i, [96, GT]) for i in range(2)]; b_qn = [Buf(), Buf()]
            qto = sb(pb2, "qto", [96, 16, GT], BF16); b_qto = Buf()
            def gen_headsB2(m, par):
                for h in range(par, 16, 2):
                    s_ = h % 2
                    pt, pb = PS.next()
                    for kc in range(6):
                        S.op("pe", lambda e, pt=pt, h=h, kc=kc: e.matmul(pt[0:96, :], lhsT=wuq[:, kc, h * 96:(h + 1) * 96], rhs=qcn[:, kc, :], start=(kc == 0), stop=(kc == 5)),
                             reads=[b_wuq, b_qcn], writes=[pb])
                    S.op("act", lambda e, pt=pt, s_=s_: e.activation(out=qs[s_][:, :], in_=pt[0:96, :], func=AF.Copy), reads=[pb], writes=[b_qs[s_]])
                    S.op("act", lambda e, pt=pt, s_=s_: e.activation(out=qq[s_][:, :], in_=pt[0:96, :], func=AF.Square), reads=[pb], writes=[b_qq[s_]])
                    yield
                    pt2, pb2_ = PS.next()
                    S.op("pe", lambda e, pt2=pt2, s_=s_: e.matmul(pt2[0:96, :], lhsT=ones_f[0:96, 0:96], rhs=qq[s_][:, :], start=True, stop=True), reads=[b_qq[s_], b_const], writes=[pb2_])
                    S.op("act", lambda e, pt2=pt2, s_=s_: e.activation(out=qr[s_][:, :], in_=pt2[0:96, :], func=AF.Sqrt, scale=1.0 / 96, bias=eps_t[0:96, 0:1]), reads=[pb2_], writes=[b_qr[s_]])
                    S.op("dve", lambda e, s_=s_: e.reciprocal(out=qr[s_][:, :], in_=qr[s_][:, :]), reads=[b_qr[s_]], writes=[b_qr[s_]])
                    yield
                    qng = ppc("qng", 0, 96)
                    S.op("dve", lambda e, s_=s_, qng=qng: e.scalar_tensor_tensor(out=qn[s_][:, :], in0=qs[s_][:, :], scalar=qng, in1=qr[s_][:, :], op0=ALU.mult, op1=ALU.mult),
                         reads=[b_qs[s_], b_qr[s_], b_pp], writes=[b_qn[s_]])
                    pt3, pb3 = PS.next()
                    S.op("pe", lambda e, pt3=pt3, s_=s_: e.matmul(pt3[0:96, :], lhsT=rot96[:, :], rhs=qn[s_][:, :], start=True, stop=True), reads=[b_qn[s_], b_const], writes=[pb3])
                    S.op("dve", lambda e, pt3=pt3, s_=s_, m=m: e.tensor_tensor(out=qq[s_][:, :], in0=pt3[0:96, :], in1=sinf[:, m * GT:(m + 1) * GT], op=ALU.mult), reads=[pb3, b_cs], writes=[b_qq[s_]])
                    yield
                    S.op("dve", lambda e, s_=s_, m=m: e.tensor_tensor(out=qn[s_][:, :], in0=qn[s_][:, :], in1=cosf[:, m * GT:(m + 1) * GT], op=ALU.mult), reads=[b_qn[s_], b_cs], writes=[b_qn[s_]])
                    S.op("dve", lambda e, s_=s_: e.tensor_tensor(out=qn[s_][:, :], in0=qn[s_][:, :], in1=qq[s_][:, :], op=ALU.add), reads=[b_qn[s_], b_qq[s_]], writes=[b_qn[s_]])
                    gkf = ppc("gkfold", 0, 96)
                    S.op("act", lambda e, s_=s_, h=h, gkf=gkf: e.activation(out=qto[:, h, :], in_=qn[s_][:, :], func=AF.Copy, scale=gkf), reads=[b_qn[s_], b_pp], writes=[b_qto])
                    yield

            for _ in gen_headB(0, xtb, b_xtb, ssb, b_ssb, uTo, b_uTo):
                pass
            for m in range(NOWN):
                u = uTo[m % 2]; bu = b_uTo[m % 2]
                for jc in range(6):
                    pt, pb = PS.next()
                    for kc in range(8):
                        S.op("pe", lambda e, pt=pt, jc=jc, kc=kc, u=u: e.matmul(pt[:, :], lhsT=winQ[:, kc, jc * 128:(jc + 1) * 128], rhs=u[:, kc, :], start=(kc == 0), stop=(kc == 7)),
                             reads=[b_winQ, bu], writes=[pb])
                    S.op("act", lambda e, pt=pt, jc=jc: e.activation(out=qc[:, jc, :], in_=pt[:, :], func=AF.Copy), reads=[pb], writes=[b_qc])
                    S.op("dve", lambda e, pt=pt, jc=jc: e.tensor_tensor(out=qsq[:, jc, :], in0=pt[:, :], in1=qc[:, jc, :], op=ALU.mult), reads=[pb, b_qc], writes=[b_qsq])
                pt, pb = PS.next()
                for jc in range(6):
                    S.op("pe", lambda e, pt=pt, jc=jc: e.matmul(pt[:, :], lhsT=ones_f[:, :], rhs=qsq[:, jc, :], start=(jc == 0), stop=(jc == 5)), reads=[b_qsq, b_const], writes=[pb])
                S.op("act", lambda e, pt=pt: e.activation(out=rbq[:, :], in_=pt[:, :], func=AF.Sqrt, scale=1.0 / 768, bias=eps_t[:, 0:1]), reads=[pb], writes=[b_rbq])
                S.op("dve", lambda e: e.reciprocal(out=rbq[:, :], in_=rbq[:, :]), reads=[b_rbq], writes=[b_rbq])
                for jc in range(6):
                    S.op("dve", lambda e, jc=jc: e.tensor_tensor(out=qcn[:, jc, :], in0=qc[:, jc, :], in1=rbq[:, :], op=ALU.mult), reads=[b_qc, b_rbq], writes=[b_qcn])
                gl = [gen_headsB2(m, 0), gen_headsB2(m, 1)]
                if m + 1 < NOWN:
                    gl.append(gen_headB(m + 1, xtb, b_xtb, ssb, b_ssb, uTo, b_uTo))
                interleave(*gl)
                S.dma("sp", lambda e, m=m: e.dma_start(out=qt_d[:, :, m * GT:(m + 1) * GT], in_=qto[:]), reads=[b_qto], writes=[b_qt_d])
            S.barrier()

        if stop_after not in ("A1", "A", "B") and "C" in phases:
          with ExitStack() as pc:
            masks = sb(pc, "masks", [128, 32, GT], BF16); b_masks = Buf()
            S.dma("sp", lambda e: e.dma_start(out=masks[:], in_=masks_d), writes=[b_masks])
            rk = sb(pc, "rkC", [128, SEQ // 128, 16]); b_rkc = Buf()
            S.dma("sp", lambda e: e.dma_start(out=rk[:].rearrange("p a b -> p (a b)"), in_=rk_d), reads=[b_rk_d], writes=[b_rkc])
            qth = [sb(pc, "qth%d" % i, [96, TOWN], BF16) for i in range(2)]; b_qth = [Buf(), Buf()]
            NR = 4
            kp = [sb(pc, "kp%d" % i, [96, GT], BF16) for i in range(NR)]; b_kp = [Buf() for _ in range(NR)]
            vp = [sb(pc, "vp%d" % i, [128, 4, 128], BF16) for i in range(NR)]; b_vp = [Buf() for _ in range(NR)]
            NPB = 4
            pbuf = [sb(pc, "pb%d" % i, [128, GT], BF16) for i in range(NPB)]; b_pbuf = [Buf() for _ in range(NPB)]
            gat = sb(pc, "gat", [128, TOWN], BF16); b_gat = Buf()
            sgt = sb(pc, "sgt", [128, TOWN], BF16); b_sgt = Buf()
            mgt = sb(pc, "mgt", [128, TOWN], BF16); b_mgt = Buf()
            rl = sb(pc, "rl", [128, GT]); b_rl = Buf()
            rl2 = sb(pc, "rl2", [128, GT]); b_rl2 = Buf()
            ybn = sb(pc, "ybn", [128, GT]); b_ybn = Buf()
            ybs = sb(pc, "ybs", [128, GT]); b_ybs = Buf()
            (s_banks, s_idx) = PS.reserve(4)
            (o_banks, o_idx) = PS.reserve(4)
            n_heads_c = 16 if stop_after != "C1" else 2
            piece = 0
            stepno = 0
            for h in range(n_heads_c):
                q_ = qth[h % 2]; bq_ = b_qth[h % 2]
                S.dma("sp", lambda e, h=h, q_=q_: e.dma_start(out=q_[:, :], in_=qt_d[:, h, :]), reads=[b_qt_d], writes=[bq_])
                hr = (h % 2) * 64
                kcx = h // 2
                S.dma("sp", lambda e, hr=hr, kcx=kcx: e.dma_start(out=gat[hr:hr + 64, :], in_=gaya_d[hr:hr + 64, kcx, :]), reads=[b_gaya_d], writes=[b_gat])
                S.dma("sp", lambda e, hr=hr, kcx=kcx: e.dma_start(out=sgt[hr:hr + 64, :], in_=sgb_d[hr:hr + 64, kcx, :]), reads=[b_sgb_d], writes=[b_sgt])
                pendq = []

                def emit_pv(t_):
                    (ot2, ob2, vt2, bvt2, blk2, pbf2, bpbf2, f2, l2) = t_
                    S.op("pe", lambda e: e.matmul(ot2[:, :], lhsT=vt2[:, blk2, :], rhs=pbf2[:, :], start=f2, stop=l2),
                         reads=[bvt2, bpbf2], writes=[ob2])
                for G in range(NG):
                    r = piece % NR; piece += 1
                    kt = kp[r]; bkt = b_kp[r]; vt = vp[r]; bvt = b_vp[r]
                    S.dma("sp", lambda e, h=h, G=G, kt=kt: e.dma_start(out=kt[0:64, :], in_=ktn_d[h * 64:(h + 1) * 64, G * GT:(G + 1) * GT]), reads=[b_ktn_d], writes=[bkt])
                    S.dma("sp", lambda e, G=G, kt=kt: e.dma_start(out=kt[64:96, :], in_=kpe_d[:, G * GT:(G + 1) * GT]), reads=[b_kpe_d], writes=[bkt])
                    S.dma("sp", lambda e, h=h, G=G, vt=vt: e.dma_start(out=vt[:].rearrange("p b c -> p (b c)"), in_=v_d[h, G, :, :]), reads=[b_v_d], writes=[bvt])
                    for m in range(G // 8, NOWN):
                        ot, ob = o_banks[m]
                        for blk in range(4):
                            si = stepno % 4; stepno += 1
                            st_, sb_ = s_banks[si]
                            pbf = pbuf[si]; bpbf = b_pbuf[si]
                            S.op("pe", lambda e, st_=st_, kt=kt, blk=blk, q_=q_, m=m: e.matmul(st_[:, :], lhsT=kt[:, blk * 128:(blk + 1) * 128], rhs=q_[:, m * GT:(m + 1) * GT], start=True, stop=True),
                                 reads=[bkt, bq_], writes=[sb_])
                            sc = rk[:, G * 4 + blk, h:h + 1]
                            S.op("act", lambda e, st_=st_, pbf=pbf, sc=sc: e.activation(out=pbf[:, :], in_=st_[:, :], func=AF.Exp, scale=sc), reads=[sb_, b_rkc], writes=[bpbf])
                            if G // 8 == m:
                                mj = (G % 8) * 4 + blk
                                S.op("dve", lambda e, pbf=pbf, mj=mj: e.tensor_tensor(out=pbf[:, :], in0=pbf[:, :], in1=masks[:, mj, :], op=ALU.mult), reads=[bpbf, b_masks], writes=[bpbf])
                            first = (G == 0 and blk == 0)
                            last = (G == 8 * m + 7 and blk == 3)
                            pendq.append((ot, ob, vt, bvt, blk, pbf, bpbf, first, last))
                            if len(pendq) > 2:
                                emit_pv(pendq.pop(0))
                    if G % 8 == 7:
                        while pendq:
                            emit_pv(pendq.pop(0))
                        m = G // 8
                        ot, ob = o_banks[m]
                        cs = slice(m * GT, (m + 1) * GT)
                        S.op("dve", lambda e, ot=ot: e.reciprocal(out=rl[64:128, :], in_=ot[64:128, :]), reads=[ob], writes=[b_rl])
                        S.op("dve", lambda e: e.tensor_copy(out=rl2[0:64, :], in_=rl[64:128, :]), reads=[b_rl], writes=[b_rl2])
                        S.op("dve", lambda e, ot=ot: e.tensor_tensor(out=ybn[0:64, :], in0=ot[0:64, :], in1=rl2[0:64, :], op=ALU.mult), reads=[ob, b_rl2], writes=[b_ybn])
                        if hr == 0:
                            ysrc = ybn; bys = b_ybn
                        else:
                            S.op("dve", lambda e: e.tensor_copy(out=ybs[64:128, :], in_=ybn[0:64, :]), reads=[b_ybn], writes=[b_ybs])
                            ysrc = ybs; bys = b_ybs
                        if debug:
                            S.dma("sp", lambda e, ysrc=ysrc, hr=hr, h=h, cs=cs: e.dma_start(out=dbg["yb"][h * 64:(h + 1) * 64, cs], in_=ysrc[hr:hr + 64, :]), reads=[bys], writes=[b_out])
                        S.op("dve", lambda e, ysrc=ysrc, hr=hr, cs=cs: e.tensor_tensor(out=ysrc[hr:hr + 64, :], in0=ysrc[hr:hr + 64, :], in1=sgt[hr:hr + 64, cs], op=ALU.mult), reads=[bys, b_sgt], writes=[bys])
                        S.op("dve", lambda e, ysrc=ysrc, hr=hr, cs=cs: e.tensor_tensor(out=mgt[hr:hr + 64, cs], in0=ysrc[hr:hr + 64, :], in1=gat[hr:hr + 64, cs], op=ALU.add), reads=[bys, b_gat], writes=[b_mgt])
                S.dma("sp", lambda e, hr=hr, kcx=kcx: e.dma_start(out=mg_d[hr:hr + 64, kcx, :], in_=mgt[hr:hr + 64, :]), reads=[b_mgt], writes=[b_mg_d])
            PS.release(s_idx); PS.release(o_idx)
            S.barrier()

        if stop_after not in ("A1", "A", "B", "C", "C1") and "D" in phases:
          with ExitStack() as pd:
            woutb = sb(pd, "woutb", [128, 8, D], BF16); b_woutb = Buf()
            with ExitStack() as wp:
                load_weight_cols(wp, woutb, b_woutb, wout_d, [(0, D)], 8, None, "wO", D)
                S.barrier()
            wr = sb(pd, "wr", [128, 8, 36]); b_wr = Buf()
            S.dma("sp", lambda e: e.dma_start(out=wr[:], in_=wr_d.rearrange("(kc p) n -> p kc n", p=128)), writes=[b_wr])
            rbias = sb(pd, "rbias", [128, 36]); gfb = sb(pd, "gfb", [128, D]); b_rb = Buf()
            S.dma("sp", lambda e: e.dma_start(out=rbias[:], in_=rbias_d), writes=[b_rb])
            S.dma("sp", lambda e: e.dma_start(out=gfb[:], in_=gffnb_d), writes=[b_rb])
            mgs = [sb(pd, "mgs%d" % i, [128, 8, GT], BF16) for i in range(2)]; b_mgs = [Buf(), Buf()]
            xtd = [sb(pd, "xtD%d" % i, [128, D]) for i in range(2)]; b_xtd = [Buf(), Buf()]
            hm = [sb(pd, "hm%d" % i, [128, D]) for i in range(2)]; b_hm = [Buf(), Buf()]
            xn = [sb(pd, "xn%d" % i, [128, D]) for i in range(2)]; b_xn = [Buf(), Buf()]
            ssd = [sb(pd, "ssD%d" % i, [128, 4]) for i in range(2)]; b_ssd = [Buf(), Buf()]
            xnT = [sb(pd, "xnT%d" % i, [128, 8, 128]) for i in range(2)]; b_xnT = [Buf(), Buf()]
            xgs = sb(pd, "xgs", [128, 8, GT], BF16); b_xgs = Buf()
            cts = sb(pd, "cts", [32, GT]); b_cts = Buf()
            R = {}
            for nm, w in [("lg", 36), ("gmax", 1), ("goh", 4), ("ngm", 1), ("ge", 4), ("gsum", 1), ("pg", 1), ("gpen", 4), ("em", 32),
                          ("t1", 1), ("m1", 32), ("em2", 32), ("t2", 1), ("m2", 32), ("dd", 1), ("ed", 1), ("w1", 1), ("w2", 1), ("comb", 32)]:
                R[nm] = [sb(pd, "r_%s%d" % (nm, i), [128, w]) for i in range(2)]
            b_R = [Buf(), Buf()]
            BIG = 1.0e9
            (ct_l, ct_idx) = PS.reserve(1)
            ctp, ctb = ct_l[0]
            for m in range(NOWN):
                mg_ = mgs[m % 2]; bmg_ = b_mgs[m % 2]
                S.dma("sp", lambda e, m=m, mg_=mg_: e.dma_start(out=mg_[:], in_=mg_d[:, :, m * GT:(m + 1) * GT]), reads=[b_mg_d], writes=[bmg_])
                for t4 in range(4):
                    tt = m * 4 + t4
                    i = tt % 2
                    r0 = tt * 128
                    S.dma("sp", lambda e, r0=r0, i=i: e.dma_start(out=xtd[i][:], in_=x_own[r0:r0 + 128, :]), writes=[b_xtd[i]])
                    for nh in range(2):
                        pt, pb = PS.next()
                        for kc in range(8):
                            S.op("pe", lambda e, pt=pt, kc=kc, nh=nh, t4=t4, mg_=mg_: e.matmul(pt[:, :], lhsT=mg_[:, kc, t4 * 128:(t4 + 1) * 128], rhs=woutb[:, kc, nh * 512:(nh + 1) * 512], start=(kc == 0), stop=(kc == 7)),
                                 reads=[bmg_, b_woutb], writes=[pb])
                        S.op("dve", lambda e, pt=pt, nh=nh, i=i: e.tensor_tensor(out=hm[i][:, nh * 512:(nh + 1) * 512], in0=pt[:, :], in1=xtd[i][:, nh * 512:(nh + 1) * 512], op=ALU.add),
                             reads=[pb, b_xtd[i]], writes=[b_hm[i]])
                    S.dma("sp", lambda e, r0=r0, i=i: e.dma_start(out=hmid_d[r0:r0 + 128, :], in_=hm[i][:]), reads=[b_hm[i]], writes=[b_hmid_d])
                    if debug:
                        S.dma("sp", lambda e, r0=r0, i=i: e.dma_start(out=dbg["hmid"][r0:r0 + 128, :], in_=hm[i][:]), reads=[b_hm[i]], writes=[b_out])
                    ss = ssd[i]; bss = b_ssd[i]
                    S.op("act", lambda e, i=i, ss=ss: e.activation(out=junk[:], in_=hm[i][:], func=AF.Square, accum_out=ss[:, 0:1]), reads=[b_hm[i]], writes=[bss, b_junk])
                    S.op("act", lambda e, ss=ss: e.activation(out=ss[:, 1:2], in_=ss[:, 0:1], func=AF.Sqrt, scale=1.0 / D, bias=eps_t[:, 0:1]), reads=[bss], writes=[bss])
                    S.op("dve", lambda e, ss=ss: e.reciprocal(out=ss[:, 2:3], in_=ss[:, 1:2]), reads=[bss], writes=[bss])
                    S.op("dve", lambda e, ss=ss, i=i: e.scalar_tensor_tensor(out=xn[i][:], in0=hm[i][:], scalar=ss[:, 2:3], in1=gfb[:], op0=ALU.mult, op1=ALU.mult),
                         reads=[bss, b_hm[i], b_rb], writes=[b_xn[i]])
                    for half in range(2):
                        pt, pb = PS.next()
                        for q in range(4):
                            kc = half * 4 + q
                            S.op("pe", lambda e, pt=pt, q=q, kc=kc, i=i: e.transpose(out=pt[:, q * 128:(q + 1) * 128], in_=xn[i][:, kc * 128:(kc + 1) * 128], identity=ident[:]),
                                 reads=[b_xn[i], b_const], writes=[pb])
                        dst = xnT[i][:, half * 4:half * 4 + 4, :]
                        src = pt[:].rearrange("p (q t) -> p q t", q=4)
                        if half == 0:
                            S.op("act", lambda e, dst=dst, src=src: e.activation(out=dst, in_=src, func=AF.Copy), reads=[pb], writes=[b_xnT[i]])
                        else:
                            S.op("dve", lambda e, dst=dst, src=src: e.tensor_copy(out=dst, in_=src), reads=[pb], writes=[b_xnT[i]])
                    S.op("pool", lambda e, i=i, t4=t4: e.tensor_copy(out=xgs[:, :, t4 * 128:(t4 + 1) * 128], in_=xnT[i][:, :, :]), reads=[b_xnT[i]], writes=[b_xgs])
                    pt, pb = PS.next()
                    for kc in range(8):
                        S.op("pe", lambda e, pt=pt, kc=kc, i=i: e.matmul(pt[:, 0:36], lhsT=xnT[i][:, kc, :], rhs=wr[:, kc, :], start=(kc == 0), stop=(kc == 7)),
                             reads=[b_xnT[i], b_wr], writes=[pb])
                    r = {k: v[i] for k, v in R.items()}
                    br = b_R[i]
                    rd = [br, b_rb]
                    S.op("dve", lambda e, pt=pt, r=r: e.tensor_tensor(out=r["lg"][:, :], in0=pt[:, 0:36], in1=rbias[:, :], op=ALU.add), reads=[pb, b_rb, br], writes=[br])
                    S.op("dve", lambda e, r=r: e.tensor_reduce(out=r["gmax"][:, :], in_=r["lg"][:, 0:4], axis=AX.X, op=ALU.max), reads=rd, writes=[br])
                    S.op("dve", lambda e, r=r: e.tensor_scalar(out=r["goh"][:, :], in0=r["lg"][:, 0:4], scalar1=r["gmax"][:, 0:1], scalar2=None, op0=ALU.is_ge), reads=rd, writes=[br])
                    S.op("dve", lambda e, r=r: e.tensor_scalar(out=r["ngm"][:, :], in0=r["gmax"][:, :], scalar1=-1.0, scalar2=None, op0=ALU.mult), reads=rd, writes=[br])
                    S.op("act", lambda e, r=r: e.activation(out=r["ge"][:, :], in_=r["lg"][:, 0:4], func=AF.Exp, bias=r["ngm"][:, 0:1], accum_out=r["gsum"][:, 0:1]), reads=rd, writes=[br])
                    S.op("dve", lambda e, r=r: e.reciprocal(out=r["pg"][:, :], in_=r["gsum"][:, :]), reads=rd, writes=[br])
                    S.op("dve", lambda e, r=r: e.tensor_scalar(out=r["gpen"][:, :], in0=r["goh"][:, :], scalar1=BIG, scalar2=-BIG, op0=ALU.mult, op1=ALU.add), reads=rd, writes=[br])
                    for g in range(4):
                        S.op("dve", lambda e, r=r, g=g: e.tensor_scalar(out=r["em"][:, g * 8:(g + 1) * 8], in0=r["lg"][:, 4 + g * 8:4 + (g + 1) * 8], scalar1=r["gpen"][:, g:g + 1], scalar2=None, op0=ALU.add), reads=rd, writes=[br])
                    S.op("dve", lambda e, r=r: e.tensor_reduce(out=r["t1"][:, :], in_=r["em"][:, :], axis=AX.X, op=ALU.max), reads=rd, writes=[br])
                    S.op("dve", lambda e, r=r: e.tensor_scalar(out=r["m1"][:, :], in0=r["em"][:, :], scalar1=r["t1"][:, 0:1], scalar2=None, op0=ALU.is_ge), reads=rd, writes=[br])
                    S.op("dve", lambda e, r=r: e.scalar_tensor_tensor(out=r["em2"][:, :], in0=r["m1"][:, :], scalar=-BIG, in1=r["em"][:, :], op0=ALU.mult, op1=ALU.add), reads=rd, writes=[br])
                    S.op("dve", lambda e, r=r: e.tensor_reduce(out=r["t2"][:, :], in_=r["em2"][:, :], axis=AX.X, op=ALU.max), reads=rd, writes=[br])
                    S.op("dve", lambda e, r=r: e.tensor_scalar(out=r["m2"][:, :], in0=r["em2"][:, :], scalar1=r["t2"][:, 0:1], scalar2=None, op0=ALU.is_ge), reads=rd, writes=[br])
                    S.op("dve", lambda e, r=r: e.tensor_tensor(out=r["dd"][:, :], in0=r["t2"][:, :], in1=r["t1"][:, :], op=ALU.subtract), reads=rd, writes=[br])
                    S.op("act", lambda e, r=r: e.activation(out=r["ed"][:, :], in_=r["dd"][:, :], func=AF.Exp), reads=rd, writes=[br])
                    S.op("dve", lambda e, r=r: e.tensor_scalar(out=r["w1"][:, :], in0=r["ed"][:, :], scalar1=1.0, scalar2=None, op0=ALU.add), reads=rd, writes=[br])
                    S.op("dve", lambda e, r=r: e.reciprocal(out=r["w1"][:, :], in_=r["w1"][:, :]), reads=rd, writes=[br])
                    S.op("dve", lambda e, r=r: e.tensor_tensor(out=r["w2"][:, :], in0=r["ed"][:, :], in1=r["w1"][:, :], op=ALU.mult), reads=rd, writes=[br])
                    S.op("dve", lambda e, r=r: e.tensor_tensor(out=r["w1"][:, :], in0=r["w1"][:, :], in1=r["pg"][:, :], op=ALU.mult), reads=rd, writes=[br])
                    S.op("dve", lambda e, r=r: e.tensor_tensor(out=r["w2"][:, :], in0=r["w2"][:, :], in1=r["pg"][:, :], op=ALU.mult), reads=rd, writes=[br])
                    S.op("dve", lambda e, r=r: e.tensor_scalar(out=r["comb"][:, :], in0=r["m1"][:, :], scalar1=r["w1"][:, 0:1], scalar2=None, op0=ALU.mult), reads=rd, writes=[br])
                    S.op("dve", lambda e, r=r: e.scalar_tensor_tensor(out=r["comb"][:, :], in0=r["m2"][:, :], scalar=r["w2"][:, 0:1], in1=r["comb"][:, :], op0=ALU.mult, op1=ALU.add), reads=rd, writes=[br])
                    if debug:
                        S.dma("sp", lambda e, r=r, r0=r0: e.dma_start(out=dbg["comb"][r0:r0 + 128, :], in_=r["comb"][:, :]), reads=[br], writes=[b_out])
                    S.op("pe", lambda e, r=r, t4=t4: e.transpose(out=ctp[0:32, t4 * 128:(t4 + 1) * 128], in_=r["comb"][:, 0:32], identity=ident[:]), reads=[br, b_const], writes=[ctb])
                S.op("act", lambda e: e.activation(out=cts[:, :], in_=ctp[0:32, :], func=AF.Copy), reads=[ctb], writes=[b_cts])
                S.dma("sp", lambda e, m=m: e.dma_start(out=combt_d[:, m * GT:(m + 1) * GT], in_=cts[:, :]), reads=[b_cts], writes=[b_combt_d])
                S.dma("sp", lambda e, m=m: e.dma_start(out=xgt_d[:, :, m * GT:(m + 1) * GT], in_=xgs[:]), reads=[b_xgs], writes=[b_xgt_d])
            PS.release(ct_idx)
            S.barrier()

        if stop_after not in ("A1", "A", "B", "C", "C1", "D") and "E" in phases:
          with ExitStack() as pe_:
            yacc = sb(pe_, "yacc", [128, 16, D]); b_yacc = [Buf() for _ in range(16)]
            S.dma("sp", lambda e: e.dma_start(out=yacc[:], in_=hmid_d.rearrange("(t p) f -> p t f", p=128)), reads=[b_hmid_d], writes=b_yacc)
            xg = sb(pe_, "xg", [128, 8, TOWN], BF16); b_xg = Buf()
            S.dma("sp", lambda e: e.dma_start(out=xg[:], in_=xgt_d), reads=[b_xgt_d], writes=[b_xg])
            combT = sb(pe_, "combT", [32, TOWN]); b_combT = Buf()
            S.dma("sp", lambda e: e.dma_start(out=combT[:], in_=combt_d), reads=[b_combt_d], writes=[b_combT])
            sel = [sb(pe_, "sel%d" % i, [32, 128]) for i in range(2)]; b_sel = [Buf(), Buf()]
            wgs2 = [sb(pe_, "wgs%d" % i, [128, 8, DEXP]) for i in range(2)]; wus2 = [sb(pe_, "wus%d" % i, [128, 8, DEXP]) for i in range(2)]
            wds2 = [sb(pe_, "wds%d" % i, [128, 2, D]) for i in range(2)]
            b_wgs2 = [Buf(), Buf()]; b_wus2 = [Buf(), Buf()]; b_wds2 = [Buf(), Buf()]
            wgb = [sb(pe_, "wgb%d" % i, [128, 8, DEXP], BF16) for i in range(2)]
            wub = [sb(pe_, "wub%d" % i, [128, 8, DEXP], BF16) for i in range(2)]
            wdb = [sb(pe_, "wdb%d" % i, [128, 2, D], BF16) for i in range(2)]
            b_wgb = [Buf(), Buf()]; b_wub = [Buf(), Buf()]; b_wdb = [Buf(), Buf()]
            cb = sb(pe_, "cb", [128, GT]); b_cb = Buf()
            sg = [sb(pe_, "sg%d" % i, [128, GT]) for i in range(2)]; b_sg = [Buf(), Buf()]
            tu = [sb(pe_, "tu%d" % i, [128, GT]) for i in range(2)]; b_tu = [Buf(), Buf()]
            hb = [sb(pe_, "hb%d" % i, [128, 2, GT], BF16) for i in range(2)]; b_hb = [Buf(), Buf()]
            n_exp = NEXP if stop_after != "E1" else 2
            for ex in range(n_exp):
                w = ex % 2
                wgs = wgs2[w]; wus = wus2[w]; wds = wds2[w]; b_wgs = b_wgs2[w]; b_wus = b_wus2[w]; b_wds = b_wds2[w]
                S.dma("sp", lambda e, ex=ex, wgs=wgs: e.dma_start(out=wgs[:], in_=wg_d[ex].rearrange("(kc p) j -> p kc j", p=128)), writes=[b_wgs])
                S.dma("sp", lambda e, ex=ex, wus=wus: e.dma_start(out=wus[:], in_=wu_d[ex].rearrange("(kc p) j -> p kc j", p=128)), writes=[b_wus])
                S.dma("sp", lambda e, ex=ex, wds=wds: e.dma_start(out=wds[:], in_=wd_d[ex].rearrange("(jc p) n -> p jc n", p=128)), writes=[b_wds])
                S.op("act", lambda e, w=w, wgs=wgs: e.activation(out=wgb[w][:], in_=wgs[:], func=AF.Copy), reads=[b_wgs], writes=[b_wgb[w]])
                S.op("act", lambda e, w=w, wus=wus: e.activation(out=wub[w][:], in_=wus[:], func=AF.Copy), reads=[b_wus], writes=[b_wub[w]])
                S.op("act", lambda e, w=w, wds=wds: e.activation(out=wdb[w][:], in_=wds[:], func=AF.Copy), reads=[b_wds], writes=[b_wdb[w]])
                S.op("dve", lambda e, w=w, ex=ex: e.tensor_copy(out=sel[w][:, :], in_=ident[0:32, ex:ex + 1].to_broadcast([32, 128])), reads=[b_const], writes=[b_sel[w]])
                for m in range(NOWN):
                    cs = slice(m * GT, (m + 1) * GT)
                    h_ = hb[m % 2]; bh_ = b_hb[m % 2]
                    pt, pb = PS.next()
                    S.op("pe", lambda e, pt=pt, w=w, cs=cs: e.matmul(pt[:, :], lhsT=sel[w][:, :], rhs=combT[:, cs], start=True, stop=True), reads=[b_sel[w], b_combT], writes=[pb])
                    S.op("act", lambda e, pt=pt: e.activation(out=cb[:, :], in_=pt[:, :], func=AF.Copy), reads=[pb], writes=[b_cb])
                    for jc in range(2):
                        s_ = jc
                        ptg, pbg = PS.next()
                        for kc in range(8):
                            S.op("pe", lambda e, ptg=ptg, kc=kc, jc=jc, w=w, cs=cs: e.matmul(ptg[:, :], lhsT=wgb[w][:, kc, jc * 128:(jc + 1) * 128], rhs=xg[:, kc, cs], start=(kc == 0), stop=(kc == 7)),
                                 reads=[b_wgb[w], b_xg], writes=[pbg])
                        ptu, pbu = PS.next()
                        for kc in range(8):
                            S.op("pe", lambda e, ptu=ptu, kc=kc, jc=jc, w=w, cs=cs: e.matmul(ptu[:, :], lhsT=wub[w][:, kc, jc * 128:(jc + 1) * 128], rhs=xg[:, kc, cs], start=(kc == 0), stop=(kc == 7)),
                                 reads=[b_wub[w], b_xg], writes=[pbu])
                        S.op("act", lambda e, ptg=ptg, s_=s_: e.activation(out=sg[s_][:, :], in_=ptg[:, :], func=AF.Silu), reads=[pbg], writes=[b_sg[s_]])
                        S.op("dve", lambda e, ptu=ptu, s_=s_: e.tensor_tensor(out=tu[s_][:, :], in0=ptu[:, :], in1=sg[s_][:, :], op=ALU.mult), reads=[pbu, b_sg[s_]], writes=[b_tu[s_]])
                        S.op("dve", lambda e, s_=s_, jc=jc, h_=h_: e.tensor_tensor(out=h_[:, jc, :], in0=tu[s_][:, :], in1=cb[:, :], op=ALU.mult), reads=[b_tu[s_], b_cb], writes=[bh_])
                    for t4 in range(4):
                        tt = m * 4 + t4
                        for nh in range(2):
                            pty, pby = PS.next()
                            for jc in range(2):
                                S.op("pe", lambda e, pty=pty, jc=jc, t4=t4, nh=nh, w=w, h_=h_: e.matmul(pty[:, :], lhsT=h_[:, jc, t4 * 128:(t4 + 1) * 128], rhs=wdb[w][:, jc, nh * 512:(nh + 1) * 512], start=(jc == 0), stop=(jc == 1)),
                                     reads=[bh_, b_wdb[w]], writes=[pby])
                            S.op("dve", lambda e, pty=pty, tt=tt, nh=nh: e.tensor_tensor(out=yacc[:, tt, nh * 512:(nh + 1) * 512], in0=pty[:, :], in1=yacc[:, tt, nh * 512:(nh + 1) * 512], op=ALU.add),
                                 reads=[pby, b_yacc[tt]], writes=[b_yacc[tt]])
            S.dma("sp", lambda e: e.dma_start(out=out_d.rearrange("(t p) f -> p t f", p=128), in_=yacc[:]), reads=b_yacc, writes=[b_out])
            S.barrier()

        S.barrier()
        for e_ in ("sp", "pool"):
            pass
        S.emit()
    return nc


b_out = Buf("out")


def _pcol(v, nchunk):
    return np.ascontiguousarray(np.asarray(v, np.float32).reshape(nchunk, 128).T)


def _prep_common(inp):
    f = lambda k: np.asarray(inp[k], np.float32)[0]
    pp = np.zeros((128, PPW), np.float32)
    pp[:, PP["gmix"]:PP["gmix"] + 8] = _pcol(f("norm_mix_g"), 8)
    cw = f("conv_w")
    pp[:, PP["convw"]:PP["convw"] + 32] = cw.reshape(4, 8, 128).transpose(2, 1, 0).reshape(128, 32)
    pp[:, PP["convb"]:PP["convb"] + 8] = _pcol(f("conv_b"), 8)
    pp[:, PP["ba"]:PP["ba"] + 8] = _pcol(f("lru_ba"), 8)
    pp[:, PP["bx"]:PP["bx"] + 8] = _pcol(f("lru_bx"), 8)
    pp[:, PP["lam"]:PP["lam"] + 8] = _pcol(f("lru_lambda"), 8)
    pp[:, PP["qag"]:PP["qag"] + 6] = _pcol(f("q_a_g"), 6)
    pp[:, PP["kvag"]:PP["kvag"] + 2] = _pcol(f("kv_a_g"), 2)
    pp[0:96, PP["qng"]] = f("q_norm_g")
    kng = f("k_norm_g")
    pp[0:64, PP["gkfold"]] = kng[0:64]
    pp[64:96, PP["gkfold"]] = 1.0
    pp[0:32, PP["gkpe"]] = kng[64:96]
    freq = (10000.0 ** (-np.arange(16, dtype=np.float32) / 16.0)).astype(np.float32)
    fr32 = np.concatenate([freq, freq])
    pp[0:32, PP["freq64"]] = fr32
    pp[32:64, PP["freq64"]] = fr32
    pp[32:64, PP["phase64"]] = np.float32(math.pi / 2)
    pp[0:64, PP["blockones"]] = 1.0
    pp[64:128, PP["blockones"] + 1] = 1.0
    pp[:, PP["gffn"]:PP["gffn"] + 8] = _pcol(f("norm_ffn_g"), 8)
    ident = np.eye(128, dtype=np.float32)
    rot32 = np.zeros((32, 32), np.float32)
    for m in range(16):
        rot32[m + 16, m] = -1.0
        rot32[m, m + 16] = 1.0
    rot96 = np.zeros((96, 96), np.float32)
    rot96[64:96, 64:96] = rot32
    wr = np.concatenate([f("router_group_w"), f("router_expert_w")], axis=1)
    rb = np.concatenate([f("router_group_b"), f("router_expert_b")])[None, :]
    rbias = np.ascontiguousarray(np.broadcast_to(rb, (128, 36))).astype(np.float32)
    gffnb = np.ascontiguousarray(np.broadcast_to(f("norm_ffn_g")[None, :], (128, D))).astype(np.float32)
    common = dict(gffnb=gffnb, ident=ident, rot96=rot96, rot32=rot32, rbias=rbias, w_in=f("w_in"), lru_wa=f("lru_wa"),
                  lru_wx=f("lru_wx"), w_uq=f("w_uq"), w_ukv=f("w_ukv"), w_out=f("w_out"), w_router=np.ascontiguousarray(wr),
                  w_gate=f("w_gate"), w_up=f("w_up"), w_down=f("w_down"))
    return pp, common


def _masks_for(c):
    m = np.zeros((128, 32, GT), np.float32)
    q = np.arange(GT)[None, :]
    for cp in range(8):
        for i in range(4):
            j = cp * 4 + i
            if cp < c:
                m[:, j, :] = 1.0
            elif cp == c:
                m[0:64, j, :] = (q >= 128 * i)
                m[64:128, j, :] = (q >= 128 * i + 64)
    return m.astype(ml_dtypes.bfloat16)


def make_in_maps(inp, names=None):
    pp, common = _prep_common(inp)
    x = np.asarray(inp["x"], np.float32)[0]
    pos = np.asarray(inp["positions"], np.int32)[0]
    posb_all = np.ascontiguousarray(np.broadcast_to(pos[None, :], (64, SEQ)))
    maps = []
    for c in range(NCORES):
        rows = np.concatenate([np.arange((8 * m + c) * GT, (8 * m + c + 1) * GT) for m in range(NOWN)])
        ppc_ = pp.copy()
        ppc_[:, PP["onehot"] + c] = 1.0
        d = dict(common)
        d.update(x_all=x, x_own=np.ascontiguousarray(x[rows]), posb_all=posb_all,
                 posb_own=np.ascontiguousarray(posb_all[:, rows]), pp=ppc_, masks=_masks_for(c))
        if names is not None:
            d = {k: d[k] for k in names}
        maps.append(d)
    return maps


def own_rows(c):
    return np.concatenate([np.arange((8 * m + c) * GT, (8 * m + c + 1) * GT) for m in range(NOWN)])


def kernel(**inputs):
    nc = build_program()
    maps = make_in_maps(inputs, nc._declared_inputs)
    res = run_bass_kernel_spmd(nc, maps, core_ids=list(range(NCORES)))
    out = np.zeros((1, SEQ, D), np.float32)
    for c in range(NCORES):
        out[0, own_rows(c)] = res.results[c]["out"]
    return out
```
